# Optimizing a Trainium2 kernel written in Bass

```python
import jax, jax.numpy as jnp
from jax import lax
import numpy as np

D_MODEL = 1024
BATCH = 8
SEQ = 2048
DEPTH = 1

CHUNK = 64
N_META = 16
Q_BLOCK = 128
N_HEADS_A = 8
HEAD_DIM_A = 64
ATTN_WIDTH = N_HEADS_A * HEAD_DIM_A
KV_LATENT = 128
IDX_HEADS = 8
IDX_DIM = 32
TOPK_MAX = 256
ATTN_SCALE = HEAD_DIM_A ** -0.5
IDX_SCALE = (IDX_HEADS ** -0.5) * (IDX_DIM ** -0.5)
POOL_WINDOWS = (2, 4, 8, 16)
POOL_WIDTH = 512
POOL_GROUP = POOL_WIDTH // len(POOL_WINDOWS)
IN_SIZES = (ATTN_WIDTH, KV_LATENT, IDX_HEADS * IDX_DIM, IDX_DIM, IDX_HEADS, POOL_WIDTH, 2 * D_MODEL)
IN_WIDTH = sum(IN_SIZES)
N_GROUPS = 4
EXPERTS_PER_GROUP = 8
TOP_E = 2
EXPERT_HIDDEN = 256
EPS = 1e-6

kernel_name = 'hybrid_dsa_pool_hmoe_stream_block'


def _rmsnorm(x, g):
    xf = x.astype(jnp.float32)
    y = xf * lax.rsqrt(jnp.mean(xf * xf, axis=-1, keepdims=True) + EPS)
    return (y * g.astype(jnp.float32)).astype(x.dtype)


def _chunk_ids(n):
    p = jnp.arange(n, dtype=jnp.int32)
    return jnp.where(p < N_META, 0, 1 + (p - N_META) // CHUNK)


def _sparse_attention(q_abs, iq, iw, c, ik, w_uv, k_top):
    B, T, _ = c.shape
    nb = -(-T // Q_BLOCK)
    pad = nb * Q_BLOCK - T
    key_chunk = _chunk_ids(T)
    q_chunk = _chunk_ids(nb * Q_BLOCK).reshape(nb, Q_BLOCK)

    def to_blocks(a):
        a = jnp.pad(a, [(0, 0), (0, pad)] + [(0, 0)] * (a.ndim - 2))
        return jnp.moveaxis(a.reshape((B, nb, Q_BLOCK) + a.shape[2:]), 1, 0)

    def block(args):
        qa, qi, qw, qc = args
        admissible = key_chunk[None, :] <= qc[:, None]
        rel = jax.nn.relu(jnp.einsum('bqhd,bsd->bqhs', qi, ik))
        score = jnp.einsum('bqhs,bqh->bqs', rel, qw)
        score = jnp.where(admissible[None], score, -jnp.inf)
        _, sel = lax.top_k(score, k_top)
        valid = key_chunk[sel] <= qc[None, :, None]
        c_sel = jax.vmap(lambda cb, ib: cb[ib])(c, sel)
        logits = jnp.einsum('bqhc,bqkc->bqhk', qa, c_sel).astype(jnp.float32) * ATTN_SCALE
        logits = jnp.where(valid[:, :, None, :], logits, -jnp.inf)
        p = jax.nn.softmax(logits, axis=-1).astype(c.dtype)
        o_lat = jnp.einsum('bqhk,bqkc->bqhc', p, c_sel)
        o = jnp.einsum('bqhc,chd->bqhd', o_lat, w_uv)
        return o.reshape(B, Q_BLOCK, ATTN_WIDTH)

    out = lax.map(block, (to_blocks(q_abs), to_blocks(iq), to_blocks(iw), q_chunk))
    return jnp.moveaxis(out, 0, 1).reshape(B, nb * Q_BLOCK, ATTN_WIDTH)[:, :T]


def _multiscale_pool(v, w_pool, pool_scale):
    B, T, _ = v.shape
    G = len(POOL_WINDOWS)
    vg = v.reshape(B, T, G, POOL_GROUP).astype(jnp.float32)
    cs = jnp.concatenate([jnp.zeros((B, 1, G, POOL_GROUP), jnp.float32), jnp.cumsum(vg, axis=1)], axis=1)
    pos = jnp.arange(1, T + 1, dtype=jnp.float32)
    outs = []
    for g, w in enumerate(POOL_WINDOWS):
        csg = cs[:, :, g]
        lower = jnp.concatenate([jnp.zeros((B, w - 1, POOL_GROUP), jnp.float32), csg[:, :T - w + 1]], axis=1)
        mean = (csg[:, 1:] - lower) / jnp.minimum(pos, float(w))[None, :, None]
        outs.append(mean - vg[:, :, g])
    d = jnp.stack(outs, axis=2).astype(v.dtype)
    y = jnp.einsum('btgc,gcd->btgd', d, w_pool).reshape(B, T, POOL_WIDTH)
    return y * pool_scale


def _hier_moe(u, w_gr, b_gr, w_er, b_er, w_gate, w_up, w_down):
    B, T, D = u.shape
    uf = u.reshape(B * T, D)
    g_logits = (uf @ w_gr).astype(jnp.float32) + b_gr.astype(jnp.float32)
    g_probs = jax.nn.softmax(g_logits, axis=-1)
    g_sel = jnp.argmax(g_logits, axis=-1)
    p_g = jnp.take_along_axis(g_probs, g_sel[:, None], axis=1)[:, 0]
    e_logits = jnp.einsum('nd,dge->nge', uf, w_er).astype(jnp.float32) + b_er.astype(jnp.float32)
    e_sel = jnp.take_along_axis(e_logits, g_sel[:, None, None], axis=1)[:, 0]
    top_v, top_i = lax.top_k(e_sel, TOP_E)
    p_e = jax.nn.softmax(top_v, axis=-1)
    within = jnp.sum(jax.nn.one_hot(top_i, EXPERTS_PER_GROUP, dtype=jnp.float32) * p_e[..., None], axis=1)
    gate = (jax.nn.one_hot(g_sel, N_GROUPS, dtype=jnp.float32)[:, :, None]
            * within[:, None, :] * p_g[:, None, None]).astype(u.dtype)
    y = jnp.zeros_like(uf)
    for g in range(N_GROUPS):
        hg = jax.nn.silu(jnp.einsum('nd,edf->nef', uf, w_gate[g])) * jnp.einsum('nd,edf->nef', uf, w_up[g])
        y = y + jnp.einsum('nef,efd->nd', hg * gate[:, g, :, None], w_down[g])
    return y.reshape(B, T, D)


def setup_inputs(seed: int = 0) -> dict:
    key = jax.random.key(seed)
    ks = jax.random.split(key, 21)
    f32 = jnp.float32
    D, L = D_MODEL, DEPTH
    nrm = lambda k, shape, fan: jax.random.normal(k, shape, f32) * (fan ** -0.5)
    gain = lambda k, shape: 1.0 + 0.02 * jax.random.normal(k, shape, f32)
    return {
        'x': jax.random.normal(ks[0], (BATCH, SEQ, D), f32),
        'meta_tokens': jax.random.normal(ks[1], (N_META, D), f32),
        'norm1_g': gain(ks[2], (L, D)),
        'w_in': nrm(ks[3], (L, D, IN_WIDTH), D),
        'kv_norm_g': gain(ks[4], (L, KV_LATENT)),
        'w_uk': nrm(ks[5], (L, KV_LATENT, N_HEADS_A, HEAD_DIM_A), KV_LATENT),
        'w_uv': nrm(ks[6], (L, KV_LATENT, N_HEADS_A, HEAD_DIM_A), KV_LATENT),
        'w_pool': nrm(ks[7], (L, len(POOL_WINDOWS), POOL_GROUP, POOL_GROUP), POOL_GROUP),
        'pool_scale': gain(ks[8], (L, POOL_WIDTH)),
        'w_branch_attn': nrm(ks[9], (L, ATTN_WIDTH, D), ATTN_WIDTH),
        'w_branch_pool': nrm(ks[10], (L, POOL_WIDTH, D), POOL_WIDTH),
        'w_out': nrm(ks[11], (L, D, D), D),
        'norm2_g': gain(ks[12], (L, D)),
        'w_group_router': nrm(ks[13], (L, D, N_GROUPS), D),
        'b_group_router': 0.01 * jax.random.normal(ks[14], (L, N_GROUPS), f32),
        'w_expert_router': nrm(ks[15], (L, D, N_GROUPS, EXPERTS_PER_GROUP), D),
        'b_expert_router': 0.01 * jax.random.normal(ks[16], (L, N_GROUPS, EXPERTS_PER_GROUP), f32),
        'w_expert_gate': nrm(ks[17], (L, N_GROUPS, EXPERTS_PER_GROUP, D, EXPERT_HIDDEN), D),
        'w_expert_up': nrm(ks[18], (L, N_GROUPS, EXPERTS_PER_GROUP, D, EXPERT_HIDDEN), D),
        'w_expert_down': nrm(ks[19], (L, N_GROUPS, EXPERTS_PER_GROUP, EXPERT_HIDDEN, D), EXPERT_HIDDEN),
        'final_norm_g': gain(ks[20], (D,)),
    }


def reference(x, meta_tokens, norm1_g, w_in, kv_norm_g, w_uk, w_uv, w_pool, pool_scale,
              w_branch_attn, w_branch_pool, w_out, norm2_g, w_group_router, b_group_router,
              w_expert_router, b_expert_router, w_expert_gate, w_expert_up, w_expert_down,
              final_norm_g):
    B, S, D = x.shape
    k_top = min(TOPK_MAX, S // 4)
    meta = jnp.broadcast_to(meta_tokens.astype(x.dtype)[None], (B, N_META, D))
    h = jnp.concatenate([meta, x], axis=1)
    T = S + N_META
    split_at = [int(v) for v in np.cumsum(IN_SIZES)[:-1]]
    for l in range(DEPTH):
        u = _rmsnorm(h, norm1_g[l])
        q, c, iq, ik, iw, pv, gates = jnp.split(u @ w_in[l], split_at, axis=-1)
        q = q.reshape(B, T, N_HEADS_A, HEAD_DIM_A)
        c = _rmsnorm(c, kv_norm_g[l])
        q_abs = jnp.einsum('bthd,chd->bthc', q, w_uk[l])
        iq = iq.reshape(B, T, IDX_HEADS, IDX_DIM)
        iw = iw * IDX_SCALE
        attn = _sparse_attention(q_abs, iq, iw, c, ik, w_uv[l], k_top)
        pool = _multiscale_pool(pv, w_pool[l], pool_scale[l])
        g_attn, g_pool = jnp.split(jax.nn.sigmoid(gates), 2, axis=-1)
        merged = g_attn * (attn @ w_branch_attn[l]) + g_pool * (pool @ w_branch_pool[l])
        h = h + merged @ w_out[l]
        h = h + _hier_moe(_rmsnorm(h, norm2_g[l]), w_group_router[l], b_group_router[l],
                          w_expert_router[l], b_expert_router[l], w_expert_gate[l],
                          w_expert_up[l], w_expert_down[l])
    return _rmsnorm(h, final_norm_g)[:, N_META:]
```

```python
import math
from contextlib import ExitStack

import numpy as np
import concourse.bass as bass
import concourse.mybir as mybir
from concourse.bass_utils import run_bass_kernel_spmd

F32 = mybir.dt.float32
BF16 = mybir.dt.bfloat16
ALU = mybir.AluOpType
AF = mybir.ActivationFunctionType
AX = mybir.AxisListType

P = 128
D = 1024
KC = 8
NT = 16
TX = 2048
T = 2064
EPS = 1e-6
ATTN_SCALE = 64 ** -0.5
IDX_SCALE = (8 ** -0.5) * (32 ** -0.5)
KTOP = 256
NIT = 16
NEG = -30000.0
BIG = 1.0e30
N_EXP = 32
ENGS = ['pe', 'act', 'dve', 'pool', 'sp']
NDS = 24


class Buf:
    __slots__ = ('name', 'lw', 'rd')

    def __init__(self, name):
        self.name = name
        self.lw = None
        self.rd = {}


class Op:
    __slots__ = ('fn', 'waits', 'dma')

    def __init__(self, fn, waits, dma):
        self.fn = fn
        self.waits = waits
        self.dma = dma


class Prog:
    def __init__(self, nc):
        self.nc = nc
        self.ops = {e: [] for e in ENGS}
        self.seen = {e: {} for e in ENGS}
        self.seen_dma = {e: set() for e in ENGS}
        self.dma_info = []
        self.dma_uses = [0] * NDS
        self.dma_hist = {}
        self.bufs = {}

    def B(self, *key):
        b = self.bufs.get(key)
        if b is None:
            b = Buf(key)
            self.bufs[key] = b
        return b

    def _deps(self, reads, writes):
        toks = set()
        for b in reads:
            if b.lw is not None:
                toks.add(b.lw)
        for b in writes:
            if b.lw is not None:
                toks.add(b.lw)
            toks.update(b.rd.values())
        return toks

    def _resolve(self, eng, toks):
        best = {}
        out = []
        for t in toks:
            if t[0] == 'c':
                if t[1] == eng and eng == 'pe':
                    continue
                if best.get(t[1], -1) < t[2]:
                    best[t[1]] = t[2]
            else:
                if t[1] not in self.seen_dma[eng]:
                    self.seen_dma[eng].add(t[1])
                    out.append(t)
        for pe_, i in best.items():
            if self.seen[eng].get(pe_, -1) >= i:
                continue
            self.seen[eng][pe_] = i
            out.append(('c', pe_, i))
        return out

    def _commit(self, tok, key, reads, writes):
        for b in reads:
            b.rd[key] = tok
        for b in writes:
            b.lw = tok
            b.rd = {}

    def op(self, eng, fn, reads=(), writes=()):
        ps = [b for b in reads if b.name[0] == 'psum']
        if ps:
            writes = list(writes) + [b for b in ps if b not in writes]
            reads = [b for b in reads if b.name[0] != 'psum']
        idx = len(self.ops[eng])
        tok = ('c', eng, idx)
        waits = self._resolve(eng, self._deps(reads, writes))
        self.ops[eng].append(Op(fn, waits, None))
        self._commit(tok, eng, reads, writes)
        return tok

    def dma(self, q, out, in_, reads=(), writes=(), **kw):
        did = len(self.dma_info)
        half = NDS // 2
        base = half if q == 'pool' else 0
        hist = self.dma_hist.setdefault(base, [])
        s = base + len(hist) % half
        self.dma_uses[s] += 1
        self.dma_info.append((s, 16 * self.dma_uses[s]))
        toks = self._deps(reads, writes)
        if len(hist) >= half:
            toks.add(('d', hist[len(hist) - half]))
        hist.append(did)
        waits = self._resolve(q, toks)
        self.ops[q].append(Op(lambda e, o=out, i=in_, k=kw: e.dma_start(out=o, in_=i, **k), waits, did))
        tok = ('d', did)
        self._commit(tok, tok, reads, writes)
        return tok

    def barrier(self):
        toks = set()
        for e in ENGS:
            if e != 'sp' and self.ops[e]:
                for i in range(len(self.ops[e]) - 1, -1, -1):
                    if self.ops[e][i].dma is None and self.ops[e][i].fn is not None:
                        toks.add(('c', e, i))
                        break
        for hist in self.dma_hist.values():
            for d in hist[-(NDS // 2):]:
                toks.add(('d', d))
        for e in ENGS:
            mine = set(t for t in toks if not (t[0] == 'c' and t[1] == e and e == 'pe'))
            waits = self._resolve(e, mine)
            if waits:
                self.ops[e].append(Op(None, waits, None))

    def emit(self, esem, dsem):
        sig = {e: set() for e in ENGS}
        for e in ENGS:
            for o in self.ops[e]:
                for t in o.waits:
                    if t[0] == 'c':
                        sig[t[1]].add(t[2])
        signo = {e: {} for e in ENGS}
        for e in ENGS:
            for n, i in enumerate(sorted(sig[e])):
                signo[e][i] = n + 1
        prog = self

        def stream(ename, h):
            for i, o in enumerate(prog.ops[ename]):
                for t in o.waits:
                    if t[0] == 'c':
                        h.wait_ge(esem[t[1]], signo[t[1]][t[2]])
                    else:
                        s, val = prog.dma_info[t[1]]
                        h.wait_ge(dsem[s], val)
                if o.fn is None:
                    continue
                inst = o.fn(h)
                if o.dma is not None:
                    inst.then_inc(dsem[prog.dma_info[o.dma][0]], 16)
                elif i in sig[ename]:
                    inst.then_inc(esem[ename], 1)

        with self.nc.Block() as block:
            @block.tensor
            def _(h):
                stream('pe', h)

            @block.scalar
            def _(h):
                stream('act', h)

            @block.vector
            def _(h):
                stream('dve', h)

            @block.gpsimd
            def _(h):
                stream('pool', h)

            @block.sync
            def _(h):
                stream('sp', h)


def mk_ap(base, extra_off, dims):
    return bass.AP(base.tensor, base.offset + extra_off, dims)


def pstep(ap):
    return ap.ap[0][0]


def build(debug=None):
    debug = debug or {}
    stop_after = debug.get('stop_after', 99)
    nc = bass.Bass("TRN2", target_bir_lowering=False)

    def din(name, shape):
        return nc.dram_tensor(name, list(shape), F32, kind="ExternalInput").ap()

    x_d = din("x", [TX, D])
    meta_d = din("meta", [16, D])
    g1_d = din("norm1_g", [1, D])
    win_d = din("w_in", [D, 3496])
    gkv_d = din("kv_norm_g", [1, 128])
    wuk_d = din("w_uk", [128, 512])
    wuv_d = din("w_uv", [128, 512])
    wpool_d = din("w_pool", [4, 128, 128])
    pscale_d = din("pool_scale", [1, 512])
    wba_d = din("w_ba", [512, D])
    wbp_d = din("w_bp", [512, D])
    wout_d = din("w_out", [D, D])
    g2_d = din("norm2_g", [1, D])
    wgr_d = din("w_gr", [D, 4])
    bgr_d = din("b_gr", [1, 4])
    wer_d = din("w_er", [D, 32])
    ber_d = din("b_er", [1, 32])
    weg_d = din("w_eg", [N_EXP, D, 256])
    weu_d = din("w_eu", [N_EXP, D, 256])
    wed_d = din("w_ed", [N_EXP, 256, D])
    gf_d = din("final_g", [1, D])
    cid_d = din("c_ident", [128, 128])
    cbb_d = din("c_blockbias", [128, 128])
    csel_d = din("c_sel8", [32, 8 * 128])
    cinv_d = din("c_invcnt", [128, 16])
    cpow_d = din("c_pow2", [128, NIT + 2])
    out_d = nc.dram_tensor("out", [TX, D], F32, kind="ExternalOutput").ap()
    dbg_out = {}

    pg = Prog(nc)
    B = pg.B

    with ExitStack() as es:
        esem = {e: es.enter_context(nc.semaphore("sem_" + e)) for e in ENGS}
        dsem = [es.enter_context(nc.semaphore("dsem%d" % i)) for i in range(NDS)]

        R0_B, R1_B, R2_B, R3_B = 62 * 1024, 64 * 1024, 42 * 1024, 34 * 1024
        arenas = {}
        for nm, nb in (('R0', R0_B), ('R1', R1_B), ('R2', R2_B), ('R3', R3_B)):
            arenas[nm] = (es.enter_context(nc.sbuf_tensor(nm, [P, nb // 4], F32)), nb)
        cursor = {}

        def reset(nm):
            cursor[nm] = 0

        def carve(nm, dtype, shape):
            h, nb = arenas[nm]
            esz = 4 if dtype == F32 else 2
            n = 1
            for s in shape[1:]:
                n *= s
            off = (cursor[nm] + 63) // 64 * 64
            assert off + n * esz <= nb, (nm, off, n * esz, nb, shape)
            cursor[nm] = off + n * esz
            hv = h if dtype == F32 else h.bitcast(dtype)
            ap = hv[0:shape[0], off // esz: off // esz + n]
            if len(shape) == 3:
                ap = ap.rearrange("p (a b) -> p a b", a=shape[1])
            elif len(shape) == 4:
                ap = ap.rearrange("p (a b c) -> p a b c", a=shape[1], b=shape[2])
            return ap

        def carve_at(nm, off, dtype, shape):
            save = cursor[nm]
            cursor[nm] = off
            ap = carve(nm, dtype, shape)
            assert (off + 63) // 64 * 64 == off
            cursor[nm] = save
            return ap

        for nm in arenas:
            reset(nm)

        banks = [es.enter_context(nc.psum_tensor("ps%d" % i, [P, 512], F32)) for i in range(8)]
        banks_bf = [b.bitcast(BF16) for b in banks]
        PB = [B('psum', i) for i in range(8)]

        uT = carve('R0', BF16, [P, KC, T])
        off_idle = (cursor['R0'] + 63) // 64 * 64
        gbc = [carve('R0', F32, [P, D]) for _ in range(2)]
        xt = [carve('R0', F32, [P, D]) for _ in range(2)]
        xn = [carve('R0', BF16, [P, D]) for _ in range(2)]
        junkbf = carve('R0', BF16, [P, T])
        ident_f = carve('R0', F32, [P, 128])
        ident_bf = carve('R0', BF16, [P, 128])
        stats = carve('R0', F32, [P, 512])
        stat_i = [0]

        def stat(n=1):
            i = stat_i[0]
            if i + n > 512:
                i = 0
            stat_i[0] = i + n
            return stats[:, i:i + n], B('stat', i, n)

        def stat_bufs(i, n):
            return [B('statc', c) for c in range(i, i + n)]

        def stat2(n=1):
            i = stat_i[0]
            if i + n > 512:
                i = 0
            stat_i[0] = i + n
            return stats[:, i:i + n], stat_bufs(i, n)

        ckeys = carve('R1', BF16, [P, 17, 129])
        cnT = carve('R1', BF16, [P, T])
        ikT = carve('R1', BF16, [P, T])
        poolT = carve('R1', BF16, [P, 4, TX])
        attnT = carve('R1', BF16, [P, 4, TX])
        iw_all = carve('R1', F32, [P, NT, 8])
        gkv_bc = carve('R1', F32, [P, 128])
        blockbias = carve('R1', F32, [P, 128])
        sel8 = carve('R1', BF16, [P, 8, 128])
        invcnt = carve('R1', F32, [P, 16])
        pow2 = carve('R1', F32, [P, NIT + 2])
        pscale = carve('R1', F32, [P, 4])
        cmaxsel = carve('R1', BF16, [P, 8, 32])
        cmax = carve('R1', F32, [P, 1])
        ones_bf = carve('R1', BF16, [P, 128])
        iqT_all = carve('R1', BF16, [P, 3, TX])
        cmaxB = carve('R1', BF16, [P, 128])

        pg.dma('sp', ident_f, cid_d, writes=[B('ident_f')])
        pg.op('dve', lambda e: e.tensor_copy(out=ident_bf, in_=ident_f), reads=[B('ident_f')], writes=[B('ident_bf')])
        pg.dma('pool', sel8[0:32].rearrange("p a b -> p (a b)"), csel_d, writes=[B('sel8')])
        pg.dma('sp', gbc[0], g1_d.partition_broadcast(128), writes=[B('gbc', 0)])
        pg.op('pool', lambda e: e.memset(ckeys[:, 16, 0:128], 0.0), writes=[B('ckeys', 16)])
        pg.op('pool', lambda e: e.memset(ckeys[:, :, 128:129], 1.0), writes=[B('ckeys_ones')])

        def late_consts():
            pg.dma('sp', blockbias, cbb_d, writes=[B('blockbias')])
            pg.dma('sp', invcnt, cinv_d, writes=[B('invcnt')])
            pg.dma('sp', pow2, cpow_d, writes=[B('pow2')])
            pg.dma('sp', pscale, pscale_d.rearrange("o (g p) -> p (o g)", p=128), writes=[B('pscale')],
                   allow_slow_non_contiguous=True)
            pg.dma('sp', gkv_bc, gkv_d.partition_broadcast(128), writes=[B('gkv_bc')])

        def perm3(ap_2d, rows):
            return ap_2d[0:rows].rearrange("t (p k) -> t p k", k=KC)

        def permout(ap_2d, rows):
            return ap_2d[0:rows].rearrange("t (k p) -> t p k", k=KC)

        rot = {}

        def nxt(name, lst):
            i = rot.get(name, 0)
            rot[name] = i + 1
            return lst[i % len(lst)]

        def rmsnorm_to_uT(src2d, src_bufs, rows, col0, gb, gb_buf, tag):
            k = nxt('xn', [0, 1])
            ss, ssb = stat2()
            rt, rtb = stat2()
            rs, rsb = stat2()
            pg.op('act', lambda e: e.activation(out=junkbf[0:rows, 0:D], in_=src2d[0:rows], func=AF.Square,
                                                accum_out=ss[0:rows]),
                  reads=src_bufs, writes=ssb + [B('junkbf')])
            pg.op('act', lambda e: e.activation(out=rt[0:rows], in_=ss[0:rows], func=AF.Sqrt, scale=1.0 / D,
                                                bias=eps_t[0:rows]),
                  reads=ssb + [B('eps')], writes=rtb)
            pg.op('dve', lambda e: e.reciprocal(out=rs[0:rows], in_=rt[0:rows]), reads=rtb, writes=rsb)
            pg.op('dve', lambda e: e.scalar_tensor_tensor(out=xn[k][0:rows], in0=src2d[0:rows],
                                                          scalar=rs[0:rows], in1=gb[0:rows],
                                                          op0=ALU.mult, op1=ALU.mult),
                  reads=src_bufs + rsb + [gb_buf], writes=[B('xn', k)])
            bk = nxt('tp', [0, 1])

            def tps(e):
                last = None
                for kc in range(KC):
                    last = e.transpose(out=banks_bf[bk][:, kc * 128: kc * 128 + rows],
                                       in_=xn[k][0:rows].rearrange("t (p k) -> t k p", k=KC)[:, kc, :],
                                       identity=ident_bf[0:rows, 0:rows])
                return last
            pg.op('pe', tps, reads=[B('xn', k), B('ident_bf')], writes=[PB[bk]])
            src = banks_bf[bk][:, 0:1024].rearrange("p (a b) -> p a b", a=KC)[:, :, 0:rows]

            def back():
                pg.op('act', lambda e: e.copy(out=uT[:, :, col0:col0 + rows], in_=src),
                      reads=[PB[bk]], writes=[B('uT', tag)])
            return back

        eps_t = carve('R0', F32, [P, 1])
        pg.op('pool', lambda e: e.memset(eps_t, EPS), writes=[B('eps')])

        reset('R2')
        reset('R3')
        pvT = carve('R3', F32, [P, 4, T])
        wslab = carve('R2', BF16, [P, KC, 512])
        wsmall = carve('R2', BF16, [P, KC, 136])
        wik = carve('R2', BF16, [P, KC, 96])
        w_iq2 = carve('R2', BF16, [P, KC, 256])
        wpool = carve('R2', BF16, [P, 4, 128])
        ptmp = [carve('R2', F32, [P, T]) for _ in range(2)]
        dT = [carve('R2', BF16, [P, T]) for _ in range(2)]
        tmpc = carve('R2', F32, [P, 16])

        xt4 = xt + [ptmp[0][:, 0:D], ptmp[1][:, 0:D]]
        tiles = [-1] + list(range(NT))
        pend1 = None
        for j in tiles:
            rows = 16 if j < 0 else 128
            col0 = 0 if j < 0 else 16 + 128 * j
            k = nxt('xt4', [0, 1, 2, 3])
            src = meta_d if j < 0 else x_d[128 * j:128 * (j + 1), :]
            pg.dma('sp', xt4[k][0:rows], src, writes=[B('xt', k)])
            if j == 1:
                late_consts()
            bk_ = rmsnorm_to_uT(xt4[k], [B('xt', k)], rows, col0, gbc[0], B('gbc', 0), j)
            if pend1 is not None:
                pend1()
            pend1 = bk_
        pend1()

        def uT_bufs(c0, c1):
            res = []
            if c0 < 16:
                res.append(B('uT', -1))
            for j in range(NT):
                a, b_ = 16 + 128 * j, 16 + 128 * (j + 1)
                if a < c1 and b_ > c0:
                    res.append(B('uT', j))
            return res

        win_p = win_d.rearrange("(p k) n -> p k n", k=KC)

        pg.dma('pool', wsmall[:, :, 0:128], win_p[:, :, 512:640], writes=[B('wsmall')])
        pg.dma('pool', wsmall[:, :, 128:136], win_p[:, :, 928:936], reads=[], writes=[B('wsmall2')])
        for r_ in range(3):
            pg.dma('pool', wik[:, :, 32 * r_:32 * r_ + 32], win_p[:, :, 896:928], writes=[B('wik', r_)])
        pg.dma('pool', w_iq2, win_p[:, :, 640:896], writes=[B('w_iq2')])
        pg.dma('pool', wslab, win_p[:, :, 936:1448], writes=[B('wslab')])
        pg.dma('pool', wpool, wpool_d.rearrange("g c d -> c g d"), writes=[B('wpool')])

        def p2a_f1(j):
            rows = 16 if j < 0 else 128
            col0 = 0 if j < 0 else 16 + 128 * j
            chunk = 16 if j < 0 else j
            bk = nxt('p2a', [2, 3])

            def mm(e):
                last = None
                for kc in range(KC):
                    last = e.matmul(banks[bk][0:rows, 0:136], lhsT=uT[:, kc, col0:col0 + rows],
                                    rhs=wsmall[:, kc, :], start=(kc == 0), stop=(kc == KC - 1))
                return last
            pg.op('pe', mm, reads=[B('uT', j), B('wsmall'), B('wsmall2')], writes=[PB[bk]])
            ss, ssb = stat2()
            rt, rtb = stat2()
            rs, rsb = stat2()
            pg.op('act', lambda e: e.activation(
                out=junkbf[0:rows, 0:128], in_=banks[bk][0:rows, 0:128], func=AF.Square, accum_out=ss[0:rows]),
                reads=[PB[bk]], writes=ssb + [B('junkbf')])
            pg.op('act', lambda e: e.activation(
                out=rt[0:rows], in_=ss[0:rows], func=AF.Sqrt, scale=1.0 / 128, bias=eps_t[0:rows]),
                reads=ssb + [B('eps')], writes=rtb)
            pg.op('dve', lambda e: e.reciprocal(out=rs[0:rows], in_=rt[0:rows]), reads=rtb, writes=rsb)
            pg.op('dve', lambda e: e.scalar_tensor_tensor(
                out=ckeys[0:rows, chunk, 0:128], in0=banks[bk][0:rows, 0:128], scalar=rs[0:rows],
                in1=gkv_bc[0:rows], op0=ALU.mult, op1=ALU.mult),
                reads=[PB[bk], B('gkv_bc')] + rsb, writes=[B('ckeys', chunk)])
            if j >= 0:
                pg.op('dve', lambda e: e.tensor_scalar(
                    out=iw_all[:, j, :], in0=banks[bk][:, 128:136], scalar1=IDX_SCALE, scalar2=None,
                    op0=ALU.mult), reads=[PB[bk]], writes=[B('iw', j)])

            def f2():
                bt = nxt('p2at', [4, 5])
                pg.op('pe', lambda e: e.transpose(
                    out=banks_bf[bt][:, 0:rows], in_=ckeys[0:rows, chunk, 0:128], identity=ident_bf[0:rows, 0:rows]),
                    reads=[B('ckeys', chunk), B('ident_bf')], writes=[PB[bt]])

                def f3():
                    pg.op('act', lambda e: e.copy(
                        out=cnT[:, col0:col0 + rows], in_=banks_bf[bt][:, 0:rows]),
                        reads=[PB[bt]], writes=[B('cnT', j)])
                return f3
            return f2

        p_f2, p_f3 = None, None
        for j in tiles:
            f2 = p2a_f1(j)
            f3 = p_f2() if p_f2 is not None else None
            if p_f3 is not None:
                p_f3()
            p_f2, p_f3 = f2, f3
        f3 = p_f2()
        if p_f3 is not None:
            p_f3()
        f3()

        blocks = [(0, 16)] + [(16 + 512 * b, 16 + 512 * (b + 1)) for b in range(4)]
        allb = list(range(8))
        evac_i = [0]

        def evac_copy(out_ap, in_ap, reads, writes):
            evac_i[0] += 1
            if evac_i[0] % 2 == 0:
                pg.op('act', lambda e: e.copy(out=out_ap, in_=in_ap), reads=reads, writes=writes)
            else:
                pg.op('dve', lambda e: e.tensor_copy(out=out_ap, in_=in_ap), reads=reads, writes=writes)

        def act_evac(out_ap, in_ap, reads, writes):
            pg.op('act', lambda e: e.copy(out=out_ap, in_=in_ap), reads=reads, writes=writes)

        def p2b_ik():
            for bi, (c0, c1) in enumerate(blocks):
                bk = nxt('gen', allb)
                n = c1 - c0

                def mm(e, bk=bk, c0=c0, c1=c1, n=n):
                    last = None
                    for kc in range(KC):
                        last = e.matmul(banks[bk][0:96, 0:n], lhsT=wik[:, kc, :], rhs=uT[:, kc, c0:c1],
                                        start=(kc == 0), stop=(kc == KC - 1))
                    return last
                pg.op('pe', mm, reads=uT_bufs(c0, c1) + [B('wik', 0), B('wik', 1), B('wik', 2)], writes=[PB[bk]])
                act_evac(ikT[0:96, c0:c1], banks[bk][0:96, 0:n], [PB[bk]], [B('ikT', bi)])

        def p2b_iq():
            for g in range(3):
                M = 96 if g < 2 else 64
                for b in range(4):
                    bk = nxt('gen', allb)
                    c0 = 16 + 512 * b

                    def mmi(e, bk=bk, g=g, M=M, c0=c0):
                        last = None
                        for kc in range(KC):
                            last = e.matmul(banks[bk][0:M, 0:512], lhsT=w_iq2[:, kc, 96 * g:96 * g + M],
                                            rhs=uT[:, kc, c0:c0 + 512], start=(kc == 0), stop=(kc == KC - 1))
                        return last
                    pg.op('pe', mmi, reads=uT_bufs(c0, c0 + 512) + [B('w_iq2')], writes=[PB[bk]])
                    act_evac(iqT_all[0:M, g, 512 * b:512 * (b + 1)], banks[bk][0:M, 0:512], [PB[bk]],
                             [B('iqT_all', g, b)])

        def p2b_pv(m):
            for bi, (c0, c1) in enumerate(blocks):
                bk = nxt('gen', allb)
                n = c1 - c0

                def mm(e, bk=bk, c0=c0, c1=c1, n=n):
                    last = None
                    for kc in range(KC):
                        last = e.matmul(banks[bk][:, 0:n], lhsT=wslab[:, kc, m * 128:(m + 1) * 128],
                                        rhs=uT[:, kc, c0:c1], start=(kc == 0), stop=(kc == KC - 1))
                    return last
                pg.op('pe', mm, reads=uT_bufs(c0, c1) + [B('wslab')], writes=[PB[bk]])
                act_evac(pvT[:, m, c0:c1], banks[bk][:, 0:n], [PB[bk]], [B('pvT', m)])

        p3_di = {}

        def p3_chain(g):
            w = 2 << g
            src = pvT[:, g, :]
            cur = src
            curb = [B('pvT', g)]
            for k in [1, 2, 4, 8][:g + 1]:
                pi = nxt('ptmp', [0, 1])
                dst = ptmp[pi]
                pg.op('pool', lambda e, dst=dst, cur=cur, k=k: e.tensor_tensor(
                    out=dst[:, k:1024], in0=cur[:, k:1024], in1=cur[:, 0:1024 - k], op=ALU.add),
                    reads=curb, writes=[B('ptmp', pi), B('xt', 2 + pi)])
                pg.op('dve', lambda e, dst=dst, cur=cur, k=k: e.tensor_tensor(
                    out=dst[:, 1024:T], in0=cur[:, 1024:T], in1=cur[:, 1024 - k:T - k], op=ALU.add),
                    reads=curb, writes=[B('ptmp', pi, 'hi')])
                pg.op('pool', lambda e, dst=dst, cur=cur, k=k: e.tensor_copy(out=dst[:, 0:k], in_=cur[:, 0:k]),
                      reads=curb, writes=[B('ptmp', pi, 'head'), B('xt', 2 + pi)])
                cur = dst
                curb = [B('ptmp', pi), B('ptmp', pi, 'hi'), B('ptmp', pi, 'head')]
            di = nxt('dT', [0, 1])
            p3_di[g] = di
            cb = curb
            pg.op('dve', lambda e: e.scalar_tensor_tensor(
                out=dT[di][:, w - 1:T], in0=cur[:, w - 1:T], scalar=1.0 / w, in1=src[:, w - 1:T],
                op0=ALU.mult, op1=ALU.subtract),
                reads=cb + [B('pvT', g)], writes=[B('dT', di)])
            pg.op('dve', lambda e: e.tensor_tensor(
                out=tmpc[:, 0:w - 1], in0=cur[:, 0:w - 1], in1=invcnt[:, 0:w - 1], op=ALU.mult),
                reads=cb + [B('invcnt')], writes=[B('tmpc')])
            pg.op('dve', lambda e: e.tensor_tensor(
                out=dT[di][:, 0:w - 1], in0=tmpc[:, 0:w - 1], in1=src[:, 0:w - 1], op=ALU.subtract),
                reads=[B('tmpc'), B('pvT', g)], writes=[B('dT', di, 'head')])

        def p3_mm(g):
            di = p3_di[g]
            for b in range(4):
                bk = nxt('gen', allb)
                pg.op('pe', lambda e, bk=bk, b=b: e.matmul(
                    banks[bk][:, 0:512], lhsT=wpool[:, g, :], rhs=dT[di][:, 16 + 512 * b:16 + 512 * (b + 1)],
                    start=True, stop=True),
                    reads=[B('wpool'), B('dT', di), B('dT', di, 'head')], writes=[PB[bk]])
                pg.op('act', lambda e, bk=bk, b=b: e.activation(
                    out=poolT[:, g, 512 * b:512 * (b + 1)], in_=banks[bk][:, 0:512], func=AF.Identity,
                    scale=pscale[:, g:g + 1]),
                    reads=[PB[bk], B('pscale')], writes=[B('poolT', b)])

        W_Q_OFF = 25344
        w_q = carve_at('R3', W_Q_OFF, BF16, [P, KC, 512])
        p2b_pv(3)
        p2b_pv(2)
        p3_chain(3)
        p2b_pv(1)
        p3_chain(2)
        if stop_after >= 4:
            pg.dma('pool', w_q, win_p[:, :, 0:512], writes=[B('w_q'), B('pvT', 3)])
        p2b_pv(0)
        p2b_ik()
        p2b_iq()
        p3_mm(3)
        p3_mm(2)
        p3_chain(1)
        p3_mm(1)
        p3_chain(0)
        p3_mm(0)

        if stop_after <= 3:
            pass
        pg.barrier()
        reset('R2')
        reset('R3')
        w_ba = carve_at('R0', off_idle, BF16, [P, 4, D])
        w_bp = carve_at('R0', off_idle + 8192, BF16, [P, 4, D])
        score = carve('R3', F32, [P, T])
        mb = carve('R3', BF16, [P, T])
        mbT = carve('R3', BF16, [P, 17, 128])
        ukT_pad = carve('R2', BF16, [P, 8, 128])
        uv_pad = carve('R2', BF16, [P, 8, 128])
        wuk_bf = carve('R2', BF16, [P, 512])
        qT_t = carve('R2', BF16, [P, 4, 128])
        qabsT_t = carve('R2', BF16, [P, 8, 128])
        absq = carve('R2', BF16, [P, 8, 128])
        iqT_t = carve('R2', BF16, [P, 8, 128])
        rbuf = [carve('R2', BF16, [P, 512]) for _ in range(4)]
        pT = [carve('R2', BF16, [P, 512]) for _ in range(3)]
        diag = carve('R2', BF16, [P, 8, 128])
        olatT_t = carve('R2', BF16, [P, 8, 128])
        negm8 = carve('R2', BF16, [P, 128])
        bis = carve('R2', F32, [P, 4 * (NIT + 2)])
        rsum = carve('R2', F32, [P, 8])
        thr = carve('R2', F32, [P, 2])

        if stop_after >= 4:
            pg.dma('pool', wuk_bf, wuk_d, writes=[B('wuk_bf')])
            pg.dma('pool', w_ba, wba_d.rearrange("(m p) n -> p m n", p=128),
                   writes=[B('w_ba'), B('gbc', 0), B('gbc', 1)])
            pg.dma('pool', w_bp, wbp_d.rearrange("(m p) n -> p m n", p=128),
                   writes=[B('w_bp'), B('xt', 0), B('xt', 1)])
            pg.op('pool', lambda e: e.memset(ukT_pad, 0.0), writes=[B('ukT_pad')])
            pg.op('pool', lambda e: e.memset(uv_pad, 0.0), writes=[B('uv_pad')])
            for hp in range(2):
                src = wuv_d.rearrange("c (m two d) -> c m two d", two=2, d=64)[:, :, hp, :]
                dst = uv_pad.rearrange("c (m two) n -> c m two n", two=2)[:, :, hp, 64 * hp:64 * hp + 64]
                pg.dma('pool', dst, src, reads=[B('uv_pad')], writes=[B('uv_pad', hp)])
            for m in range(4):
                bk = nxt('gen', allb)
                pg.op('pe', lambda e, bk=bk, m=m: e.transpose(
                    out=banks_bf[bk][:, 0:128], in_=wuk_bf[:, m * 128:(m + 1) * 128], identity=ident_bf),
                    reads=[B('wuk_bf'), B('ident_bf')], writes=[PB[bk]])
                for hp in range(2):
                    h = 2 * m + hp
                    pg.op('dve', lambda e, bk=bk, h=h, hp=hp: e.tensor_copy(
                        out=ukT_pad[64 * hp:64 * hp + 64, h, :], in_=banks_bf[bk][64 * hp:64 * hp + 64, 0:128]),
                        reads=[PB[bk], B('ukT_pad')], writes=[B('ukT_pad', h)])
            pg.op('dve', lambda e: e.tensor_reduce(out=cmax, in_=cnT, axis=AX.X, op=ALU.max,
                                                   apply_absolute_value=True),
                  reads=[B('cnT', j) for j in tiles], writes=[B('cmax')])
            pg.op('pool', lambda e: e.memset(ones_bf, 1.0), writes=[B('ones_bf')])
            pg.op('dve', lambda e: e.tensor_copy(out=cmaxB, in_=mk_ap(cmax, 0, [[pstep(cmax), 128], [0, 128]])),
                  reads=[B('cmax')], writes=[B('cmaxB')])
            pg.op('pool', lambda e: e.memset(cmaxsel, 0.0), writes=[B('cmaxsel')])
            pg.op('pool', lambda e: e.memset(mbT[:, 16, :], NEG), writes=[B('mbT', 16)])
            for h in range(8):
                pg.op('dve', lambda e, h=h: e.tensor_copy(out=cmaxsel[:, h, h:h + 1], in_=cmax),
                      reads=[B('cmax'), B('cmaxsel')], writes=[B('cmaxsel', h)])
        ukb = [B('ukT_pad')] + [B('ukT_pad', h) for h in range(8)]
        uvb = [B('uv_pad'), B('uv_pad', 0), B('uv_pad', 1)]
        cmb = [B('cmaxsel')] + [B('cmaxsel', h) for h in range(8)]
        ikb = [B('ikT', bi) for bi in range(5)]
        cnb = [B('cnT', j) for j in tiles]
        ckb = [B('ckeys', c) for c in range(17)] + [B('ckeys_ones')]

        mids = bis[:, 0:NIT + 2]
        cnts = bis[:, NIT + 2:2 * (NIT + 2)]
        dds = bis[:, 2 * (NIT + 2):3 * (NIT + 2)]
        Wc = bis[:, 3 * (NIT + 2):4 * (NIT + 2)]

        n_attn_tiles = NT if stop_after >= 4 else 0
        n_attn_tiles = debug.get('n_attn_tiles', n_attn_tiles)
        qabs2 = [qabsT_t, carve('R2', BF16, [P, 8, 128]), carve('R2', BF16, [P, 8, 128])]
        mb2 = [mb, carve('R3', BF16, [P, T])]
        assert cursor['R3'] <= W_Q_OFF, cursor['R3']
        score2 = [score, carve('R2', F32, [P, T])]
        negsh = carve('R2', F32, [P, 8])
        lnS = carve('R2', F32, [P, 512])
        rcpS = carve('R2', F32, [P, 512])
        ROT4 = [0, 1, 2, 3]
        PSS = [6, 7]
        ACC = [4, 5]

        def act_copy(out_ap, in_ap, reads, writes):
            pg.op('act', lambda e: e.copy(out=out_ap, in_=in_ap), reads=reads, writes=writes)

        def chunks_of(j):
            S = 16 + 128 * (j + 1)
            return [(0, 16)] + [(16 + 512 * m, min(16 + 512 * (m + 1), S)) for m in range((S - 16 + 511) // 512)]

        def stageA(j, tick=None):
            par = j % 2
            par3 = j % 3
            sc, qa = score2[par], qabs2[par3]
            qc = 16 + 128 * j
            ub = [B('uT', j)]
            for h in range(8):
                pg.op('act', lambda e, h=h, j=j: e.activation(out=diag[:, h, :], in_=ident_f, func=AF.Identity,
                                                              scale=iw_all[:, j, h:h + 1]),
                      reads=[B('ident_f'), B('iw', j)], writes=[B('diag', h)])
            items = []
            for (c0, c1) in chunks_of(j):
                bs = nxt('pss', PSS)
                for hp_ in range(4):
                    items.append((c0, c1, bs, hp_))

            def emit_x(it):
                c0, c1, bs, hp_ = it
                n = c1 - c0
                res = []
                for hh in range(2):
                    h = 2 * hp_ + hh
                    bx = nxt('rot4', ROT4)
                    g_, a_ = h // 3, h % 3
                    pg.op('pe', lambda e, bx=bx, g_=g_, a_=a_, c0=c0, c1=c1, n=n: e.matmul(
                        banks[bx][:, 0:n], lhsT=iqT_all[32 * a_:32 * a_ + 32, g_, 128 * j:128 * (j + 1)],
                        rhs=ikT[32 * a_:32 * a_ + 32, c0:c1], start=True, stop=True),
                        reads=[B('iqT_all', g_, j // 4)] + ikb, writes=[PB[bx]])
                    ri = nxt('rbuf', [0, 1, 2, 3])
                    pg.op('act', lambda e, bx=bx, ri=ri, n=n: e.activation(
                        out=rbuf[ri][:, 0:n], in_=banks[bx][:, 0:n], func=AF.Relu),
                        reads=[PB[bx]], writes=[B('rbuf', ri)])
                    res.append((h, ri))
                return res

            def emit_d(it, res):
                c0, c1, bs, hp_ = it
                n = c1 - c0
                for (h, ri) in res:
                    pg.op('pe', lambda e, bs=bs, h=h, ri=ri, n=n: e.matmul(
                        banks[bs][:, 0:n], lhsT=diag[:, h, :], rhs=rbuf[ri][:, 0:n], start=(h == 0), stop=(h == 7)),
                        reads=[B('diag', h), B('rbuf', ri)], writes=[PB[bs]])
                if hp_ == 3:
                    act_copy(sc[:, c0:c1], banks[bs][:, 0:n], [PB[bs]], [B('score', par, c0)])

            prev = None
            for it in items:
                res = emit_x(it)
                if prev is not None:
                    emit_d(*prev)
                prev = (it, res)
                if tick is not None:
                    tick()
            emit_d(*prev)
            bk = nxt('rot4', ROT4)

            def mmq(e, bk=bk, qc=qc):
                last = None
                for m in range(4):
                    for kc in range(KC):
                        last = e.matmul(banks[bk][:, m * 128:(m + 1) * 128], lhsT=w_q[:, kc, m * 128:(m + 1) * 128],
                                        rhs=uT[:, kc, qc:qc + 128], start=(kc == 0), stop=(kc == KC - 1))
                return last
            pg.op('pe', mmq, reads=ub + [B('w_q')], writes=[PB[bk]])
            act_copy(qT_t, banks[bk][:, 0:512].rearrange("p (a b) -> p a b", a=4), [PB[bk]], [B('qT_t')])
            for half in range(2):
                bk = nxt('rot4', ROT4)

                def mma(e, bk=bk, half=half):
                    last = None
                    for hh in range(4):
                        h = 4 * half + hh
                        last = e.matmul(banks[bk][:, hh * 128:(hh + 1) * 128], lhsT=ukT_pad[:, h, :],
                                        rhs=qT_t[:, h // 2, :], start=True, stop=True)
                    return last
                pg.op('pe', mma, reads=ukb + [B('qT_t')], writes=[PB[bk]])
                src3 = banks[bk][:, 0:512].rearrange("p (a b) -> p a b", a=4)
                act_copy(qa[:, 4 * half:4 * half + 4, :], src3, [PB[bk]], [B('qabsT_t', par3, half)])
                pg.op('dve', lambda e, half=half, qa=qa: e.scalar_tensor_tensor(
                    out=absq[:, 4 * half:4 * half + 4, :], in0=qa[:, 4 * half:4 * half + 4, :], scalar=-1.0,
                    in1=qa[:, 4 * half:4 * half + 4, :], op0=ALU.mult, op1=ALU.max),
                    reads=[B('qabsT_t', par3, half)], writes=[B('absq', half)])

        def stageA2(j):
            par = j % 3
            for half in range(2):
                bk = nxt('rot4', ROT4)

                def mmb(e, bk=bk, half=half):
                    last = None
                    for hh in range(4):
                        last = e.matmul(banks[bk][:, hh * 128:(hh + 1) * 128], lhsT=cmaxB,
                                        rhs=absq[:, 4 * half + hh, :], start=True, stop=True)
                    return last
                pg.op('pe', mmb, reads=[B('cmaxB'), B('absq', half)], writes=[PB[bk]])
                pg.op('dve', lambda e, bk=bk, half=half: e.tensor_reduce(
                    out=negsh[:, 4 + half:5 + half], in_=banks[bk][:, 0:512], axis=AX.X, op=ALU.max),
                    reads=[PB[bk]], writes=[B('negsh_t', half)])
            pg.op('dve', lambda e: e.tensor_tensor(out=negsh[:, 6:7], in0=negsh[:, 4:5], in1=negsh[:, 5:6], op=ALU.max),
                  reads=[B('negsh_t', 0), B('negsh_t', 1)], writes=[B('negsh_m')])
            pg.op('dve', lambda e, par=par: e.tensor_scalar(out=negsh[:, par:par + 1], in0=negsh[:, 6:7],
                                                            scalar1=-ATTN_SCALE, scalar2=None, op0=ALU.mult),
                  reads=[B('negsh_m')], writes=[B('negsh', par)])

        def scbufs(j):
            return [B('score', j % 2, c0) for (c0, c1) in chunks_of(j)]

        def stageB_init(j):
            S = 16 + 128 * (j + 1)
            sc = score2[j % 2]
            scb = scbufs(j)
            Rr, Rb = stat2()
            pg.op('dve', lambda e: e.tensor_reduce(out=Rr, in_=sc[:, 0:S], axis=AX.X, op=ALU.max,
                                                   apply_absolute_value=True),
                  reads=scb, writes=Rb)
            pg.op('dve', lambda e: e.tensor_tensor(out=sc[:, S - 128:S], in0=sc[:, S - 128:S],
                                                   in1=blockbias, op=ALU.add),
                  reads=scb + [B('blockbias')], writes=[B('score', j % 2, 16 + 512 * (j // 4))])
            pg.op('dve', lambda e: e.tensor_scalar(out=Wc, in0=pow2, scalar1=Rr, scalar2=1e-30,
                                                   op0=ALU.mult, op1=ALU.add),
                  reads=Rb + [B('pow2')], writes=[B('Wc')])
            pg.op('dve', lambda e: e.memset(mids[:, 0:1], 0.0), writes=[B('mid', 0)])

        def stageB_iter(j, n_):
            S = 16 + 128 * (j + 1)
            sc = score2[j % 2]
            pg.op('dve', lambda e: e.tensor_scalar(
                out=junkbf[:, 0:S], in0=sc[:, 0:S], scalar1=mids[:, n_ - 1:n_], scalar2=None,
                op0=ALU.is_ge, op1=ALU.add, accum_out=cnts[:, n_ - 1:n_]),
                reads=scbufs(j) + [B('mid', n_ - 1)], writes=[B('junkbf'), B('cnt', n_)])
            pg.op('dve', lambda e: e.scalar_tensor_tensor(
                out=dds[:, n_ - 1:n_], in0=cnts[:, n_ - 1:n_], scalar=KTOP - 0.5, in1=Wc[:, n_ - 1:n_],
                op0=ALU.is_ge, op1=ALU.mult),
                reads=[B('cnt', n_), B('Wc')], writes=[B('dd', n_)])
            pg.op('dve', lambda e: e.scalar_tensor_tensor(
                out=mids[:, n_:n_ + 1], in0=dds[:, n_ - 1:n_], scalar=Wc[:, n_:n_ + 1], in1=mids[:, n_ - 1:n_],
                op0=ALU.subtract, op1=ALU.add),
                reads=[B('dd', n_), B('Wc'), B('mid', n_ - 1)], writes=[B('mid', n_)])

        def stageB_fin(j):
            pg.op('dve', lambda e: e.tensor_tensor(out=thr[:, 0:1], in0=mids[:, NIT:NIT + 1], in1=Wc[:, NIT:NIT + 1],
                                                   op=ALU.subtract),
                  reads=[B('mid', NIT), B('Wc')], writes=[B('thr')])
            S = 16 + 128 * (j + 1)
            sc = score2[j % 2]
            mbj = mb2[j % 2]
            pg.op('dve', lambda e: e.tensor_scalar(out=mbj[:, 0:S], in0=sc[:, 0:S], scalar1=thr[:, 0:1],
                                                   scalar2=NEG, op0=ALU.is_lt, op1=ALU.mult),
                  reads=scbufs(j) + [B('thr')], writes=[B('mb', j % 2)])

        def stageC(j):
            mbj = mb2[j % 2]
            klist = [16] + list(range(j + 1))
            for g0 in range(0, len(klist), 8):
                grp = klist[g0:g0 + 8]
                bk = nxt('rot4', ROT4)

                def tp(e, grp=grp, bk=bk):
                    last = None
                    for si, i in enumerate(grp):
                        c_ = 0 if i == 16 else 16 + 128 * i
                        last = e.transpose(out=banks_bf[bk][:, si * 128:(si + 1) * 128], in_=mbj[:, c_:c_ + 128],
                                           identity=ident_bf)
                    return last
                pg.op('pe', tp, reads=[B('mb', j % 2), B('ident_bf')], writes=[PB[bk]])
                si0 = 0
                if grp[0] == 16:
                    pg.op('dve', lambda e, bk=bk: e.tensor_copy(out=mbT[0:16, 16, :], in_=banks_bf[bk][0:16, 0:128]),
                          reads=[PB[bk]], writes=[B('mbT', 16)])
                    si0 = 1
                nn_ = len(grp) - si0
                if nn_ > 0:
                    i0 = grp[si0]
                    srcv = banks_bf[bk][:, si0 * 128:(si0 + nn_) * 128].rearrange("p (a b) -> p a b", a=nn_)
                    pg.op('dve', lambda e, i0=i0, nn_=nn_, srcv=srcv: e.tensor_copy(out=mbT[:, i0:i0 + nn_, :], in_=srcv),
                          reads=[PB[bk]], writes=[B('mbT', i) for i in range(i0, i0 + nn_)])

        def stageD_lg(j, q, ci):
            par = j % 3
            qa = qabs2[par]
            xch = [16] + list(range(j + 1))
            i = xch[ci]
            bl = nxt('rot4', ROT4)
            kc0 = 0 if i == 16 else 16 + 128 * i
            mrow = mk_ap(mbT, i * 128, [[pstep(mbT), 128], [0, 4], [1, 128]])
            qrhs = qa[:, 4 * q:4 * q + 4, :]

            def lg(e):
                e.matmul(banks[bl][:, 0:512], lhsT=cnT[:, kc0:kc0 + 128], rhs=qrhs, start=True, stop=False)
                return e.matmul(banks[bl][:, 0:512], lhsT=ident_bf, rhs=mrow, start=False, stop=True)
            pg.op('pe', lg, reads=cnb + [B('qabsT_t', par, q), B('ident_bf'), B('mbT', i)], writes=[PB[bl]])
            pi = nxt('pT', [0, 1, 2])
            pg.op('act', lambda e: e.activation(out=pT[pi][:, 0:512], in_=banks[bl][:, 0:512], func=AF.Exp,
                                                scale=ATTN_SCALE, bias=negsh[:, par:par + 1]),
                  reads=[PB[bl], B('negsh', par)], writes=[B('pT', pi)])
            return pi

        def stageD_pv(j, q, ci, pi):
            xch = [16] + list(range(j + 1))
            i = xch[ci]
            bO, bS = ACC
            first, last_ = (ci == 0), (ci == len(xch) - 1)

            def pv(e):
                e.matmul(banks[bO][:, 0:512], lhsT=ckeys[:, i, 0:128], rhs=pT[pi][:, 0:512], start=first, stop=last_)
                return e.matmul(banks[bS][:, 0:512], lhsT=ones_bf, rhs=pT[pi][:, 0:512], start=first, stop=last_)
            pg.op('pe', pv, reads=[B('pT', pi), B('ones_bf'), B('ckeys', i)], writes=[PB[bO], PB[bS]])

        def stageD_qfin(j, q):
            bO, bS = ACC
            pg.op('act', lambda e: e.activation(out=lnS, in_=banks[bS][:, 0:512], func=AF.Ln),
                  reads=[PB[bS]], writes=[B('lnS')])
            pg.op('act', lambda e: e.activation(out=rcpS, in_=lnS, func=AF.Exp, scale=-1.0),
                  reads=[B('lnS')], writes=[B('rcpS')])
            pg.op('dve', lambda e: e.tensor_tensor(
                out=olatT_t[:, 4 * q:4 * q + 4, :], in0=banks[bO][:, 0:512].rearrange("p (a b) -> p a b", a=4),
                in1=rcpS.rearrange("p (a b) -> p a b", a=4), op=ALU.mult),
                reads=[PB[bO], B('rcpS')], writes=[B('olatT_t', q)])

        def stageD_fin(j):
            bk = nxt('rot4', ROT4)

            def mmo(e, bk=bk):
                last = None
                for m in range(4):
                    o = banks[bk][:, m * 128:(m + 1) * 128]
                    e.matmul(o, lhsT=uv_pad[:, 2 * m, :], rhs=olatT_t[:, 2 * m, :], start=True, stop=False)
                    last = e.matmul(o, lhsT=uv_pad[:, 2 * m + 1, :], rhs=olatT_t[:, 2 * m + 1, :], start=False, stop=True)
                return last
            pg.op('pe', mmo, reads=uvb + [B('olatT_t', 0), B('olatT_t', 1)], writes=[PB[bk]])
            act_copy(attnT[:, :, 128 * j:128 * (j + 1)], banks[bk][:, 0:512].rearrange("p (a b) -> p a b", a=4),
                     [PB[bk]], [B('attnT', j)])

        nA = n_attn_tiles

        def fullB(j):
            stageB_init(j)
            if 16 + 128 * (j + 1) <= KTOP:
                pg.op('dve', lambda e: e.tensor_reduce(out=cnts[:, 0:1], in_=Wc[:, 1:NIT + 1], axis=AX.X, op=ALU.add),
                      reads=[B('Wc')], writes=[B('cnt', 1)])
                pg.op('dve', lambda e: e.tensor_scalar(out=mids[:, NIT:NIT + 1], in0=cnts[:, 0:1], scalar1=-1.0,
                                                       scalar2=None, op0=ALU.mult),
                      reads=[B('cnt', 1)], writes=[B('mid', NIT)])
            else:
                for n_ in range(1, NIT + 1):
                    stageB_iter(j, n_)
            stageB_fin(j)

        if nA > 2:
            stageA(0)
            fullB(0)
            stageA(1)
            stageB_init(1)
            stageA2(0)
            for n_ in (1, 2, 3):
                stageB_iter(1, n_)
            stageA2(1)
            for n_ in (4, 5, 6):
                stageB_iter(1, n_)
            stageC(0)
            for n_ in (7, 8):
                stageB_iter(1, n_)
            assert 4 * len(chunks_of(2)) == 8
            pro_n = [8]

            def pro_tick():
                pro_n[0] += 1
                if pro_n[0] <= NIT:
                    stageB_iter(1, pro_n[0])
            stageA(2, pro_tick)
            for n_ in range(pro_n[0] + 1, NIT + 1):
                stageB_iter(1, n_)
            stageA2(2)
            stageB_fin(1)
        elif nA > 0:
            stageA(0)
            fullB(0)
            if nA > 1:
                stageA(1)
            stageA2(0)
            if nA > 1:
                stageA2(1)
            stageC(0)
            if nA > 1:
                fullB(1)
        for i in range(nA):
            hasB = i + 2 < nA
            hasA = i + 3 < nA
            nch = i + 2
            seq = [(q, ci) for q in range(2) for ci in range(nch)]
            steps = len(seq)
            n_items = 4 * len(chunks_of(i + 3)) if hasA else 0
            wD, wA = 1.0 * steps, 1.7 * n_items
            ticks = [('D', k) for k in range(steps)] + [('A', k) for k in range(n_items)]
            wts = [1.0] * steps + [1.7] * n_items
            tot = sum(wts)
            sched = {}
            acc, done = 0.0, 0
            for tk, w in zip(ticks, wts):
                acc += w
                upto = int(round(NIT * acc / tot))
                sched[tk] = list(range(done + 1, upto + 1))
                done = upto
            if hasB:
                stageB_init(i + 2)
            pis = {}
            pis[0] = stageD_lg(i, *seq[0])
            for k, (q, ci) in enumerate(seq):
                if k + 1 < steps:
                    pis[k + 1] = stageD_lg(i, *seq[k + 1])
                stageD_pv(i, q, ci, pis[k])
                if hasB:
                    for n_ in sched[('D', k)]:
                        stageB_iter(i + 2, n_)
                if ci == nch - 1:
                    stageD_qfin(i, q)
            stageD_fin(i)
            if hasA:
                cnt = [0]

                def tick(cnt=cnt):
                    if hasB:
                        for n_ in sched[('A', cnt[0])]:
                            stageB_iter(i + 2, n_)
                    cnt[0] += 1
                stageA(i + 3, tick)
            if hasB:
                stageB_fin(i + 2)
            if i + 1 < nA:
                stageC(i + 1)
            if hasA:
                stageA2(i + 3)

        pg.barrier()
        reset('R2')
        reset('R3')
        mergedT = carve('R3', BF16, [P, 8, TX])
        wgA2 = [carve('R2', BF16, [P, KC, 256]) for _ in range(2)]
        wgP2 = [carve('R2', BF16, [P, KC, 256]) for _ in range(2)]
        w_out = carve('R2', BF16, [P, 8, D])
        off_wout_end = cursor['R2']
        sA = [carve('R2', F32, [P, 512]) for _ in range(2)]
        sP = [carve('R2', F32, [P, 512]) for _ in range(2)]
        run_tail = stop_after >= 5

        def load_wg(grp):
            s_ = grp % 2
            pg.dma('pool', wgA2[s_], win_p[:, :, 1448 + 256 * grp:1448 + 256 * (grp + 1)], writes=[B('wgA', s_)])
            pg.dma('pool', wgP2[s_], win_p[:, :, 2472 + 256 * grp:2472 + 256 * (grp + 1)], writes=[B('wgP', s_)])

        if run_tail:
            load_wg(0)
            load_wg(1)
            pg.dma('pool', w_out, wout_d.rearrange("(m p) n -> p m n", p=128), writes=[B('w_out')])
            for grp in range(4):
                gs = grp % 2
                wgA, wgP = wgA2[gs], wgP2[gs]
                if 1 <= grp < 3:
                    load_wg(grp + 1)
                for n4 in range(2):
                    nn = 2 * grp + n4
                    for b in range(4):
                        bset = nxt('tailA', [[0, 1, 2, 3], [4, 5, 6, 7]])
                        c0 = 16 + 512 * b
                        bA, bP, bgA, bgP = bset

                        def mmA(e, bA=bA, nn=nn, b=b):
                            last = None
                            for m in range(4):
                                last = e.matmul(banks[bA][:, 0:512], lhsT=w_ba[:, m, nn * 128:(nn + 1) * 128],
                                                rhs=attnT[:, m, 512 * b:512 * (b + 1)], start=(m == 0), stop=(m == 3))
                            return last
                        pg.op('pe', mmA, reads=[B('w_ba')] + [B('attnT', 4 * b + q) for q in range(4)], writes=[PB[bA]])

                        def mmP(e, bP=bP, nn=nn, b=b):
                            last = None
                            for m in range(4):
                                last = e.matmul(banks[bP][:, 0:512], lhsT=w_bp[:, m, nn * 128:(nn + 1) * 128],
                                                rhs=poolT[:, m, 512 * b:512 * (b + 1)], start=(m == 0), stop=(m == 3))
                            return last
                        pg.op('pe', mmP, reads=[B('w_bp'), B('poolT', b)], writes=[PB[bP]])

                        def mmg(e, bk, wt, n4=n4, c0=c0):
                            last = None
                            for kc in range(KC):
                                last = e.matmul(banks[bk][:, 0:512], lhsT=wt[:, kc, n4 * 128:(n4 + 1) * 128],
                                                rhs=uT[:, kc, c0:c0 + 512], start=(kc == 0), stop=(kc == KC - 1))
                            return last
                        pg.op('pe', lambda e, bgA=bgA, f=mmg, wgA=wgA: f(e, bgA, wgA),
                              reads=uT_bufs(c0, c0 + 512) + [B('wgA', gs)], writes=[PB[bgA]])
                        pg.op('pe', lambda e, bgP=bgP, f=mmg, wgP=wgP: f(e, bgP, wgP),
                              reads=uT_bufs(c0, c0 + 512) + [B('wgP', gs)], writes=[PB[bgP]])
                        si = nxt('sAP', [0, 1])
                        pg.op('act', lambda e, bgA=bgA, si=si: e.activation(out=sA[si], in_=banks[bgA][:, 0:512], func=AF.Sigmoid),
                              reads=[PB[bgA]], writes=[B('sA', si)])
                        pg.op('act', lambda e, bgP=bgP, si=si: e.activation(out=sP[si], in_=banks[bgP][:, 0:512], func=AF.Sigmoid),
                              reads=[PB[bgP]], writes=[B('sP', si)])
                        pg.op('dve', lambda e, bA=bA, si=si: e.tensor_tensor(out=sA[si], in0=sA[si], in1=banks[bA][:, 0:512], op=ALU.mult),
                              reads=[B('sA', si), PB[bA]], writes=[B('sA', si)])
                        pg.op('dve', lambda e, bP=bP, si=si: e.tensor_tensor(out=sP[si], in0=sP[si], in1=banks[bP][:, 0:512], op=ALU.mult),
                              reads=[B('sP', si), PB[bP]], writes=[B('sP', si)])
                        pg.op('dve', lambda e, si=si, nn=nn, b=b: e.tensor_tensor(
                            out=mergedT[:, nn, 512 * b:512 * (b + 1)], in0=sA[si], in1=sP[si], op=ALU.add),
                            reads=[B('sA', si), B('sP', si)], writes=[B('mergedT', nn, b)])

        pg.barrier()
        reset('R1')
        reset('R2')
        hacc = carve('R1', F32, [P, NT, D])
        w_r = carve('R2', BF16, [P, KC, 36])
        rbias = carve('R2', F32, [P, 36])
        gate_all = carve('R2', F32, [P, NT, 32])
        rt_ = carve('R2', F32, [P, 256])
        cur_save = cursor['R2']
        lg_all = carve('R2', F32, [P, NT, 36])
        ssq = carve('R2', F32, [P, NT])
        rstd2 = carve('R2', F32, [P, NT])
        rv = carve('R2', F32, [P, 1600])
        assert cursor['R2'] <= off_wout_end - 16384, cursor['R2']
        if run_tail:
            pg.dma('pool', w_r[:, :, 0:4], wgr_d.rearrange("(p k) n -> p k n", k=KC), writes=[B('w_r', 0)])
            pg.dma('pool', w_r[:, :, 4:36], wer_d.rearrange("(p k) n -> p k n", k=KC), writes=[B('w_r', 1)])
            pg.dma('sp', rbias[:, 0:4], bgr_d.partition_broadcast(128), writes=[B('rbias', 0)])
            pg.dma('sp', rbias[:, 4:36], ber_d.partition_broadcast(128), writes=[B('rbias', 1)])
            pg.dma('sp', gbc[1], g2_d.partition_broadcast(128), writes=[B('gbc', 1)])
            for j in range(NT):
                b = j // 4
                k = nxt('xt', [0, 1])
                pg.dma('sp', xt[k], x_d[128 * j:128 * (j + 1), :], writes=[B('xt', k)])
                for hh in range(2):
                    bk = nxt('tailB', [0, 1, 2, 3])

                    def mmh(e, bk=bk, j=j, hh=hh):
                        last = None
                        for nn in range(8):
                            last = e.matmul(banks[bk][:, 0:512], lhsT=mergedT[:, nn, 128 * j:128 * (j + 1)],
                                            rhs=w_out[:, nn, 512 * hh:512 * (hh + 1)], start=(nn == 0), stop=(nn == 7))
                        return last
                    pg.op('pe', mmh, reads=[B('mergedT', nn, b) for nn in range(8)] + [B('w_out')] + [B('R3a', q) for q in range(6)],
                          writes=[PB[bk]])
                    pg.op('dve', lambda e, bk=bk, j=j, hh=hh, k=k: e.tensor_tensor(
                        out=hacc[:, j, 512 * hh:512 * (hh + 1)], in0=banks[bk][:, 0:512], in1=xt[k][:, 512 * hh:512 * (hh + 1)],
                        op=ALU.add), reads=[PB[bk], B('xt', k)], writes=[B('hacc', j, hh)])
                pg.op('act', lambda e, j=j: e.activation(out=junkbf[:, 0:D], in_=hacc[:, j, :], func=AF.Square,
                                                         accum_out=ssq[:, j:j + 1]),
                      reads=[B('hacc', j, 0), B('hacc', j, 1)], writes=[B('junkbf'), B('ssq', j)])
            pg.op('act', lambda e: e.activation(out=rstd2, in_=ssq, func=AF.Sqrt, scale=1.0 / D, bias=eps_t),
                  reads=[B('ssq', j) for j in range(NT)] + [B('eps')], writes=[B('rstd2s')])
            pg.op('dve', lambda e: e.reciprocal(out=rstd2, in_=rstd2), reads=[B('rstd2s')], writes=[B('rstd2')])
            def s2_front(j):
                k = nxt('xn', [0, 1])
                pg.op('dve', lambda e: e.scalar_tensor_tensor(
                    out=xn[k], in0=hacc[:, j, :], scalar=rstd2[:, j:j + 1], in1=gbc[1], op0=ALU.mult, op1=ALU.mult),
                    reads=[B('hacc', j, 0), B('hacc', j, 1), B('rstd2'), B('gbc', 1)], writes=[B('xn', k)])
                bk = nxt('tp', [4, 5])

                def tps(e):
                    last = None
                    for kc in range(KC):
                        last = e.transpose(out=banks_bf[bk][:, kc * 128:(kc + 1) * 128],
                                           in_=xn[k].rearrange("t (p k) -> t k p", k=KC)[:, kc, :], identity=ident_bf)
                    return last
                pg.op('pe', tps, reads=[B('xn', k), B('ident_bf')], writes=[PB[bk]])
                srcv = banks_bf[bk][:, 0:1024].rearrange("p (a b) -> p a b", a=KC)
                pg.op('act', lambda e: e.copy(out=uT[:, :, 16 + 128 * j:16 + 128 * (j + 1)], in_=srcv),
                      reads=[PB[bk]], writes=[B('uT', j)])

            s2_bk = {}

            def s2_back(j):
                bk2 = nxt('rt', [6, 7])
                s2_bk[j] = bk2

                def mmr(e):
                    last = None
                    for kc in range(KC):
                        last = e.matmul(banks[bk2][:, 0:36], lhsT=uT[:, kc, 16 + 128 * j:16 + 128 * (j + 1)],
                                        rhs=w_r[:, kc, :], start=(kc == 0), stop=(kc == KC - 1))
                    return last
                pg.op('pe', mmr, reads=[B('uT', j), B('w_r', 0), B('w_r', 1)], writes=[PB[bk2]])

            def s2_add(j):
                bk2 = s2_bk[j]
                pg.op('dve', lambda e: e.tensor_tensor(out=lg_all[:, j, :], in0=banks[bk2][:, 0:36], in1=rbias,
                                                       op=ALU.add),
                      reads=[PB[bk2], B('rbias', 0), B('rbias', 1)], writes=[B('lg_all', j)])

            for t_ in range(NT + 3):
                if t_ >= 3:
                    s2_add(t_ - 3)
                if 2 <= t_ < NT + 2:
                    s2_back(t_ - 2)
                if t_ < NT:
                    s2_front(t_)
            RV = B('rv')
            off = [0]

            def rvv(n):
                a_ = rv[:, off[0]:off[0] + n]
                off[0] += n
                return a_
            gmax, gsum, pgp, m1, m2, dm, ed, den, p1, p2 = [rvv(NT) for _ in range(10)]
            goh, gex = rvv(NT * 4), rvv(NT * 4)
            esel, oh1, esel2, oh2, t1, t2 = [rvv(NT * 8) for _ in range(6)]
            tmp32 = rvv(NT * 32)
            pl = pstep(lg_all)
            pr = pstep(rv)

            def v3(ap, n):
                return ap.rearrange("p (a b) -> p a b", a=NT)

            def bc_last(ap, n):
                return mk_ap(ap, 0, [[pr, 128], [1, NT], [0, n]])
            gl = lg_all[:, :, 0:4]
            el4 = mk_ap(lg_all, 4, [[pl, 128], [36, NT], [8, 4], [1, 8]])
            lgb = [B('lg_all', j) for j in range(NT)]

            def dv(fn, extra=()):
                pg.op('dve', fn, reads=[RV] + list(extra), writes=[RV])
            dv(lambda e: e.tensor_reduce(out=gmax, in_=gl, axis=AX.X, op=ALU.max), lgb)
            dv(lambda e: e.tensor_tensor(out=v3(goh, 4), in0=gl, in1=bc_last(gmax, 4), op=ALU.is_ge), lgb)
            dv(lambda e: e.tensor_tensor(out=v3(gex, 4), in0=gl, in1=bc_last(gmax, 4), op=ALU.subtract), lgb)
            pg.op('act', lambda e: e.activation(out=gex, in_=gex, func=AF.Exp), reads=[RV], writes=[RV])
            dv(lambda e: e.tensor_reduce(out=gsum, in_=v3(gex, 4), axis=AX.X, op=ALU.add))
            dv(lambda e: e.reciprocal(out=pgp, in_=gsum))
            goh4 = mk_ap(goh, 0, [[pr, 128], [4, NT], [1, 4], [0, 8]])
            t4 = mk_ap(tmp32, 0, [[pr, 128], [32, NT], [8, 4], [1, 8]])
            dv(lambda e: e.tensor_tensor(out=t4, in0=el4, in1=goh4, op=ALU.mult), lgb)
            t4T = mk_ap(tmp32, 0, [[pr, 128], [32, NT], [1, 8], [8, 4]])
            dv(lambda e: e.tensor_reduce(out=v3(esel, 8), in_=t4T, axis=AX.X, op=ALU.add))
            dv(lambda e: e.tensor_reduce(out=m1, in_=v3(esel, 8), axis=AX.X, op=ALU.max))
            dv(lambda e: e.tensor_tensor(out=v3(oh1, 8), in0=v3(esel, 8), in1=bc_last(m1, 8), op=ALU.is_ge))
            dv(lambda e: e.scalar_tensor_tensor(out=esel2, in0=oh1, scalar=-BIG, in1=esel, op0=ALU.mult, op1=ALU.add))
            dv(lambda e: e.tensor_reduce(out=m2, in_=v3(esel2, 8), axis=AX.X, op=ALU.max))
            dv(lambda e: e.tensor_tensor(out=v3(oh2, 8), in0=v3(esel2, 8), in1=bc_last(m2, 8), op=ALU.is_ge))
            dv(lambda e: e.tensor_tensor(out=dm, in0=m2, in1=m1, op=ALU.subtract))
            pg.op('act', lambda e: e.activation(out=ed, in_=dm, func=AF.Exp), reads=[RV], writes=[RV])
            dv(lambda e: e.tensor_scalar(out=den, in0=ed, scalar1=1.0, scalar2=None, op0=ALU.add))
            dv(lambda e: e.reciprocal(out=p1, in_=den))
            dv(lambda e: e.tensor_tensor(out=p2, in0=ed, in1=p1, op=ALU.mult))
            dv(lambda e: e.tensor_tensor(out=p1, in0=p1, in1=pgp, op=ALU.mult))
            dv(lambda e: e.tensor_tensor(out=p2, in0=p2, in1=pgp, op=ALU.mult))
            dv(lambda e: e.tensor_tensor(out=v3(t1, 8), in0=v3(oh1, 8), in1=bc_last(p1, 8), op=ALU.mult))
            dv(lambda e: e.tensor_tensor(out=v3(t2, 8), in0=v3(oh2, 8), in1=bc_last(p2, 8), op=ALU.mult))
            dv(lambda e: e.tensor_tensor(out=t1, in0=t1, in1=t2, op=ALU.add))
            wpg4 = mk_ap(t1, 0, [[pr, 128], [8, NT], [0, 4], [1, 8]])
            gout = gate_all.rearrange("p a (g e) -> p a g e", g=4)
            pg.op('dve', lambda e: e.tensor_tensor(out=gout, in0=goh4, in1=wpg4, op=ALU.mult),
                  reads=[RV], writes=[B('gate', j) for j in range(NT)])
        cursor['R2'] = cur_save

        reset('R3')
        n_exp = debug.get('n_exp', N_EXP if stop_after >= 6 else 0)
        wg_e = [carve('R3', BF16, [P, KC, 256]) for _ in range(2)]
        wu_e = [carve('R3', BF16, [P, KC, 256]) for _ in range(2)]
        wd_e = [carve('R3', BF16, [P, 2, D]) for _ in range(2)]
        silt = [carve('R3', F32, [P, 512]) for _ in range(2)]
        hid = [carve('R2', BF16, [P, 2, TX]) for _ in range(2)]

        def load_gu(e_):
            s = e_ % 2
            al = (lambda q: [B('R3a', q)]) if e_ < 2 else (lambda q: [])
            pg.dma('pool', wg_e[s], weg_d[e_].rearrange("(p k) n -> p k n", k=KC), writes=[B('wg_e', s)] + al(2 * s))
            pg.dma('pool', wu_e[s], weu_d[e_].rearrange("(p k) n -> p k n", k=KC), writes=[B('wu_e', s)] + al(2 * s + 1))

        def load_d(e_):
            s = e_ % 2
            pg.dma('pool', wd_e[s], wed_d[e_].rearrange("(f p) n -> p f n", p=128),
                   writes=[B('wd_e', s)] + ([B('R3a', 4 + s)] if e_ < 2 else []))

        def gu_group(e_, b, fh):
            s = e_ % 2
            c0 = 16 + 512 * b
            bG, bU = nxt('moeGU', [[0, 1], [2, 3]])

            def mmGU(e, bk, wt):
                last = None
                for kc in range(KC):
                    last = e.matmul(banks[bk][:, 0:512], lhsT=wt[:, kc, fh * 128:(fh + 1) * 128],
                                    rhs=uT[:, kc, c0:c0 + 512], start=(kc == 0), stop=(kc == KC - 1))
                return last
            pg.op('pe', lambda e: mmGU(e, bG, wg_e[s]), reads=uT_bufs(c0, c0 + 512) + [B('wg_e', s)],
                  writes=[PB[bG]])
            pg.op('pe', lambda e: mmGU(e, bU, wu_e[s]), reads=uT_bufs(c0, c0 + 512) + [B('wu_e', s)],
                  writes=[PB[bU]])
            si = nxt('silt', [0, 1])
            pg.op('act', lambda e: e.activation(out=silt[si], in_=banks[bG][:, 0:512], func=AF.Silu),
                  reads=[PB[bG]], writes=[B('silt', si)])
            pg.op('dve', lambda e: e.tensor_tensor(
                out=hid[s][:, fh, 512 * b:512 * (b + 1)], in0=silt[si], in1=banks[bU][:, 0:512], op=ALU.mult),
                reads=[B('silt', si), PB[bU]], writes=[B('hid', s, b)])

        def y_tile(e_, j):
            s = e_ % 2
            for hh in range(2):
                bk = nxt('moeY', [4, 5, 6, 7])

                def mmy(e, bk=bk, hh=hh):
                    e.matmul(banks[bk][:, 0:512], lhsT=hid[s][:, 0, 128 * j:128 * (j + 1)],
                             rhs=wd_e[s][:, 0, 512 * hh:512 * (hh + 1)], start=True, stop=False)
                    return e.matmul(banks[bk][:, 0:512], lhsT=hid[s][:, 1, 128 * j:128 * (j + 1)],
                                    rhs=wd_e[s][:, 1, 512 * hh:512 * (hh + 1)], start=False, stop=True)
                pg.op('pe', mmy, reads=[B('hid', s, j // 4), B('wd_e', s)], writes=[PB[bk]])
                pg.op('dve', lambda e, bk=bk, hh=hh: e.scalar_tensor_tensor(
                    out=hacc[:, j, 512 * hh:512 * (hh + 1)], in0=banks[bk][:, 0:512], scalar=gate_all[:, j, e_:e_ + 1],
                    in1=hacc[:, j, 512 * hh:512 * (hh + 1)], op0=ALU.mult, op1=ALU.add),
                    reads=[PB[bk], B('gate', j), B('hacc', j, hh)], writes=[B('hacc', j, hh)])

        if n_exp > 0:
            load_gu(0)
            load_d(0)
        for e_ in range(n_exp + 1 if n_exp > 0 else 0):
            if e_ + 1 < n_exp:
                load_gu(e_ + 1)
            for gi in range(8):
                if e_ < n_exp:
                    gu_group(e_, gi // 2, gi % 2)
                if e_ >= 1:
                    y_tile(e_ - 1, 2 * gi)
                    y_tile(e_ - 1, 2 * gi + 1)
            if e_ + 1 < n_exp:
                load_d(e_ + 1)

        if run_tail:
            pg.dma('sp', gbc[0], gf_d.partition_broadcast(128), writes=[B('gbc', 0)])
            for j in range(NT):
                hb = [B('hacc', j, 0), B('hacc', j, 1)]
                pg.op('act', lambda e, j=j: e.activation(out=junkbf[:, 0:D], in_=hacc[:, j, :], func=AF.Square,
                                                         accum_out=ssq[:, j:j + 1]),
                      reads=hb, writes=[B('ssq', j), B('junkbf')])
            pg.op('act', lambda e: e.activation(out=rstd2, in_=ssq, func=AF.Sqrt, scale=1.0 / D, bias=eps_t),
                  reads=[B('ssq', j) for j in range(NT)] + [B('eps')], writes=[B('rstd2s')])
            pg.op('dve', lambda e: e.reciprocal(out=rstd2, in_=rstd2), reads=[B('rstd2s')], writes=[B('rstd2')])
            obuf = [carve_at('R0', 4096 * i, F32, [P, D]) for i in range(8)]
            for j in range(NT):
                hb = [B('hacc', j, 0), B('hacc', j, 1)]
                k = j % 8
                if j % 3 == 2:
                    pg.op('act', lambda e, j=j, k=k: e.activation(
                        out=obuf[k], in_=hacc[:, j, :], func=AF.Identity, scale=rstd2[:, j:j + 1]),
                        reads=hb + [B('rstd2')], writes=[B('obuf', k)])
                    pg.op('pool', lambda e, k=k: e.tensor_tensor(out=obuf[k], in0=obuf[k], in1=gbc[0], op=ALU.mult),
                          reads=[B('obuf', k), B('gbc', 0)], writes=[B('obuf', k)])
                else:
                    pg.op('dve', lambda e, j=j, k=k: e.scalar_tensor_tensor(
                        out=obuf[k], in0=hacc[:, j, :], scalar=rstd2[:, j:j + 1], in1=gbc[0], op0=ALU.mult, op1=ALU.mult),
                        reads=hb + [B('rstd2'), B('gbc', 0)], writes=[B('obuf', k)])
                pg.dma('sp', out_d[128 * j:128 * (j + 1), :], obuf[k], reads=[B('obuf', k)], writes=[B('out', j)])

        pg.barrier()
        local = dict(uT=uT, ckeys=ckeys, cnT=cnT, ikT=ikT, poolT=poolT, attnT=attnT, iw_all=iw_all, score=score,
                     mb=mb, hacc=hacc, gate_all=gate_all, mergedT=mergedT, qabsT_t=qabsT_t,
                     bis=bis, thr=thr, iqT_t=iqT_t, diag=diag, mbT=mbT)
        for name in debug.get('dump', []):
            ap = local[name]
            shp = list(ap.shape)
            n = 1
            for s_ in shp[1:]:
                n *= s_
            dd = nc.dram_tensor("dbg_" + name, [shp[0], n], F32, kind="ExternalOutput").ap()
            flat = ap
            if len(shp) == 3:
                flat = ap.rearrange("p a b -> p (a b)")
            for c0 in range(0, n, 2048):
                c1 = min(n, c0 + 2048)
                pg.dma('pool', dd[:, c0:c1], flat[:, c0:c1], reads=[], writes=[B('dbg', name, c0)])
        pg.barrier()
        pg.emit(esem, dsem)
    return nc


_CACHE = {}


def _consts():
    ident = np.eye(128, dtype=np.float32)
    p = np.arange(128)[:, None]
    s = np.arange(128)[None, :]
    blockbias = np.where((s < 64) | (p >= 64), 0.0, -BIG).astype(np.float32)
    sel8 = np.zeros((32, 8, 128), np.float32)
    for h in range(8):
        sel8[h, h, :] = 1.0
    invcnt = np.broadcast_to((1.0 / np.arange(1, 17, dtype=np.float32))[None, :], (128, 16)).copy()
    pow2 = np.broadcast_to((1.0001 * 2.0 ** (-np.arange(NIT + 2, dtype=np.float64))).astype(np.float32)[None, :],
                           (128, NIT + 2)).copy()
    return dict(c_ident=ident, c_blockbias=blockbias, c_sel8=sel8.reshape(32, 1024), c_invcnt=invcnt, c_pow2=pow2)


def make_in_maps(inputs):
    f = lambda a: np.ascontiguousarray(np.asarray(a, dtype=np.float32))
    shared = dict(
        meta=f(inputs['meta_tokens']),
        norm1_g=f(inputs['norm1_g']).reshape(1, D),
        w_in=f(inputs['w_in']).reshape(D, 3496),
        kv_norm_g=f(inputs['kv_norm_g']).reshape(1, 128),
        w_uk=f(inputs['w_uk']).reshape(128, 512),
        w_uv=f(inputs['w_uv']).reshape(128, 512),
        w_pool=f(inputs['w_pool']).reshape(4, 128, 128),
        pool_scale=f(inputs['pool_scale']).reshape(1, 512),
        w_ba=f(inputs['w_branch_attn']).reshape(512, D),
        w_bp=f(inputs['w_branch_pool']).reshape(512, D),
        w_out=f(inputs['w_out']).reshape(D, D),
        norm2_g=f(inputs['norm2_g']).reshape(1, D),
        w_gr=f(inputs['w_group_router']).reshape(D, 4),
        b_gr=f(inputs['b_group_router']).reshape(1, 4),
        w_er=f(inputs['w_expert_router']).reshape(D, 32),
        b_er=f(inputs['b_expert_router']).reshape(1, 32),
        w_eg=f(inputs['w_expert_gate']).reshape(N_EXP, D, 256),
        w_eu=f(inputs['w_expert_up']).reshape(N_EXP, D, 256),
        w_ed=f(inputs['w_expert_down']).reshape(N_EXP, 256, D),
        final_g=f(inputs['final_norm_g']).reshape(1, D),
    )
    shared.update(_consts())
    x = f(inputs['x'])
    return [dict(shared, x=x[b]) for b in range(8)]


def kernel(**inputs):
    if 'nc' not in _CACHE:
        _CACHE['nc'] = build()
    nc = _CACHE['nc']
    in_maps = make_in_maps(inputs)
    res = run_bass_kernel_spmd(nc, in_maps, core_ids=list(range(8)))
    return np.stack([np.asarray(r["out"], dtype=np.float32).reshape(TX, D) for r in res.results], axis=0)
```

```python
import math
from contextlib import ExitStack

import numpy as np
import concourse.bass as bass
import concourse.mybir as mybir
from concourse.bass_utils import run_bass_kernel_spmd

F32 = mybir.dt.float32
BF16 = mybir.dt.bfloat16
ALU = mybir.AluOpType
AF = mybir.ActivationFunctionType
AX = mybir.AxisListType

P = 128
D = 1024
KC = 8
NT = 16
TX = 2048
T = 2064
EPS = 1e-6
ATTN_SCALE = 64 ** -0.5
IDX_SCALE = (8 ** -0.5) * (32 ** -0.5)
KTOP = 256
NIT = 16
NEG = -30000.0
BIG = 1.0e30
N_EXP = 32
ENGS = ['pe', 'act', 'dve', 'pool', 'sp']
NDS = 24


class Buf:
    __slots__ = ('name', 'lw', 'rd')

    def __init__(self, name):
        self.name = name
        self.lw = None
        self.rd = {}


class Op:
    __slots__ = ('fn', 'waits', 'dma')

    def __init__(self, fn, waits, dma):
        self.fn = fn
        self.waits = waits
        self.dma = dma


class Prog:
    def __init__(self, nc):
        self.nc = nc
        self.ops = {e: [] for e in ENGS}
        self.seen = {e: {} for e in ENGS}
        self.seen_dma = {e: set() for e in ENGS}
        self.dma_info = []
        self.dma_uses = [0] * NDS
        self.dma_hist = {}
        self.bufs = {}

    def B(self, *key):
        b = self.bufs.get(key)
        if b is None:
            b = Buf(key)
            self.bufs[key] = b
        return b

    def _deps(self, reads, writes):
        toks = set()
        for b in reads:
            if b.lw is not None:
                toks.add(b.lw)
        for b in writes:
            if b.lw is not None:
                toks.add(b.lw)
            toks.update(b.rd.values())
        return toks

    def _resolve(self, eng, toks):
        best = {}
        out = []
        for t in toks:
            if t[0] == 'c':
                if t[1] == eng and eng == 'pe':
                    continue
                if best.get(t[1], -1) < t[2]:
                    best[t[1]] = t[2]
            else:
                if t[1] not in self.seen_dma[eng]:
                    self.seen_dma[eng].add(t[1])
                    out.append(t)
        for pe_, i in best.items():
            if self.seen[eng].get(pe_, -1) >= i:
                continue
            self.seen[eng][pe_] = i
            out.append(('c', pe_, i))
        return out

    def _commit(self, tok, key, reads, writes):
        for b in reads:
            b.rd[key] = tok
        for b in writes:
            b.lw = tok
            b.rd = {}

    def op(self, eng, fn, reads=(), writes=()):
        ps = [b for b in reads if b.name[0] == 'psum']
        if ps:
            writes = list(writes) + [b for b in ps if b not in writes]
            reads = [b for b in reads if b.name[0] != 'psum']
        idx = len(self.ops[eng])
        tok = ('c', eng, idx)
        waits = self._resolve(eng, self._deps(reads, writes))
        self.ops[eng].append(Op(fn, waits, None))
        self._commit(tok, eng, reads, writes)
        return tok

    def dma(self, q, out, in_, reads=(), writes=(), **kw):
        did = len(self.dma_info)
        half = NDS // 2
        base = half if q == 'pool' else 0
        hist = self.dma_hist.setdefault(base, [])
        s = base + len(hist) % half
        self.dma_uses[s] += 1
        self.dma_info.append((s, 16 * self.dma_uses[s]))
        toks = self._deps(reads, writes)
        if len(hist) >= half:
            toks.add(('d', hist[len(hist) - half]))
        hist.append(did)
        waits = self._resolve(q, toks)
        self.ops[q].append(Op(lambda e, o=out, i=in_, k=kw: e.dma_start(out=o, in_=i, **k), waits, did))
        tok = ('d', did)
        self._commit(tok, tok, reads, writes)
        return tok

    def barrier(self):
        toks = set()
        for e in ENGS:
            if e != 'sp' and self.ops[e]:
                for i in range(len(self.ops[e]) - 1, -1, -1):
                    if self.ops[e][i].dma is None and self.ops[e][i].fn is not None:
                        toks.add(('c', e, i))
                        break
        for hist in self.dma_hist.values():
            for d in hist[-(NDS // 2):]:
                toks.add(('d', d))
        for e in ENGS:
            mine = set(t for t in toks if not (t[0] == 'c' and t[1] == e and e == 'pe'))
            waits = self._resolve(e, mine)
            if waits:
                self.ops[e].append(Op(None, waits, None))

    def emit(self, esem, dsem):
        sig = {e: set() for e in ENGS}
        for e in ENGS:
            for o in self.ops[e]:
                for t in o.waits:
                    if t[0] == 'c':
                        sig[t[1]].add(t[2])
        signo = {e: {} for e in ENGS}
        for e in ENGS:
            for n, i in enumerate(sorted(sig[e])):
                signo[e][i] = n + 1
        prog = self

        def stream(ename, h):
            for i, o in enumerate(prog.ops[ename]):
                for t in o.waits:
                    if t[0] == 'c':
                        h.wait_ge(esem[t[1]], signo[t[1]][t[2]])
                    else:
                        s, val = prog.dma_info[t[1]]
                        h.wait_ge(dsem[s], val)
                if o.fn is None:
                    continue
                inst = o.fn(h)
                if o.dma is not None:
                    inst.then_inc(dsem[prog.dma_info[o.dma][0]], 16)
                elif i in sig[ename]:
                    inst.then_inc(esem[ename], 1)

        with self.nc.Block() as block:
            @block.tensor
            def _(h):
                stream('pe', h)

            @block.scalar
            def _(h):
                stream('act', h)

            @block.vector
            def _(h):
                stream('dve', h)

            @block.gpsimd
            def _(h):
                stream('pool', h)

            @block.sync
            def _(h):
                stream('sp', h)


def mk_ap(base, extra_off, dims):
    return bass.AP(base.tensor, base.offset + extra_off, dims)


def pstep(ap):
    return ap.ap[0][0]


def build(debug=None):
    debug = debug or {}
    stop_after = debug.get('stop_after', 99)
    nc = bass.Bass("TRN2", target_bir_lowering=False)

    def din(name, shape):
        return nc.dram_tensor(name, list(shape), F32, kind="ExternalInput").ap()

    x_d = din("x", [TX, D])
    meta_d = din("meta", [16, D])
    g1_d = din("norm1_g", [1, D])
    win_d = din("w_in", [D, 3496])
    gkv_d = din("kv_norm_g", [1, 128])
    wuk_d = din("w_uk", [128, 512])
    wuv_d = din("w_uv", [128, 512])
    wpool_d = din("w_pool", [4, 128, 128])
    pscale_d = din("pool_scale", [1, 512])
    wba_d = din("w_ba", [512, D])
    wbp_d = din("w_bp", [512, D])
    wout_d = din("w_out", [D, D])
    g2_d = din("norm2_g", [1, D])
    wgr_d = din("w_gr", [D, 4])
    bgr_d = din("b_gr", [1, 4])
    wer_d = din("w_er", [D, 32])
    ber_d = din("b_er", [1, 32])
    weg_d = din("w_eg", [N_EXP, D, 256])
    weu_d = din("w_eu", [N_EXP, D, 256])
    wed_d = din("w_ed", [N_EXP, 256, D])
    gf_d = din("final_g", [1, D])
    cid_d = din("c_ident", [128, 128])
    cbb_d = din("c_blockbias", [128, 128])
    csel_d = din("c_sel8", [32, 8 * 128])
    cinv_d = din("c_invcnt", [128, 16])
    cpow_d = din("c_pow2", [128, NIT + 2])
    out_d = nc.dram_tensor("out", [TX, D], F32, kind="ExternalOutput").ap()
    dbg_out = {}

    pg = Prog(nc)
    B = pg.B

    with ExitStack() as es:
        esem = {e: es.enter_context(nc.semaphore("sem_" + e)) for e in ENGS}
        dsem = [es.enter_context(nc.semaphore("dsem%d" % i)) for i in range(NDS)]

        R0_B, R1_B, R2_B, R3_B = 62 * 1024, 64 * 1024, 42 * 1024, 34 * 1024
        arenas = {}
        for nm, nb in (('R0', R0_B), ('R1', R1_B), ('R2', R2_B), ('R3', R3_B)):
            arenas[nm] = (es.enter_context(nc.sbuf_tensor(nm, [P, nb // 4], F32)), nb)
        cursor = {}

        def reset(nm):
            cursor[nm] = 0

        def carve(nm, dtype, shape):
            h, nb = arenas[nm]
            esz = 4 if dtype == F32 else 2
            n = 1
            for s in shape[1:]:
                n *= s
            off = (cursor[nm] + 63) // 64 * 64
            assert off + n * esz <= nb, (nm, off, n * esz, nb, shape)
            cursor[nm] = off + n * esz
            hv = h if dtype == F32 else h.bitcast(dtype)
            ap = hv[0:shape[0], off // esz: off // esz + n]
            if len(shape) == 3:
                ap = ap.rearrange("p (a b) -> p a b", a=shape[1])
            elif len(shape) == 4:
                ap = ap.rearrange("p (a b c) -> p a b c", a=shape[1], b=shape[2])
            return ap

        def carve_at(nm, off, dtype, shape):
            save = cursor[nm]
            cursor[nm] = off
            ap = carve(nm, dtype, shape)
            assert (off + 63) // 64 * 64 == off
            cursor[nm] = save
            return ap

        for nm in arenas:
            reset(nm)

        banks = [es.enter_context(nc.psum_tensor("ps%d" % i, [P, 512], F32)) for i in range(8)]
        banks_bf = [b.bitcast(BF16) for b in banks]
        PB = [B('psum', i) for i in range(8)]

        uT = carve('R0', BF16, [P, KC, T])
        off_idle = (cursor['R0'] + 63) // 64 * 64
        gbc = [carve('R0', F32, [P, D]) for _ in range(2)]
        xt = [carve('R0', F32, [P, D]) for _ in range(2)]
        xn = [carve('R0', BF16, [P, D]) for _ in range(2)]
        junkbf = carve('R0', BF16, [P, T])
        ident_f = carve('R0', F32, [P, 128])
        ident_bf = carve('R0', BF16, [P, 128])
        stats = carve('R0', F32, [P, 512])
        stat_i = [0]

        def stat(n=1):
            i = stat_i[0]
            if i + n > 512:
                i = 0
            stat_i[0] = i + n
            return stats[:, i:i + n], B('stat', i, n)

        def stat_bufs(i, n):
            return [B('statc', c) for c in range(i, i + n)]

        def stat2(n=1):
            i = stat_i[0]
            if i + n > 512:
                i = 0
            stat_i[0] = i + n
            return stats[:, i:i + n], stat_bufs(i, n)

        ckeys = carve('R1', BF16, [P, 17, 129])
        cnT = carve('R1', BF16, [P, T])
        ikT = carve('R1', BF16, [P, T])
        poolT = carve('R1', BF16, [P, 4, TX])
        attnT = carve('R1', BF16, [P, 4, TX])
        iw_all = carve('R1', F32, [P, NT, 8])
        gkv_bc = carve('R1', F32, [P, 128])
        blockbias = carve('R1', F32, [P, 128])
        sel8 = carve('R1', BF16, [P, 8, 128])
        invcnt = carve('R1', F32, [P, 16])
        pow2 = carve('R1', F32, [P, NIT + 2])
        pscale = carve('R1', F32, [P, 4])
        cmaxsel = carve('R1', BF16, [P, 8, 32])
        cmax = carve('R1', F32, [P, 1])
        ones_bf = carve('R1', BF16, [P, 128])
        iqT_all = carve('R1', BF16, [P, 3, TX])
        cmaxB = carve('R1', BF16, [P, 128])

        pg.dma('sp', ident_f, cid_d, writes=[B('ident_f')])
        pg.op('dve', lambda e: e.tensor_copy(out=ident_bf, in_=ident_f), reads=[B('ident_f')], writes=[B('ident_bf')])
        pg.dma('pool', sel8[0:32].rearrange("p a b -> p (a b)"), csel_d, writes=[B('sel8')])
        pg.dma('sp', gbc[0], g1_d.partition_broadcast(128), writes=[B('gbc', 0)])
        pg.op('pool', lambda e: e.memset(ckeys[:, 16, 0:128], 0.0), writes=[B('ckeys', 16)])
        pg.op('pool', lambda e: e.memset(ckeys[:, :, 128:129], 1.0), writes=[B('ckeys_ones')])

        def late_consts():
            pg.dma('sp', blockbias, cbb_d, writes=[B('blockbias')])
            pg.dma('sp', invcnt, cinv_d, writes=[B('invcnt')])
            pg.dma('sp', pow2, cpow_d, writes=[B('pow2')])
            pg.dma('sp', pscale, pscale_d.rearrange("o (g p) -> p (o g)", p=128), writes=[B('pscale')],
                   allow_slow_non_contiguous=True)
            pg.dma('sp', gkv_bc, gkv_d.partition_broadcast(128), writes=[B('gkv_bc')])

        def perm3(ap_2d, rows):
            return ap_2d[0:rows].rearrange("t (p k) -> t p k", k=KC)

        def permout(ap_2d, rows):
            return ap_2d[0:rows].rearrange("t (k p) -> t p k", k=KC)

        rot = {}

        def nxt(name, lst):
            i = rot.get(name, 0)
            rot[name] = i + 1
            return lst[i % len(lst)]

        def rmsnorm_to_uT(src2d, src_bufs, rows, col0, gb, gb_buf, tag):
            k = nxt('xn', [0, 1])
            ss, ssb = stat2()
            rt, rtb = stat2()
            rs, rsb = stat2()
            pg.op('act', lambda e: e.activation(out=junkbf[0:rows, 0:D], in_=src2d[0:rows], func=AF.Square,
                                                accum_out=ss[0:rows]),
                  reads=src_bufs, writes=ssb + [B('junkbf')])
            pg.op('act', lambda e: e.activation(out=rt[0:rows], in_=ss[0:rows], func=AF.Sqrt, scale=1.0 / D,
                                                bias=eps_t[0:rows]),
                  reads=ssb + [B('eps')], writes=rtb)
            pg.op('dve', lambda e: e.reciprocal(out=rs[0:rows], in_=rt[0:rows]), reads=rtb, writes=rsb)
            pg.op('dve', lambda e: e.scalar_tensor_tensor(out=xn[k][0:rows], in0=src2d[0:rows],
                                                          scalar=rs[0:rows], in1=gb[0:rows],
                                                          op0=ALU.mult, op1=ALU.mult),
                  reads=src_bufs + rsb + [gb_buf], writes=[B('xn', k)])
            bk = nxt('tp', [0, 1])

            def tps(e):
                last = None
                for kc in range(KC):
                    last = e.transpose(out=banks_bf[bk][:, kc * 128: kc * 128 + rows],
                                       in_=xn[k][0:rows].rearrange("t (p k) -> t k p", k=KC)[:, kc, :],
                                       identity=ident_bf[0:rows, 0:rows])
                return last
            pg.op('pe', tps, reads=[B('xn', k), B('ident_bf')], writes=[PB[bk]])
            src = banks_bf[bk][:, 0:1024].rearrange("p (a b) -> p a b", a=KC)[:, :, 0:rows]

            def back():
                pg.op('act', lambda e: e.copy(out=uT[:, :, col0:col0 + rows], in_=src),
                      reads=[PB[bk]], writes=[B('uT', tag)])
            return back

        eps_t = carve('R0', F32, [P, 1])
        pg.op('pool', lambda e: e.memset(eps_t, EPS), writes=[B('eps')])

        reset('R2')
        reset('R3')
        pvT = carve('R3', F32, [P, 4, T])
        wslab = carve('R2', BF16, [P, KC, 512])
        wsmall = carve('R2', BF16, [P, KC, 136])
        wik = carve('R2', BF16, [P, KC, 96])
        w_iq2 = carve('R2', BF16, [P, KC, 256])
        wpool = carve('R2', BF16, [P, 4, 128])
        ptmp = [carve('R2', F32, [P, T]) for _ in range(2)]
        dT = [carve('R2', BF16, [P, T]) for _ in range(2)]
        tmpc = carve('R2', F32, [P, 16])

        xt4 = xt + [ptmp[0][:, 0:D], ptmp[1][:, 0:D]]
        tiles = [-1] + list(range(NT))
        pend1 = None
        for j in tiles:
            rows = 16 if j < 0 else 128
            col0 = 0 if j < 0 else 16 + 128 * j
            k = nxt('xt4', [0, 1, 2, 3])
            src = meta_d if j < 0 else x_d[128 * j:128 * (j + 1), :]
            pg.dma('sp', xt4[k][0:rows], src, writes=[B('xt', k)])
            if j == 1:
                late_consts()
            bk_ = rmsnorm_to_uT(xt4[k], [B('xt', k)], rows, col0, gbc[0], B('gbc', 0), j)
            if pend1 is not None:
                pend1()
            pend1 = bk_
        pend1()

        def uT_bufs(c0, c1):
            res = []
            if c0 < 16:
                res.append(B('uT', -1))
            for j in range(NT):
                a, b_ = 16 + 128 * j, 16 + 128 * (j + 1)
                if a < c1 and b_ > c0:
                    res.append(B('uT', j))
            return res

        win_p = win_d.rearrange("(p k) n -> p k n", k=KC)

        pg.dma('pool', wsmall[:, :, 0:128], win_p[:, :, 512:640], writes=[B('wsmall')])
        pg.dma('pool', wsmall[:, :, 128:136], win_p[:, :, 928:936], reads=[], writes=[B('wsmall2')])
        for r_ in range(3):
            pg.dma('pool', wik[:, :, 32 * r_:32 * r_ + 32], win_p[:, :, 896:928], writes=[B('wik', r_)])
        pg.dma('pool', w_iq2, win_p[:, :, 640:896], writes=[B('w_iq2')])
        pg.dma('pool', wslab, win_p[:, :, 936:1448], writes=[B('wslab')])
        pg.dma('pool', wpool, wpool_d.rearrange("g c d -> c g d"), writes=[B('wpool')])

        def p2a_f1(j):
            rows = 16 if j < 0 else 128
            col0 = 0 if j < 0 else 16 + 128 * j
            chunk = 16 if j < 0 else j
            bk = nxt('p2a', [2, 3])

            def mm(e):
                last = None
                for kc in range(KC):
                    last = e.matmul(banks[bk][0:rows, 0:136], lhsT=uT[:, kc, col0:col0 + rows],
                                    rhs=wsmall[:, kc, :], start=(kc == 0), stop=(kc == KC - 1))
                return last
            pg.op('pe', mm, reads=[B('uT', j), B('wsmall'), B('wsmall2')], writes=[PB[bk]])
            ss, ssb = stat2()
            rt, rtb = stat2()
            rs, rsb = stat2()
            pg.op('act', lambda e: e.activation(
                out=junkbf[0:rows, 0:128], in_=banks[bk][0:rows, 0:128], func=AF.Square, accum_out=ss[0:rows]),
                reads=[PB[bk]], writes=ssb + [B('junkbf')])
            pg.op('act', lambda e: e.activation(
                out=rt[0:rows], in_=ss[0:rows], func=AF.Sqrt, scale=1.0 / 128, bias=eps_t[0:rows]),
                reads=ssb + [B('eps')], writes=rtb)
            pg.op('dve', lambda e: e.reciprocal(out=rs[0:rows], in_=rt[0:rows]), reads=rtb, writes=rsb)
            pg.op('dve', lambda e: e.scalar_tensor_tensor(
                out=ckeys[0:rows, chunk, 0:128], in0=banks[bk][0:rows, 0:128], scalar=rs[0:rows],
                in1=gkv_bc[0:rows], op0=ALU.mult, op1=ALU.mult),
                reads=[PB[bk], B('gkv_bc')] + rsb, writes=[B('ckeys', chunk)])
            if j >= 0:
                pg.op('dve', lambda e: e.tensor_scalar(
                    out=iw_all[:, j, :], in0=banks[bk][:, 128:136], scalar1=IDX_SCALE, scalar2=None,
                    op0=ALU.mult), reads=[PB[bk]], writes=[B('iw', j)])

            def f2():
                bt = nxt('p2at', [4, 5])
                pg.op('pe', lambda e: e.transpose(
                    out=banks_bf[bt][:, 0:rows], in_=ckeys[0:rows, chunk, 0:128], identity=ident_bf[0:rows, 0:rows]),
                    reads=[B('ckeys', chunk), B('ident_bf')], writes=[PB[bt]])

                def f3():
                    pg.op('act', lambda e: e.copy(
                        out=cnT[:, col0:col0 + rows], in_=banks_bf[bt][:, 0:rows]),
                        reads=[PB[bt]], writes=[B('cnT', j)])
                return f3
            return f2

        p_f2, p_f3 = None, None
        for j in tiles:
            f2 = p2a_f1(j)
            f3 = p_f2() if p_f2 is not None else None
            if p_f3 is not None:
                p_f3()
            p_f2, p_f3 = f2, f3
        f3 = p_f2()
        if p_f3 is not None:
            p_f3()
        f3()

        blocks = [(0, 16)] + [(16 + 512 * b, 16 + 512 * (b + 1)) for b in range(4)]
        allb = list(range(8))
        evac_i = [0]

        def evac_copy(out_ap, in_ap, reads, writes):
            evac_i[0] += 1
            if evac_i[0] % 2 == 0:
                pg.op('act', lambda e: e.copy(out=out_ap, in_=in_ap), reads=reads, writes=writes)
            else:
                pg.op('dve', lambda e: e.tensor_copy(out=out_ap, in_=in_ap), reads=reads, writes=writes)

        def act_evac(out_ap, in_ap, reads, writes):
            pg.op('act', lambda e: e.copy(out=out_ap, in_=in_ap), reads=reads, writes=writes)

        def p2b_ik():
            for bi, (c0, c1) in enumerate(blocks):
                bk = nxt('gen', allb)
                n = c1 - c0

                def mm(e, bk=bk, c0=c0, c1=c1, n=n):
                    last = None
                    for kc in range(KC):
                        last = e.matmul(banks[bk][0:96, 0:n], lhsT=wik[:, kc, :], rhs=uT[:, kc, c0:c1],
                                        start=(kc == 0), stop=(kc == KC - 1))
                    return last
                pg.op('pe', mm, reads=uT_bufs(c0, c1) + [B('wik', 0), B('wik', 1), B('wik', 2)], writes=[PB[bk]])
                act_evac(ikT[0:96, c0:c1], banks[bk][0:96, 0:n], [PB[bk]], [B('ikT', bi)])

        def p2b_iq():
            for g in range(3):
                M = 96 if g < 2 else 64
                for b in range(4):
                    bk = nxt('gen', allb)
                    c0 = 16 + 512 * b

                    def mmi(e, bk=bk, g=g, M=M, c0=c0):
                        last = None
                        for kc in range(KC):
                            last = e.matmul(banks[bk][0:M, 0:512], lhsT=w_iq2[:, kc, 96 * g:96 * g + M],
                                            rhs=uT[:, kc, c0:c0 + 512], start=(kc == 0), stop=(kc == KC - 1))
                        return last
                    pg.op('pe', mmi, reads=uT_bufs(c0, c0 + 512) + [B('w_iq2')], writes=[PB[bk]])
                    act_evac(iqT_all[0:M, g, 512 * b:512 * (b + 1)], banks[bk][0:M, 0:512], [PB[bk]],
                             [B('iqT_all', g, b)])

        def p2b_pv(m):
            for bi, (c0, c1) in enumerate(blocks):
                bk = nxt('gen', allb)
                n = c1 - c0

                def mm(e, bk=bk, c0=c0, c1=c1, n=n):
                    last = None
                    for kc in range(KC):
                        last = e.matmul(banks[bk][:, 0:n], lhsT=wslab[:, kc, m * 128:(m + 1) * 128],
                                        rhs=uT[:, kc, c0:c1], start=(kc == 0), stop=(kc == KC - 1))
                    return last
                pg.op('pe', mm, reads=uT_bufs(c0, c1) + [B('wslab')], writes=[PB[bk]])
                act_evac(pvT[:, m, c0:c1], banks[bk][:, 0:n], [PB[bk]], [B('pvT', m)])

        p3_di = {}

        def p3_chain(g):
            w = 2 << g
            src = pvT[:, g, :]
            cur = src
            curb = [B('pvT', g)]
            for k in [1, 2, 4, 8][:g + 1]:
                pi = nxt('ptmp', [0, 1])
                dst = ptmp[pi]
                pg.op('pool', lambda e, dst=dst, cur=cur, k=k: e.tensor_tensor(
                    out=dst[:, k:1024], in0=cur[:, k:1024], in1=cur[:, 0:1024 - k], op=ALU.add),
                    reads=curb, writes=[B('ptmp', pi), B('xt', 2 + pi)])
                pg.op('dve', lambda e, dst=dst, cur=cur, k=k: e.tensor_tensor(
                    out=dst[:, 1024:T], in0=cur[:, 1024:T], in1=cur[:, 1024 - k:T - k], op=ALU.add),
                    reads=curb, writes=[B('ptmp', pi, 'hi')])
                pg.op('pool', lambda e, dst=dst, cur=cur, k=k: e.tensor_copy(out=dst[:, 0:k], in_=cur[:, 0:k]),
                      reads=curb, writes=[B('ptmp', pi, 'head'), B('xt', 2 + pi)])
                cur = dst
                curb = [B('ptmp', pi), B('ptmp', pi, 'hi'), B('ptmp', pi, 'head')]
            di = nxt('dT', [0, 1])
            p3_di[g] = di
            cb = curb
            pg.op('dve', lambda e: e.scalar_tensor_tensor(
                out=dT[di][:, w - 1:T], in0=cur[:, w - 1:T], scalar=1.0 / w, in1=src[:, w - 1:T],
                op0=ALU.mult, op1=ALU.subtract),
                reads=cb + [B('pvT', g)], writes=[B('dT', di)])
            pg.op('dve', lambda e: e.tensor_tensor(
                out=tmpc[:, 0:w - 1], in0=cur[:, 0:w - 1], in1=invcnt[:, 0:w - 1], op=ALU.mult),
                reads=cb + [B('invcnt')], writes=[B('tmpc')])
            pg.op('dve', lambda e: e.tensor_tensor(
                out=dT[di][:, 0:w - 1], in0=tmpc[:, 0:w - 1], in1=src[:, 0:w - 1], op=ALU.subtract),
                reads=[B('tmpc'), B('pvT', g)], writes=[B('dT', di, 'head')])

        def p3_mm(g):
            di = p3_di[g]
            for b in range(4):
                bk = nxt('gen', allb)
                pg.op('pe', lambda e, bk=bk, b=b: e.matmul(
                    banks[bk][:, 0:512], lhsT=wpool[:, g, :], rhs=dT[di][:, 16 + 512 * b:16 + 512 * (b + 1)],
                    start=True, stop=True),
                    reads=[B('wpool'), B('dT', di), B('dT', di, 'head')], writes=[PB[bk]])
                pg.op('act', lambda e, bk=bk, b=b: e.activation(
                    out=poolT[:, g, 512 * b:512 * (b + 1)], in_=banks[bk][:, 0:512], func=AF.Identity,
                    scale=pscale[:, g:g + 1]),
                    reads=[PB[bk], B('pscale')], writes=[B('poolT', b)])

        W_Q_OFF = 25344
        w_q = carve_at('R3', W_Q_OFF, BF16, [P, KC, 512])
        p2b_pv(3)
        p2b_pv(2)
        p3_chain(3)
        p2b_pv(1)
        p3_chain(2)
        if stop_after >= 4:
            pg.dma('pool', w_q, win_p[:, :, 0:512], writes=[B('w_q'), B('pvT', 3)])
        p2b_pv(0)
        p2b_ik()
        p2b_iq()
        p3_mm(3)
        p3_mm(2)
        p3_chain(1)
        p3_mm(1)
        p3_chain(0)
        p3_mm(0)

        if stop_after <= 3:
            pass
        pg.barrier()
        reset('R2')
        reset('R3')
        w_ba = carve_at('R0', off_idle, BF16, [P, 4, D])
        w_bp = carve_at('R0', off_idle + 8192, BF16, [P, 4, D])
        score = carve('R3', F32, [P, T])
        mb = carve('R3', BF16, [P, T])
        mbT = carve('R3', BF16, [P, 17, 128])
        ukT_pad = carve('R2', BF16, [P, 8, 128])
        uv_pad = carve('R2', BF16, [P, 8, 128])
        wuk_bf = carve('R2', BF16, [P, 512])
        qT_t = carve('R2', BF16, [P, 4, 128])
        qabsT_t = carve('R2', BF16, [P, 8, 128])
        absq = carve('R2', BF16, [P, 8, 128])
        iqT_t = carve('R2', BF16, [P, 8, 128])
        rbuf = [carve('R2', BF16, [P, 512]) for _ in range(4)]
        pT = [carve('R2', BF16, [P, 512]) for _ in range(3)]
        diag = carve('R2', BF16, [P, 8, 128])
        olatT_t = carve('R2', BF16, [P, 8, 128])
        negm8 = carve('R2', BF16, [P, 128])
        bis = carve('R2', F32, [P, 4 * (NIT + 2)])
        rsum = carve('R2', F32, [P, 8])
        thr = carve('R2', F32, [P, 2])

        if stop_after >= 4:
            pg.dma('pool', wuk_bf, wuk_d, writes=[B('wuk_bf')])
            pg.dma('pool', w_ba, wba_d.rearrange("(m p) n -> p m n", p=128),
                   writes=[B('w_ba'), B('gbc', 0), B('gbc', 1)])
            pg.dma('pool', w_bp, wbp_d.rearrange("(m p) n -> p m n", p=128),
                   writes=[B('w_bp'), B('xt', 0), B('xt', 1)])
            pg.op('pool', lambda e: e.memset(ukT_pad, 0.0), writes=[B('ukT_pad')])
            pg.op('pool', lambda e: e.memset(uv_pad, 0.0), writes=[B('uv_pad')])
            for hp in range(2):
                src = wuv_d.rearrange("c (m two d) -> c m two d", two=2, d=64)[:, :, hp, :]
                dst = uv_pad.rearrange("c (m two) n -> c m two n", two=2)[:, :, hp, 64 * hp:64 * hp + 64]
                pg.dma('pool', dst, src, reads=[B('uv_pad')], writes=[B('uv_pad', hp)])
            for m in range(4):
                bk = nxt('gen', allb)
                pg.op('pe', lambda e, bk=bk, m=m: e.transpose(
                    out=banks_bf[bk][:, 0:128], in_=wuk_bf[:, m * 128:(m + 1) * 128], identity=ident_bf),
                    reads=[B('wuk_bf'), B('ident_bf')], writes=[PB[bk]])
                for hp in range(2):
                    h = 2 * m + hp
                    pg.op('dve', lambda e, bk=bk, h=h, hp=hp: e.tensor_copy(
                        out=ukT_pad[64 * hp:64 * hp + 64, h, :], in_=banks_bf[bk][64 * hp:64 * hp + 64, 0:128]),
                        reads=[PB[bk], B('ukT_pad')], writes=[B('ukT_pad', h)])
            pg.op('dve', lambda e: e.tensor_reduce(out=cmax, in_=cnT, axis=AX.X, op=ALU.max,
                                                   apply_absolute_value=True),
                  reads=[B('cnT', j) for j in tiles], writes=[B('cmax')])
            pg.op('pool', lambda e: e.memset(ones_bf, 1.0), writes=[B('ones_bf')])
            pg.op('dve', lambda e: e.tensor_copy(out=cmaxB, in_=mk_ap(cmax, 0, [[pstep(cmax), 128], [0, 128]])),
                  reads=[B('cmax')], writes=[B('cmaxB')])
            pg.op('pool', lambda e: e.memset(cmaxsel, 0.0), writes=[B('cmaxsel')])
            pg.op('pool', lambda e: e.memset(mbT[:, 16, :], NEG), writes=[B('mbT', 16)])
            for h in range(8):
                pg.op('dve', lambda e, h=h: e.tensor_copy(out=cmaxsel[:, h, h:h + 1], in_=cmax),
                      reads=[B('cmax'), B('cmaxsel')], writes=[B('cmaxsel', h)])
        ukb = [B('ukT_pad')] + [B('ukT_pad', h) for h in range(8)]
        uvb = [B('uv_pad'), B('uv_pad', 0), B('uv_pad', 1)]
        cmb = [B('cmaxsel')] + [B('cmaxsel', h) for h in range(8)]
        ikb = [B('ikT', bi) for bi in range(5)]
        cnb = [B('cnT', j) for j in tiles]
        ckb = [B('ckeys', c) for c in range(17)] + [B('ckeys_ones')]

        mids = bis[:, 0:NIT + 2]
        cnts = bis[:, NIT + 2:2 * (NIT + 2)]
        dds = bis[:, 2 * (NIT + 2):3 * (NIT + 2)]
        Wc = bis[:, 3 * (NIT + 2):4 * (NIT + 2)]

        n_attn_tiles = NT if stop_after >= 4 else 0
        n_attn_tiles = debug.get('n_attn_tiles', n_attn_tiles)
        qabs2 = [qabsT_t, carve('R2', BF16, [P, 8, 128]), carve('R2', BF16, [P, 8, 128])]
        mb2 = [mb, carve('R3', BF16, [P, T])]
        assert cursor['R3'] <= W_Q_OFF, cursor['R3']
        score2 = [score, carve('R2', F32, [P, T])]
        negsh = carve('R2', F32, [P, 8])
        lnS = carve('R2', F32, [P, 512])
        rcpS = carve('R2', F32, [P, 512])
        ROT4 = [0, 1, 2, 3]
        PSS = [6, 7]
        ACC = [4, 5]

        def act_copy(out_ap, in_ap, reads, writes):
            pg.op('act', lambda e: e.copy(out=out_ap, in_=in_ap), reads=reads, writes=writes)

        def chunks_of(j):
            S = 16 + 128 * (j + 1)
            return [(0, 16)] + [(16 + 512 * m, min(16 + 512 * (m + 1), S)) for m in range((S - 16 + 511) // 512)]

        def stageA(j, tick=None):
            par = j % 2
            par3 = j % 3
            sc, qa = score2[par], qabs2[par3]
            qc = 16 + 128 * j
            ub = [B('uT', j)]
            for h in range(8):
                pg.op('act', lambda e, h=h, j=j: e.activation(out=diag[:, h, :], in_=ident_f, func=AF.Identity,
                                                              scale=iw_all[:, j, h:h + 1]),
                      reads=[B('ident_f'), B('iw', j)], writes=[B('diag', h)])
            items = []
            for (c0, c1) in chunks_of(j):
                bs = nxt('pss', PSS)
                for hp_ in range(4):
                    items.append((c0, c1, bs, hp_))

            def emit_x(it):
                c0, c1, bs, hp_ = it
                n = c1 - c0
                res = []
                for hh in range(2):
                    h = 2 * hp_ + hh
                    bx = nxt('rot4', ROT4)
                    g_, a_ = h // 3, h % 3
                    pg.op('pe', lambda e, bx=bx, g_=g_, a_=a_, c0=c0, c1=c1, n=n: e.matmul(
                        banks[bx][:, 0:n], lhsT=iqT_all[32 * a_:32 * a_ + 32, g_, 128 * j:128 * (j + 1)],
                        rhs=ikT[32 * a_:32 * a_ + 32, c0:c1], start=True, stop=True),
                        reads=[B('iqT_all', g_, j // 4)] + ikb, writes=[PB[bx]])
                    ri = nxt('rbuf', [0, 1, 2, 3])
                    pg.op('act', lambda e, bx=bx, ri=ri, n=n: e.activation(
                        out=rbuf[ri][:, 0:n], in_=banks[bx][:, 0:n], func=AF.Relu),
                        reads=[PB[bx]], writes=[B('rbuf', ri)])
                    res.append((h, ri))
                return res

            def emit_d(it, res):
                c0, c1, bs, hp_ = it
                n = c1 - c0
                for (h, ri) in res:
                    pg.op('pe', lambda e, bs=bs, h=h, ri=ri, n=n: e.matmul(
                        banks[bs][:, 0:n], lhsT=diag[:, h, :], rhs=rbuf[ri][:, 0:n], start=(h == 0), stop=(h == 7)),
                        reads=[B('diag', h), B('rbuf', ri)], writes=[PB[bs]])
                if hp_ == 3:
                    act_copy(sc[:, c0:c1], banks[bs][:, 0:n], [PB[bs]], [B('score', par, c0)])

            prev = None
            for it in items:
                res = emit_x(it)
                if prev is not None:
                    emit_d(*prev)
                prev = (it, res)
                if tick is not None:
                    tick()
            emit_d(*prev)
            bk = nxt('rot4', ROT4)

            def mmq(e, bk=bk, qc=qc):
                last = None
                for m in range(4):
                    for kc in range(KC):
                        last = e.matmul(banks[bk][:, m * 128:(m + 1) * 128], lhsT=w_q[:, kc, m * 128:(m + 1) * 128],
                                        rhs=uT[:, kc, qc:qc + 128], start=(kc == 0), stop=(kc == KC - 1))
                return last
            pg.op('pe', mmq, reads=ub + [B('w_q')], writes=[PB[bk]])
            act_copy(qT_t, banks[bk][:, 0:512].rearrange("p (a b) -> p a b", a=4), [PB[bk]], [B('qT_t')])
            for half in range(2):
                bk = nxt('rot4', ROT4)

                def mma(e, bk=bk, half=half):
                    last = None
                    for hh in range(4):
                        h = 4 * half + hh
                        last = e.matmul(banks[bk][:, hh * 128:(hh + 1) * 128], lhsT=ukT_pad[:, h, :],
                                        rhs=qT_t[:, h // 2, :], start=True, stop=True)
                    return last
                pg.op('pe', mma, reads=ukb + [B('qT_t')], writes=[PB[bk]])
                src3 = banks[bk][:, 0:512].rearrange("p (a b) -> p a b", a=4)
                act_copy(qa[:, 4 * half:4 * half + 4, :], src3, [PB[bk]], [B('qabsT_t', par3, half)])
                pg.op('dve', lambda e, half=half, qa=qa: e.scalar_tensor_tensor(
                    out=absq[:, 4 * half:4 * half + 4, :], in0=qa[:, 4 * half:4 * half + 4, :], scalar=-1.0,
                    in1=qa[:, 4 * half:4 * half + 4, :], op0=ALU.mult, op1=ALU.max),
                    reads=[B('qabsT_t', par3, half)], writes=[B('absq', half)])

        def stageA2(j):
            par = j % 3
            for half in range(2):
                bk = nxt('rot4', ROT4)

                def mmb(e, bk=bk, half=half):
                    last = None
                    for hh in range(4):
                        last = e.matmul(banks[bk][:, hh * 128:(hh + 1) * 128], lhsT=cmaxB,
                                        rhs=absq[:, 4 * half + hh, :], start=True, stop=True)
                    return last
                pg.op('pe', mmb, reads=[B('cmaxB'), B('absq', half)], writes=[PB[bk]])
                pg.op('dve', lambda e, bk=bk, half=half: e.tensor_reduce(
                    out=negsh[:, 4 + half:5 + half], in_=banks[bk][:, 0:512], axis=AX.X, op=ALU.max),
                    reads=[PB[bk]], writes=[B('negsh_t', half)])
            pg.op('dve', lambda e: e.tensor_tensor(out=negsh[:, 6:7], in0=negsh[:, 4:5], in1=negsh[:, 5:6], op=ALU.max),
                  reads=[B('negsh_t', 0), B('negsh_t', 1)], writes=[B('negsh_m')])
            pg.op('dve', lambda e, par=par: e.tensor_scalar(out=negsh[:, par:par + 1], in0=negsh[:, 6:7],
                                                            scalar1=-ATTN_SCALE, scalar2=None, op0=ALU.mult),
                  reads=[B('negsh_m')], writes=[B('negsh', par)])

        def scbufs(j):
            return [B('score', j % 2, c0) for (c0, c1) in chunks_of(j)]

        def stageB_init(j):
            S = 16 + 128 * (j + 1)
            sc = score2[j % 2]
            scb = scbufs(j)
            Rr, Rb = stat2()
            pg.op('dve', lambda e: e.tensor_reduce(out=Rr, in_=sc[:, 0:S], axis=AX.X, op=ALU.max,
                                                   apply_absolute_value=True),
                  reads=scb, writes=Rb)
            pg.op('dve', lambda e: e.tensor_tensor(out=sc[:, S - 128:S], in0=sc[:, S - 128:S],
                                                   in1=blockbias, op=ALU.add),
                  reads=scb + [B('blockbias')], writes=[B('score', j % 2, 16 + 512 * (j // 4))])
            pg.op('dve', lambda e: e.tensor_scalar(out=Wc, in0=pow2, scalar1=Rr, scalar2=1e-30,
                                                   op0=ALU.mult, op1=ALU.add),
                  reads=Rb + [B('pow2')], writes=[B('Wc')])
            pg.op('dve', lambda e: e.memset(mids[:, 0:1], 0.0), writes=[B('mid', 0)])

        def stageB_iter(j, n_):
            S = 16 + 128 * (j + 1)
            sc = score2[j % 2]
            pg.op('dve', lambda e: e.tensor_scalar(
                out=junkbf[:, 0:S], in0=sc[:, 0:S], scalar1=mids[:, n_ - 1:n_], scalar2=None,
                op0=ALU.is_ge, op1=ALU.add, accum_out=cnts[:, n_ - 1:n_]),
                reads=scbufs(j) + [B('mid', n_ - 1)], writes=[B('junkbf'), B('cnt', n_)])
            pg.op('dve', lambda e: e.scalar_tensor_tensor(
                out=dds[:, n_ - 1:n_], in0=cnts[:, n_ - 1:n_], scalar=KTOP - 0.5, in1=Wc[:, n_ - 1:n_],
                op0=ALU.is_ge, op1=ALU.mult),
                reads=[B('cnt', n_), B('Wc')], writes=[B('dd', n_)])
            pg.op('dve', lambda e: e.scalar_tensor_tensor(
                out=mids[:, n_:n_ + 1], in0=dds[:, n_ - 1:n_], scalar=Wc[:, n_:n_ + 1], in1=mids[:, n_ - 1:n_],
                op0=ALU.subtract, op1=ALU.add),
                reads=[B('dd', n_), B('Wc'), B('mid', n_ - 1)], writes=[B('mid', n_)])

        def stageB_fin(j):
            pg.op('dve', lambda e: e.tensor_tensor(out=thr[:, 0:1], in0=mids[:, NIT:NIT + 1], in1=Wc[:, NIT:NIT + 1],
                                                   op=ALU.subtract),
                  reads=[B('mid', NIT), B('Wc')], writes=[B('thr')])
            S = 16 + 128 * (j + 1)
            sc = score2[j % 2]
            mbj = mb2[j % 2]
            pg.op('dve', lambda e: e.tensor_scalar(out=mbj[:, 0:S], in0=sc[:, 0:S], scalar1=thr[:, 0:1],
                                                   scalar2=NEG, op0=ALU.is_lt, op1=ALU.mult),
                  reads=scbufs(j) + [B('thr')], writes=[B('mb', j % 2)])

        def stageC(j):
            mbj = mb2[j % 2]
            klist = [16] + list(range(j + 1))
            for g0 in range(0, len(klist), 8):
                grp = klist[g0:g0 + 8]
                bk = nxt('rot4', ROT4)

                def tp(e, grp=grp, bk=bk):
                    last = None
                    for si, i in enumerate(grp):
                        c_ = 0 if i == 16 else 16 + 128 * i
                        last = e.transpose(out=banks_bf[bk][:, si * 128:(si + 1) * 128], in_=mbj[:, c_:c_ + 128],
                                           identity=ident_bf)
                    return last
                pg.op('pe', tp, reads=[B('mb', j % 2), B('ident_bf')], writes=[PB[bk]])
                si0 = 0
                if grp[0] == 16:
                    pg.op('dve', lambda e, bk=bk: e.tensor_copy(out=mbT[0:16, 16, :], in_=banks_bf[bk][0:16, 0:128]),
                          reads=[PB[bk]], writes=[B('mbT', 16)])
                    si0 = 1
                nn_ = len(grp) - si0
                if nn_ > 0:
                    i0 = grp[si0]
                    srcv = banks_bf[bk][:, si0 * 128:(si0 + nn_) * 128].rearrange("p (a b) -> p a b", a=nn_)
                    pg.op('dve', lambda e, i0=i0, nn_=nn_, srcv=srcv: e.tensor_copy(out=mbT[:, i0:i0 + nn_, :], in_=srcv),
                          reads=[PB[bk]], writes=[B('mbT', i) for i in range(i0, i0 + nn_)])

        def stageD_lg(j, q, ci):
            par = j % 3
            qa = qabs2[par]
            xch = [16] + list(range(j + 1))
            i = xch[ci]
            bl = nxt('rot4', ROT4)
            kc0 = 0 if i == 16 else 16 + 128 * i
            mrow = mk_ap(mbT, i * 128, [[pstep(mbT), 128], [0, 4], [1, 128]])
            qrhs = qa[:, 4 * q:4 * q + 4, :]

            def lg(e):
                e.matmul(banks[bl][:, 0:512], lhsT=cnT[:, kc0:kc0 + 128], rhs=qrhs, start=True, stop=False)
                return e.matmul(banks[bl][:, 0:512], lhsT=ident_bf, rhs=mrow, start=False, stop=True)
            pg.op('pe', lg, reads=cnb + [B('qabsT_t', par, q), B('ident_bf'), B('mbT', i)], writes=[PB[bl]])
            pi = nxt('pT', [0, 1, 2])
            pg.op('act', lambda e: e.activation(out=pT[pi][:, 0:512], in_=banks[bl][:, 0:512], func=AF.Exp,
                                                scale=ATTN_SCALE, bias=negsh[:, par:par + 1]),
                  reads=[PB[bl], B('negsh', par)], writes=[B('pT', pi)])
            return pi

        def stageD_pv(j, q, ci, pi):
            xch = [16] + list(range(j + 1))
            i = xch[ci]
            bO, bS = ACC
            first, last_ = (ci == 0), (ci == len(xch) - 1)

            def pv(e):
                e.matmul(banks[bO][:, 0:512], lhsT=ckeys[:, i, 0:128], rhs=pT[pi][:, 0:512], start=first, stop=last_)
                return e.matmul(banks[bS][:, 0:512], lhsT=ones_bf, rhs=pT[pi][:, 0:512], start=first, stop=last_)
            pg.op('pe', pv, reads=[B('pT', pi), B('ones_bf'), B('ckeys', i)], writes=[PB[bO], PB[bS]])

        def stageD_qfin(j, q):
            bO, bS = ACC
            pg.op('act', lambda e: e.activation(out=lnS, in_=banks[bS][:, 0:512], func=AF.Ln),
                  reads=[PB[bS]], writes=[B('lnS')])
            pg.op('act', lambda e: e.activation(out=rcpS, in_=lnS, func=AF.Exp, scale=-1.0),
                  reads=[B('lnS')], writes=[B('rcpS')])
            pg.op('dve', lambda e: e.tensor_tensor(
                out=olatT_t[:, 4 * q:4 * q + 4, :], in0=banks[bO][:, 0:512].rearrange("p (a b) -> p a b", a=4),
                in1=rcpS.rearrange("p (a b) -> p a b", a=4), op=ALU.mult),
                reads=[PB[bO], B('rcpS')], writes=[B('olatT_t', q)])

        def stageD_fin(j):
            bk = nxt('rot4', ROT4)

            def mmo(e, bk=bk):
                last = None
                for m in range(4):
                    o = banks[bk][:, m * 128:(m + 1) * 128]
                    e.matmul(o, lhsT=uv_pad[:, 2 * m, :], rhs=olatT_t[:, 2 * m, :], start=True, stop=False)
                    last = e.matmul(o, lhsT=uv_pad[:, 2 * m + 1, :], rhs=olatT_t[:, 2 * m + 1, :], start=False, stop=True)
                return last
            pg.op('pe', mmo, reads=uvb + [B('olatT_t', 0), B('olatT_t', 1)], writes=[PB[bk]])
            act_copy(attnT[:, :, 128 * j:128 * (j + 1)], banks[bk][:, 0:512].rearrange("p (a b) -> p a b", a=4),
                     [PB[bk]], [B('attnT', j)])

        nA = n_attn_tiles

        def fullB(j):
            stageB_init(j)
            if 16 + 128 * (j + 1) <= KTOP:
                pg.op('dve', lambda e: e.tensor_reduce(out=cnts[:, 0:1], in_=Wc[:, 1:NIT + 1], axis=AX.X, op=ALU.add),
                      reads=[B('Wc')], writes=[B('cnt', 1)])
                pg.op('dve', lambda e: e.tensor_scalar(out=mids[:, NIT:NIT + 1], in0=cnts[:, 0:1], scalar1=-1.0,
                                                       scalar2=None, op0=ALU.mult),
                      reads=[B('cnt', 1)], writes=[B('mid', NIT)])
            else:
                for n_ in range(1, NIT + 1):
                    stageB_iter(j, n_)
            stageB_fin(j)

        if nA > 2:
            stageA(0)
            fullB(0)
            stageA(1)
            stageB_init(1)
            stageA2(0)
            for n_ in (1, 2, 3):
                stageB_iter(1, n_)
            stageA2(1)
            for n_ in (4, 5, 6):
                stageB_iter(1, n_)
            stageC(0)
            for n_ in (7, 8):
                stageB_iter(1, n_)
            assert 4 * len(chunks_of(2)) == 8
            pro_n = [8]

            def pro_tick():
                pro_n[0] += 1
                if pro_n[0] <= NIT:
                    stageB_iter(1, pro_n[0])
            stageA(2, pro_tick)
            for n_ in range(pro_n[0] + 1, NIT + 1):
                stageB_iter(1, n_)
            stageA2(2)
            stageB_fin(1)
        elif nA > 0:
            stageA(0)
            fullB(0)
            if nA > 1:
                stageA(1)
            stageA2(0)
            if nA > 1:
                stageA2(1)
            stageC(0)
            if nA > 1:
                fullB(1)
        carry_sched = None
        for i in range(nA):
            hasB = i + 2 < nA
            hasA = i + 3 < nA
            nch = i + 2
            seq = [(q, ci) for q in range(2) for ci in range(nch)]
            steps = len(seq)
            n_items = 4 * len(chunks_of(i + 3)) if hasA else 0
            wD, wA = 1.0 * steps, 1.7 * n_items
            ticks = [('D', k) for k in range(steps)] + [('A', k) for k in range(n_items)]
            wts = [1.0] * steps + [1.7] * n_items
            split_last = hasB and not hasA and i + 1 < nA
            if split_last:
                steps2 = 2 * (i + 3)
                ticks = ticks + [('E', k) for k in range(steps2)]
                wts = wts + [1.0] * steps2
            tot = sum(wts)
            sched = {}
            acc, done = 0.0, 0
            for tk, w in zip(ticks, wts):
                acc += w
                upto = int(round(NIT * acc / tot))
                sched[tk] = list(range(done + 1, upto + 1))
                done = upto
            if hasB:
                stageB_init(i + 2)
            pis = {}
            pis[0] = stageD_lg(i, *seq[0])
            for k, (q, ci) in enumerate(seq):
                if k + 1 < steps:
                    pis[k + 1] = stageD_lg(i, *seq[k + 1])
                stageD_pv(i, q, ci, pis[k])
                if hasB:
                    for n_ in sched[('D', k)]:
                        stageB_iter(i + 2, n_)
                if carry_sched is not None:
                    for n_ in carry_sched[('E', k)]:
                        stageB_iter(i + 1, n_)
                if ci == nch - 1:
                    stageD_qfin(i, q)
            stageD_fin(i)
            if hasA:
                cnt = [0]

                def tick(cnt=cnt):
                    if hasB:
                        for n_ in sched[('A', cnt[0])]:
                            stageB_iter(i + 2, n_)
                    cnt[0] += 1
                stageA(i + 3, tick)
            if hasB and not split_last:
                stageB_fin(i + 2)
            if carry_sched is not None:
                stageB_fin(i + 1)
            carry_sched = sched if split_last else None
            if i + 1 < nA:
                stageC(i + 1)
            if hasA:
                stageA2(i + 3)

        pg.barrier()
        reset('R2')
        reset('R3')
        mergedT = carve('R3', BF16, [P, 8, TX])
        wgA2 = [carve('R2', BF16, [P, KC, 256]) for _ in range(2)]
        wgP2 = [carve('R2', BF16, [P, KC, 256]) for _ in range(2)]
        w_out = carve('R2', BF16, [P, 8, D])
        off_wout_end = cursor['R2']
        sA = [carve('R2', F32, [P, 512]) for _ in range(2)]
        sP = [carve('R2', F32, [P, 512]) for _ in range(2)]
        run_tail = stop_after >= 5

        def load_wg(grp):
            s_ = grp % 2
            pg.dma('pool', wgA2[s_], win_p[:, :, 1448 + 256 * grp:1448 + 256 * (grp + 1)], writes=[B('wgA', s_)])
            pg.dma('pool', wgP2[s_], win_p[:, :, 2472 + 256 * grp:2472 + 256 * (grp + 1)], writes=[B('wgP', s_)])

        if run_tail:
            load_wg(0)
            load_wg(1)
            pg.dma('pool', w_out, wout_d.rearrange("(m p) n -> p m n", p=128), writes=[B('w_out')])
            for grp in range(4):
                gs = grp % 2
                wgA, wgP = wgA2[gs], wgP2[gs]
                if 1 <= grp < 3:
                    load_wg(grp + 1)
                for n4 in range(2):
                    nn = 2 * grp + n4
                    for b in range(4):
                        bset = nxt('tailA', [[0, 1, 2, 3], [4, 5, 6, 7]])
                        c0 = 16 + 512 * b
                        bA, bP, bgA, bgP = bset

                        def mmA(e, bA=bA, nn=nn, b=b):
                            last = None
                            for m in range(4):
                                last = e.matmul(banks[bA][:, 0:512], lhsT=w_ba[:, m, nn * 128:(nn + 1) * 128],
                                                rhs=attnT[:, m, 512 * b:512 * (b + 1)], start=(m == 0), stop=(m == 3))
                            return last
                        pg.op('pe', mmA, reads=[B('w_ba')] + [B('attnT', 4 * b + q) for q in range(4)], writes=[PB[bA]])

                        def mmP(e, bP=bP, nn=nn, b=b):
                            last = None
                            for m in range(4):
                                last = e.matmul(banks[bP][:, 0:512], lhsT=w_bp[:, m, nn * 128:(nn + 1) * 128],
                                                rhs=poolT[:, m, 512 * b:512 * (b + 1)], start=(m == 0), stop=(m == 3))
                            return last
                        pg.op('pe', mmP, reads=[B('w_bp'), B('poolT', b)], writes=[PB[bP]])

                        def mmg(e, bk, wt, n4=n4, c0=c0):
                            last = None
                            for kc in range(KC):
                                last = e.matmul(banks[bk][:, 0:512], lhsT=wt[:, kc, n4 * 128:(n4 + 1) * 128],
                                                rhs=uT[:, kc, c0:c0 + 512], start=(kc == 0), stop=(kc == KC - 1))
                            return last
                        pg.op('pe', lambda e, bgA=bgA, f=mmg, wgA=wgA: f(e, bgA, wgA),
                              reads=uT_bufs(c0, c0 + 512) + [B('wgA', gs)], writes=[PB[bgA]])
                        pg.op('pe', lambda e, bgP=bgP, f=mmg, wgP=wgP: f(e, bgP, wgP),
                              reads=uT_bufs(c0, c0 + 512) + [B('wgP', gs)], writes=[PB[bgP]])
                        si = nxt('sAP', [0, 1])
                        pg.op('act', lambda e, bgA=bgA, si=si: e.activation(out=sA[si], in_=banks[bgA][:, 0:512], func=AF.Sigmoid),
                              reads=[PB[bgA]], writes=[B('sA', si)])
                        pg.op('act', lambda e, bgP=bgP, si=si: e.activation(out=sP[si], in_=banks[bgP][:, 0:512], func=AF.Sigmoid),
                              reads=[PB[bgP]], writes=[B('sP', si)])
                        pg.op('dve', lambda e, bA=bA, si=si: e.tensor_tensor(out=sA[si], in0=sA[si], in1=banks[bA][:, 0:512], op=ALU.mult),
                              reads=[B('sA', si), PB[bA]], writes=[B('sA', si)])
                        pg.op('dve', lambda e, bP=bP, si=si: e.tensor_tensor(out=sP[si], in0=sP[si], in1=banks[bP][:, 0:512], op=ALU.mult),
                              reads=[B('sP', si), PB[bP]], writes=[B('sP', si)])
                        pg.op('dve', lambda e, si=si, nn=nn, b=b: e.tensor_tensor(
                            out=mergedT[:, nn, 512 * b:512 * (b + 1)], in0=sA[si], in1=sP[si], op=ALU.add),
                            reads=[B('sA', si), B('sP', si)], writes=[B('mergedT', nn, b)])

        pg.barrier()
        reset('R1')
        reset('R2')
        hacc = carve('R1', F32, [P, NT, D])
        w_r = carve('R2', BF16, [P, KC, 36])
        rbias = carve('R2', F32, [P, 36])
        gate_all = carve('R2', F32, [P, NT, 32])
        rt_ = carve('R2', F32, [P, 256])
        cur_save = cursor['R2']
        lg_all = carve('R2', F32, [P, NT, 36])
        ssq = carve('R2', F32, [P, NT])
        rstd2 = carve('R2', F32, [P, NT])
        rv = carve('R2', F32, [P, 1600])
        assert cursor['R2'] <= off_wout_end - 16384, cursor['R2']
        if run_tail:
            pg.dma('pool', w_r[:, :, 0:4], wgr_d.rearrange("(p k) n -> p k n", k=KC), writes=[B('w_r', 0)])
            pg.dma('pool', w_r[:, :, 4:36], wer_d.rearrange("(p k) n -> p k n", k=KC), writes=[B('w_r', 1)])
            pg.dma('sp', rbias[:, 0:4], bgr_d.partition_broadcast(128), writes=[B('rbias', 0)])
            pg.dma('sp', rbias[:, 4:36], ber_d.partition_broadcast(128), writes=[B('rbias', 1)])
            pg.dma('sp', gbc[1], g2_d.partition_broadcast(128), writes=[B('gbc', 1)])
            for j in range(NT):
                b = j // 4
                k = nxt('xt', [0, 1])
                pg.dma('sp', xt[k], x_d[128 * j:128 * (j + 1), :], writes=[B('xt', k)])
                for hh in range(2):
                    bk = nxt('tailB', [0, 1, 2, 3])

                    def mmh(e, bk=bk, j=j, hh=hh):
                        last = None
                        for nn in range(8):
                            last = e.matmul(banks[bk][:, 0:512], lhsT=mergedT[:, nn, 128 * j:128 * (j + 1)],
                                            rhs=w_out[:, nn, 512 * hh:512 * (hh + 1)], start=(nn == 0), stop=(nn == 7))
                        return last
                    pg.op('pe', mmh, reads=[B('mergedT', nn, b) for nn in range(8)] + [B('w_out')] + [B('R3a', q) for q in range(6)],
                          writes=[PB[bk]])
                    pg.op('dve', lambda e, bk=bk, j=j, hh=hh, k=k: e.tensor_tensor(
                        out=hacc[:, j, 512 * hh:512 * (hh + 1)], in0=banks[bk][:, 0:512], in1=xt[k][:, 512 * hh:512 * (hh + 1)],
                        op=ALU.add), reads=[PB[bk], B('xt', k)], writes=[B('hacc', j, hh)])
                pg.op('act', lambda e, j=j: e.activation(out=junkbf[:, 0:D], in_=hacc[:, j, :], func=AF.Square,
                                                         accum_out=ssq[:, j:j + 1]),
                      reads=[B('hacc', j, 0), B('hacc', j, 1)], writes=[B('junkbf'), B('ssq', j)])
            pg.op('act', lambda e: e.activation(out=rstd2, in_=ssq, func=AF.Sqrt, scale=1.0 / D, bias=eps_t),
                  reads=[B('ssq', j) for j in range(NT)] + [B('eps')], writes=[B('rstd2s')])
            pg.op('dve', lambda e: e.reciprocal(out=rstd2, in_=rstd2), reads=[B('rstd2s')], writes=[B('rstd2')])
            def s2_front(j):
                k = nxt('xn', [0, 1])
                pg.op('dve', lambda e: e.scalar_tensor_tensor(
                    out=xn[k], in0=hacc[:, j, :], scalar=rstd2[:, j:j + 1], in1=gbc[1], op0=ALU.mult, op1=ALU.mult),
                    reads=[B('hacc', j, 0), B('hacc', j, 1), B('rstd2'), B('gbc', 1)], writes=[B('xn', k)])
                bk = nxt('tp', [4, 5])

                def tps(e):
                    last = None
                    for kc in range(KC):
                        last = e.transpose(out=banks_bf[bk][:, kc * 128:(kc + 1) * 128],
                                           in_=xn[k].rearrange("t (p k) -> t k p", k=KC)[:, kc, :], identity=ident_bf)
                    return last
                pg.op('pe', tps, reads=[B('xn', k), B('ident_bf')], writes=[PB[bk]])
                srcv = banks_bf[bk][:, 0:1024].rearrange("p (a b) -> p a b", a=KC)
                pg.op('act', lambda e: e.copy(out=uT[:, :, 16 + 128 * j:16 + 128 * (j + 1)], in_=srcv),
                      reads=[PB[bk]], writes=[B('uT', j)])

            s2_bk = {}

            def s2_back(j):
                bk2 = nxt('rt', [6, 7])
                s2_bk[j] = bk2

                def mmr(e):
                    last = None
                    for kc in range(KC):
                        last = e.matmul(banks[bk2][:, 0:36], lhsT=uT[:, kc, 16 + 128 * j:16 + 128 * (j + 1)],
                                        rhs=w_r[:, kc, :], start=(kc == 0), stop=(kc == KC - 1))
                    return last
                pg.op('pe', mmr, reads=[B('uT', j), B('w_r', 0), B('w_r', 1)], writes=[PB[bk2]])

            def s2_add(j):
                bk2 = s2_bk[j]
                pg.op('dve', lambda e: e.tensor_tensor(out=lg_all[:, j, :], in0=banks[bk2][:, 0:36], in1=rbias,
                                                       op=ALU.add),
                      reads=[PB[bk2], B('rbias', 0), B('rbias', 1)], writes=[B('lg_all', j)])

            for t_ in range(NT + 3):
                if t_ >= 3:
                    s2_add(t_ - 3)
                if 2 <= t_ < NT + 2:
                    s2_back(t_ - 2)
                if t_ < NT:
                    s2_front(t_)
            RV = B('rv')
            off = [0]

            def rvv(n):
                a_ = rv[:, off[0]:off[0] + n]
                off[0] += n
                return a_
            gmax, gsum, pgp, m1, m2, dm, ed, den, p1, p2 = [rvv(NT) for _ in range(10)]
            goh, gex = rvv(NT * 4), rvv(NT * 4)
            esel, oh1, esel2, oh2, t1, t2 = [rvv(NT * 8) for _ in range(6)]
            tmp32 = rvv(NT * 32)
            pl = pstep(lg_all)
            pr = pstep(rv)

            def v3(ap, n):
                return ap.rearrange("p (a b) -> p a b", a=NT)

            def bc_last(ap, n):
                return mk_ap(ap, 0, [[pr, 128], [1, NT], [0, n]])
            gl = lg_all[:, :, 0:4]
            el4 = mk_ap(lg_all, 4, [[pl, 128], [36, NT], [8, 4], [1, 8]])
            lgb = [B('lg_all', j) for j in range(NT)]

            def dv(fn, extra=()):
                pg.op('dve', fn, reads=[RV] + list(extra), writes=[RV])
            dv(lambda e: e.tensor_reduce(out=gmax, in_=gl, axis=AX.X, op=ALU.max), lgb)
            dv(lambda e: e.tensor_tensor(out=v3(goh, 4), in0=gl, in1=bc_last(gmax, 4), op=ALU.is_ge), lgb)
            dv(lambda e: e.tensor_tensor(out=v3(gex, 4), in0=gl, in1=bc_last(gmax, 4), op=ALU.subtract), lgb)
            pg.op('act', lambda e: e.activation(out=gex, in_=gex, func=AF.Exp), reads=[RV], writes=[RV])
            dv(lambda e: e.tensor_reduce(out=gsum, in_=v3(gex, 4), axis=AX.X, op=ALU.add))
            dv(lambda e: e.reciprocal(out=pgp, in_=gsum))
            goh4 = mk_ap(goh, 0, [[pr, 128], [4, NT], [1, 4], [0, 8]])
            t4 = mk_ap(tmp32, 0, [[pr, 128], [32, NT], [8, 4], [1, 8]])
            dv(lambda e: e.tensor_tensor(out=t4, in0=el4, in1=goh4, op=ALU.mult), lgb)
            t4T = mk_ap(tmp32, 0, [[pr, 128], [32, NT], [1, 8], [8, 4]])
            dv(lambda e: e.tensor_reduce(out=v3(esel, 8), in_=t4T, axis=AX.X, op=ALU.add))
            dv(lambda e: e.tensor_reduce(out=m1, in_=v3(esel, 8), axis=AX.X, op=ALU.max))
            dv(lambda e: e.tensor_tensor(out=v3(oh1, 8), in0=v3(esel, 8), in1=bc_last(m1, 8), op=ALU.is_ge))
            dv(lambda e: e.scalar_tensor_tensor(out=esel2, in0=oh1, scalar=-BIG, in1=esel, op0=ALU.mult, op1=ALU.add))
            dv(lambda e: e.tensor_reduce(out=m2, in_=v3(esel2, 8), axis=AX.X, op=ALU.max))
            dv(lambda e: e.tensor_tensor(out=v3(oh2, 8), in0=v3(esel2, 8), in1=bc_last(m2, 8), op=ALU.is_ge))
            dv(lambda e: e.tensor_tensor(out=dm, in0=m2, in1=m1, op=ALU.subtract))
            pg.op('act', lambda e: e.activation(out=ed, in_=dm, func=AF.Exp), reads=[RV], writes=[RV])
            dv(lambda e: e.tensor_scalar(out=den, in0=ed, scalar1=1.0, scalar2=None, op0=ALU.add))
            dv(lambda e: e.reciprocal(out=p1, in_=den))
            dv(lambda e: e.tensor_tensor(out=p2, in0=ed, in1=p1, op=ALU.mult))
            dv(lambda e: e.tensor_tensor(out=p1, in0=p1, in1=pgp, op=ALU.mult))
            dv(lambda e: e.tensor_tensor(out=p2, in0=p2, in1=pgp, op=ALU.mult))
            dv(lambda e: e.tensor_tensor(out=v3(t1, 8), in0=v3(oh1, 8), in1=bc_last(p1, 8), op=ALU.mult))
            dv(lambda e: e.tensor_tensor(out=v3(t2, 8), in0=v3(oh2, 8), in1=bc_last(p2, 8), op=ALU.mult))
            dv(lambda e: e.tensor_tensor(out=t1, in0=t1, in1=t2, op=ALU.add))
            wpg4 = mk_ap(t1, 0, [[pr, 128], [8, NT], [0, 4], [1, 8]])
            gout = gate_all.rearrange("p a (g e) -> p a g e", g=4)
            pg.op('dve', lambda e: e.tensor_tensor(out=gout, in0=goh4, in1=wpg4, op=ALU.mult),
                  reads=[RV], writes=[B('gate', j) for j in range(NT)])
        cursor['R2'] = cur_save

        reset('R3')
        n_exp = debug.get('n_exp', N_EXP if stop_after >= 6 else 0)
        wg_e = [carve('R3', BF16, [P, KC, 256]) for _ in range(2)]
        wu_e = [carve('R3', BF16, [P, KC, 256]) for _ in range(2)]
        wd_e = [carve('R3', BF16, [P, 2, D]) for _ in range(2)]
        silt = [carve('R3', F32, [P, 512]) for _ in range(2)]
        hid = [carve('R2', BF16, [P, 2, TX]) for _ in range(2)]

        def load_gu(e_):
            s = e_ % 2
            al = (lambda q: [B('R3a', q)]) if e_ < 2 else (lambda q: [])
            pg.dma('pool', wg_e[s], weg_d[e_].rearrange("(p k) n -> p k n", k=KC), writes=[B('wg_e', s)] + al(2 * s))
            pg.dma('pool', wu_e[s], weu_d[e_].rearrange("(p k) n -> p k n", k=KC), writes=[B('wu_e', s)] + al(2 * s + 1))

        def load_d(e_):
            s = e_ % 2
            pg.dma('pool', wd_e[s], wed_d[e_].rearrange("(f p) n -> p f n", p=128),
                   writes=[B('wd_e', s)] + ([B('R3a', 4 + s)] if e_ < 2 else []))

        def gu_group(e_, b, fh):
            s = e_ % 2
            c0 = 16 + 512 * b
            bG, bU = nxt('moeGU', [[0, 1], [2, 3]])

            def mmGU(e, bk, wt):
                last = None
                for kc in range(KC):
                    last = e.matmul(banks[bk][:, 0:512], lhsT=wt[:, kc, fh * 128:(fh + 1) * 128],
                                    rhs=uT[:, kc, c0:c0 + 512], start=(kc == 0), stop=(kc == KC - 1))
                return last
            pg.op('pe', lambda e: mmGU(e, bG, wg_e[s]), reads=uT_bufs(c0, c0 + 512) + [B('wg_e', s)],
                  writes=[PB[bG]])
            pg.op('pe', lambda e: mmGU(e, bU, wu_e[s]), reads=uT_bufs(c0, c0 + 512) + [B('wu_e', s)],
                  writes=[PB[bU]])
            si = nxt('silt', [0, 1])
            pg.op('act', lambda e: e.activation(out=silt[si], in_=banks[bG][:, 0:512], func=AF.Silu),
                  reads=[PB[bG]], writes=[B('silt', si)])
            pg.op('dve', lambda e: e.tensor_tensor(
                out=hid[s][:, fh, 512 * b:512 * (b + 1)], in0=silt[si], in1=banks[bU][:, 0:512], op=ALU.mult),
                reads=[B('silt', si), PB[bU]], writes=[B('hid', s, b)])

        def y_tile(e_, j):
            s = e_ % 2
            for hh in range(2):
                bk = nxt('moeY', [4, 5, 6, 7])

                def mmy(e, bk=bk, hh=hh):
                    e.matmul(banks[bk][:, 0:512], lhsT=hid[s][:, 0, 128 * j:128 * (j + 1)],
                             rhs=wd_e[s][:, 0, 512 * hh:512 * (hh + 1)], start=True, stop=False)
                    return e.matmul(banks[bk][:, 0:512], lhsT=hid[s][:, 1, 128 * j:128 * (j + 1)],
                                    rhs=wd_e[s][:, 1, 512 * hh:512 * (hh + 1)], start=False, stop=True)
                pg.op('pe', mmy, reads=[B('hid', s, j // 4), B('wd_e', s)], writes=[PB[bk]])
                pg.op('dve', lambda e, bk=bk, hh=hh: e.scalar_tensor_tensor(
                    out=hacc[:, j, 512 * hh:512 * (hh + 1)], in0=banks[bk][:, 0:512], scalar=gate_all[:, j, e_:e_ + 1],
                    in1=hacc[:, j, 512 * hh:512 * (hh + 1)], op0=ALU.mult, op1=ALU.add),
                    reads=[PB[bk], B('gate', j), B('hacc', j, hh)], writes=[B('hacc', j, hh)])

        if n_exp > 0:
            load_gu(0)
            load_d(0)
        for e_ in range(n_exp + 1 if n_exp > 0 else 0):
            if e_ + 1 < n_exp:
                load_gu(e_ + 1)
            for gi in range(8):
                if e_ < n_exp:
                    gu_group(e_, gi // 2, gi % 2)
                if e_ >= 1:
                    y_tile(e_ - 1, 2 * gi)
                    y_tile(e_ - 1, 2 * gi + 1)
            if e_ + 1 < n_exp:
                load_d(e_ + 1)

        if run_tail:
            pg.dma('sp', gbc[0], gf_d.partition_broadcast(128), writes=[B('gbc', 0)])
            for j in range(NT):
                hb = [B('hacc', j, 0), B('hacc', j, 1)]
                pg.op('act', lambda e, j=j: e.activation(out=junkbf[:, 0:D], in_=hacc[:, j, :], func=AF.Square,
                                                         accum_out=ssq[:, j:j + 1]),
                      reads=hb, writes=[B('ssq', j), B('junkbf')])
            pg.op('act', lambda e: e.activation(out=rstd2, in_=ssq, func=AF.Sqrt, scale=1.0 / D, bias=eps_t),
                  reads=[B('ssq', j) for j in range(NT)] + [B('eps')], writes=[B('rstd2s')])
            pg.op('dve', lambda e: e.reciprocal(out=rstd2, in_=rstd2), reads=[B('rstd2s')], writes=[B('rstd2')])
            obuf = [carve_at('R0', 4096 * i, F32, [P, D]) for i in range(8)]
            for j in range(NT):
                hb = [B('hacc', j, 0), B('hacc', j, 1)]
                k = j % 8
                if j % 3 == 2:
                    pg.op('act', lambda e, j=j, k=k: e.activation(
                        out=obuf[k], in_=hacc[:, j, :], func=AF.Identity, scale=rstd2[:, j:j + 1]),
                        reads=hb + [B('rstd2')], writes=[B('obuf', k)])
                    pg.op('pool', lambda e, k=k: e.tensor_tensor(out=obuf[k], in0=obuf[k], in1=gbc[0], op=ALU.mult),
                          reads=[B('obuf', k), B('gbc', 0)], writes=[B('obuf', k)])
                else:
                    pg.op('dve', lambda e, j=j, k=k: e.scalar_tensor_tensor(
                        out=obuf[k], in0=hacc[:, j, :], scalar=rstd2[:, j:j + 1], in1=gbc[0], op0=ALU.mult, op1=ALU.mult),
                        reads=hb + [B('rstd2'), B('gbc', 0)], writes=[B('obuf', k)])
                pg.dma('sp', out_d[128 * j:128 * (j + 1), :], obuf[k], reads=[B('obuf', k)], writes=[B('out', j)])

        pg.barrier()
        local = dict(uT=uT, ckeys=ckeys, cnT=cnT, ikT=ikT, poolT=poolT, attnT=attnT, iw_all=iw_all, score=score,
                     mb=mb, hacc=hacc, gate_all=gate_all, mergedT=mergedT, qabsT_t=qabsT_t,
                     bis=bis, thr=thr, iqT_t=iqT_t, diag=diag, mbT=mbT)
        for name in debug.get('dump', []):
            ap = local[name]
            shp = list(ap.shape)
            n = 1
            for s_ in shp[1:]:
                n *= s_
            dd = nc.dram_tensor("dbg_" + name, [shp[0], n], F32, kind="ExternalOutput").ap()
            flat = ap
            if len(shp) == 3:
                flat = ap.rearrange("p a b -> p (a b)")
            for c0 in range(0, n, 2048):
                c1 = min(n, c0 + 2048)
                pg.dma('pool', dd[:, c0:c1], flat[:, c0:c1], reads=[], writes=[B('dbg', name, c0)])
        pg.barrier()
        pg.emit(esem, dsem)
    return nc


_CACHE = {}


def _consts():
    ident = np.eye(128, dtype=np.float32)
    p = np.arange(128)[:, None]
    s = np.arange(128)[None, :]
    blockbias = np.where((s < 64) | (p >= 64), 0.0, -BIG).astype(np.float32)
    sel8 = np.zeros((32, 8, 128), np.float32)
    for h in range(8):
        sel8[h, h, :] = 1.0
    invcnt = np.broadcast_to((1.0 / np.arange(1, 17, dtype=np.float32))[None, :], (128, 16)).copy()
    pow2 = np.broadcast_to((1.0001 * 2.0 ** (-np.arange(NIT + 2, dtype=np.float64))).astype(np.float32)[None, :],
                           (128, NIT + 2)).copy()
    return dict(c_ident=ident, c_blockbias=blockbias, c_sel8=sel8.reshape(32, 1024), c_invcnt=invcnt, c_pow2=pow2)


def make_in_maps(inputs):
    f = lambda a: np.ascontiguousarray(np.asarray(a, dtype=np.float32))
    shared = dict(
        meta=f(inputs['meta_tokens']),
        norm1_g=f(inputs['norm1_g']).reshape(1, D),
        w_in=f(inputs['w_in']).reshape(D, 3496),
        kv_norm_g=f(inputs['kv_norm_g']).reshape(1, 128),
        w_uk=f(inputs['w_uk']).reshape(128, 512),
        w_uv=f(inputs['w_uv']).reshape(128, 512),
        w_pool=f(inputs['w_pool']).reshape(4, 128, 128),
        pool_scale=f(inputs['pool_scale']).reshape(1, 512),
        w_ba=f(inputs['w_branch_attn']).reshape(512, D),
        w_bp=f(inputs['w_branch_pool']).reshape(512, D),
        w_out=f(inputs['w_out']).reshape(D, D),
        norm2_g=f(inputs['norm2_g']).reshape(1, D),
        w_gr=f(inputs['w_group_router']).reshape(D, 4),
        b_gr=f(inputs['b_group_router']).reshape(1, 4),
        w_er=f(inputs['w_expert_router']).reshape(D, 32),
        b_er=f(inputs['b_expert_router']).reshape(1, 32),
        w_eg=f(inputs['w_expert_gate']).reshape(N_EXP, D, 256),
        w_eu=f(inputs['w_expert_up']).reshape(N_EXP, D, 256),
        w_ed=f(inputs['w_expert_down']).reshape(N_EXP, 256, D),
        final_g=f(inputs['final_norm_g']).reshape(1, D),
    )
    shared.update(_consts())
    x = f(inputs['x'])
    return [dict(shared, x=x[b]) for b in range(8)]


def kernel(**inputs):
    if 'nc' not in _CACHE:
        _CACHE['nc'] = build()
    nc = _CACHE['nc']
    in_maps = make_in_maps(inputs)
    res = run_bass_kernel_spmd(nc, in_maps, core_ids=list(range(8)))
    return np.stack([np.asarray(r["out"], dtype=np.float32).reshape(TX, D) for r in res.results], axis=0)
```

```python
import math
from contextlib import ExitStack

import numpy as np
import concourse.bass as bass
import concourse.mybir as mybir
from concourse.bass_utils import run_bass_kernel_spmd

F32 = mybir.dt.float32
BF16 = mybir.dt.bfloat16
ALU = mybir.AluOpType
AF = mybir.ActivationFunctionType
AX = mybir.AxisListType

P = 128
D = 1024
KC = 8
NT = 16
TX = 2048
T = 2064
EPS = 1e-6
ATTN_SCALE = 64 ** -0.5
IDX_SCALE = (8 ** -0.5) * (32 ** -0.5)
KTOP = 256
NIT = 16
NEG = -30000.0
BIG = 1.0e30
N_EXP = 32
ENGS = ['pe', 'act', 'dve', 'pool', 'sp']
NDS = 24


class Buf:
    __slots__ = ('name', 'lw', 'rd')

    def __init__(self, name):
        self.name = name
        self.lw = None
        self.rd = {}


class Op:
    __slots__ = ('fn', 'waits', 'dma')

    def __init__(self, fn, waits, dma):
        self.fn = fn
        self.waits = waits
        self.dma = dma


class Prog:
    def __init__(self, nc):
        self.nc = nc
        self.ops = {e: [] for e in ENGS}
        self.seen = {e: {} for e in ENGS}
        self.seen_dma = {e: set() for e in ENGS}
        self.dma_info = []
        self.dma_uses = [0] * NDS
        self.dma_hist = {}
        self.bufs = {}

    def B(self, *key):
        b = self.bufs.get(key)
        if b is None:
            b = Buf(key)
            self.bufs[key] = b
        return b

    def _deps(self, reads, writes):
        toks = set()
        for b in reads:
            if b.lw is not None:
                toks.add(b.lw)
        for b in writes:
            if b.lw is not None:
                toks.add(b.lw)
            toks.update(b.rd.values())
        return toks

    def _resolve(self, eng, toks):
        best = {}
        out = []
        for t in toks:
            if t[0] == 'c':
                if t[1] == eng and eng == 'pe':
                    continue
                if best.get(t[1], -1) < t[2]:
                    best[t[1]] = t[2]
            else:
                if t[1] not in self.seen_dma[eng]:
                    self.seen_dma[eng].add(t[1])
                    out.append(t)
        for pe_, i in best.items():
            if self.seen[eng].get(pe_, -1) >= i:
                continue
            self.seen[eng][pe_] = i
            out.append(('c', pe_, i))
        return out

    def _commit(self, tok, key, reads, writes):
        for b in reads:
            b.rd[key] = tok
        for b in writes:
            b.lw = tok
            b.rd = {}

    def op(self, eng, fn, reads=(), writes=()):
        ps = [b for b in reads if b.name[0] == 'psum']
        if ps:
            writes = list(writes) + [b for b in ps if b not in writes]
            reads = [b for b in reads if b.name[0] != 'psum']
        idx = len(self.ops[eng])
        tok = ('c', eng, idx)
        waits = self._resolve(eng, self._deps(reads, writes))
        self.ops[eng].append(Op(fn, waits, None))
        self._commit(tok, eng, reads, writes)
        return tok

    def dma(self, q, out, in_, reads=(), writes=(), **kw):
        did = len(self.dma_info)
        half = NDS // 2
        base = half if q == 'pool' else 0
        hist = self.dma_hist.setdefault(base, [])
        s = base + len(hist) % half
        self.dma_uses[s] += 1
        self.dma_info.append((s, 16 * self.dma_uses[s]))
        toks = self._deps(reads, writes)
        if len(hist) >= half:
            toks.add(('d', hist[len(hist) - half]))
        hist.append(did)
        waits = self._resolve(q, toks)
        self.ops[q].append(Op(lambda e, o=out, i=in_, k=kw: e.dma_start(out=o, in_=i, **k), waits, did))
        tok = ('d', did)
        self._commit(tok, tok, reads, writes)
        return tok

    def barrier(self):
        toks = set()
        for e in ENGS:
            if e != 'sp' and self.ops[e]:
                for i in range(len(self.ops[e]) - 1, -1, -1):
                    if self.ops[e][i].dma is None and self.ops[e][i].fn is not None:
                        toks.add(('c', e, i))
                        break
        for hist in self.dma_hist.values():
            for d in hist[-(NDS // 2):]:
                toks.add(('d', d))
        for e in ENGS:
            mine = set(t for t in toks if not (t[0] == 'c' and t[1] == e and e == 'pe'))
            waits = self._resolve(e, mine)
            if waits:
                self.ops[e].append(Op(None, waits, None))

    def emit(self, esem, dsem):
        sig = {e: set() for e in ENGS}
        for e in ENGS:
            for o in self.ops[e]:
                for t in o.waits:
                    if t[0] == 'c':
                        sig[t[1]].add(t[2])
        signo = {e: {} for e in ENGS}
        for e in ENGS:
            for n, i in enumerate(sorted(sig[e])):
                signo[e][i] = n + 1
        prog = self

        def stream(ename, h):
            for i, o in enumerate(prog.ops[ename]):
                for t in o.waits:
                    if t[0] == 'c':
                        h.wait_ge(esem[t[1]], signo[t[1]][t[2]])
                    else:
                        s, val = prog.dma_info[t[1]]
                        h.wait_ge(dsem[s], val)
                if o.fn is None:
                    continue
                inst = o.fn(h)
                if o.dma is not None:
                    inst.then_inc(dsem[prog.dma_info[o.dma][0]], 16)
                elif i in sig[ename]:
                    inst.then_inc(esem[ename], 1)

        with self.nc.Block() as block:
            @block.tensor
            def _(h):
                stream('pe', h)

            @block.scalar
            def _(h):
                stream('act', h)

            @block.vector
            def _(h):
                stream('dve', h)

            @block.gpsimd
            def _(h):
                stream('pool', h)

            @block.sync
            def _(h):
                stream('sp', h)


def mk_ap(base, extra_off, dims):
    return bass.AP(base.tensor, base.offset + extra_off, dims)


def pstep(ap):
    return ap.ap[0][0]


def build(debug=None):
    debug = debug or {}
    stop_after = debug.get('stop_after', 99)
    nc = bass.Bass("TRN2", target_bir_lowering=False)

    def din(name, shape):
        return nc.dram_tensor(name, list(shape), F32, kind="ExternalInput").ap()

    x_d = din("x", [TX, D])
    meta_d = din("meta", [16, D])
    g1_d = din("norm1_g", [1, D])
    win_d = din("w_in", [D, 3496])
    gkv_d = din("kv_norm_g", [1, 128])
    wuk_d = din("w_uk", [128, 512])
    wuv_d = din("w_uv", [128, 512])
    wpool_d = din("w_pool", [4, 128, 128])
    pscale_d = din("pool_scale", [1, 512])
    wba_d = din("w_ba", [512, D])
    wbp_d = din("w_bp", [512, D])
    wout_d = din("w_out", [D, D])
    g2_d = din("norm2_g", [1, D])
    wgr_d = din("w_gr", [D, 4])
    bgr_d = din("b_gr", [1, 4])
    wer_d = din("w_er", [D, 32])
    ber_d = din("b_er", [1, 32])
    weg_d = din("w_eg", [N_EXP, D, 256])
    weu_d = din("w_eu", [N_EXP, D, 256])
    wed_d = din("w_ed", [N_EXP, 256, D])
    gf_d = din("final_g", [1, D])
    cid_d = din("c_ident", [128, 128])
    cbb_d = din("c_blockbias", [128, 128])
    csel_d = din("c_sel8", [32, 8 * 128])
    cinv_d = din("c_invcnt", [128, 16])
    cpow_d = din("c_pow2", [128, NIT + 2])
    out_d = nc.dram_tensor("out", [TX, D], F32, kind="ExternalOutput").ap()
    dbg_out = {}

    pg = Prog(nc)
    B = pg.B

    with ExitStack() as es:
        esem = {e: es.enter_context(nc.semaphore("sem_" + e)) for e in ENGS}
        dsem = [es.enter_context(nc.semaphore("dsem%d" % i)) for i in range(NDS)]

        R0_B, R1_B, R2_B, R3_B = 62 * 1024, 64 * 1024, 42 * 1024, 34 * 1024
        arenas = {}
        for nm, nb in (('R0', R0_B), ('R1', R1_B), ('R2', R2_B), ('R3', R3_B)):
            arenas[nm] = (es.enter_context(nc.sbuf_tensor(nm, [P, nb // 4], F32)), nb)
        cursor = {}

        def reset(nm):
            cursor[nm] = 0

        def carve(nm, dtype, shape):
            h, nb = arenas[nm]
            esz = 4 if dtype == F32 else 2
            n = 1
            for s in shape[1:]:
                n *= s
            off = (cursor[nm] + 63) // 64 * 64
            assert off + n * esz <= nb, (nm, off, n * esz, nb, shape)
            cursor[nm] = off + n * esz
            hv = h if dtype == F32 else h.bitcast(dtype)
            ap = hv[0:shape[0], off // esz: off // esz + n]
            if len(shape) == 3:
                ap = ap.rearrange("p (a b) -> p a b", a=shape[1])
            elif len(shape) == 4:
                ap = ap.rearrange("p (a b c) -> p a b c", a=shape[1], b=shape[2])
            return ap

        def carve_at(nm, off, dtype, shape):
            save = cursor[nm]
            cursor[nm] = off
            ap = carve(nm, dtype, shape)
            assert (off + 63) // 64 * 64 == off
            cursor[nm] = save
            return ap

        for nm in arenas:
            reset(nm)

        banks = [es.enter_context(nc.psum_tensor("ps%d" % i, [P, 512], F32)) for i in range(8)]
        banks_bf = [b.bitcast(BF16) for b in banks]
        PB = [B('psum', i) for i in range(8)]

        uT = carve('R0', BF16, [P, KC, T])
        off_idle = (cursor['R0'] + 63) // 64 * 64
        gbc = [carve('R0', F32, [P, D]) for _ in range(2)]
        xt = [carve('R0', F32, [P, D]) for _ in range(2)]
        xn = [carve('R0', BF16, [P, D]) for _ in range(2)]
        junkbf = carve('R0', BF16, [P, T])
        ident_f = carve('R0', F32, [P, 128])
        ident_bf = carve('R0', BF16, [P, 128])
        stats = carve('R0', F32, [P, 512])
        stat_i = [0]

        def stat(n=1):
            i = stat_i[0]
            if i + n > 512:
                i = 0
            stat_i[0] = i + n
            return stats[:, i:i + n], B('stat', i, n)

        def stat_bufs(i, n):
            return [B('statc', c) for c in range(i, i + n)]

        def stat2(n=1):
            i = stat_i[0]
            if i + n > 512:
                i = 0
            stat_i[0] = i + n
            return stats[:, i:i + n], stat_bufs(i, n)

        ckeys = carve('R1', BF16, [P, 17, 129])
        cnT = carve('R1', BF16, [P, T])
        ikT = carve('R1', BF16, [P, T])
        poolT = carve('R1', BF16, [P, 4, TX])
        attnT = carve('R1', BF16, [P, 4, TX])
        iw_all = carve('R1', F32, [P, NT, 8])
        gkv_bc = carve('R1', F32, [P, 128])
        blockbias = carve('R1', F32, [P, 128])
        sel8 = carve('R1', BF16, [P, 8, 128])
        invcnt = carve('R1', F32, [P, 16])
        pow2 = carve('R1', F32, [P, NIT + 2])
        pscale = carve('R1', F32, [P, 4])
        cmaxsel = carve('R1', BF16, [P, 8, 32])
        cmax = carve('R1', F32, [P, 1])
        ones_bf = carve('R1', BF16, [P, 128])
        iqT_all = carve('R1', BF16, [P, 3, TX])
        cmaxB = carve('R1', BF16, [P, 128])

        pg.dma('sp', ident_f, cid_d, writes=[B('ident_f')])
        pg.op('dve', lambda e: e.tensor_copy(out=ident_bf, in_=ident_f), reads=[B('ident_f')], writes=[B('ident_bf')])
        pg.dma('pool', sel8[0:32].rearrange("p a b -> p (a b)"), csel_d, writes=[B('sel8')])
        pg.dma('sp', gbc[0], g1_d.partition_broadcast(128), writes=[B('gbc', 0)])
        pg.op('pool', lambda e: e.memset(ckeys[:, 16, 0:128], 0.0), writes=[B('ckeys', 16)])
        pg.op('pool', lambda e: e.memset(ckeys[:, :, 128:129], 1.0), writes=[B('ckeys_ones')])

        def late_consts():
            pg.dma('sp', blockbias, cbb_d, writes=[B('blockbias')])
            pg.dma('sp', invcnt, cinv_d, writes=[B('invcnt')])
            pg.dma('sp', pow2, cpow_d, writes=[B('pow2')])
            pg.dma('sp', pscale, pscale_d.rearrange("o (g p) -> p (o g)", p=128), writes=[B('pscale')],
                   allow_slow_non_contiguous=True)
            pg.dma('sp', gkv_bc, gkv_d.partition_broadcast(128), writes=[B('gkv_bc')])

        def perm3(ap_2d, rows):
            return ap_2d[0:rows].rearrange("t (p k) -> t p k", k=KC)

        def permout(ap_2d, rows):
            return ap_2d[0:rows].rearrange("t (k p) -> t p k", k=KC)

        rot = {}

        def nxt(name, lst):
            i = rot.get(name, 0)
            rot[name] = i + 1
            return lst[i % len(lst)]

        def rmsnorm_to_uT(src2d, src_bufs, rows, col0, gb, gb_buf, tag):
            k = nxt('xn', [0, 1])
            ss, ssb = stat2()
            rt, rtb = stat2()
            rs, rsb = stat2()
            pg.op('act', lambda e: e.activation(out=junkbf[0:rows, 0:D], in_=src2d[0:rows], func=AF.Square,
                                                accum_out=ss[0:rows]),
                  reads=src_bufs, writes=ssb + [B('junkbf')])
            pg.op('act', lambda e: e.activation(out=rt[0:rows], in_=ss[0:rows], func=AF.Sqrt, scale=1.0 / D,
                                                bias=eps_t[0:rows]),
                  reads=ssb + [B('eps')], writes=rtb)
            pg.op('dve', lambda e: e.reciprocal(out=rs[0:rows], in_=rt[0:rows]), reads=rtb, writes=rsb)
            pg.op('dve', lambda e: e.scalar_tensor_tensor(out=xn[k][0:rows], in0=src2d[0:rows],
                                                          scalar=rs[0:rows], in1=gb[0:rows],
                                                          op0=ALU.mult, op1=ALU.mult),
                  reads=src_bufs + rsb + [gb_buf], writes=[B('xn', k)])
            bk = nxt('tp', [0, 1])

            def tps(e):
                last = None
                for kc in range(KC):
                    last = e.transpose(out=banks_bf[bk][:, kc * 128: kc * 128 + rows],
                                       in_=xn[k][0:rows].rearrange("t (p k) -> t k p", k=KC)[:, kc, :],
                                       identity=ident_bf[0:rows, 0:rows])
                return last
            pg.op('pe', tps, reads=[B('xn', k), B('ident_bf')], writes=[PB[bk]])
            src = banks_bf[bk][:, 0:1024].rearrange("p (a b) -> p a b", a=KC)[:, :, 0:rows]

            def back():
                pg.op('act', lambda e: e.copy(out=uT[:, :, col0:col0 + rows], in_=src),
                      reads=[PB[bk]], writes=[B('uT', tag)])
            return back

        eps_t = carve('R0', F32, [P, 1])
        pg.op('pool', lambda e: e.memset(eps_t, EPS), writes=[B('eps')])

        reset('R2')
        reset('R3')
        pvT = carve('R3', F32, [P, 4, T])
        wslab = carve('R2', BF16, [P, KC, 512])
        wsmall = carve('R2', BF16, [P, KC, 136])
        wik = carve('R2', BF16, [P, KC, 96])
        w_iq2 = carve('R2', BF16, [P, KC, 256])
        wpool = carve('R2', BF16, [P, 4, 128])
        ptmp = [carve('R2', F32, [P, T]) for _ in range(2)]
        dT = [carve('R2', BF16, [P, T]) for _ in range(2)]
        tmpc = carve('R2', F32, [P, 16])

        xt4 = xt + [ptmp[0][:, 0:D], ptmp[1][:, 0:D]]
        tiles = [-1] + list(range(NT))
        pend1 = None
        for j in tiles:
            rows = 16 if j < 0 else 128
            col0 = 0 if j < 0 else 16 + 128 * j
            k = nxt('xt4', [0, 1, 2, 3])
            src = meta_d if j < 0 else x_d[128 * j:128 * (j + 1), :]
            pg.dma('sp', xt4[k][0:rows], src, writes=[B('xt', k)])
            if j == 1:
                late_consts()
            bk_ = rmsnorm_to_uT(xt4[k], [B('xt', k)], rows, col0, gbc[0], B('gbc', 0), j)
            if pend1 is not None:
                pend1()
            pend1 = bk_
        pend1()

        def uT_bufs(c0, c1):
            res = []
            if c0 < 16:
                res.append(B('uT', -1))
            for j in range(NT):
                a, b_ = 16 + 128 * j, 16 + 128 * (j + 1)
                if a < c1 and b_ > c0:
                    res.append(B('uT', j))
            return res

        win_p = win_d.rearrange("(p k) n -> p k n", k=KC)

        pg.dma('pool', wsmall[:, :, 0:128], win_p[:, :, 512:640], writes=[B('wsmall')])
        pg.dma('pool', wsmall[:, :, 128:136], win_p[:, :, 928:936], reads=[], writes=[B('wsmall2')])
        for r_ in range(3):
            pg.dma('pool', wik[:, :, 32 * r_:32 * r_ + 32], win_p[:, :, 896:928], writes=[B('wik', r_)])
        pg.dma('pool', w_iq2, win_p[:, :, 640:896], writes=[B('w_iq2')])
        pg.dma('pool', wslab, win_p[:, :, 936:1448], writes=[B('wslab')])
        pg.dma('pool', wpool, wpool_d.rearrange("g c d -> c g d"), writes=[B('wpool')])

        def p2a_f1(j):
            rows = 16 if j < 0 else 128
            col0 = 0 if j < 0 else 16 + 128 * j
            chunk = 16 if j < 0 else j
            bk = nxt('p2a', [2, 3])

            def mm(e):
                last = None
                for kc in range(KC):
                    last = e.matmul(banks[bk][0:rows, 0:136], lhsT=uT[:, kc, col0:col0 + rows],
                                    rhs=wsmall[:, kc, :], start=(kc == 0), stop=(kc == KC - 1))
                return last
            pg.op('pe', mm, reads=[B('uT', j), B('wsmall'), B('wsmall2')], writes=[PB[bk]])
            ss, ssb = stat2()
            rt, rtb = stat2()
            rs, rsb = stat2()
            pg.op('act', lambda e: e.activation(
                out=junkbf[0:rows, 0:128], in_=banks[bk][0:rows, 0:128], func=AF.Square, accum_out=ss[0:rows]),
                reads=[PB[bk]], writes=ssb + [B('junkbf')])
            pg.op('act', lambda e: e.activation(
                out=rt[0:rows], in_=ss[0:rows], func=AF.Sqrt, scale=1.0 / 128, bias=eps_t[0:rows]),
                reads=ssb + [B('eps')], writes=rtb)
            pg.op('dve', lambda e: e.reciprocal(out=rs[0:rows], in_=rt[0:rows]), reads=rtb, writes=rsb)
            pg.op('dve', lambda e: e.scalar_tensor_tensor(
                out=ckeys[0:rows, chunk, 0:128], in0=banks[bk][0:rows, 0:128], scalar=rs[0:rows],
                in1=gkv_bc[0:rows], op0=ALU.mult, op1=ALU.mult),
                reads=[PB[bk], B('gkv_bc')] + rsb, writes=[B('ckeys', chunk)])
            if j >= 0:
                pg.op('dve', lambda e: e.tensor_scalar(
                    out=iw_all[:, j, :], in0=banks[bk][:, 128:136], scalar1=IDX_SCALE, scalar2=None,
                    op0=ALU.mult), reads=[PB[bk]], writes=[B('iw', j)])

            def f2():
                bt = nxt('p2at', [4, 5])
                pg.op('pe', lambda e: e.transpose(
                    out=banks_bf[bt][:, 0:rows], in_=ckeys[0:rows, chunk, 0:128], identity=ident_bf[0:rows, 0:rows]),
                    reads=[B('ckeys', chunk), B('ident_bf')], writes=[PB[bt]])

                def f3():
                    pg.op('act', lambda e: e.copy(
                        out=cnT[:, col0:col0 + rows], in_=banks_bf[bt][:, 0:rows]),
                        reads=[PB[bt]], writes=[B('cnT', j)])
                return f3
            return f2

        p_f2, p_f3 = None, None
        for j in tiles:
            f2 = p2a_f1(j)
            f3 = p_f2() if p_f2 is not None else None
            if p_f3 is not None:
                p_f3()
            p_f2, p_f3 = f2, f3
        f3 = p_f2()
        if p_f3 is not None:
            p_f3()
        f3()

        blocks = [(0, 16)] + [(16 + 512 * b, 16 + 512 * (b + 1)) for b in range(4)]
        allb = list(range(8))
        evac_i = [0]

        def evac_copy(out_ap, in_ap, reads, writes):
            evac_i[0] += 1
            if evac_i[0] % 2 == 0:
                pg.op('act', lambda e: e.copy(out=out_ap, in_=in_ap), reads=reads, writes=writes)
            else:
                pg.op('dve', lambda e: e.tensor_copy(out=out_ap, in_=in_ap), reads=reads, writes=writes)

        def act_evac(out_ap, in_ap, reads, writes):
            pg.op('act', lambda e: e.copy(out=out_ap, in_=in_ap), reads=reads, writes=writes)

        def p2b_ik():
            for bi, (c0, c1) in enumerate(blocks):
                bk = nxt('gen', allb)
                n = c1 - c0

                def mm(e, bk=bk, c0=c0, c1=c1, n=n):
                    last = None
                    for kc in range(KC):
                        last = e.matmul(banks[bk][0:96, 0:n], lhsT=wik[:, kc, :], rhs=uT[:, kc, c0:c1],
                                        start=(kc == 0), stop=(kc == KC - 1))
                    return last
                pg.op('pe', mm, reads=uT_bufs(c0, c1) + [B('wik', 0), B('wik', 1), B('wik', 2)], writes=[PB[bk]])
                act_evac(ikT[0:96, c0:c1], banks[bk][0:96, 0:n], [PB[bk]], [B('ikT', bi)])

        def p2b_iq():
            for g in range(3):
                M = 96 if g < 2 else 64
                for b in range(4):
                    bk = nxt('gen', allb)
                    c0 = 16 + 512 * b

                    def mmi(e, bk=bk, g=g, M=M, c0=c0):
                        last = None
                        for kc in range(KC):
                            last = e.matmul(banks[bk][0:M, 0:512], lhsT=w_iq2[:, kc, 96 * g:96 * g + M],
                                            rhs=uT[:, kc, c0:c0 + 512], start=(kc == 0), stop=(kc == KC - 1))
                        return last
                    pg.op('pe', mmi, reads=uT_bufs(c0, c0 + 512) + [B('w_iq2')], writes=[PB[bk]])
                    act_evac(iqT_all[0:M, g, 512 * b:512 * (b + 1)], banks[bk][0:M, 0:512], [PB[bk]],
                             [B('iqT_all', g, b)])

        def p2b_pv(m):
            for bi, (c0, c1) in enumerate(blocks):
                bk = nxt('gen', allb)
                n = c1 - c0

                def mm(e, bk=bk, c0=c0, c1=c1, n=n):
                    last = None
                    for kc in range(KC):
                        last = e.matmul(banks[bk][:, 0:n], lhsT=wslab[:, kc, m * 128:(m + 1) * 128],
                                        rhs=uT[:, kc, c0:c1], start=(kc == 0), stop=(kc == KC - 1))
                    return last
                pg.op('pe', mm, reads=uT_bufs(c0, c1) + [B('wslab')], writes=[PB[bk]])
                act_evac(pvT[:, m, c0:c1], banks[bk][:, 0:n], [PB[bk]], [B('pvT', m)])

        p3_di = {}

        def p3_chain(g):
            w = 2 << g
            src = pvT[:, g, :]
            cur = src
            curb = [B('pvT', g)]
            for k in [1, 2, 4, 8][:g + 1]:
                pi = nxt('ptmp', [0, 1])
                dst = ptmp[pi]
                pg.op('pool', lambda e, dst=dst, cur=cur, k=k: e.tensor_tensor(
                    out=dst[:, k:1024], in0=cur[:, k:1024], in1=cur[:, 0:1024 - k], op=ALU.add),
                    reads=curb, writes=[B('ptmp', pi), B('xt', 2 + pi)])
                pg.op('dve', lambda e, dst=dst, cur=cur, k=k: e.tensor_tensor(
                    out=dst[:, 1024:T], in0=cur[:, 1024:T], in1=cur[:, 1024 - k:T - k], op=ALU.add),
                    reads=curb, writes=[B('ptmp', pi, 'hi')])
                pg.op('pool', lambda e, dst=dst, cur=cur, k=k: e.tensor_copy(out=dst[:, 0:k], in_=cur[:, 0:k]),
                      reads=curb, writes=[B('ptmp', pi, 'head'), B('xt', 2 + pi)])
                cur = dst
                curb = [B('ptmp', pi), B('ptmp', pi, 'hi'), B('ptmp', pi, 'head')]
            di = nxt('dT', [0, 1])
            p3_di[g] = di
            cb = curb
            pg.op('dve', lambda e: e.scalar_tensor_tensor(
                out=dT[di][:, w - 1:T], in0=cur[:, w - 1:T], scalar=1.0 / w, in1=src[:, w - 1:T],
                op0=ALU.mult, op1=ALU.subtract),
                reads=cb + [B('pvT', g)], writes=[B('dT', di)])
            pg.op('dve', lambda e: e.tensor_tensor(
                out=tmpc[:, 0:w - 1], in0=cur[:, 0:w - 1], in1=invcnt[:, 0:w - 1], op=ALU.mult),
                reads=cb + [B('invcnt')], writes=[B('tmpc')])
            pg.op('dve', lambda e: e.tensor_tensor(
                out=dT[di][:, 0:w - 1], in0=tmpc[:, 0:w - 1], in1=src[:, 0:w - 1], op=ALU.subtract),
                reads=[B('tmpc'), B('pvT', g)], writes=[B('dT', di, 'head')])

        def p3_mm(g):
            di = p3_di[g]
            for b in range(4):
                bk = nxt('gen', allb)
                pg.op('pe', lambda e, bk=bk, b=b: e.matmul(
                    banks[bk][:, 0:512], lhsT=wpool[:, g, :], rhs=dT[di][:, 16 + 512 * b:16 + 512 * (b + 1)],
                    start=True, stop=True),
                    reads=[B('wpool'), B('dT', di), B('dT', di, 'head')], writes=[PB[bk]])
                pg.op('act', lambda e, bk=bk, b=b: e.activation(
                    out=poolT[:, g, 512 * b:512 * (b + 1)], in_=banks[bk][:, 0:512], func=AF.Identity,
                    scale=pscale[:, g:g + 1]),
                    reads=[PB[bk], B('pscale')], writes=[B('poolT', b)])

        W_Q_OFF = 25344
        w_q = carve_at('R3', W_Q_OFF, BF16, [P, KC, 512])
        p2b_pv(3)
        p2b_pv(2)
        p3_chain(3)
        p2b_pv(1)
        p3_chain(2)
        if stop_after >= 4:
            pg.dma('pool', w_q, win_p[:, :, 0:512], writes=[B('w_q'), B('pvT', 3)])
        p2b_pv(0)
        p2b_ik()
        p2b_iq()
        p3_mm(3)
        p3_mm(2)
        p3_chain(1)
        p3_mm(1)
        p3_chain(0)
        p3_mm(0)

        if stop_after <= 3:
            pass
        pg.barrier()
        reset('R2')
        reset('R3')
        w_ba = carve_at('R0', off_idle, BF16, [P, 4, D])
        w_bp = carve_at('R0', off_idle + 8192, BF16, [P, 4, D])
        score = carve('R3', F32, [P, T])
        mb = carve('R3', BF16, [P, T])
        mbT = carve('R3', BF16, [P, 17, 128])
        ukT_pad = carve('R2', BF16, [P, 8, 128])
        uv_pad = carve('R2', BF16, [P, 8, 128])
        wuk_bf = carve('R2', BF16, [P, 512])
        qT_t = carve('R2', BF16, [P, 4, 128])
        qabsT_t = carve('R2', BF16, [P, 8, 128])
        absq = carve('R2', BF16, [P, 8, 128])
        iqT_t = carve('R2', BF16, [P, 8, 128])
        rbuf = [carve('R2', BF16, [P, 512]) for _ in range(4)]
        pT = [carve('R2', BF16, [P, 512]) for _ in range(3)]
        diag = carve('R2', BF16, [P, 8, 128])
        olatT_t = carve('R2', BF16, [P, 8, 128])
        negm8 = carve('R2', BF16, [P, 128])
        bis = carve('R2', F32, [P, 4 * (NIT + 2)])
        rsum = carve('R2', F32, [P, 8])
        thr = carve('R2', F32, [P, 2])

        if stop_after >= 4:
            pg.dma('pool', wuk_bf, wuk_d, writes=[B('wuk_bf')])
            pg.dma('pool', w_ba, wba_d.rearrange("(m p) n -> p m n", p=128),
                   writes=[B('w_ba'), B('gbc', 0), B('gbc', 1)])
            pg.dma('pool', w_bp, wbp_d.rearrange("(m p) n -> p m n", p=128),
                   writes=[B('w_bp'), B('xt', 0), B('xt', 1)])
            pg.op('pool', lambda e: e.memset(ukT_pad, 0.0), writes=[B('ukT_pad')])
            pg.op('pool', lambda e: e.memset(uv_pad, 0.0), writes=[B('uv_pad')])
            for hp in range(2):
                src = wuv_d.rearrange("c (m two d) -> c m two d", two=2, d=64)[:, :, hp, :]
                dst = uv_pad.rearrange("c (m two) n -> c m two n", two=2)[:, :, hp, 64 * hp:64 * hp + 64]
                pg.dma('pool', dst, src, reads=[B('uv_pad')], writes=[B('uv_pad', hp)])
            for m in range(4):
                bk = nxt('gen', allb)
                pg.op('pe', lambda e, bk=bk, m=m: e.transpose(
                    out=banks_bf[bk][:, 0:128], in_=wuk_bf[:, m * 128:(m + 1) * 128], identity=ident_bf),
                    reads=[B('wuk_bf'), B('ident_bf')], writes=[PB[bk]])
                for hp in range(2):
                    h = 2 * m + hp
                    pg.op('dve', lambda e, bk=bk, h=h, hp=hp: e.tensor_copy(
                        out=ukT_pad[64 * hp:64 * hp + 64, h, :], in_=banks_bf[bk][64 * hp:64 * hp + 64, 0:128]),
                        reads=[PB[bk], B('ukT_pad')], writes=[B('ukT_pad', h)])
            pg.op('dve', lambda e: e.tensor_reduce(out=cmax, in_=cnT, axis=AX.X, op=ALU.max,
                                                   apply_absolute_value=True),
                  reads=[B('cnT', j) for j in tiles], writes=[B('cmax')])
            pg.op('pool', lambda e: e.memset(ones_bf, 1.0), writes=[B('ones_bf')])
            pg.op('dve', lambda e: e.tensor_copy(out=cmaxB, in_=mk_ap(cmax, 0, [[pstep(cmax), 128], [0, 128]])),
                  reads=[B('cmax')], writes=[B('cmaxB')])
            pg.op('pool', lambda e: e.memset(cmaxsel, 0.0), writes=[B('cmaxsel')])
            pg.op('pool', lambda e: e.memset(mbT[:, 16, :], NEG), writes=[B('mbT', 16)])
            for h in range(8):
                pg.op('dve', lambda e, h=h: e.tensor_copy(out=cmaxsel[:, h, h:h + 1], in_=cmax),
                      reads=[B('cmax'), B('cmaxsel')], writes=[B('cmaxsel', h)])
        ukb = [B('ukT_pad')] + [B('ukT_pad', h) for h in range(8)]
        uvb = [B('uv_pad'), B('uv_pad', 0), B('uv_pad', 1)]
        cmb = [B('cmaxsel')] + [B('cmaxsel', h) for h in range(8)]
        ikb = [B('ikT', bi) for bi in range(5)]
        cnb = [B('cnT', j) for j in tiles]
        ckb = [B('ckeys', c) for c in range(17)] + [B('ckeys_ones')]

        mids = bis[:, 0:NIT + 2]
        cnts = bis[:, NIT + 2:2 * (NIT + 2)]
        dds = bis[:, 2 * (NIT + 2):3 * (NIT + 2)]
        Wc = bis[:, 3 * (NIT + 2):4 * (NIT + 2)]

        n_attn_tiles = NT if stop_after >= 4 else 0
        n_attn_tiles = debug.get('n_attn_tiles', n_attn_tiles)
        qabs2 = [qabsT_t, carve('R2', BF16, [P, 8, 128]), carve('R2', BF16, [P, 8, 128])]
        mb2 = [mb, carve('R3', BF16, [P, T])]
        assert cursor['R3'] <= W_Q_OFF, cursor['R3']
        score2 = [score, carve('R2', F32, [P, T])]
        negsh = carve('R2', F32, [P, 8])
        lnS = carve('R2', F32, [P, 512])
        rcpS = carve('R2', F32, [P, 512])
        ROT4 = [0, 1, 2, 3]
        PSS = [6, 7]
        ACC = [4, 5]

        def act_copy(out_ap, in_ap, reads, writes):
            pg.op('act', lambda e: e.copy(out=out_ap, in_=in_ap), reads=reads, writes=writes)

        def chunks_of(j):
            S = 16 + 128 * (j + 1)
            return [(0, 16)] + [(16 + 512 * m, min(16 + 512 * (m + 1), S)) for m in range((S - 16 + 511) // 512)]

        def stageA(j, tick=None):
            par = j % 2
            par3 = j % 3
            sc, qa = score2[par], qabs2[par3]
            qc = 16 + 128 * j
            ub = [B('uT', j)]
            for h in range(8):
                pg.op('act', lambda e, h=h, j=j: e.activation(out=diag[:, h, :], in_=ident_f, func=AF.Identity,
                                                              scale=iw_all[:, j, h:h + 1]),
                      reads=[B('ident_f'), B('iw', j)], writes=[B('diag', h)])
            items = []
            for (c0, c1) in chunks_of(j):
                bs = nxt('pss', PSS)
                for hp_ in range(4):
                    items.append((c0, c1, bs, hp_))

            def emit_x(it):
                c0, c1, bs, hp_ = it
                n = c1 - c0
                res = []
                for hh in range(2):
                    h = 2 * hp_ + hh
                    bx = nxt('rot4', ROT4)
                    g_, a_ = h // 3, h % 3
                    pg.op('pe', lambda e, bx=bx, g_=g_, a_=a_, c0=c0, c1=c1, n=n: e.matmul(
                        banks[bx][:, 0:n], lhsT=iqT_all[32 * a_:32 * a_ + 32, g_, 128 * j:128 * (j + 1)],
                        rhs=ikT[32 * a_:32 * a_ + 32, c0:c1], start=True, stop=True),
                        reads=[B('iqT_all', g_, j // 4)] + ikb, writes=[PB[bx]])
                    ri = nxt('rbuf', [0, 1, 2, 3])
                    pg.op('act', lambda e, bx=bx, ri=ri, n=n: e.activation(
                        out=rbuf[ri][:, 0:n], in_=banks[bx][:, 0:n], func=AF.Relu),
                        reads=[PB[bx]], writes=[B('rbuf', ri)])
                    res.append((h, ri))
                return res

            def emit_d(it, res):
                c0, c1, bs, hp_ = it
                n = c1 - c0
                for (h, ri) in res:
                    pg.op('pe', lambda e, bs=bs, h=h, ri=ri, n=n: e.matmul(
                        banks[bs][:, 0:n], lhsT=diag[:, h, :], rhs=rbuf[ri][:, 0:n], start=(h == 0), stop=(h == 7)),
                        reads=[B('diag', h), B('rbuf', ri)], writes=[PB[bs]])
                if hp_ == 3:
                    act_copy(sc[:, c0:c1], banks[bs][:, 0:n], [PB[bs]], [B('score', par, c0)])

            prev = None
            for it in items:
                res = emit_x(it)
                if prev is not None:
                    emit_d(*prev)
                prev = (it, res)
                if tick is not None:
                    tick()
            emit_d(*prev)
            bk = nxt('rot4', ROT4)

            def mmq(e, bk=bk, qc=qc):
                last = None
                for m in range(4):
                    for kc in range(KC):
                        last = e.matmul(banks[bk][:, m * 128:(m + 1) * 128], lhsT=w_q[:, kc, m * 128:(m + 1) * 128],
                                        rhs=uT[:, kc, qc:qc + 128], start=(kc == 0), stop=(kc == KC - 1))
                return last
            pg.op('pe', mmq, reads=ub + [B('w_q')], writes=[PB[bk]])
            act_copy(qT_t, banks[bk][:, 0:512].rearrange("p (a b) -> p a b", a=4), [PB[bk]], [B('qT_t')])
            for half in range(2):
                bk = nxt('rot4', ROT4)

                def mma(e, bk=bk, half=half):
                    last = None
                    for hh in range(4):
                        h = 4 * half + hh
                        last = e.matmul(banks[bk][:, hh * 128:(hh + 1) * 128], lhsT=ukT_pad[:, h, :],
                                        rhs=qT_t[:, h // 2, :], start=True, stop=True)
                    return last
                pg.op('pe', mma, reads=ukb + [B('qT_t')], writes=[PB[bk]])
                src3 = banks[bk][:, 0:512].rearrange("p (a b) -> p a b", a=4)
                act_copy(qa[:, 4 * half:4 * half + 4, :], src3, [PB[bk]], [B('qabsT_t', par3, half)])
                pg.op('dve', lambda e, half=half, qa=qa: e.scalar_tensor_tensor(
                    out=absq[:, 4 * half:4 * half + 4, :], in0=qa[:, 4 * half:4 * half + 4, :], scalar=-1.0,
                    in1=qa[:, 4 * half:4 * half + 4, :], op0=ALU.mult, op1=ALU.max),
                    reads=[B('qabsT_t', par3, half)], writes=[B('absq', half)])

        def stageA2(j):
            par = j % 3
            for half in range(2):
                bk = nxt('rot4', ROT4)

                def mmb(e, bk=bk, half=half):
                    last = None
                    for hh in range(4):
                        last = e.matmul(banks[bk][:, hh * 128:(hh + 1) * 128], lhsT=cmaxB,
                                        rhs=absq[:, 4 * half + hh, :], start=True, stop=True)
                    return last
                pg.op('pe', mmb, reads=[B('cmaxB'), B('absq', half)], writes=[PB[bk]])
                pg.op('dve', lambda e, bk=bk, half=half: e.tensor_reduce(
                    out=negsh[:, 4 + half:5 + half], in_=banks[bk][:, 0:512], axis=AX.X, op=ALU.max),
                    reads=[PB[bk]], writes=[B('negsh_t', half)])
            pg.op('dve', lambda e: e.tensor_tensor(out=negsh[:, 6:7], in0=negsh[:, 4:5], in1=negsh[:, 5:6], op=ALU.max),
                  reads=[B('negsh_t', 0), B('negsh_t', 1)], writes=[B('negsh_m')])
            pg.op('dve', lambda e, par=par: e.tensor_scalar(out=negsh[:, par:par + 1], in0=negsh[:, 6:7],
                                                            scalar1=-ATTN_SCALE, scalar2=None, op0=ALU.mult),
                  reads=[B('negsh_m')], writes=[B('negsh', par)])

        def scbufs(j):
            return [B('score', j % 2, c0) for (c0, c1) in chunks_of(j)]

        def stageB_init(j):
            S = 16 + 128 * (j + 1)
            sc = score2[j % 2]
            scb = scbufs(j)
            Rr, Rb = stat2()
            pg.op('dve', lambda e: e.tensor_reduce(out=Rr, in_=sc[:, 0:S], axis=AX.X, op=ALU.max,
                                                   apply_absolute_value=True),
                  reads=scb, writes=Rb)
            pg.op('dve', lambda e: e.tensor_tensor(out=sc[:, S - 128:S], in0=sc[:, S - 128:S],
                                                   in1=blockbias, op=ALU.add),
                  reads=scb + [B('blockbias')], writes=[B('score', j % 2, 16 + 512 * (j // 4))])
            pg.op('dve', lambda e: e.tensor_scalar(out=Wc, in0=pow2, scalar1=Rr, scalar2=1e-30,
                                                   op0=ALU.mult, op1=ALU.add),
                  reads=Rb + [B('pow2')], writes=[B('Wc')])
            pg.op('dve', lambda e: e.memset(mids[:, 0:1], 0.0), writes=[B('mid', 0)])

        def stageB_iter(j, n_):
            S = 16 + 128 * (j + 1)
            sc = score2[j % 2]
            pg.op('dve', lambda e: e.tensor_scalar(
                out=junkbf[:, 0:S], in0=sc[:, 0:S], scalar1=mids[:, n_ - 1:n_], scalar2=None,
                op0=ALU.is_ge, op1=ALU.add, accum_out=cnts[:, n_ - 1:n_]),
                reads=scbufs(j) + [B('mid', n_ - 1)], writes=[B('junkbf'), B('cnt', n_)])
            pg.op('dve', lambda e: e.scalar_tensor_tensor(
                out=dds[:, n_ - 1:n_], in0=cnts[:, n_ - 1:n_], scalar=KTOP - 0.5, in1=Wc[:, n_ - 1:n_],
                op0=ALU.is_ge, op1=ALU.mult),
                reads=[B('cnt', n_), B('Wc')], writes=[B('dd', n_)])
            pg.op('dve', lambda e: e.scalar_tensor_tensor(
                out=mids[:, n_:n_ + 1], in0=dds[:, n_ - 1:n_], scalar=Wc[:, n_:n_ + 1], in1=mids[:, n_ - 1:n_],
                op0=ALU.subtract, op1=ALU.add),
                reads=[B('dd', n_), B('Wc'), B('mid', n_ - 1)], writes=[B('mid', n_)])

        def stageB_fin(j):
            pg.op('dve', lambda e: e.tensor_tensor(out=thr[:, 0:1], in0=mids[:, NIT:NIT + 1], in1=Wc[:, NIT:NIT + 1],
                                                   op=ALU.subtract),
                  reads=[B('mid', NIT), B('Wc')], writes=[B('thr')])
            S = 16 + 128 * (j + 1)
            sc = score2[j % 2]
            mbj = mb2[j % 2]
            pg.op('dve', lambda e: e.tensor_scalar(out=mbj[:, 0:S], in0=sc[:, 0:S], scalar1=thr[:, 0:1],
                                                   scalar2=NEG, op0=ALU.is_lt, op1=ALU.mult),
                  reads=scbufs(j) + [B('thr')], writes=[B('mb', j % 2)])

        def stageC(j):
            mbj = mb2[j % 2]
            klist = [16] + list(range(j + 1))
            for g0 in range(0, len(klist), 8):
                grp = klist[g0:g0 + 8]
                bk = nxt('rot4', ROT4)

                def tp(e, grp=grp, bk=bk):
                    last = None
                    for si, i in enumerate(grp):
                        c_ = 0 if i == 16 else 16 + 128 * i
                        last = e.transpose(out=banks_bf[bk][:, si * 128:(si + 1) * 128], in_=mbj[:, c_:c_ + 128],
                                           identity=ident_bf)
                    return last
                pg.op('pe', tp, reads=[B('mb', j % 2), B('ident_bf')], writes=[PB[bk]])
                si0 = 0
                if grp[0] == 16:
                    pg.op('dve', lambda e, bk=bk: e.tensor_copy(out=mbT[0:16, 16, :], in_=banks_bf[bk][0:16, 0:128]),
                          reads=[PB[bk]], writes=[B('mbT', 16)])
                    si0 = 1
                nn_ = len(grp) - si0
                if nn_ > 0:
                    i0 = grp[si0]
                    srcv = banks_bf[bk][:, si0 * 128:(si0 + nn_) * 128].rearrange("p (a b) -> p a b", a=nn_)
                    pg.op('dve', lambda e, i0=i0, nn_=nn_, srcv=srcv: e.tensor_copy(out=mbT[:, i0:i0 + nn_, :], in_=srcv),
                          reads=[PB[bk]], writes=[B('mbT', i) for i in range(i0, i0 + nn_)])

        def stageD_lg(j, q, ci):
            par = j % 3
            qa = qabs2[par]
            xch = [16] + list(range(j + 1))
            i = xch[ci]
            bl = nxt('rot4', ROT4)
            kc0 = 0 if i == 16 else 16 + 128 * i
            mrow = mk_ap(mbT, i * 128, [[pstep(mbT), 128], [0, 4], [1, 128]])
            qrhs = qa[:, 4 * q:4 * q + 4, :]

            def lg(e):
                e.matmul(banks[bl][:, 0:512], lhsT=cnT[:, kc0:kc0 + 128], rhs=qrhs, start=True, stop=False)
                return e.matmul(banks[bl][:, 0:512], lhsT=ident_bf, rhs=mrow, start=False, stop=True)
            pg.op('pe', lg, reads=cnb + [B('qabsT_t', par, q), B('ident_bf'), B('mbT', i)], writes=[PB[bl]])
            pi = nxt('pT', [0, 1, 2])
            pg.op('act', lambda e: e.activation(out=pT[pi][:, 0:512], in_=banks[bl][:, 0:512], func=AF.Exp,
                                                scale=ATTN_SCALE, bias=negsh[:, par:par + 1]),
                  reads=[PB[bl], B('negsh', par)], writes=[B('pT', pi)])
            return pi

        def stageD_pv(j, q, ci, pi):
            xch = [16] + list(range(j + 1))
            i = xch[ci]
            bO, bS = ACC
            first, last_ = (ci == 0), (ci == len(xch) - 1)

            def pv(e):
                e.matmul(banks[bO][:, 0:512], lhsT=ckeys[:, i, 0:128], rhs=pT[pi][:, 0:512], start=first, stop=last_)
                return e.matmul(banks[bS][:, 0:512], lhsT=ones_bf, rhs=pT[pi][:, 0:512], start=first, stop=last_)
            pg.op('pe', pv, reads=[B('pT', pi), B('ones_bf'), B('ckeys', i)], writes=[PB[bO], PB[bS]])

        def stageD_qfin(j, q):
            bO, bS = ACC
            pg.op('act', lambda e: e.activation(out=lnS, in_=banks[bS][:, 0:512], func=AF.Ln),
                  reads=[PB[bS]], writes=[B('lnS')])
            pg.op('act', lambda e: e.activation(out=rcpS, in_=lnS, func=AF.Exp, scale=-1.0),
                  reads=[B('lnS')], writes=[B('rcpS')])
            pg.op('dve', lambda e: e.tensor_tensor(
                out=olatT_t[:, 4 * q:4 * q + 4, :], in0=banks[bO][:, 0:512].rearrange("p (a b) -> p a b", a=4),
                in1=rcpS.rearrange("p (a b) -> p a b", a=4), op=ALU.mult),
                reads=[PB[bO], B('rcpS')], writes=[B('olatT_t', q)])

        def stageD_fin(j):
            bk = nxt('rot4', ROT4)

            def mmo(e, bk=bk):
                last = None
                for m in range(4):
                    o = banks[bk][:, m * 128:(m + 1) * 128]
                    e.matmul(o, lhsT=uv_pad[:, 2 * m, :], rhs=olatT_t[:, 2 * m, :], start=True, stop=False)
                    last = e.matmul(o, lhsT=uv_pad[:, 2 * m + 1, :], rhs=olatT_t[:, 2 * m + 1, :], start=False, stop=True)
                return last
            pg.op('pe', mmo, reads=uvb + [B('olatT_t', 0), B('olatT_t', 1)], writes=[PB[bk]])
            act_copy(attnT[:, :, 128 * j:128 * (j + 1)], banks[bk][:, 0:512].rearrange("p (a b) -> p a b", a=4),
                     [PB[bk]], [B('attnT', j)])

        nA = n_attn_tiles

        def fullB(j):
            stageB_init(j)
            if 16 + 128 * (j + 1) <= KTOP:
                pg.op('dve', lambda e: e.tensor_reduce(out=cnts[:, 0:1], in_=Wc[:, 1:NIT + 1], axis=AX.X, op=ALU.add),
                      reads=[B('Wc')], writes=[B('cnt', 1)])
                pg.op('dve', lambda e: e.tensor_scalar(out=mids[:, NIT:NIT + 1], in0=cnts[:, 0:1], scalar1=-1.0,
                                                       scalar2=None, op0=ALU.mult),
                      reads=[B('cnt', 1)], writes=[B('mid', NIT)])
            else:
                for n_ in range(1, NIT + 1):
                    stageB_iter(j, n_)
            stageB_fin(j)

        if nA > 2:
            stageA(0)
            fullB(0)
            stageA(1)
            stageB_init(1)
            stageA2(0)
            for n_ in (1, 2, 3):
                stageB_iter(1, n_)
            stageA2(1)
            for n_ in (4, 5, 6):
                stageB_iter(1, n_)
            stageC(0)
            for n_ in (7, 8):
                stageB_iter(1, n_)
            assert 4 * len(chunks_of(2)) == 8
            pro_n = [8]

            def pro_tick():
                pro_n[0] += 1
                if pro_n[0] <= NIT:
                    stageB_iter(1, pro_n[0])
            stageA(2, pro_tick)
            for n_ in range(pro_n[0] + 1, NIT + 1):
                stageB_iter(1, n_)
            stageA2(2)
            stageB_fin(1)
        elif nA > 0:
            stageA(0)
            fullB(0)
            if nA > 1:
                stageA(1)
            stageA2(0)
            if nA > 1:
                stageA2(1)
            stageC(0)
            if nA > 1:
                fullB(1)
        carry_sched = None
        for i in range(nA):
            hasB = i + 2 < nA
            hasA = i + 3 < nA
            nch = i + 2
            seq = [(q, ci) for q in range(2) for ci in range(nch)]
            steps = len(seq)
            n_items = 4 * len(chunks_of(i + 3)) if hasA else 0
            wD, wA = 1.0 * steps, 1.7 * n_items
            ticks = [('D', k) for k in range(steps)] + [('A', k) for k in range(n_items)]
            wts = [1.0] * steps + [1.7] * n_items
            split_last = hasB and not hasA and i + 1 < nA
            if split_last:
                steps2 = 2 * (i + 3)
                ticks = ticks + [('E', k) for k in range(steps2)]
                wts = wts + [1.0] * steps2
            tot = sum(wts)
            sched = {}
            acc, done = 0.0, 0
            for tk, w in zip(ticks, wts):
                acc += w
                upto = int(round(NIT * acc / tot))
                sched[tk] = list(range(done + 1, upto + 1))
                done = upto
            if hasB:
                stageB_init(i + 2)
            pis = {}
            pis[0] = stageD_lg(i, *seq[0])
            for k, (q, ci) in enumerate(seq):
                if k + 1 < steps:
                    pis[k + 1] = stageD_lg(i, *seq[k + 1])
                stageD_pv(i, q, ci, pis[k])
                if hasB:
                    for n_ in sched[('D', k)]:
                        stageB_iter(i + 2, n_)
                if carry_sched is not None:
                    for n_ in carry_sched[('E', k)]:
                        stageB_iter(i + 1, n_)
                if ci == nch - 1:
                    stageD_qfin(i, q)
            stageD_fin(i)
            if hasA:
                cnt = [0]

                def tick(cnt=cnt):
                    if hasB:
                        for n_ in sched[('A', cnt[0])]:
                            stageB_iter(i + 2, n_)
                    cnt[0] += 1
                stageA(i + 3, tick)
            if hasB and not split_last:
                stageB_fin(i + 2)
            if carry_sched is not None:
                stageB_fin(i + 1)
            carry_sched = sched if split_last else None
            if i + 1 < nA:
                stageC(i + 1)
            if hasA:
                stageA2(i + 3)

        pg.barrier()
        reset('R2')
        reset('R3')
        mergedT = carve('R3', BF16, [P, 8, TX])
        wgA2 = [carve('R2', BF16, [P, KC, 256]) for _ in range(2)]
        wgP2 = [carve('R2', BF16, [P, KC, 256]) for _ in range(2)]
        w_out = carve('R2', BF16, [P, 8, D])
        off_wout_end = cursor['R2']
        sA = [carve('R2', F32, [P, 512]) for _ in range(2)]
        sP = [carve('R2', F32, [P, 512]) for _ in range(2)]
        run_tail = stop_after >= 5

        def load_wg(grp):
            s_ = grp % 2
            pg.dma('pool', wgA2[s_], win_p[:, :, 1448 + 256 * grp:1448 + 256 * (grp + 1)], writes=[B('wgA', s_)])
            pg.dma('pool', wgP2[s_], win_p[:, :, 2472 + 256 * grp:2472 + 256 * (grp + 1)], writes=[B('wgP', s_)])

        if run_tail:
            load_wg(0)
            load_wg(1)
            pg.dma('pool', w_out, wout_d.rearrange("(m p) n -> p m n", p=128), writes=[B('w_out')])
            for grp in range(4):
                gs = grp % 2
                wgA, wgP = wgA2[gs], wgP2[gs]
                if 1 <= grp < 3:
                    load_wg(grp + 1)
                for n4 in range(2):
                    nn = 2 * grp + n4
                    for b in range(4):
                        bset = nxt('tailA', [[0, 1, 2, 3], [4, 5, 6, 7]])
                        c0 = 16 + 512 * b
                        bA, bP, bgA, bgP = bset

                        def mmA(e, bA=bA, nn=nn, b=b):
                            last = None
                            for m in range(4):
                                last = e.matmul(banks[bA][:, 0:512], lhsT=w_ba[:, m, nn * 128:(nn + 1) * 128],
                                                rhs=attnT[:, m, 512 * b:512 * (b + 1)], start=(m == 0), stop=(m == 3))
                            return last
                        pg.op('pe', mmA, reads=[B('w_ba')] + [B('attnT', 4 * b + q) for q in range(4)], writes=[PB[bA]])

                        def mmP(e, bP=bP, nn=nn, b=b):
                            last = None
                            for m in range(4):
                                last = e.matmul(banks[bP][:, 0:512], lhsT=w_bp[:, m, nn * 128:(nn + 1) * 128],
                                                rhs=poolT[:, m, 512 * b:512 * (b + 1)], start=(m == 0), stop=(m == 3))
                            return last
                        pg.op('pe', mmP, reads=[B('w_bp'), B('poolT', b)], writes=[PB[bP]])

                        def mmg(e, bk, wt, n4=n4, c0=c0):
                            last = None
                            for kc in range(KC):
                                last = e.matmul(banks[bk][:, 0:512], lhsT=wt[:, kc, n4 * 128:(n4 + 1) * 128],
                                                rhs=uT[:, kc, c0:c0 + 512], start=(kc == 0), stop=(kc == KC - 1))
                            return last
                        pg.op('pe', lambda e, bgA=bgA, f=mmg, wgA=wgA: f(e, bgA, wgA),
                              reads=uT_bufs(c0, c0 + 512) + [B('wgA', gs)], writes=[PB[bgA]])
                        pg.op('pe', lambda e, bgP=bgP, f=mmg, wgP=wgP: f(e, bgP, wgP),
                              reads=uT_bufs(c0, c0 + 512) + [B('wgP', gs)], writes=[PB[bgP]])
                        si = nxt('sAP', [0, 1])
                        pg.op('act', lambda e, bgA=bgA, si=si: e.activation(out=sA[si], in_=banks[bgA][:, 0:512], func=AF.Sigmoid),
                              reads=[PB[bgA]], writes=[B('sA', si)])
                        pg.op('act', lambda e, bgP=bgP, si=si: e.activation(out=sP[si], in_=banks[bgP][:, 0:512], func=AF.Sigmoid),
                              reads=[PB[bgP]], writes=[B('sP', si)])
                        pg.op('dve', lambda e, bA=bA, si=si: e.tensor_tensor(out=sA[si], in0=sA[si], in1=banks[bA][:, 0:512], op=ALU.mult),
                              reads=[B('sA', si), PB[bA]], writes=[B('sA', si)])
                        pg.op('dve', lambda e, bP=bP, si=si: e.tensor_tensor(out=sP[si], in0=sP[si], in1=banks[bP][:, 0:512], op=ALU.mult),
                              reads=[B('sP', si), PB[bP]], writes=[B('sP', si)])
                        pg.op('dve', lambda e, si=si, nn=nn, b=b: e.tensor_tensor(
                            out=mergedT[:, nn, 512 * b:512 * (b + 1)], in0=sA[si], in1=sP[si], op=ALU.add),
                            reads=[B('sA', si), B('sP', si)], writes=[B('mergedT', nn, b)])

        pg.barrier()
        reset('R1')
        reset('R2')
        hacc = carve('R1', F32, [P, NT, D])
        w_r = carve('R2', BF16, [P, KC, 36])
        rbias = carve('R2', F32, [P, 36])
        gate_all = carve('R2', F32, [P, NT, 32])
        rt_ = carve('R2', F32, [P, 256])
        cur_save = cursor['R2']
        lg_all = carve('R2', F32, [P, NT, 36])
        ssq = carve('R2', F32, [P, NT])
        rstd2 = carve('R2', F32, [P, NT])
        rv = carve('R2', F32, [P, 1600])
        assert cursor['R2'] <= off_wout_end - 16384, cursor['R2']
        if run_tail:
            pg.dma('pool', w_r[:, :, 0:4], wgr_d.rearrange("(p k) n -> p k n", k=KC), writes=[B('w_r', 0)])
            pg.dma('pool', w_r[:, :, 4:36], wer_d.rearrange("(p k) n -> p k n", k=KC), writes=[B('w_r', 1)])
            pg.dma('sp', rbias[:, 0:4], bgr_d.partition_broadcast(128), writes=[B('rbias', 0)])
            pg.dma('sp', rbias[:, 4:36], ber_d.partition_broadcast(128), writes=[B('rbias', 1)])
            pg.dma('sp', gbc[1], g2_d.partition_broadcast(128), writes=[B('gbc', 1)])
            for j in range(NT):
                b = j // 4
                k = nxt('xt', [0, 1])
                pg.dma('sp', xt[k], x_d[128 * j:128 * (j + 1), :], writes=[B('xt', k)])
                for hh in range(2):
                    bk = nxt('tailB', [0, 1, 2, 3])

                    def mmh(e, bk=bk, j=j, hh=hh):
                        last = None
                        for nn in range(8):
                            last = e.matmul(banks[bk][:, 0:512], lhsT=mergedT[:, nn, 128 * j:128 * (j + 1)],
                                            rhs=w_out[:, nn, 512 * hh:512 * (hh + 1)], start=(nn == 0), stop=(nn == 7))
                        return last
                    pg.op('pe', mmh, reads=[B('mergedT', nn, b) for nn in range(8)] + [B('w_out')] + [B('R3a', q) for q in range(6)],
                          writes=[PB[bk]])
                    pg.op('dve', lambda e, bk=bk, j=j, hh=hh, k=k: e.tensor_tensor(
                        out=hacc[:, j, 512 * hh:512 * (hh + 1)], in0=banks[bk][:, 0:512], in1=xt[k][:, 512 * hh:512 * (hh + 1)],
                        op=ALU.add), reads=[PB[bk], B('xt', k)], writes=[B('hacc', j, hh)])
                pg.op('act', lambda e, j=j: e.activation(out=junkbf[:, 0:D], in_=hacc[:, j, :], func=AF.Square,
                                                         accum_out=ssq[:, j:j + 1]),
                      reads=[B('hacc', j, 0), B('hacc', j, 1)], writes=[B('junkbf'), B('ssq', j)])
            pg.op('act', lambda e: e.activation(out=rstd2, in_=ssq, func=AF.Sqrt, scale=1.0 / D, bias=eps_t),
                  reads=[B('ssq', j) for j in range(NT)] + [B('eps')], writes=[B('rstd2s')])
            pg.op('dve', lambda e: e.reciprocal(out=rstd2, in_=rstd2), reads=[B('rstd2s')], writes=[B('rstd2')])
            def s2_front(j):
                k = nxt('xn', [0, 1])
                pg.op('dve', lambda e: e.scalar_tensor_tensor(
                    out=xn[k], in0=hacc[:, j, :], scalar=rstd2[:, j:j + 1], in1=gbc[1], op0=ALU.mult, op1=ALU.mult),
                    reads=[B('hacc', j, 0), B('hacc', j, 1), B('rstd2'), B('gbc', 1)], writes=[B('xn', k)])
                bk = nxt('tp', [4, 5])

                def tps(e):
                    last = None
                    for kc in range(KC):
                        last = e.transpose(out=banks_bf[bk][:, kc * 128:(kc + 1) * 128],
                                           in_=xn[k].rearrange("t (p k) -> t k p", k=KC)[:, kc, :], identity=ident_bf)
                    return last
                pg.op('pe', tps, reads=[B('xn', k), B('ident_bf')], writes=[PB[bk]])
                srcv = banks_bf[bk][:, 0:1024].rearrange("p (a b) -> p a b", a=KC)
                pg.op('act', lambda e: e.copy(out=uT[:, :, 16 + 128 * j:16 + 128 * (j + 1)], in_=srcv),
                      reads=[PB[bk]], writes=[B('uT', j)])

            s2_bk = {}

            def s2_back(j):
                bk2 = nxt('rt', [6, 7])
                s2_bk[j] = bk2

                def mmr(e):
                    last = None
                    for kc in range(KC):
                        last = e.matmul(banks[bk2][:, 0:36], lhsT=uT[:, kc, 16 + 128 * j:16 + 128 * (j + 1)],
                                        rhs=w_r[:, kc, :], start=(kc == 0), stop=(kc == KC - 1))
                    return last
                pg.op('pe', mmr, reads=[B('uT', j), B('w_r', 0), B('w_r', 1)], writes=[PB[bk2]])

            def s2_add(j):
                bk2 = s2_bk[j]
                pg.op('dve', lambda e: e.tensor_tensor(out=lg_all[:, j, :], in0=banks[bk2][:, 0:36], in1=rbias,
                                                       op=ALU.add),
                      reads=[PB[bk2], B('rbias', 0), B('rbias', 1)], writes=[B('lg_all', j)])

            for t_ in range(NT + 3):
                if t_ >= 3:
                    s2_add(t_ - 3)
                if 2 <= t_ < NT + 2:
                    s2_back(t_ - 2)
                if t_ < NT:
                    s2_front(t_)
            RV = B('rv')
            off = [0]

            def rvv(n):
                a_ = rv[:, off[0]:off[0] + n]
                off[0] += n
                return a_
            gmax, gsum, pgp, m1, m2, dm, ed, den, p1, p2 = [rvv(NT) for _ in range(10)]
            goh, gex = rvv(NT * 4), rvv(NT * 4)
            esel, oh1, esel2, oh2, t1, t2 = [rvv(NT * 8) for _ in range(6)]
            tmp32 = rvv(NT * 32)
            pl = pstep(lg_all)
            pr = pstep(rv)

            def v3(ap, n):
                return ap.rearrange("p (a b) -> p a b", a=NT)

            def bc_last(ap, n):
                return mk_ap(ap, 0, [[pr, 128], [1, NT], [0, n]])
            gl = lg_all[:, :, 0:4]
            el4 = mk_ap(lg_all, 4, [[pl, 128], [36, NT], [8, 4], [1, 8]])
            lgb = [B('lg_all', j) for j in range(NT)]

            def dv(fn, extra=()):
                pg.op('dve', fn, reads=[RV] + list(extra), writes=[RV])
            dv(lambda e: e.tensor_reduce(out=gmax, in_=gl, axis=AX.X, op=ALU.max), lgb)
            dv(lambda e: e.tensor_tensor(out=v3(goh, 4), in0=gl, in1=bc_last(gmax, 4), op=ALU.is_ge), lgb)
            dv(lambda e: e.tensor_tensor(out=v3(gex, 4), in0=gl, in1=bc_last(gmax, 4), op=ALU.subtract), lgb)
            pg.op('act', lambda e: e.activation(out=gex, in_=gex, func=AF.Exp), reads=[RV], writes=[RV])
            dv(lambda e: e.tensor_reduce(out=gsum, in_=v3(gex, 4), axis=AX.X, op=ALU.add))
            dv(lambda e: e.reciprocal(out=pgp, in_=gsum))
            goh4 = mk_ap(goh, 0, [[pr, 128], [4, NT], [1, 4], [0, 8]])
            t4 = mk_ap(tmp32, 0, [[pr, 128], [32, NT], [8, 4], [1, 8]])
            dv(lambda e: e.tensor_tensor(out=t4, in0=el4, in1=goh4, op=ALU.mult), lgb)
            t4T = mk_ap(tmp32, 0, [[pr, 128], [32, NT], [1, 8], [8, 4]])
            dv(lambda e: e.tensor_reduce(out=v3(esel, 8), in_=t4T, axis=AX.X, op=ALU.add))
            dv(lambda e: e.tensor_reduce(out=m1, in_=v3(esel, 8), axis=AX.X, op=ALU.max))
            dv(lambda e: e.tensor_tensor(out=v3(oh1, 8), in0=v3(esel, 8), in1=bc_last(m1, 8), op=ALU.is_ge))
            dv(lambda e: e.scalar_tensor_tensor(out=esel2, in0=oh1, scalar=-BIG, in1=esel, op0=ALU.mult, op1=ALU.add))
            dv(lambda e: e.tensor_reduce(out=m2, in_=v3(esel2, 8), axis=AX.X, op=ALU.max))
            dv(lambda e: e.tensor_tensor(out=v3(oh2, 8), in0=v3(esel2, 8), in1=bc_last(m2, 8), op=ALU.is_ge))
            dv(lambda e: e.tensor_tensor(out=dm, in0=m2, in1=m1, op=ALU.subtract))
            pg.op('act', lambda e: e.activation(out=ed, in_=dm, func=AF.Exp), reads=[RV], writes=[RV])
            dv(lambda e: e.tensor_scalar(out=den, in0=ed, scalar1=1.0, scalar2=None, op0=ALU.add))
            dv(lambda e: e.reciprocal(out=p1, in_=den))
            dv(lambda e: e.tensor_tensor(out=p2, in0=ed, in1=p1, op=ALU.mult))
            dv(lambda e: e.tensor_tensor(out=p1, in0=p1, in1=pgp, op=ALU.mult))
            dv(lambda e: e.tensor_tensor(out=p2, in0=p2, in1=pgp, op=ALU.mult))
            dv(lambda e: e.tensor_tensor(out=v3(t1, 8), in0=v3(oh1, 8), in1=bc_last(p1, 8), op=ALU.mult))
            dv(lambda e: e.tensor_tensor(out=v3(t2, 8), in0=v3(oh2, 8), in1=bc_last(p2, 8), op=ALU.mult))
            dv(lambda e: e.tensor_tensor(out=t1, in0=t1, in1=t2, op=ALU.add))
            wpg4 = mk_ap(t1, 0, [[pr, 128], [8, NT], [0, 4], [1, 8]])
            gout = gate_all.rearrange("p a (g e) -> p a g e", g=4)
            pg.op('dve', lambda e: e.tensor_tensor(out=gout, in0=goh4, in1=wpg4, op=ALU.mult),
                  reads=[RV], writes=[B('gate', j) for j in range(NT)])
        cursor['R2'] = cur_save

        reset('R3')
        n_exp = debug.get('n_exp', N_EXP if stop_after >= 6 else 0)
        wg_e = [carve('R3', BF16, [P, KC, 256]) for _ in range(2)]
        wu_e = [carve('R3', BF16, [P, KC, 256]) for _ in range(2)]
        wd_e = [carve('R3', BF16, [P, 2, D]) for _ in range(2)]
        silt = [carve('R3', F32, [P, 512]) for _ in range(2)]
        hid = [carve('R2', BF16, [P, 2, TX]) for _ in range(2)]

        def load_gu(e_):
            s = e_ % 2
            al = (lambda q: [B('R3a', q)]) if e_ < 2 else (lambda q: [])
            pg.dma('pool', wg_e[s], weg_d[e_].rearrange("(p k) n -> p k n", k=KC), writes=[B('wg_e', s)] + al(2 * s))
            pg.dma('pool', wu_e[s], weu_d[e_].rearrange("(p k) n -> p k n", k=KC), writes=[B('wu_e', s)] + al(2 * s + 1))

        def load_d(e_):
            s = e_ % 2
            pg.dma('pool', wd_e[s], wed_d[e_].rearrange("(f p) n -> p f n", p=128),
                   writes=[B('wd_e', s)] + ([B('R3a', 4 + s)] if e_ < 2 else []))

        def gu_group(e_, b, fh):
            s = e_ % 2
            c0 = 16 + 512 * b
            bG, bU = nxt('moeGU', [[0, 1], [2, 3]])

            def mmGU(e, bk, wt):
                last = None
                for kc in range(KC):
                    last = e.matmul(banks[bk][:, 0:512], lhsT=wt[:, kc, fh * 128:(fh + 1) * 128],
                                    rhs=uT[:, kc, c0:c0 + 512], start=(kc == 0), stop=(kc == KC - 1))
                return last
            pg.op('pe', lambda e: mmGU(e, bG, wg_e[s]), reads=uT_bufs(c0, c0 + 512) + [B('wg_e', s)],
                  writes=[PB[bG]])
            pg.op('pe', lambda e: mmGU(e, bU, wu_e[s]), reads=uT_bufs(c0, c0 + 512) + [B('wu_e', s)],
                  writes=[PB[bU]])
            si = nxt('silt', [0, 1])
            pg.op('act', lambda e: e.activation(out=silt[si], in_=banks[bG][:, 0:512], func=AF.Silu),
                  reads=[PB[bG]], writes=[B('silt', si)])
            pg.op('dve', lambda e: e.tensor_tensor(
                out=hid[s][:, fh, 512 * b:512 * (b + 1)], in0=silt[si], in1=banks[bU][:, 0:512], op=ALU.mult),
                reads=[B('silt', si), PB[bU]], writes=[B('hid', s, b)])

        def y_tile(e_, j):
            s = e_ % 2
            for hh in range(2):
                bk = nxt('moeY', [4, 5, 6, 7])

                def mmy(e, bk=bk, hh=hh):
                    e.matmul(banks[bk][:, 0:512], lhsT=hid[s][:, 0, 128 * j:128 * (j + 1)],
                             rhs=wd_e[s][:, 0, 512 * hh:512 * (hh + 1)], start=True, stop=False)
                    return e.matmul(banks[bk][:, 0:512], lhsT=hid[s][:, 1, 128 * j:128 * (j + 1)],
                                    rhs=wd_e[s][:, 1, 512 * hh:512 * (hh + 1)], start=False, stop=True)
                pg.op('pe', mmy, reads=[B('hid', s, j // 4), B('wd_e', s)], writes=[PB[bk]])
                pg.op('dve', lambda e, bk=bk, hh=hh: e.scalar_tensor_tensor(
                    out=hacc[:, j, 512 * hh:512 * (hh + 1)], in0=banks[bk][:, 0:512], scalar=gate_all[:, j, e_:e_ + 1],
                    in1=hacc[:, j, 512 * hh:512 * (hh + 1)], op0=ALU.mult, op1=ALU.add),
                    reads=[PB[bk], B('gate', j), B('hacc', j, hh)], writes=[B('hacc', j, hh)])

        if n_exp > 0:
            load_gu(0)
            load_d(0)
        for e_ in range(n_exp + 1 if n_exp > 0 else 0):
            if e_ + 1 < n_exp:
                load_gu(e_ + 1)
            for gi in range(8):
                if e_ < n_exp:
                    gu_group(e_, gi // 2, gi % 2)
                if e_ >= 1:
                    y_tile(e_ - 1, 2 * gi)
                    y_tile(e_ - 1, 2 * gi + 1)
            if e_ + 1 < n_exp:
                load_d(e_ + 1)

        if run_tail:
            pg.dma('sp', gbc[0], gf_d.partition_broadcast(128), writes=[B('gbc', 0)])
            for j in range(NT):
                hb = [B('hacc', j, 0), B('hacc', j, 1)]
                pg.op('act', lambda e, j=j: e.activation(out=junkbf[:, 0:D], in_=hacc[:, j, :], func=AF.Square,
                                                         accum_out=ssq[:, j:j + 1]),
                      reads=hb, writes=[B('ssq', j), B('junkbf')])
            pg.op('act', lambda e: e.activation(out=rstd2, in_=ssq, func=AF.Sqrt, scale=1.0 / D, bias=eps_t),
                  reads=[B('ssq', j) for j in range(NT)] + [B('eps')], writes=[B('rstd2s')])
            pg.op('dve', lambda e: e.reciprocal(out=rstd2, in_=rstd2), reads=[B('rstd2s')], writes=[B('rstd2')])
            obuf = [carve_at('R0', 4096 * i, F32, [P, D]) for i in range(8)]
            for j in range(NT):
                hb = [B('hacc', j, 0), B('hacc', j, 1)]
                k = j % 8
                if False:
                    pg.op('act', lambda e, j=j, k=k: e.activation(
                        out=obuf[k], in_=hacc[:, j, :], func=AF.Identity, scale=rstd2[:, j:j + 1]),
                        reads=hb + [B('rstd2')], writes=[B('obuf', k)])
                    pg.op('pool', lambda e, k=k: e.tensor_tensor(out=obuf[k], in0=obuf[k], in1=gbc[0], op=ALU.mult),
                          reads=[B('obuf', k), B('gbc', 0)], writes=[B('obuf', k)])
                else:
                    pg.op('dve', lambda e, j=j, k=k: e.scalar_tensor_tensor(
                        out=obuf[k], in0=hacc[:, j, :], scalar=rstd2[:, j:j + 1], in1=gbc[0], op0=ALU.mult, op1=ALU.mult),
                        reads=hb + [B('rstd2'), B('gbc', 0)], writes=[B('obuf', k)])
                pg.dma('sp', out_d[128 * j:128 * (j + 1), :], obuf[k], reads=[B('obuf', k)], writes=[B('out', j)])

        pg.barrier()
        local = dict(uT=uT, ckeys=ckeys, cnT=cnT, ikT=ikT, poolT=poolT, attnT=attnT, iw_all=iw_all, score=score,
                     mb=mb, hacc=hacc, gate_all=gate_all, mergedT=mergedT, qabsT_t=qabsT_t,
                     bis=bis, thr=thr, iqT_t=iqT_t, diag=diag, mbT=mbT)
        for name in debug.get('dump', []):
            ap = local[name]
            shp = list(ap.shape)
            n = 1
            for s_ in shp[1:]:
                n *= s_
            dd = nc.dram_tensor("dbg_" + name, [shp[0], n], F32, kind="ExternalOutput").ap()
            flat = ap
            if len(shp) == 3:
                flat = ap.rearrange("p a b -> p (a b)")
            for c0 in range(0, n, 2048):
                c1 = min(n, c0 + 2048)
                pg.dma('pool', dd[:, c0:c1], flat[:, c0:c1], reads=[], writes=[B('dbg', name, c0)])
        pg.barrier()
        pg.emit(esem, dsem)
    return nc


_CACHE = {}


def _consts():
    ident = np.eye(128, dtype=np.float32)
    p = np.arange(128)[:, None]
    s = np.arange(128)[None, :]
    blockbias = np.where((s < 64) | (p >= 64), 0.0, -BIG).astype(np.float32)
    sel8 = np.zeros((32, 8, 128), np.float32)
    for h in range(8):
        sel8[h, h, :] = 1.0
    invcnt = np.broadcast_to((1.0 / np.arange(1, 17, dtype=np.float32))[None, :], (128, 16)).copy()
    pow2 = np.broadcast_to((1.0001 * 2.0 ** (-np.arange(NIT + 2, dtype=np.float64))).astype(np.float32)[None, :],
                           (128, NIT + 2)).copy()
    return dict(c_ident=ident, c_blockbias=blockbias, c_sel8=sel8.reshape(32, 1024), c_invcnt=invcnt, c_pow2=pow2)


def make_in_maps(inputs):
    f = lambda a: np.ascontiguousarray(np.asarray(a, dtype=np.float32))
    shared = dict(
        meta=f(inputs['meta_tokens']),
        norm1_g=f(inputs['norm1_g']).reshape(1, D),
        w_in=f(inputs['w_in']).reshape(D, 3496),
        kv_norm_g=f(inputs['kv_norm_g']).reshape(1, 128),
        w_uk=f(inputs['w_uk']).reshape(128, 512),
        w_uv=f(inputs['w_uv']).reshape(128, 512),
        w_pool=f(inputs['w_pool']).reshape(4, 128, 128),
        pool_scale=f(inputs['pool_scale']).reshape(1, 512),
        w_ba=f(inputs['w_branch_attn']).reshape(512, D),
        w_bp=f(inputs['w_branch_pool']).reshape(512, D),
        w_out=f(inputs['w_out']).reshape(D, D),
        norm2_g=f(inputs['norm2_g']).reshape(1, D),
        w_gr=f(inputs['w_group_router']).reshape(D, 4),
        b_gr=f(inputs['b_group_router']).reshape(1, 4),
        w_er=f(inputs['w_expert_router']).reshape(D, 32),
        b_er=f(inputs['b_expert_router']).reshape(1, 32),
        w_eg=f(inputs['w_expert_gate']).reshape(N_EXP, D, 256),
        w_eu=f(inputs['w_expert_up']).reshape(N_EXP, D, 256),
        w_ed=f(inputs['w_expert_down']).reshape(N_EXP, 256, D),
        final_g=f(inputs['final_norm_g']).reshape(1, D),
    )
    shared.update(_consts())
    x = f(inputs['x'])
    return [dict(shared, x=x[b]) for b in range(8)]


def kernel(**inputs):
    if 'nc' not in _CACHE:
        _CACHE['nc'] = build()
    nc = _CACHE['nc']
    in_maps = make_in_maps(inputs)
    res = run_bass_kernel_spmd(nc, in_maps, core_ids=list(range(8)))
    return np.stack([np.asarray(r["out"], dtype=np.float32).reshape(TX, D) for r in res.results], axis=0)
```

```python
import math
from contextlib import ExitStack

import numpy as np
import concourse.bass as bass
import concourse.mybir as mybir
from concourse.bass_utils import run_bass_kernel_spmd

F32 = mybir.dt.float32
BF16 = mybir.dt.bfloat16
ALU = mybir.AluOpType
AF = mybir.ActivationFunctionType
AX = mybir.AxisListType

P = 128
D = 1024
KC = 8
NT = 16
TX = 2048
T = 2064
EPS = 1e-6
ATTN_SCALE = 64 ** -0.5
IDX_SCALE = (8 ** -0.5) * (32 ** -0.5)
KTOP = 256
NIT = 16
NEG = -30000.0
BIG = 1.0e30
N_EXP = 32
ENGS = ['pe', 'act', 'dve', 'pool', 'sp']
NDS = 24


class Buf:
    __slots__ = ('name', 'lw', 'rd')

    def __init__(self, name):
        self.name = name
        self.lw = None
        self.rd = {}


class Op:
    __slots__ = ('fn', 'waits', 'dma')

    def __init__(self, fn, waits, dma):
        self.fn = fn
        self.waits = waits
        self.dma = dma


class Prog:
    def __init__(self, nc):
        self.nc = nc
        self.ops = {e: [] for e in ENGS}
        self.seen = {e: {} for e in ENGS}
        self.seen_dma = {e: set() for e in ENGS}
        self.dma_info = []
        self.dma_uses = [0] * NDS
        self.dma_hist = {}
        self.bufs = {}

    def B(self, *key):
        b = self.bufs.get(key)
        if b is None:
            b = Buf(key)
            self.bufs[key] = b
        return b

    def _deps(self, reads, writes):
        toks = set()
        for b in reads:
            if b.lw is not None:
                toks.add(b.lw)
        for b in writes:
            if b.lw is not None:
                toks.add(b.lw)
            toks.update(b.rd.values())
        return toks

    def _resolve(self, eng, toks):
        best = {}
        out = []
        for t in toks:
            if t[0] == 'c':
                if t[1] == eng and eng == 'pe':
                    continue
                if best.get(t[1], -1) < t[2]:
                    best[t[1]] = t[2]
            else:
                if t[1] not in self.seen_dma[eng]:
                    self.seen_dma[eng].add(t[1])
                    out.append(t)
        for pe_, i in best.items():
            if self.seen[eng].get(pe_, -1) >= i:
                continue
            self.seen[eng][pe_] = i
            out.append(('c', pe_, i))
        return out

    def _commit(self, tok, key, reads, writes):
        for b in reads:
            b.rd[key] = tok
        for b in writes:
            b.lw = tok
            b.rd = {}

    def op(self, eng, fn, reads=(), writes=()):
        ps = [b for b in reads if b.name[0] == 'psum']
        if ps:
            writes = list(writes) + [b for b in ps if b not in writes]
            reads = [b for b in reads if b.name[0] != 'psum']
        idx = len(self.ops[eng])
        tok = ('c', eng, idx)
        waits = self._resolve(eng, self._deps(reads, writes))
        self.ops[eng].append(Op(fn, waits, None))
        self._commit(tok, eng, reads, writes)
        return tok

    def dma(self, q, out, in_, reads=(), writes=(), **kw):
        did = len(self.dma_info)
        half = NDS // 2
        base = half if q == 'pool' else 0
        hist = self.dma_hist.setdefault(base, [])
        s = base + len(hist) % half
        self.dma_uses[s] += 1
        self.dma_info.append((s, 16 * self.dma_uses[s]))
        toks = self._deps(reads, writes)
        if len(hist) >= half:
            toks.add(('d', hist[len(hist) - half]))
        hist.append(did)
        waits = self._resolve(q, toks)
        self.ops[q].append(Op(lambda e, o=out, i=in_, k=kw: e.dma_start(out=o, in_=i, **k), waits, did))
        tok = ('d', did)
        self._commit(tok, tok, reads, writes)
        return tok

    def barrier(self):
        toks = set()
        for e in ENGS:
            if e != 'sp' and self.ops[e]:
                for i in range(len(self.ops[e]) - 1, -1, -1):
                    if self.ops[e][i].dma is None and self.ops[e][i].fn is not None:
                        toks.add(('c', e, i))
                        break
        for hist in self.dma_hist.values():
            for d in hist[-(NDS // 2):]:
                toks.add(('d', d))
        for e in ENGS:
            mine = set(t for t in toks if not (t[0] == 'c' and t[1] == e and e == 'pe'))
            waits = self._resolve(e, mine)
            if waits:
                self.ops[e].append(Op(None, waits, None))

    def emit(self, esem, dsem):
        sig = {e: set() for e in ENGS}
        for e in ENGS:
            for o in self.ops[e]:
                for t in o.waits:
                    if t[0] == 'c':
                        sig[t[1]].add(t[2])
        signo = {e: {} for e in ENGS}
        for e in ENGS:
            for n, i in enumerate(sorted(sig[e])):
                signo[e][i] = n + 1
        prog = self

        def stream(ename, h):
            for i, o in enumerate(prog.ops[ename]):
                for t in o.waits:
                    if t[0] == 'c':
                        h.wait_ge(esem[t[1]], signo[t[1]][t[2]])
                    else:
                        s, val = prog.dma_info[t[1]]
                        h.wait_ge(dsem[s], val)
                if o.fn is None:
                    continue
                inst = o.fn(h)
                if o.dma is not None:
                    inst.then_inc(dsem[prog.dma_info[o.dma][0]], 16)
                elif i in sig[ename]:
                    inst.then_inc(esem[ename], 1)

        with self.nc.Block() as block:
            @block.tensor
            def _(h):
                stream('pe', h)

            @block.scalar
            def _(h):
                stream('act', h)

            @block.vector
            def _(h):
                stream('dve', h)

            @block.gpsimd
            def _(h):
                stream('pool', h)

            @block.sync
            def _(h):
                stream('sp', h)


def mk_ap(base, extra_off, dims):
    return bass.AP(base.tensor, base.offset + extra_off, dims)


def pstep(ap):
    return ap.ap[0][0]


def build(debug=None):
    debug = debug or {}
    stop_after = debug.get('stop_after', 99)
    nc = bass.Bass("TRN2", target_bir_lowering=False)

    def din(name, shape):
        return nc.dram_tensor(name, list(shape), F32, kind="ExternalInput").ap()

    x_d = din("x", [TX, D])
    meta_d = din("meta", [16, D])
    g1_d = din("norm1_g", [1, D])
    win_d = din("w_in", [D, 3496])
    gkv_d = din("kv_norm_g", [1, 128])
    wuk_d = din("w_uk", [128, 512])
    wuv_d = din("w_uv", [128, 512])
    wpool_d = din("w_pool", [4, 128, 128])
    pscale_d = din("pool_scale", [1, 512])
    wba_d = din("w_ba", [512, D])
    wbp_d = din("w_bp", [512, D])
    wout_d = din("w_out", [D, D])
    g2_d = din("norm2_g", [1, D])
    wgr_d = din("w_gr", [D, 4])
    bgr_d = din("b_gr", [1, 4])
    wer_d = din("w_er", [D, 32])
    ber_d = din("b_er", [1, 32])
    weg_d = din("w_eg", [N_EXP, D, 256])
    weu_d = din("w_eu", [N_EXP, D, 256])
    wed_d = din("w_ed", [N_EXP, 256, D])
    gf_d = din("final_g", [1, D])
    cid_d = din("c_ident", [128, 128])
    cbb_d = din("c_blockbias", [128, 128])
    csel_d = din("c_sel8", [32, 8 * 128])
    cinv_d = din("c_invcnt", [128, 16])
    cpow_d = din("c_pow2", [128, NIT + 2])
    out_d = nc.dram_tensor("out", [TX, D], F32, kind="ExternalOutput").ap()
    dbg_out = {}

    pg = Prog(nc)
    B = pg.B

    with ExitStack() as es:
        esem = {e: es.enter_context(nc.semaphore("sem_" + e)) for e in ENGS}
        dsem = [es.enter_context(nc.semaphore("dsem%d" % i)) for i in range(NDS)]

        R0_B, R1_B, R2_B, R3_B = 62 * 1024, 64 * 1024, 42 * 1024, 34 * 1024
        arenas = {}
        for nm, nb in (('R0', R0_B), ('R1', R1_B), ('R2', R2_B), ('R3', R3_B)):
            arenas[nm] = (es.enter_context(nc.sbuf_tensor(nm, [P, nb // 4], F32)), nb)
        cursor = {}

        def reset(nm):
            cursor[nm] = 0

        def carve(nm, dtype, shape):
            h, nb = arenas[nm]
            esz = 4 if dtype == F32 else 2
            n = 1
            for s in shape[1:]:
                n *= s
            off = (cursor[nm] + 63) // 64 * 64
            assert off + n * esz <= nb, (nm, off, n * esz, nb, shape)
            cursor[nm] = off + n * esz
            hv = h if dtype == F32 else h.bitcast(dtype)
            ap = hv[0:shape[0], off // esz: off // esz + n]
            if len(shape) == 3:
                ap = ap.rearrange("p (a b) -> p a b", a=shape[1])
            elif len(shape) == 4:
                ap = ap.rearrange("p (a b c) -> p a b c", a=shape[1], b=shape[2])
            return ap

        def carve_at(nm, off, dtype, shape):
            save = cursor[nm]
            cursor[nm] = off
            ap = carve(nm, dtype, shape)
            assert (off + 63) // 64 * 64 == off
            cursor[nm] = save
            return ap

        for nm in arenas:
            reset(nm)

        banks = [es.enter_context(nc.psum_tensor("ps%d" % i, [P, 512], F32)) for i in range(8)]
        banks_bf = [b.bitcast(BF16) for b in banks]
        PB = [B('psum', i) for i in range(8)]

        uT = carve('R0', BF16, [P, KC, T])
        off_idle = (cursor['R0'] + 63) // 64 * 64
        gbc = [carve('R0', F32, [P, D]) for _ in range(2)]
        xt = [carve('R0', F32, [P, D]) for _ in range(2)]
        xn = [carve('R0', BF16, [P, D]) for _ in range(2)]
        junkbf = carve('R0', BF16, [P, T])
        ident_f = carve('R0', F32, [P, 128])
        ident_bf = carve('R0', BF16, [P, 128])
        stats = carve('R0', F32, [P, 512])
        stat_i = [0]

        def stat(n=1):
            i = stat_i[0]
            if i + n > 512:
                i = 0
            stat_i[0] = i + n
            return stats[:, i:i + n], B('stat', i, n)

        def stat_bufs(i, n):
            return [B('statc', c) for c in range(i, i + n)]

        def stat2(n=1):
            i = stat_i[0]
            if i + n > 512:
                i = 0
            stat_i[0] = i + n
            return stats[:, i:i + n], stat_bufs(i, n)

        ckeys = carve('R1', BF16, [P, 17, 129])
        cnT = carve('R1', BF16, [P, T])
        ikT = carve('R1', BF16, [P, T])
        poolT = carve('R1', BF16, [P, 4, TX])
        attnT = carve('R1', BF16, [P, 4, TX])
        iw_all = carve('R1', F32, [P, NT, 8])
        gkv_bc = carve('R1', F32, [P, 128])
        blockbias = carve('R1', F32, [P, 128])
        sel8 = carve('R1', BF16, [P, 8, 128])
        invcnt = carve('R1', F32, [P, 16])
        pow2 = carve('R1', F32, [P, NIT + 2])
        pscale = carve('R1', F32, [P, 4])
        cmaxsel = carve('R1', BF16, [P, 8, 32])
        cmax = carve('R1', F32, [P, 1])
        ones_bf = carve('R1', BF16, [P, 128])
        iqT_all = carve('R1', BF16, [P, 3, TX])
        cmaxB = carve('R1', BF16, [P, 128])

        pg.dma('sp', ident_f, cid_d, writes=[B('ident_f')])
        pg.op('dve', lambda e: e.tensor_copy(out=ident_bf, in_=ident_f), reads=[B('ident_f')], writes=[B('ident_bf')])
        pg.dma('pool', sel8[0:32].rearrange("p a b -> p (a b)"), csel_d, writes=[B('sel8')])
        pg.dma('sp', gbc[0], g1_d.partition_broadcast(128), writes=[B('gbc', 0)])
        pg.op('pool', lambda e: e.memset(ckeys[:, 16, 0:128], 0.0), writes=[B('ckeys', 16)])
        pg.op('pool', lambda e: e.memset(ckeys[:, :, 128:129], 1.0), writes=[B('ckeys_ones')])

        def late_consts():
            pg.dma('sp', blockbias, cbb_d, writes=[B('blockbias')])
            pg.dma('sp', invcnt, cinv_d, writes=[B('invcnt')])
            pg.dma('sp', pow2, cpow_d, writes=[B('pow2')])
            pg.dma('sp', pscale, pscale_d.rearrange("o (g p) -> p (o g)", p=128), writes=[B('pscale')],
                   allow_slow_non_contiguous=True)
            pg.dma('sp', gkv_bc, gkv_d.partition_broadcast(128), writes=[B('gkv_bc')])

        def perm3(ap_2d, rows):
            return ap_2d[0:rows].rearrange("t (p k) -> t p k", k=KC)

        def permout(ap_2d, rows):
            return ap_2d[0:rows].rearrange("t (k p) -> t p k", k=KC)

        rot = {}

        def nxt(name, lst):
            i = rot.get(name, 0)
            rot[name] = i + 1
            return lst[i % len(lst)]

        def rmsnorm_to_uT(src2d, src_bufs, rows, col0, gb, gb_buf, tag):
            k = nxt('xn', [0, 1])
            ss, ssb = stat2()
            rt, rtb = stat2()
            rs, rsb = stat2()
            pg.op('act', lambda e: e.activation(out=junkbf[0:rows, 0:D], in_=src2d[0:rows], func=AF.Square,
                                                accum_out=ss[0:rows]),
                  reads=src_bufs, writes=ssb + [B('junkbf')])
            pg.op('act', lambda e: e.activation(out=rt[0:rows], in_=ss[0:rows], func=AF.Sqrt, scale=1.0 / D,
                                                bias=eps_t[0:rows]),
                  reads=ssb + [B('eps')], writes=rtb)
            pg.op('dve', lambda e: e.reciprocal(out=rs[0:rows], in_=rt[0:rows]), reads=rtb, writes=rsb)
            pg.op('dve', lambda e: e.scalar_tensor_tensor(out=xn[k][0:rows], in0=src2d[0:rows],
                                                          scalar=rs[0:rows], in1=gb[0:rows],
                                                          op0=ALU.mult, op1=ALU.mult),
                  reads=src_bufs + rsb + [gb_buf], writes=[B('xn', k)])
            bk = nxt('tp', [0, 1])

            def tps(e):
                last = None
                for kc in range(KC):
                    last = e.transpose(out=banks_bf[bk][:, kc * 128: kc * 128 + rows],
                                       in_=xn[k][0:rows].rearrange("t (p k) -> t k p", k=KC)[:, kc, :],
                                       identity=ident_bf[0:rows, 0:rows])
                return last
            pg.op('pe', tps, reads=[B('xn', k), B('ident_bf')], writes=[PB[bk]])
            src = banks_bf[bk][:, 0:1024].rearrange("p (a b) -> p a b", a=KC)[:, :, 0:rows]

            def back():
                pg.op('act', lambda e: e.copy(out=uT[:, :, col0:col0 + rows], in_=src),
                      reads=[PB[bk]], writes=[B('uT', tag)])
            return back

        eps_t = carve('R0', F32, [P, 1])
        pg.op('pool', lambda e: e.memset(eps_t, EPS), writes=[B('eps')])

        reset('R2')
        reset('R3')
        pvT = carve('R3', F32, [P, 4, T])
        wslab = carve('R2', BF16, [P, KC, 512])
        wsmall = carve('R2', BF16, [P, KC, 136])
        wik = carve('R2', BF16, [P, KC, 96])
        w_iq2 = carve('R2', BF16, [P, KC, 256])
        wpool = carve('R2', BF16, [P, 4, 128])
        ptmp = [carve('R2', F32, [P, T]) for _ in range(2)]
        dT = [carve('R2', BF16, [P, T]) for _ in range(2)]
        tmpc = carve('R2', F32, [P, 16])

        xt4 = xt + [ptmp[0][:, 0:D], ptmp[1][:, 0:D]]
        tiles = [-1] + list(range(NT))
        pend1 = None
        for j in tiles:
            rows = 16 if j < 0 else 128
            col0 = 0 if j < 0 else 16 + 128 * j
            k = nxt('xt4', [0, 1, 2, 3])
            src = meta_d if j < 0 else x_d[128 * j:128 * (j + 1), :]
            pg.dma('sp', xt4[k][0:rows], src, writes=[B('xt', k)])
            if j == 1:
                late_consts()
            bk_ = rmsnorm_to_uT(xt4[k], [B('xt', k)], rows, col0, gbc[0], B('gbc', 0), j)
            if pend1 is not None:
                pend1()
            pend1 = bk_
        pend1()

        def uT_bufs(c0, c1):
            res = []
            if c0 < 16:
                res.append(B('uT', -1))
            for j in range(NT):
                a, b_ = 16 + 128 * j, 16 + 128 * (j + 1)
                if a < c1 and b_ > c0:
                    res.append(B('uT', j))
            return res

        win_p = win_d.rearrange("(p k) n -> p k n", k=KC)

        pg.dma('pool', wsmall[:, :, 0:128], win_p[:, :, 512:640], writes=[B('wsmall')])
        pg.dma('pool', wsmall[:, :, 128:136], win_p[:, :, 928:936], reads=[], writes=[B('wsmall2')])
        for r_ in range(3):
            pg.dma('pool', wik[:, :, 32 * r_:32 * r_ + 32], win_p[:, :, 896:928], writes=[B('wik', r_)])
        pg.dma('pool', w_iq2, win_p[:, :, 640:896], writes=[B('w_iq2')])
        pg.dma('pool', wslab, win_p[:, :, 936:1448], writes=[B('wslab')])
        pg.dma('pool', wpool, wpool_d.rearrange("g c d -> c g d"), writes=[B('wpool')])

        def p2a_f1(j):
            rows = 16 if j < 0 else 128
            col0 = 0 if j < 0 else 16 + 128 * j
            chunk = 16 if j < 0 else j
            bk = nxt('p2a', [2, 3])

            def mm(e):
                last = None
                for kc in range(KC):
                    last = e.matmul(banks[bk][0:rows, 0:136], lhsT=uT[:, kc, col0:col0 + rows],
                                    rhs=wsmall[:, kc, :], start=(kc == 0), stop=(kc == KC - 1))
                return last
            pg.op('pe', mm, reads=[B('uT', j), B('wsmall'), B('wsmall2')], writes=[PB[bk]])
            ss, ssb = stat2()
            rt, rtb = stat2()
            rs, rsb = stat2()
            pg.op('act', lambda e: e.activation(
                out=junkbf[0:rows, 0:128], in_=banks[bk][0:rows, 0:128], func=AF.Square, accum_out=ss[0:rows]),
                reads=[PB[bk]], writes=ssb + [B('junkbf')])
            pg.op('act', lambda e: e.activation(
                out=rt[0:rows], in_=ss[0:rows], func=AF.Sqrt, scale=1.0 / 128, bias=eps_t[0:rows]),
                reads=ssb + [B('eps')], writes=rtb)
            pg.op('dve', lambda e: e.reciprocal(out=rs[0:rows], in_=rt[0:rows]), reads=rtb, writes=rsb)
            pg.op('dve', lambda e: e.scalar_tensor_tensor(
                out=ckeys[0:rows, chunk, 0:128], in0=banks[bk][0:rows, 0:128], scalar=rs[0:rows],
                in1=gkv_bc[0:rows], op0=ALU.mult, op1=ALU.mult),
                reads=[PB[bk], B('gkv_bc')] + rsb, writes=[B('ckeys', chunk)])
            if j >= 0:
                pg.op('dve', lambda e: e.tensor_scalar(
                    out=iw_all[:, j, :], in0=banks[bk][:, 128:136], scalar1=IDX_SCALE, scalar2=None,
                    op0=ALU.mult), reads=[PB[bk]], writes=[B('iw', j)])

            def f2():
                bt = nxt('p2at', [4, 5])
                pg.op('pe', lambda e: e.transpose(
                    out=banks_bf[bt][:, 0:rows], in_=ckeys[0:rows, chunk, 0:128], identity=ident_bf[0:rows, 0:rows]),
                    reads=[B('ckeys', chunk), B('ident_bf')], writes=[PB[bt]])

                def f3():
                    pg.op('act', lambda e: e.copy(
                        out=cnT[:, col0:col0 + rows], in_=banks_bf[bt][:, 0:rows]),
                        reads=[PB[bt]], writes=[B('cnT', j)])
                return f3
            return f2

        p_f2, p_f3 = None, None
        for j in tiles:
            f2 = p2a_f1(j)
            f3 = p_f2() if p_f2 is not None else None
            if p_f3 is not None:
                p_f3()
            p_f2, p_f3 = f2, f3
        f3 = p_f2()
        if p_f3 is not None:
            p_f3()
        f3()

        blocks = [(0, 16)] + [(16 + 512 * b, 16 + 512 * (b + 1)) for b in range(4)]
        allb = list(range(8))
        evac_i = [0]

        def evac_copy(out_ap, in_ap, reads, writes):
            evac_i[0] += 1
            if evac_i[0] % 2 == 0:
                pg.op('act', lambda e: e.copy(out=out_ap, in_=in_ap), reads=reads, writes=writes)
            else:
                pg.op('dve', lambda e: e.tensor_copy(out=out_ap, in_=in_ap), reads=reads, writes=writes)

        def act_evac(out_ap, in_ap, reads, writes):
            pg.op('act', lambda e: e.copy(out=out_ap, in_=in_ap), reads=reads, writes=writes)

        def p2b_ik():
            for bi, (c0, c1) in enumerate(blocks):
                bk = nxt('gen', allb)
                n = c1 - c0

                def mm(e, bk=bk, c0=c0, c1=c1, n=n):
                    last = None
                    for kc in range(KC):
                        last = e.matmul(banks[bk][0:96, 0:n], lhsT=wik[:, kc, :], rhs=uT[:, kc, c0:c1],
                                        start=(kc == 0), stop=(kc == KC - 1))
                    return last
                pg.op('pe', mm, reads=uT_bufs(c0, c1) + [B('wik', 0), B('wik', 1), B('wik', 2)], writes=[PB[bk]])
                act_evac(ikT[0:96, c0:c1], banks[bk][0:96, 0:n], [PB[bk]], [B('ikT', bi)])

        def p2b_iq():
            for g in range(3):
                M = 96 if g < 2 else 64
                for b in range(4):
                    bk = nxt('gen', allb)
                    c0 = 16 + 512 * b

                    def mmi(e, bk=bk, g=g, M=M, c0=c0):
                        last = None
                        for kc in range(KC):
                            last = e.matmul(banks[bk][0:M, 0:512], lhsT=w_iq2[:, kc, 96 * g:96 * g + M],
                                            rhs=uT[:, kc, c0:c0 + 512], start=(kc == 0), stop=(kc == KC - 1))
                        return last
                    pg.op('pe', mmi, reads=uT_bufs(c0, c0 + 512) + [B('w_iq2')], writes=[PB[bk]])
                    act_evac(iqT_all[0:M, g, 512 * b:512 * (b + 1)], banks[bk][0:M, 0:512], [PB[bk]],
                             [B('iqT_all', g, b)])

        def p2b_pv(m):
            for bi, (c0, c1) in enumerate(blocks):
                bk = nxt('gen', allb)
                n = c1 - c0

                def mm(e, bk=bk, c0=c0, c1=c1, n=n):
                    last = None
                    for kc in range(KC):
                        last = e.matmul(banks[bk][:, 0:n], lhsT=wslab[:, kc, m * 128:(m + 1) * 128],
                                        rhs=uT[:, kc, c0:c1], start=(kc == 0), stop=(kc == KC - 1))
                    return last
                pg.op('pe', mm, reads=uT_bufs(c0, c1) + [B('wslab')], writes=[PB[bk]])
                act_evac(pvT[:, m, c0:c1], banks[bk][:, 0:n], [PB[bk]], [B('pvT', m)])

        p3_di = {}

        def p3_chain(g):
            w = 2 << g
            src = pvT[:, g, :]
            cur = src
            curb = [B('pvT', g)]
            for k in [1, 2, 4, 8][:g + 1]:
                pi = nxt('ptmp', [0, 1])
                dst = ptmp[pi]
                pg.op('pool', lambda e, dst=dst, cur=cur, k=k: e.tensor_tensor(
                    out=dst[:, k:1024], in0=cur[:, k:1024], in1=cur[:, 0:1024 - k], op=ALU.add),
                    reads=curb, writes=[B('ptmp', pi), B('xt', 2 + pi)])
                pg.op('dve', lambda e, dst=dst, cur=cur, k=k: e.tensor_tensor(
                    out=dst[:, 1024:T], in0=cur[:, 1024:T], in1=cur[:, 1024 - k:T - k], op=ALU.add),
                    reads=curb, writes=[B('ptmp', pi, 'hi')])
                pg.op('pool', lambda e, dst=dst, cur=cur, k=k: e.tensor_copy(out=dst[:, 0:k], in_=cur[:, 0:k]),
                      reads=curb, writes=[B('ptmp', pi, 'head'), B('xt', 2 + pi)])
                cur = dst
                curb = [B('ptmp', pi), B('ptmp', pi, 'hi'), B('ptmp', pi, 'head')]
            di = nxt('dT', [0, 1])
            p3_di[g] = di
            cb = curb
            pg.op('dve', lambda e: e.scalar_tensor_tensor(
                out=dT[di][:, w - 1:T], in0=cur[:, w - 1:T], scalar=1.0 / w, in1=src[:, w - 1:T],
                op0=ALU.mult, op1=ALU.subtract),
                reads=cb + [B('pvT', g)], writes=[B('dT', di)])
            pg.op('dve', lambda e: e.tensor_tensor(
                out=tmpc[:, 0:w - 1], in0=cur[:, 0:w - 1], in1=invcnt[:, 0:w - 1], op=ALU.mult),
                reads=cb + [B('invcnt')], writes=[B('tmpc')])
            pg.op('dve', lambda e: e.tensor_tensor(
                out=dT[di][:, 0:w - 1], in0=tmpc[:, 0:w - 1], in1=src[:, 0:w - 1], op=ALU.subtract),
                reads=[B('tmpc'), B('pvT', g)], writes=[B('dT', di, 'head')])

        def p3_mm(g):
            di = p3_di[g]
            for b in range(4):
                bk = nxt('gen', allb)
                pg.op('pe', lambda e, bk=bk, b=b: e.matmul(
                    banks[bk][:, 0:512], lhsT=wpool[:, g, :], rhs=dT[di][:, 16 + 512 * b:16 + 512 * (b + 1)],
                    start=True, stop=True),
                    reads=[B('wpool'), B('dT', di), B('dT', di, 'head')], writes=[PB[bk]])
                pg.op('act', lambda e, bk=bk, b=b: e.activation(
                    out=poolT[:, g, 512 * b:512 * (b + 1)], in_=banks[bk][:, 0:512], func=AF.Identity,
                    scale=pscale[:, g:g + 1]),
                    reads=[PB[bk], B('pscale')], writes=[B('poolT', b)])

        W_Q_OFF = 25344
        w_q = carve_at('R3', W_Q_OFF, BF16, [P, KC, 512])
        p2b_pv(3)
        p2b_pv(2)
        p3_chain(3)
        p2b_pv(1)
        p3_chain(2)
        if stop_after >= 4:
            pg.dma('pool', w_q, win_p[:, :, 0:512], writes=[B('w_q'), B('pvT', 3)])
        p2b_pv(0)
        p2b_ik()
        p2b_iq()
        p3_mm(3)
        p3_mm(2)
        p3_chain(1)
        p3_mm(1)
        p3_chain(0)
        p3_mm(0)

        if stop_after <= 3:
            pass
        pg.barrier()
        reset('R2')
        reset('R3')
        w_ba = carve_at('R0', off_idle, BF16, [P, 4, D])
        w_bp = carve_at('R0', off_idle + 8192, BF16, [P, 4, D])
        score = carve('R3', F32, [P, T])
        mb = carve('R3', BF16, [P, T])
        mbT = carve('R3', BF16, [P, 17, 128])
        ukT_pad = carve('R2', BF16, [P, 8, 128])
        uv_pad = carve('R2', BF16, [P, 8, 128])
        wuk_bf = carve('R2', BF16, [P, 512])
        qT_t = carve('R2', BF16, [P, 4, 128])
        qabsT_t = carve('R2', BF16, [P, 8, 128])
        absq = carve('R2', BF16, [P, 8, 128])
        iqT_t = carve('R2', BF16, [P, 8, 128])
        rbuf = [carve('R2', BF16, [P, 512]) for _ in range(4)]
        pT = [carve('R2', BF16, [P, 512]) for _ in range(3)]
        diag = carve('R2', BF16, [P, 8, 128])
        olatT_t = carve('R2', BF16, [P, 8, 128])
        negm8 = carve('R2', BF16, [P, 128])
        bis = carve('R2', F32, [P, 4 * (NIT + 2)])
        rsum = carve('R2', F32, [P, 8])
        thr = carve('R2', F32, [P, 2])

        if stop_after >= 4:
            pg.dma('pool', wuk_bf, wuk_d, writes=[B('wuk_bf')])
            pg.dma('pool', w_ba, wba_d.rearrange("(m p) n -> p m n", p=128),
                   writes=[B('w_ba'), B('gbc', 0), B('gbc', 1)])
            pg.dma('pool', w_bp, wbp_d.rearrange("(m p) n -> p m n", p=128),
                   writes=[B('w_bp'), B('xt', 0), B('xt', 1)])
            pg.op('pool', lambda e: e.memset(ukT_pad, 0.0), writes=[B('ukT_pad')])
            pg.op('pool', lambda e: e.memset(uv_pad, 0.0), writes=[B('uv_pad')])
            for hp in range(2):
                src = wuv_d.rearrange("c (m two d) -> c m two d", two=2, d=64)[:, :, hp, :]
                dst = uv_pad.rearrange("c (m two) n -> c m two n", two=2)[:, :, hp, 64 * hp:64 * hp + 64]
                pg.dma('pool', dst, src, reads=[B('uv_pad')], writes=[B('uv_pad', hp)])
            for m in range(4):
                bk = nxt('gen', allb)
                pg.op('pe', lambda e, bk=bk, m=m: e.transpose(
                    out=banks_bf[bk][:, 0:128], in_=wuk_bf[:, m * 128:(m + 1) * 128], identity=ident_bf),
                    reads=[B('wuk_bf'), B('ident_bf')], writes=[PB[bk]])
                for hp in range(2):
                    h = 2 * m + hp
                    pg.op('dve', lambda e, bk=bk, h=h, hp=hp: e.tensor_copy(
                        out=ukT_pad[64 * hp:64 * hp + 64, h, :], in_=banks_bf[bk][64 * hp:64 * hp + 64, 0:128]),
                        reads=[PB[bk], B('ukT_pad')], writes=[B('ukT_pad', h)])
            pg.op('dve', lambda e: e.tensor_reduce(out=cmax, in_=cnT, axis=AX.X, op=ALU.max,
                                                   apply_absolute_value=True),
                  reads=[B('cnT', j) for j in tiles], writes=[B('cmax')])
            pg.op('pool', lambda e: e.memset(ones_bf, 1.0), writes=[B('ones_bf')])
            pg.op('dve', lambda e: e.tensor_copy(out=cmaxB, in_=mk_ap(cmax, 0, [[pstep(cmax), 128], [0, 128]])),
                  reads=[B('cmax')], writes=[B('cmaxB')])
            pg.op('pool', lambda e: e.memset(cmaxsel, 0.0), writes=[B('cmaxsel')])
            pg.op('pool', lambda e: e.memset(mbT[:, 16, :], NEG), writes=[B('mbT', 16)])
            for h in range(8):
                pg.op('dve', lambda e, h=h: e.tensor_copy(out=cmaxsel[:, h, h:h + 1], in_=cmax),
                      reads=[B('cmax'), B('cmaxsel')], writes=[B('cmaxsel', h)])
        ukb = [B('ukT_pad')] + [B('ukT_pad', h) for h in range(8)]
        uvb = [B('uv_pad'), B('uv_pad', 0), B('uv_pad', 1)]
        cmb = [B('cmaxsel')] + [B('cmaxsel', h) for h in range(8)]
        ikb = [B('ikT', bi) for bi in range(5)]
        cnb = [B('cnT', j) for j in tiles]
        ckb = [B('ckeys', c) for c in range(17)] + [B('ckeys_ones')]

        mids = bis[:, 0:NIT + 2]
        cnts = bis[:, NIT + 2:2 * (NIT + 2)]
        dds = bis[:, 2 * (NIT + 2):3 * (NIT + 2)]
        Wc = bis[:, 3 * (NIT + 2):4 * (NIT + 2)]

        n_attn_tiles = NT if stop_after >= 4 else 0
        n_attn_tiles = debug.get('n_attn_tiles', n_attn_tiles)
        qabs2 = [qabsT_t, carve('R2', BF16, [P, 8, 128]), carve('R2', BF16, [P, 8, 128])]
        mb2 = [mb, carve('R3', BF16, [P, T])]
        assert cursor['R3'] <= W_Q_OFF, cursor['R3']
        score2 = [score, carve('R2', F32, [P, T])]
        negsh = carve('R2', F32, [P, 8])
        lnS = carve('R2', F32, [P, 512])
        rcpS = carve('R2', F32, [P, 512])
        ROT4 = [0, 1, 2, 3]
        PSS = [6, 7]
        ACC = [4, 5]

        def act_copy(out_ap, in_ap, reads, writes):
            pg.op('act', lambda e: e.copy(out=out_ap, in_=in_ap), reads=reads, writes=writes)

        def chunks_of(j):
            S = 16 + 128 * (j + 1)
            return [(0, 16)] + [(16 + 512 * m, min(16 + 512 * (m + 1), S)) for m in range((S - 16 + 511) // 512)]

        def stageA(j, tick=None):
            par = j % 2
            par3 = j % 3
            sc, qa = score2[par], qabs2[par3]
            qc = 16 + 128 * j
            ub = [B('uT', j)]
            for h in range(8):
                pg.op('act', lambda e, h=h, j=j: e.activation(out=diag[:, h, :], in_=ident_f, func=AF.Identity,
                                                              scale=iw_all[:, j, h:h + 1]),
                      reads=[B('ident_f'), B('iw', j)], writes=[B('diag', h)])
            items = []
            for (c0, c1) in chunks_of(j):
                bs = nxt('pss', PSS)
                for hp_ in range(4):
                    items.append((c0, c1, bs, hp_))

            def emit_x(it):
                c0, c1, bs, hp_ = it
                n = c1 - c0
                res = []
                for hh in range(2):
                    h = 2 * hp_ + hh
                    bx = nxt('rot4', ROT4)
                    g_, a_ = h // 3, h % 3
                    pg.op('pe', lambda e, bx=bx, g_=g_, a_=a_, c0=c0, c1=c1, n=n: e.matmul(
                        banks[bx][:, 0:n], lhsT=iqT_all[32 * a_:32 * a_ + 32, g_, 128 * j:128 * (j + 1)],
                        rhs=ikT[32 * a_:32 * a_ + 32, c0:c1], start=True, stop=True),
                        reads=[B('iqT_all', g_, j // 4)] + ikb, writes=[PB[bx]])
                    ri = nxt('rbuf', [0, 1, 2, 3])
                    pg.op('act', lambda e, bx=bx, ri=ri, n=n: e.activation(
                        out=rbuf[ri][:, 0:n], in_=banks[bx][:, 0:n], func=AF.Relu),
                        reads=[PB[bx]], writes=[B('rbuf', ri)])
                    res.append((h, ri))
                return res

            def emit_d(it, res):
                c0, c1, bs, hp_ = it
                n = c1 - c0
                for (h, ri) in res:
                    pg.op('pe', lambda e, bs=bs, h=h, ri=ri, n=n: e.matmul(
                        banks[bs][:, 0:n], lhsT=diag[:, h, :], rhs=rbuf[ri][:, 0:n], start=(h == 0), stop=(h == 7)),
                        reads=[B('diag', h), B('rbuf', ri)], writes=[PB[bs]])
                if hp_ == 3:
                    act_copy(sc[:, c0:c1], banks[bs][:, 0:n], [PB[bs]], [B('score', par, c0)])

            prev = None
            for it in items:
                res = emit_x(it)
                if prev is not None:
                    emit_d(*prev)
                prev = (it, res)
                if tick is not None:
                    tick()
            emit_d(*prev)
            bk = nxt('rot4', ROT4)

            def mmq(e, bk=bk, qc=qc):
                last = None
                for m in range(4):
                    for kc in range(KC):
                        last = e.matmul(banks[bk][:, m * 128:(m + 1) * 128], lhsT=w_q[:, kc, m * 128:(m + 1) * 128],
                                        rhs=uT[:, kc, qc:qc + 128], start=(kc == 0), stop=(kc == KC - 1))
                return last
            pg.op('pe', mmq, reads=ub + [B('w_q')], writes=[PB[bk]])
            act_copy(qT_t, banks[bk][:, 0:512].rearrange("p (a b) -> p a b", a=4), [PB[bk]], [B('qT_t')])
            for half in range(2):
                bk = nxt('rot4', ROT4)

                def mma(e, bk=bk, half=half):
                    last = None
                    for hh in range(4):
                        h = 4 * half + hh
                        last = e.matmul(banks[bk][:, hh * 128:(hh + 1) * 128], lhsT=ukT_pad[:, h, :],
                                        rhs=qT_t[:, h // 2, :], start=True, stop=True)
                    return last
                pg.op('pe', mma, reads=ukb + [B('qT_t')], writes=[PB[bk]])
                src3 = banks[bk][:, 0:512].rearrange("p (a b) -> p a b", a=4)
                act_copy(qa[:, 4 * half:4 * half + 4, :], src3, [PB[bk]], [B('qabsT_t', par3, half)])
                pg.op('dve', lambda e, half=half, qa=qa: e.scalar_tensor_tensor(
                    out=absq[:, 4 * half:4 * half + 4, :], in0=qa[:, 4 * half:4 * half + 4, :], scalar=-1.0,
                    in1=qa[:, 4 * half:4 * half + 4, :], op0=ALU.mult, op1=ALU.max),
                    reads=[B('qabsT_t', par3, half)], writes=[B('absq', half)])

        def stageA2(j):
            par = j % 3
            for half in range(2):
                bk = nxt('rot4', ROT4)

                def mmb(e, bk=bk, half=half):
                    last = None
                    for hh in range(4):
                        last = e.matmul(banks[bk][:, hh * 128:(hh + 1) * 128], lhsT=cmaxB,
                                        rhs=absq[:, 4 * half + hh, :], start=True, stop=True)
                    return last
                pg.op('pe', mmb, reads=[B('cmaxB'), B('absq', half)], writes=[PB[bk]])
                pg.op('dve', lambda e, bk=bk, half=half: e.tensor_reduce(
                    out=negsh[:, 4 + half:5 + half], in_=banks[bk][:, 0:512], axis=AX.X, op=ALU.max),
                    reads=[PB[bk]], writes=[B('negsh_t', half)])
            pg.op('dve', lambda e: e.tensor_tensor(out=negsh[:, 6:7], in0=negsh[:, 4:5], in1=negsh[:, 5:6], op=ALU.max),
                  reads=[B('negsh_t', 0), B('negsh_t', 1)], writes=[B('negsh_m')])
            pg.op('dve', lambda e, par=par: e.tensor_scalar(out=negsh[:, par:par + 1], in0=negsh[:, 6:7],
                                                            scalar1=-ATTN_SCALE, scalar2=None, op0=ALU.mult),
                  reads=[B('negsh_m')], writes=[B('negsh', par)])

        def scbufs(j):
            return [B('score', j % 2, c0) for (c0, c1) in chunks_of(j)]

        def stageB_init(j):
            S = 16 + 128 * (j + 1)
            sc = score2[j % 2]
            scb = scbufs(j)
            Rr, Rb = stat2()
            pg.op('dve', lambda e: e.tensor_reduce(out=Rr, in_=sc[:, 0:S], axis=AX.X, op=ALU.max,
                                                   apply_absolute_value=True),
                  reads=scb, writes=Rb)
            pg.op('dve', lambda e: e.tensor_tensor(out=sc[:, S - 128:S], in0=sc[:, S - 128:S],
                                                   in1=blockbias, op=ALU.add),
                  reads=scb + [B('blockbias')], writes=[B('score', j % 2, 16 + 512 * (j // 4))])
            pg.op('dve', lambda e: e.tensor_scalar(out=Wc, in0=pow2, scalar1=Rr, scalar2=1e-30,
                                                   op0=ALU.mult, op1=ALU.add),
                  reads=Rb + [B('pow2')], writes=[B('Wc')])
            pg.op('dve', lambda e: e.memset(mids[:, 0:1], 0.0), writes=[B('mid', 0)])

        def stageB_iter(j, n_):
            S = 16 + 128 * (j + 1)
            sc = score2[j % 2]
            pg.op('dve', lambda e: e.tensor_scalar(
                out=junkbf[:, 0:S], in0=sc[:, 0:S], scalar1=mids[:, n_ - 1:n_], scalar2=None,
                op0=ALU.is_ge, op1=ALU.add, accum_out=cnts[:, n_ - 1:n_]),
                reads=scbufs(j) + [B('mid', n_ - 1)], writes=[B('junkbf'), B('cnt', n_)])
            pg.op('dve', lambda e: e.scalar_tensor_tensor(
                out=dds[:, n_ - 1:n_], in0=cnts[:, n_ - 1:n_], scalar=KTOP - 0.5, in1=Wc[:, n_ - 1:n_],
                op0=ALU.is_ge, op1=ALU.mult),
                reads=[B('cnt', n_), B('Wc')], writes=[B('dd', n_)])
            pg.op('dve', lambda e: e.scalar_tensor_tensor(
                out=mids[:, n_:n_ + 1], in0=dds[:, n_ - 1:n_], scalar=Wc[:, n_:n_ + 1], in1=mids[:, n_ - 1:n_],
                op0=ALU.subtract, op1=ALU.add),
                reads=[B('dd', n_), B('Wc'), B('mid', n_ - 1)], writes=[B('mid', n_)])

        def stageB_fin(j):
            pg.op('dve', lambda e: e.tensor_tensor(out=thr[:, 0:1], in0=mids[:, NIT:NIT + 1], in1=Wc[:, NIT:NIT + 1],
                                                   op=ALU.subtract),
                  reads=[B('mid', NIT), B('Wc')], writes=[B('thr')])
            S = 16 + 128 * (j + 1)
            sc = score2[j % 2]
            mbj = mb2[j % 2]
            pg.op('dve', lambda e: e.tensor_scalar(out=mbj[:, 0:S], in0=sc[:, 0:S], scalar1=thr[:, 0:1],
                                                   scalar2=NEG, op0=ALU.is_lt, op1=ALU.mult),
                  reads=scbufs(j) + [B('thr')], writes=[B('mb', j % 2)])

        def stageC(j):
            mbj = mb2[j % 2]
            klist = [16] + list(range(j + 1))
            for g0 in range(0, len(klist), 8):
                grp = klist[g0:g0 + 8]
                bk = nxt('rot4', ROT4)

                def tp(e, grp=grp, bk=bk):
                    last = None
                    for si, i in enumerate(grp):
                        c_ = 0 if i == 16 else 16 + 128 * i
                        last = e.transpose(out=banks_bf[bk][:, si * 128:(si + 1) * 128], in_=mbj[:, c_:c_ + 128],
                                           identity=ident_bf)
                    return last
                pg.op('pe', tp, reads=[B('mb', j % 2), B('ident_bf')], writes=[PB[bk]])
                si0 = 0
                if grp[0] == 16:
                    pg.op('dve', lambda e, bk=bk: e.tensor_copy(out=mbT[0:16, 16, :], in_=banks_bf[bk][0:16, 0:128]),
                          reads=[PB[bk]], writes=[B('mbT', 16)])
                    si0 = 1
                nn_ = len(grp) - si0
                if nn_ > 0:
                    i0 = grp[si0]
                    srcv = banks_bf[bk][:, si0 * 128:(si0 + nn_) * 128].rearrange("p (a b) -> p a b", a=nn_)
                    pg.op('dve', lambda e, i0=i0, nn_=nn_, srcv=srcv: e.tensor_copy(out=mbT[:, i0:i0 + nn_, :], in_=srcv),
                          reads=[PB[bk]], writes=[B('mbT', i) for i in range(i0, i0 + nn_)])

        def stageD_lg(j, q, ci):
            par = j % 3
            qa = qabs2[par]
            xch = [16] + list(range(j + 1))
            i = xch[ci]
            bl = nxt('rot4', ROT4)
            kc0 = 0 if i == 16 else 16 + 128 * i
            mrow = mk_ap(mbT, i * 128, [[pstep(mbT), 128], [0, 4], [1, 128]])
            qrhs = qa[:, 4 * q:4 * q + 4, :]

            def lg(e):
                e.matmul(banks[bl][:, 0:512], lhsT=cnT[:, kc0:kc0 + 128], rhs=qrhs, start=True, stop=False)
                return e.matmul(banks[bl][:, 0:512], lhsT=ident_bf, rhs=mrow, start=False, stop=True)
            pg.op('pe', lg, reads=cnb + [B('qabsT_t', par, q), B('ident_bf'), B('mbT', i)], writes=[PB[bl]])
            pi = nxt('pT', [0, 1, 2])
            pg.op('act', lambda e: e.activation(out=pT[pi][:, 0:512], in_=banks[bl][:, 0:512], func=AF.Exp,
                                                scale=ATTN_SCALE, bias=negsh[:, par:par + 1]),
                  reads=[PB[bl], B('negsh', par)], writes=[B('pT', pi)])
            return pi

        def stageD_pv(j, q, ci, pi):
            xch = [16] + list(range(j + 1))
            i = xch[ci]
            bO, bS = ACC
            first, last_ = (ci == 0), (ci == len(xch) - 1)

            def pv(e):
                e.matmul(banks[bO][:, 0:512], lhsT=ckeys[:, i, 0:128], rhs=pT[pi][:, 0:512], start=first, stop=last_)
                return e.matmul(banks[bS][:, 0:512], lhsT=ones_bf, rhs=pT[pi][:, 0:512], start=first, stop=last_)
            pg.op('pe', pv, reads=[B('pT', pi), B('ones_bf'), B('ckeys', i)], writes=[PB[bO], PB[bS]])

        def stageD_qfin(j, q):
            bO, bS = ACC
            pg.op('act', lambda e: e.activation(out=lnS, in_=banks[bS][:, 0:512], func=AF.Ln),
                  reads=[PB[bS]], writes=[B('lnS')])
            pg.op('act', lambda e: e.activation(out=rcpS, in_=lnS, func=AF.Exp, scale=-1.0),
                  reads=[B('lnS')], writes=[B('rcpS')])
            pg.op('dve', lambda e: e.tensor_tensor(
                out=olatT_t[:, 4 * q:4 * q + 4, :], in0=banks[bO][:, 0:512].rearrange("p (a b) -> p a b", a=4),
                in1=rcpS.rearrange("p (a b) -> p a b", a=4), op=ALU.mult),
                reads=[PB[bO], B('rcpS')], writes=[B('olatT_t', q)])

        def stageD_fin(j):
            bk = nxt('rot4', ROT4)

            def mmo(e, bk=bk):
                last = None
                for m in range(4):
                    o = banks[bk][:, m * 128:(m + 1) * 128]
                    e.matmul(o, lhsT=uv_pad[:, 2 * m, :], rhs=olatT_t[:, 2 * m, :], start=True, stop=False)
                    last = e.matmul(o, lhsT=uv_pad[:, 2 * m + 1, :], rhs=olatT_t[:, 2 * m + 1, :], start=False, stop=True)
                return last
            pg.op('pe', mmo, reads=uvb + [B('olatT_t', 0), B('olatT_t', 1)], writes=[PB[bk]])
            act_copy(attnT[:, :, 128 * j:128 * (j + 1)], banks[bk][:, 0:512].rearrange("p (a b) -> p a b", a=4),
                     [PB[bk]], [B('attnT', j)])

        nA = n_attn_tiles

        def fullB(j):
            stageB_init(j)
            if 16 + 128 * (j + 1) <= KTOP:
                pg.op('dve', lambda e: e.tensor_reduce(out=cnts[:, 0:1], in_=Wc[:, 1:NIT + 1], axis=AX.X, op=ALU.add),
                      reads=[B('Wc')], writes=[B('cnt', 1)])
                pg.op('dve', lambda e: e.tensor_scalar(out=mids[:, NIT:NIT + 1], in0=cnts[:, 0:1], scalar1=-1.0,
                                                       scalar2=None, op0=ALU.mult),
                      reads=[B('cnt', 1)], writes=[B('mid', NIT)])
            else:
                for n_ in range(1, NIT + 1):
                    stageB_iter(j, n_)
            stageB_fin(j)

        if nA > 2:
            stageA(0)
            fullB(0)
            stageA(1)
            stageB_init(1)
            stageA2(0)
            for n_ in (1, 2, 3):
                stageB_iter(1, n_)
            stageA2(1)
            for n_ in (4, 5, 6):
                stageB_iter(1, n_)
            stageC(0)
            for n_ in (7, 8):
                stageB_iter(1, n_)
            assert 4 * len(chunks_of(2)) == 8
            pro_n = [8]

            def pro_tick():
                pro_n[0] += 1
                if pro_n[0] <= NIT:
                    stageB_iter(1, pro_n[0])
            stageA(2, pro_tick)
            for n_ in range(pro_n[0] + 1, NIT + 1):
                stageB_iter(1, n_)
            stageA2(2)
            stageB_fin(1)
        elif nA > 0:
            stageA(0)
            fullB(0)
            if nA > 1:
                stageA(1)
            stageA2(0)
            if nA > 1:
                stageA2(1)
            stageC(0)
            if nA > 1:
                fullB(1)
        carry_sched = None
        for i in range(nA):
            hasB = i + 2 < nA
            hasA = i + 3 < nA
            nch = i + 2
            seq = [(q, ci) for q in range(2) for ci in range(nch)]
            steps = len(seq)
            n_items = 4 * len(chunks_of(i + 3)) if hasA else 0
            wD, wA = 1.0 * steps, 1.7 * n_items
            ticks = [('D', k) for k in range(steps)] + [('A', k) for k in range(n_items)]
            wts = [1.0] * steps + [1.7] * n_items
            split_last = hasB and not hasA and i + 1 < nA
            if split_last:
                steps2 = 2 * (i + 3)
                ticks = ticks + [('E', k) for k in range(steps2)]
                wts = wts + [1.0] * steps2
            tot = sum(wts)
            sched = {}
            acc, done = 0.0, 0
            for tk, w in zip(ticks, wts):
                acc += w
                upto = int(round(NIT * acc / tot))
                sched[tk] = list(range(done + 1, upto + 1))
                done = upto
            if hasB:
                stageB_init(i + 2)
            pis = {}
            pis[0] = stageD_lg(i, *seq[0])
            for k, (q, ci) in enumerate(seq):
                if k + 1 < steps:
                    pis[k + 1] = stageD_lg(i, *seq[k + 1])
                stageD_pv(i, q, ci, pis[k])
                if hasB:
                    for n_ in sched[('D', k)]:
                        stageB_iter(i + 2, n_)
                if carry_sched is not None:
                    for n_ in carry_sched[('E', k)]:
                        stageB_iter(i + 1, n_)
                if ci == nch - 1:
                    stageD_qfin(i, q)
            stageD_fin(i)
            if hasA:
                cnt = [0]

                def tick(cnt=cnt):
                    if hasB:
                        for n_ in sched[('A', cnt[0])]:
                            stageB_iter(i + 2, n_)
                    cnt[0] += 1
                stageA(i + 3, tick)
            if hasB and not split_last:
                stageB_fin(i + 2)
            if carry_sched is not None:
                stageB_fin(i + 1)
            carry_sched = sched if split_last else None
            if i + 1 < nA:
                stageC(i + 1)
            if hasA:
                stageA2(i + 3)

        pg.barrier()
        reset('R2')
        reset('R3')
        mergedT = carve('R3', BF16, [P, 8, TX])
        wgA2 = [carve('R2', BF16, [P, KC, 256]) for _ in range(2)]
        wgP2 = [carve('R2', BF16, [P, KC, 256]) for _ in range(2)]
        w_out = carve('R2', BF16, [P, 8, D])
        off_wout_end = cursor['R2']
        sA = [carve('R2', F32, [P, 512]) for _ in range(2)]
        sP = [carve('R2', F32, [P, 512]) for _ in range(2)]
        run_tail = stop_after >= 5

        def load_wg(grp):
            s_ = grp % 2
            pg.dma('pool', wgA2[s_], win_p[:, :, 1448 + 256 * grp:1448 + 256 * (grp + 1)], writes=[B('wgA', s_)])
            pg.dma('pool', wgP2[s_], win_p[:, :, 2472 + 256 * grp:2472 + 256 * (grp + 1)], writes=[B('wgP', s_)])

        if run_tail:
            load_wg(0)
            load_wg(1)
            pg.dma('pool', w_out, wout_d.rearrange("(m p) n -> p m n", p=128), writes=[B('w_out')])
            for grp in range(4):
                gs = grp % 2
                wgA, wgP = wgA2[gs], wgP2[gs]
                if 1 <= grp < 3:
                    load_wg(grp + 1)
                for n4 in range(2):
                    nn = 2 * grp + n4
                    for b in range(4):
                        bset = nxt('tailA', [[0, 1, 2, 3], [4, 5, 6, 7]])
                        c0 = 16 + 512 * b
                        bA, bP, bgA, bgP = bset

                        def mmA(e, bA=bA, nn=nn, b=b):
                            last = None
                            for m in range(4):
                                last = e.matmul(banks[bA][:, 0:512], lhsT=w_ba[:, m, nn * 128:(nn + 1) * 128],
                                                rhs=attnT[:, m, 512 * b:512 * (b + 1)], start=(m == 0), stop=(m == 3))
                            return last
                        pg.op('pe', mmA, reads=[B('w_ba')] + [B('attnT', 4 * b + q) for q in range(4)], writes=[PB[bA]])

                        def mmP(e, bP=bP, nn=nn, b=b):
                            last = None
                            for m in range(4):
                                last = e.matmul(banks[bP][:, 0:512], lhsT=w_bp[:, m, nn * 128:(nn + 1) * 128],
                                                rhs=poolT[:, m, 512 * b:512 * (b + 1)], start=(m == 0), stop=(m == 3))
                            return last
                        pg.op('pe', mmP, reads=[B('w_bp'), B('poolT', b)], writes=[PB[bP]])

                        def mmg(e, bk, wt, n4=n4, c0=c0):
                            last = None
                            for kc in range(KC):
                                last = e.matmul(banks[bk][:, 0:512], lhsT=wt[:, kc, n4 * 128:(n4 + 1) * 128],
                                                rhs=uT[:, kc, c0:c0 + 512], start=(kc == 0), stop=(kc == KC - 1))
                            return last
                        pg.op('pe', lambda e, bgA=bgA, f=mmg, wgA=wgA: f(e, bgA, wgA),
                              reads=uT_bufs(c0, c0 + 512) + [B('wgA', gs)], writes=[PB[bgA]])
                        pg.op('pe', lambda e, bgP=bgP, f=mmg, wgP=wgP: f(e, bgP, wgP),
                              reads=uT_bufs(c0, c0 + 512) + [B('wgP', gs)], writes=[PB[bgP]])
                        si = nxt('sAP', [0, 1])
                        pg.op('act', lambda e, bgA=bgA, si=si: e.activation(out=sA[si], in_=banks[bgA][:, 0:512], func=AF.Sigmoid),
                              reads=[PB[bgA]], writes=[B('sA', si)])
                        pg.op('act', lambda e, bgP=bgP, si=si: e.activation(out=sP[si], in_=banks[bgP][:, 0:512], func=AF.Sigmoid),
                              reads=[PB[bgP]], writes=[B('sP', si)])
                        pg.op('dve', lambda e, bA=bA, si=si: e.tensor_tensor(out=sA[si], in0=sA[si], in1=banks[bA][:, 0:512], op=ALU.mult),
                              reads=[B('sA', si), PB[bA]], writes=[B('sA', si)])
                        pg.op('dve', lambda e, bP=bP, si=si: e.tensor_tensor(out=sP[si], in0=sP[si], in1=banks[bP][:, 0:512], op=ALU.mult),
                              reads=[B('sP', si), PB[bP]], writes=[B('sP', si)])
                        pg.op('dve', lambda e, si=si, nn=nn, b=b: e.tensor_tensor(
                            out=mergedT[:, nn, 512 * b:512 * (b + 1)], in0=sA[si], in1=sP[si], op=ALU.add),
                            reads=[B('sA', si), B('sP', si)], writes=[B('mergedT', nn, b)])

        pg.barrier()
        reset('R1')
        reset('R2')
        hacc = carve('R1', F32, [P, NT, D])
        w_r = carve('R2', BF16, [P, KC, 36])
        rbias = carve('R2', F32, [P, 36])
        gate_all = carve('R2', F32, [P, NT, 32])
        rt_ = carve('R2', F32, [P, 256])
        cur_save = cursor['R2']
        lg_all = carve('R2', F32, [P, NT, 36])
        ssq = carve('R2', F32, [P, NT])
        rstd2 = carve('R2', F32, [P, NT])
        rv = carve('R2', F32, [P, 1600])
        assert cursor['R2'] <= off_wout_end - 16384, cursor['R2']
        if run_tail:
            pg.dma('pool', w_r[:, :, 0:4], wgr_d.rearrange("(p k) n -> p k n", k=KC), writes=[B('w_r', 0)])
            pg.dma('pool', w_r[:, :, 4:36], wer_d.rearrange("(p k) n -> p k n", k=KC), writes=[B('w_r', 1)])
            pg.dma('sp', rbias[:, 0:4], bgr_d.partition_broadcast(128), writes=[B('rbias', 0)])
            pg.dma('sp', rbias[:, 4:36], ber_d.partition_broadcast(128), writes=[B('rbias', 1)])
            pg.dma('sp', gbc[1], g2_d.partition_broadcast(128), writes=[B('gbc', 1)])
            for j in range(NT):
                b = j // 4
                k = nxt('xt', [0, 1])
                pg.dma('sp', xt[k], x_d[128 * j:128 * (j + 1), :], writes=[B('xt', k)])
                for hh in range(2):
                    bk = nxt('tailB', [0, 1, 2, 3])

                    def mmh(e, bk=bk, j=j, hh=hh):
                        last = None
                        for nn in range(8):
                            last = e.matmul(banks[bk][:, 0:512], lhsT=mergedT[:, nn, 128 * j:128 * (j + 1)],
                                            rhs=w_out[:, nn, 512 * hh:512 * (hh + 1)], start=(nn == 0), stop=(nn == 7))
                        return last
                    pg.op('pe', mmh, reads=[B('mergedT', nn, b) for nn in range(8)] + [B('w_out')] + [B('R3a', q) for q in range(6)],
                          writes=[PB[bk]])
                    pg.op('dve', lambda e, bk=bk, j=j, hh=hh, k=k: e.tensor_tensor(
                        out=hacc[:, j, 512 * hh:512 * (hh + 1)], in0=banks[bk][:, 0:512], in1=xt[k][:, 512 * hh:512 * (hh + 1)],
                        op=ALU.add), reads=[PB[bk], B('xt', k)], writes=[B('hacc', j, hh)])
                pg.op('act', lambda e, j=j: e.activation(out=junkbf[:, 0:D], in_=hacc[:, j, :], func=AF.Square,
                                                         accum_out=ssq[:, j:j + 1]),
                      reads=[B('hacc', j, 0), B('hacc', j, 1)], writes=[B('junkbf'), B('ssq', j)])
                if j % 8 == 7:
                    hf = j // 8
                    sl = slice(8 * hf, 8 * hf + 8)
                    pg.op('act', lambda e, sl=sl: e.activation(out=rstd2[:, sl], in_=ssq[:, sl], func=AF.Sqrt,
                                                                scale=1.0 / D, bias=eps_t),
                          reads=[B('ssq', jj) for jj in range(8 * hf, 8 * hf + 8)] + [B('eps')],
                          writes=[B('rstd2s', hf)])
                    pg.op('dve', lambda e, sl=sl: e.reciprocal(out=rstd2[:, sl], in_=rstd2[:, sl]),
                          reads=[B('rstd2s', hf)], writes=[B('rstd2', hf)])
            def s2_front(j):
                k = nxt('xn', [0, 1])
                pg.op('dve', lambda e: e.scalar_tensor_tensor(
                    out=xn[k], in0=hacc[:, j, :], scalar=rstd2[:, j:j + 1], in1=gbc[1], op0=ALU.mult, op1=ALU.mult),
                    reads=[B('hacc', j, 0), B('hacc', j, 1), B('rstd2', j // 8), B('gbc', 1)], writes=[B('xn', k)])
                bk = nxt('tp', [4, 5])

                def tps(e):
                    last = None
                    for kc in range(KC):
                        last = e.transpose(out=banks_bf[bk][:, kc * 128:(kc + 1) * 128],
                                           in_=xn[k].rearrange("t (p k) -> t k p", k=KC)[:, kc, :], identity=ident_bf)
                    return last
                pg.op('pe', tps, reads=[B('xn', k), B('ident_bf')], writes=[PB[bk]])
                srcv = banks_bf[bk][:, 0:1024].rearrange("p (a b) -> p a b", a=KC)
                pg.op('act', lambda e: e.copy(out=uT[:, :, 16 + 128 * j:16 + 128 * (j + 1)], in_=srcv),
                      reads=[PB[bk]], writes=[B('uT', j)])

            s2_bk = {}

            def s2_back(j):
                bk2 = nxt('rt', [6, 7])
                s2_bk[j] = bk2

                def mmr(e):
                    last = None
                    for kc in range(KC):
                        last = e.matmul(banks[bk2][:, 0:36], lhsT=uT[:, kc, 16 + 128 * j:16 + 128 * (j + 1)],
                                        rhs=w_r[:, kc, :], start=(kc == 0), stop=(kc == KC - 1))
                    return last
                pg.op('pe', mmr, reads=[B('uT', j), B('w_r', 0), B('w_r', 1)], writes=[PB[bk2]])

            def s2_add(j):
                bk2 = s2_bk[j]
                pg.op('dve', lambda e: e.tensor_tensor(out=lg_all[:, j, :], in0=banks[bk2][:, 0:36], in1=rbias,
                                                       op=ALU.add),
                      reads=[PB[bk2], B('rbias', 0), B('rbias', 1)], writes=[B('lg_all', j)])

            for t_ in range(NT + 3):
                if t_ >= 3:
                    s2_add(t_ - 3)
                if 2 <= t_ < NT + 2:
                    s2_back(t_ - 2)
                if t_ < NT:
                    s2_front(t_)
            RV = B('rv')
            off = [0]

            def rvv(n):
                a_ = rv[:, off[0]:off[0] + n]
                off[0] += n
                return a_
            gmax, gsum, pgp, m1, m2, dm, ed, den, p1, p2 = [rvv(NT) for _ in range(10)]
            goh, gex = rvv(NT * 4), rvv(NT * 4)
            esel, oh1, esel2, oh2, t1, t2 = [rvv(NT * 8) for _ in range(6)]
            tmp32 = rvv(NT * 32)
            pl = pstep(lg_all)
            pr = pstep(rv)

            def v3(ap, n):
                return ap.rearrange("p (a b) -> p a b", a=NT)

            def bc_last(ap, n):
                return mk_ap(ap, 0, [[pr, 128], [1, NT], [0, n]])
            gl = lg_all[:, :, 0:4]
            el4 = mk_ap(lg_all, 4, [[pl, 128], [36, NT], [8, 4], [1, 8]])
            lgb = [B('lg_all', j) for j in range(NT)]

            def dv(fn, extra=()):
                pg.op('dve', fn, reads=[RV] + list(extra), writes=[RV])
            dv(lambda e: e.tensor_reduce(out=gmax, in_=gl, axis=AX.X, op=ALU.max), lgb)
            dv(lambda e: e.tensor_tensor(out=v3(goh, 4), in0=gl, in1=bc_last(gmax, 4), op=ALU.is_ge), lgb)
            dv(lambda e: e.tensor_tensor(out=v3(gex, 4), in0=gl, in1=bc_last(gmax, 4), op=ALU.subtract), lgb)
            pg.op('act', lambda e: e.activation(out=gex, in_=gex, func=AF.Exp), reads=[RV], writes=[RV])
            dv(lambda e: e.tensor_reduce(out=gsum, in_=v3(gex, 4), axis=AX.X, op=ALU.add))
            dv(lambda e: e.reciprocal(out=pgp, in_=gsum))
            goh4 = mk_ap(goh, 0, [[pr, 128], [4, NT], [1, 4], [0, 8]])
            t4 = mk_ap(tmp32, 0, [[pr, 128], [32, NT], [8, 4], [1, 8]])
            dv(lambda e: e.tensor_tensor(out=t4, in0=el4, in1=goh4, op=ALU.mult), lgb)
            t4T = mk_ap(tmp32, 0, [[pr, 128], [32, NT], [1, 8], [8, 4]])
            dv(lambda e: e.tensor_reduce(out=v3(esel, 8), in_=t4T, axis=AX.X, op=ALU.add))
            dv(lambda e: e.tensor_reduce(out=m1, in_=v3(esel, 8), axis=AX.X, op=ALU.max))
            dv(lambda e: e.tensor_tensor(out=v3(oh1, 8), in0=v3(esel, 8), in1=bc_last(m1, 8), op=ALU.is_ge))
            dv(lambda e: e.scalar_tensor_tensor(out=esel2, in0=oh1, scalar=-BIG, in1=esel, op0=ALU.mult, op1=ALU.add))
            dv(lambda e: e.tensor_reduce(out=m2, in_=v3(esel2, 8), axis=AX.X, op=ALU.max))
            dv(lambda e: e.tensor_tensor(out=v3(oh2, 8), in0=v3(esel2, 8), in1=bc_last(m2, 8), op=ALU.is_ge))
            dv(lambda e: e.tensor_tensor(out=dm, in0=m2, in1=m1, op=ALU.subtract))
            pg.op('act', lambda e: e.activation(out=ed, in_=dm, func=AF.Exp), reads=[RV], writes=[RV])
            dv(lambda e: e.tensor_scalar(out=den, in0=ed, scalar1=1.0, scalar2=None, op0=ALU.add))
            dv(lambda e: e.reciprocal(out=p1, in_=den))
            dv(lambda e: e.tensor_tensor(out=p2, in0=ed, in1=p1, op=ALU.mult))
            dv(lambda e: e.tensor_tensor(out=p1, in0=p1, in1=pgp, op=ALU.mult))
            dv(lambda e: e.tensor_tensor(out=p2, in0=p2, in1=pgp, op=ALU.mult))
            dv(lambda e: e.tensor_tensor(out=v3(t1, 8), in0=v3(oh1, 8), in1=bc_last(p1, 8), op=ALU.mult))
            dv(lambda e: e.tensor_tensor(out=v3(t2, 8), in0=v3(oh2, 8), in1=bc_last(p2, 8), op=ALU.mult))
            dv(lambda e: e.tensor_tensor(out=t1, in0=t1, in1=t2, op=ALU.add))
            wpg4 = mk_ap(t1, 0, [[pr, 128], [8, NT], [0, 4], [1, 8]])
            gout = gate_all.rearrange("p a (g e) -> p a g e", g=4)
            pg.op('dve', lambda e: e.tensor_tensor(out=gout, in0=goh4, in1=wpg4, op=ALU.mult),
                  reads=[RV], writes=[B('gate', j) for j in range(NT)])
        cursor['R2'] = cur_save

        reset('R3')
        n_exp = debug.get('n_exp', N_EXP if stop_after >= 6 else 0)
        wg_e = [carve('R3', BF16, [P, KC, 256]) for _ in range(2)]
        wu_e = [carve('R3', BF16, [P, KC, 256]) for _ in range(2)]
        wd_e = [carve('R3', BF16, [P, 2, D]) for _ in range(2)]
        silt = [carve('R3', F32, [P, 512]) for _ in range(2)]
        hid = [carve('R2', BF16, [P, 2, TX]) for _ in range(2)]

        def load_gu(e_):
            s = e_ % 2
            al = (lambda q: [B('R3a', q)]) if e_ < 2 else (lambda q: [])
            pg.dma('pool', wg_e[s], weg_d[e_].rearrange("(p k) n -> p k n", k=KC), writes=[B('wg_e', s)] + al(2 * s))
            pg.dma('pool', wu_e[s], weu_d[e_].rearrange("(p k) n -> p k n", k=KC), writes=[B('wu_e', s)] + al(2 * s + 1))

        def load_d(e_):
            s = e_ % 2
            pg.dma('pool', wd_e[s], wed_d[e_].rearrange("(f p) n -> p f n", p=128),
                   writes=[B('wd_e', s)] + ([B('R3a', 4 + s)] if e_ < 2 else []))

        def gu_group(e_, b, fh):
            s = e_ % 2
            c0 = 16 + 512 * b
            bG, bU = nxt('moeGU', [[0, 1], [2, 3]])

            def mmGU(e, bk, wt):
                last = None
                for kc in range(KC):
                    last = e.matmul(banks[bk][:, 0:512], lhsT=wt[:, kc, fh * 128:(fh + 1) * 128],
                                    rhs=uT[:, kc, c0:c0 + 512], start=(kc == 0), stop=(kc == KC - 1))
                return last
            pg.op('pe', lambda e: mmGU(e, bG, wg_e[s]), reads=uT_bufs(c0, c0 + 512) + [B('wg_e', s)],
                  writes=[PB[bG]])
            pg.op('pe', lambda e: mmGU(e, bU, wu_e[s]), reads=uT_bufs(c0, c0 + 512) + [B('wu_e', s)],
                  writes=[PB[bU]])
            si = nxt('silt', [0, 1])
            pg.op('act', lambda e: e.activation(out=silt[si], in_=banks[bG][:, 0:512], func=AF.Silu),
                  reads=[PB[bG]], writes=[B('silt', si)])
            pg.op('dve', lambda e: e.tensor_tensor(
                out=hid[s][:, fh, 512 * b:512 * (b + 1)], in0=silt[si], in1=banks[bU][:, 0:512], op=ALU.mult),
                reads=[B('silt', si), PB[bU]], writes=[B('hid', s, b)])

        def y_tile(e_, j):
            s = e_ % 2
            for hh in range(2):
                bk = nxt('moeY', [4, 5, 6, 7])

                def mmy(e, bk=bk, hh=hh):
                    e.matmul(banks[bk][:, 0:512], lhsT=hid[s][:, 0, 128 * j:128 * (j + 1)],
                             rhs=wd_e[s][:, 0, 512 * hh:512 * (hh + 1)], start=True, stop=False)
                    return e.matmul(banks[bk][:, 0:512], lhsT=hid[s][:, 1, 128 * j:128 * (j + 1)],
                                    rhs=wd_e[s][:, 1, 512 * hh:512 * (hh + 1)], start=False, stop=True)
                pg.op('pe', mmy, reads=[B('hid', s, j // 4), B('wd_e', s)], writes=[PB[bk]])
                pg.op('dve', lambda e, bk=bk, hh=hh: e.scalar_tensor_tensor(
                    out=hacc[:, j, 512 * hh:512 * (hh + 1)], in0=banks[bk][:, 0:512], scalar=gate_all[:, j, e_:e_ + 1],
                    in1=hacc[:, j, 512 * hh:512 * (hh + 1)], op0=ALU.mult, op1=ALU.add),
                    reads=[PB[bk], B('gate', j), B('hacc', j, hh)], writes=[B('hacc', j, hh)])

        if n_exp > 0:
            load_gu(0)
            load_d(0)
        for e_ in range(n_exp + 1 if n_exp > 0 else 0):
            if e_ + 1 < n_exp:
                load_gu(e_ + 1)
            for gi in range(8):
                if e_ < n_exp:
                    gu_group(e_, gi // 2, gi % 2)
                if e_ >= 1:
                    y_tile(e_ - 1, 2 * gi)
                    y_tile(e_ - 1, 2 * gi + 1)
            if e_ + 1 < n_exp:
                load_d(e_ + 1)

        if run_tail:
            pg.dma('sp', gbc[0], gf_d.partition_broadcast(128), writes=[B('gbc', 0)])
            for j in range(NT):
                hb = [B('hacc', j, 0), B('hacc', j, 1)]
                pg.op('act', lambda e, j=j: e.activation(out=junkbf[:, 0:D], in_=hacc[:, j, :], func=AF.Square,
                                                         accum_out=ssq[:, j:j + 1]),
                      reads=hb, writes=[B('ssq', j), B('junkbf')])
            pg.op('act', lambda e: e.activation(out=rstd2, in_=ssq, func=AF.Sqrt, scale=1.0 / D, bias=eps_t),
                  reads=[B('ssq', j) for j in range(NT)] + [B('eps')], writes=[B('rstd2s')])
            pg.op('dve', lambda e: e.reciprocal(out=rstd2, in_=rstd2), reads=[B('rstd2s')], writes=[B('rstd2')])
            obuf = [carve_at('R0', 4096 * i, F32, [P, D]) for i in range(8)]
            for j in range(NT):
                hb = [B('hacc', j, 0), B('hacc', j, 1)]
                k = j % 8
                if False:
                    pg.op('act', lambda e, j=j, k=k: e.activation(
                        out=obuf[k], in_=hacc[:, j, :], func=AF.Identity, scale=rstd2[:, j:j + 1]),
                        reads=hb + [B('rstd2')], writes=[B('obuf', k)])
                    pg.op('pool', lambda e, k=k: e.tensor_tensor(out=obuf[k], in0=obuf[k], in1=gbc[0], op=ALU.mult),
                          reads=[B('obuf', k), B('gbc', 0)], writes=[B('obuf', k)])
                else:
                    pg.op('dve', lambda e, j=j, k=k: e.scalar_tensor_tensor(
                        out=obuf[k], in0=hacc[:, j, :], scalar=rstd2[:, j:j + 1], in1=gbc[0], op0=ALU.mult, op1=ALU.mult),
                        reads=hb + [B('rstd2'), B('gbc', 0)], writes=[B('obuf', k)])
                pg.dma('sp', out_d[128 * j:128 * (j + 1), :], obuf[k], reads=[B('obuf', k)], writes=[B('out', j)])

        pg.barrier()
        local = dict(uT=uT, ckeys=ckeys, cnT=cnT, ikT=ikT, poolT=poolT, attnT=attnT, iw_all=iw_all, score=score,
                     mb=mb, hacc=hacc, gate_all=gate_all, mergedT=mergedT, qabsT_t=qabsT_t,
                     bis=bis, thr=thr, iqT_t=iqT_t, diag=diag, mbT=mbT)
        for name in debug.get('dump', []):
            ap = local[name]
            shp = list(ap.shape)
            n = 1
            for s_ in shp[1:]:
                n *= s_
            dd = nc.dram_tensor("dbg_" + name, [shp[0], n], F32, kind="ExternalOutput").ap()
            flat = ap
            if len(shp) == 3:
                flat = ap.rearrange("p a b -> p (a b)")
            for c0 in range(0, n, 2048):
                c1 = min(n, c0 + 2048)
                pg.dma('pool', dd[:, c0:c1], flat[:, c0:c1], reads=[], writes=[B('dbg', name, c0)])
        pg.barrier()
        pg.emit(esem, dsem)
    return nc


_CACHE = {}


def _consts():
    ident = np.eye(128, dtype=np.float32)
    p = np.arange(128)[:, None]
    s = np.arange(128)[None, :]
    blockbias = np.where((s < 64) | (p >= 64), 0.0, -BIG).astype(np.float32)
    sel8 = np.zeros((32, 8, 128), np.float32)
    for h in range(8):
        sel8[h, h, :] = 1.0
    invcnt = np.broadcast_to((1.0 / np.arange(1, 17, dtype=np.float32))[None, :], (128, 16)).copy()
    pow2 = np.broadcast_to((1.0001 * 2.0 ** (-np.arange(NIT + 2, dtype=np.float64))).astype(np.float32)[None, :],
                           (128, NIT + 2)).copy()
    return dict(c_ident=ident, c_blockbias=blockbias, c_sel8=sel8.reshape(32, 1024), c_invcnt=invcnt, c_pow2=pow2)


def make_in_maps(inputs):
    f = lambda a: np.ascontiguousarray(np.asarray(a, dtype=np.float32))
    shared = dict(
        meta=f(inputs['meta_tokens']),
        norm1_g=f(inputs['norm1_g']).reshape(1, D),
        w_in=f(inputs['w_in']).reshape(D, 3496),
        kv_norm_g=f(inputs['kv_norm_g']).reshape(1, 128),
        w_uk=f(inputs['w_uk']).reshape(128, 512),
        w_uv=f(inputs['w_uv']).reshape(128, 512),
        w_pool=f(inputs['w_pool']).reshape(4, 128, 128),
        pool_scale=f(inputs['pool_scale']).reshape(1, 512),
        w_ba=f(inputs['w_branch_attn']).reshape(512, D),
        w_bp=f(inputs['w_branch_pool']).reshape(512, D),
        w_out=f(inputs['w_out']).reshape(D, D),
        norm2_g=f(inputs['norm2_g']).reshape(1, D),
        w_gr=f(inputs['w_group_router']).reshape(D, 4),
        b_gr=f(inputs['b_group_router']).reshape(1, 4),
        w_er=f(inputs['w_expert_router']).reshape(D, 32),
        b_er=f(inputs['b_expert_router']).reshape(1, 32),
        w_eg=f(inputs['w_expert_gate']).reshape(N_EXP, D, 256),
        w_eu=f(inputs['w_expert_up']).reshape(N_EXP, D, 256),
        w_ed=f(inputs['w_expert_down']).reshape(N_EXP, 256, D),
        final_g=f(inputs['final_norm_g']).reshape(1, D),
    )
    shared.update(_consts())
    x = f(inputs['x'])
    return [dict(shared, x=x[b]) for b in range(8)]


def kernel(**inputs):
    if 'nc' not in _CACHE:
        _CACHE['nc'] = build()
    nc = _CACHE['nc']
    in_maps = make_in_maps(inputs)
    res = run_bass_kernel_spmd(nc, in_maps, core_ids=list(range(8)))
    return np.stack([np.asarray(r["out"], dtype=np.float32).reshape(TX, D) for r in res.results], axis=0)
```

```python
import math
from contextlib import ExitStack

import numpy as np
import concourse.bass as bass
import concourse.mybir as mybir
from concourse.bass_utils import run_bass_kernel_spmd

F32 = mybir.dt.float32
BF16 = mybir.dt.bfloat16
ALU = mybir.AluOpType
AF = mybir.ActivationFunctionType
AX = mybir.AxisListType

P = 128
D = 1024
KC = 8
NT = 16
TX = 2048
T = 2064
EPS = 1e-6
ATTN_SCALE = 64 ** -0.5
IDX_SCALE = (8 ** -0.5) * (32 ** -0.5)
KTOP = 256
NIT = 16
NEG = -30000.0
BIG = 1.0e30
N_EXP = 32
ENGS = ['pe', 'act', 'dve', 'pool', 'sp']
NDS = 24


class Buf:
    __slots__ = ('name', 'lw', 'rd')

    def __init__(self, name):
        self.name = name
        self.lw = None
        self.rd = {}


class Op:
    __slots__ = ('fn', 'waits', 'dma')

    def __init__(self, fn, waits, dma):
        self.fn = fn
        self.waits = waits
        self.dma = dma


class Prog:
    def __init__(self, nc):
        self.nc = nc
        self.ops = {e: [] for e in ENGS}
        self.seen = {e: {} for e in ENGS}
        self.seen_dma = {e: set() for e in ENGS}
        self.dma_info = []
        self.dma_uses = [0] * NDS
        self.dma_hist = {}
        self.bufs = {}

    def B(self, *key):
        b = self.bufs.get(key)
        if b is None:
            b = Buf(key)
            self.bufs[key] = b
        return b

    def _deps(self, reads, writes):
        toks = set()
        for b in reads:
            if b.lw is not None:
                toks.add(b.lw)
        for b in writes:
            if b.lw is not None:
                toks.add(b.lw)
            toks.update(b.rd.values())
        return toks

    def _resolve(self, eng, toks):
        best = {}
        out = []
        for t in toks:
            if t[0] == 'c':
                if t[1] == eng and eng == 'pe':
                    continue
                if best.get(t[1], -1) < t[2]:
                    best[t[1]] = t[2]
            else:
                if t[1] not in self.seen_dma[eng]:
                    self.seen_dma[eng].add(t[1])
                    out.append(t)
        for pe_, i in best.items():
            if self.seen[eng].get(pe_, -1) >= i:
                continue
            self.seen[eng][pe_] = i
            out.append(('c', pe_, i))
        return out

    def _commit(self, tok, key, reads, writes):
        for b in reads:
            b.rd[key] = tok
        for b in writes:
            b.lw = tok
            b.rd = {}

    def op(self, eng, fn, reads=(), writes=()):
        ps = [b for b in reads if b.name[0] == 'psum']
        if ps:
            writes = list(writes) + [b for b in ps if b not in writes]
            reads = [b for b in reads if b.name[0] != 'psum']
        idx = len(self.ops[eng])
        tok = ('c', eng, idx)
        waits = self._resolve(eng, self._deps(reads, writes))
        self.ops[eng].append(Op(fn, waits, None))
        self._commit(tok, eng, reads, writes)
        return tok

    def dma(self, q, out, in_, reads=(), writes=(), **kw):
        did = len(self.dma_info)
        half = NDS // 2
        base = half if q == 'pool' else 0
        hist = self.dma_hist.setdefault(base, [])
        s = base + len(hist) % half
        self.dma_uses[s] += 1
        self.dma_info.append((s, 16 * self.dma_uses[s]))
        toks = self._deps(reads, writes)
        if len(hist) >= half:
            toks.add(('d', hist[len(hist) - half]))
        hist.append(did)
        waits = self._resolve(q, toks)
        self.ops[q].append(Op(lambda e, o=out, i=in_, k=kw: e.dma_start(out=o, in_=i, **k), waits, did))
        tok = ('d', did)
        self._commit(tok, tok, reads, writes)
        return tok

    def barrier(self):
        toks = set()
        for e in ENGS:
            if e != 'sp' and self.ops[e]:
                for i in range(len(self.ops[e]) - 1, -1, -1):
                    if self.ops[e][i].dma is None and self.ops[e][i].fn is not None:
                        toks.add(('c', e, i))
                        break
        for hist in self.dma_hist.values():
            for d in hist[-(NDS // 2):]:
                toks.add(('d', d))
        for e in ENGS:
            mine = set(t for t in toks if not (t[0] == 'c' and t[1] == e and e == 'pe'))
            waits = self._resolve(e, mine)
            if waits:
                self.ops[e].append(Op(None, waits, None))

    def emit(self, esem, dsem):
        sig = {e: set() for e in ENGS}
        for e in ENGS:
            for o in self.ops[e]:
                for t in o.waits:
                    if t[0] == 'c':
                        sig[t[1]].add(t[2])
        signo = {e: {} for e in ENGS}
        for e in ENGS:
            for n, i in enumerate(sorted(sig[e])):
                signo[e][i] = n + 1
        prog = self

        def stream(ename, h):
            for i, o in enumerate(prog.ops[ename]):
                for t in o.waits:
                    if t[0] == 'c':
                        h.wait_ge(esem[t[1]], signo[t[1]][t[2]])
                    else:
                        s, val = prog.dma_info[t[1]]
                        h.wait_ge(dsem[s], val)
                if o.fn is None:
                    continue
                inst = o.fn(h)
                if o.dma is not None:
                    inst.then_inc(dsem[prog.dma_info[o.dma][0]], 16)
                elif i in sig[ename]:
                    inst.then_inc(esem[ename], 1)

        with self.nc.Block() as block:
            @block.tensor
            def _(h):
                stream('pe', h)

            @block.scalar
            def _(h):
                stream('act', h)

            @block.vector
            def _(h):
                stream('dve', h)

            @block.gpsimd
            def _(h):
                stream('pool', h)

            @block.sync
            def _(h):
                stream('sp', h)


def mk_ap(base, extra_off, dims):
    return bass.AP(base.tensor, base.offset + extra_off, dims)


def pstep(ap):
    return ap.ap[0][0]


def build(debug=None):
    debug = debug or {}
    stop_after = debug.get('stop_after', 99)
    nc = bass.Bass("TRN2", target_bir_lowering=False)

    def din(name, shape):
        return nc.dram_tensor(name, list(shape), F32, kind="ExternalInput").ap()

    x_d = din("x", [TX, D])
    meta_d = din("meta", [16, D])
    g1_d = din("norm1_g", [1, D])
    win_d = din("w_in", [D, 3496])
    gkv_d = din("kv_norm_g", [1, 128])
    wuk_d = din("w_uk", [128, 512])
    wuv_d = din("w_uv", [128, 512])
    wpool_d = din("w_pool", [4, 128, 128])
    pscale_d = din("pool_scale", [1, 512])
    wba_d = din("w_ba", [512, D])
    wbp_d = din("w_bp", [512, D])
    wout_d = din("w_out", [D, D])
    g2_d = din("norm2_g", [1, D])
    wgr_d = din("w_gr", [D, 4])
    bgr_d = din("b_gr", [1, 4])
    wer_d = din("w_er", [D, 32])
    ber_d = din("b_er", [1, 32])
    weg_d = din("w_eg", [N_EXP, D, 256])
    weu_d = din("w_eu", [N_EXP, D, 256])
    wed_d = din("w_ed", [N_EXP, 256, D])
    gf_d = din("final_g", [1, D])
    cid_d = din("c_ident", [128, 128])
    cbb_d = din("c_blockbias", [128, 128])
    csel_d = din("c_sel8", [32, 8 * 128])
    cinv_d = din("c_invcnt", [128, 16])
    cpow_d = din("c_pow2", [128, NIT + 2])
    out_d = nc.dram_tensor("out", [TX, D], F32, kind="ExternalOutput").ap()
    dbg_out = {}

    pg = Prog(nc)
    B = pg.B

    with ExitStack() as es:
        esem = {e: es.enter_context(nc.semaphore("sem_" + e)) for e in ENGS}
        dsem = [es.enter_context(nc.semaphore("dsem%d" % i)) for i in range(NDS)]

        R0_B, R1_B, R2_B, R3_B = 62 * 1024, 64 * 1024, 42 * 1024, 34 * 1024
        arenas = {}
        for nm, nb in (('R0', R0_B), ('R1', R1_B), ('R2', R2_B), ('R3', R3_B)):
            arenas[nm] = (es.enter_context(nc.sbuf_tensor(nm, [P, nb // 4], F32)), nb)
        cursor = {}

        def reset(nm):
            cursor[nm] = 0

        def carve(nm, dtype, shape):
            h, nb = arenas[nm]
            esz = 4 if dtype == F32 else 2
            n = 1
            for s in shape[1:]:
                n *= s
            off = (cursor[nm] + 63) // 64 * 64
            assert off + n * esz <= nb, (nm, off, n * esz, nb, shape)
            cursor[nm] = off + n * esz
            hv = h if dtype == F32 else h.bitcast(dtype)
            ap = hv[0:shape[0], off // esz: off // esz + n]
            if len(shape) == 3:
                ap = ap.rearrange("p (a b) -> p a b", a=shape[1])
            elif len(shape) == 4:
                ap = ap.rearrange("p (a b c) -> p a b c", a=shape[1], b=shape[2])
            return ap

        def carve_at(nm, off, dtype, shape):
            save = cursor[nm]
            cursor[nm] = off
            ap = carve(nm, dtype, shape)
            assert (off + 63) // 64 * 64 == off
            cursor[nm] = save
            return ap

        for nm in arenas:
            reset(nm)

        banks = [es.enter_context(nc.psum_tensor("ps%d" % i, [P, 512], F32)) for i in range(8)]
        banks_bf = [b.bitcast(BF16) for b in banks]
        PB = [B('psum', i) for i in range(8)]

        uT = carve('R0', BF16, [P, KC, T])
        off_idle = (cursor['R0'] + 63) // 64 * 64
        gbc = [carve('R0', F32, [P, D]) for _ in range(2)]
        xt = [carve('R0', F32, [P, D]) for _ in range(2)]
        xn = [carve('R0', BF16, [P, D]) for _ in range(2)]
        junkbf = carve('R0', BF16, [P, T])
        ident_f = carve('R0', F32, [P, 128])
        ident_bf = carve('R0', BF16, [P, 128])
        stats = carve('R0', F32, [P, 512])
        stat_i = [0]

        def stat(n=1):
            i = stat_i[0]
            if i + n > 512:
                i = 0
            stat_i[0] = i + n
            return stats[:, i:i + n], B('stat', i, n)

        def stat_bufs(i, n):
            return [B('statc', c) for c in range(i, i + n)]

        def stat2(n=1):
            i = stat_i[0]
            if i + n > 512:
                i = 0
            stat_i[0] = i + n
            return stats[:, i:i + n], stat_bufs(i, n)

        ckeys = carve('R1', BF16, [P, 17, 129])
        cnT = carve('R1', BF16, [P, T])
        ikT = carve('R1', BF16, [P, T])
        poolT = carve('R1', BF16, [P, 4, TX])
        attnT = carve('R1', BF16, [P, 4, TX])
        iw_all = carve('R1', F32, [P, NT, 8])
        gkv_bc = carve('R1', F32, [P, 128])
        blockbias = carve('R1', F32, [P, 128])
        sel8 = carve('R1', BF16, [P, 8, 128])
        invcnt = carve('R1', F32, [P, 16])
        pow2 = carve('R1', F32, [P, NIT + 2])
        pscale = carve('R1', F32, [P, 4])
        cmaxsel = carve('R1', BF16, [P, 8, 32])
        cmax = carve('R1', F32, [P, 1])
        ones_bf = carve('R1', BF16, [P, 128])
        iqT_all = carve('R1', BF16, [P, 3, TX])
        cmaxB = carve('R1', BF16, [P, 128])

        pg.dma('sp', ident_f, cid_d, writes=[B('ident_f')])
        pg.op('dve', lambda e: e.tensor_copy(out=ident_bf, in_=ident_f), reads=[B('ident_f')], writes=[B('ident_bf')])
        pg.dma('pool', sel8[0:32].rearrange("p a b -> p (a b)"), csel_d, writes=[B('sel8')])
        pg.dma('sp', gbc[0], g1_d.partition_broadcast(128), writes=[B('gbc', 0)])
        pg.op('pool', lambda e: e.memset(ckeys[:, 16, 0:128], 0.0), writes=[B('ckeys', 16)])
        pg.op('pool', lambda e: e.memset(ckeys[:, :, 128:129], 1.0), writes=[B('ckeys_ones')])

        def late_consts():
            pg.dma('sp', blockbias, cbb_d, writes=[B('blockbias')])
            pg.dma('sp', invcnt, cinv_d, writes=[B('invcnt')])
            pg.dma('sp', pow2, cpow_d, writes=[B('pow2')])
            pg.dma('sp', pscale, pscale_d.rearrange("o (g p) -> p (o g)", p=128), writes=[B('pscale')],
                   allow_slow_non_contiguous=True)
            pg.dma('sp', gkv_bc, gkv_d.partition_broadcast(128), writes=[B('gkv_bc')])

        def perm3(ap_2d, rows):
            return ap_2d[0:rows].rearrange("t (p k) -> t p k", k=KC)

        def permout(ap_2d, rows):
            return ap_2d[0:rows].rearrange("t (k p) -> t p k", k=KC)

        rot = {}

        def nxt(name, lst):
            i = rot.get(name, 0)
            rot[name] = i + 1
            return lst[i % len(lst)]

        def rmsnorm_to_uT(src2d, src_bufs, rows, col0, gb, gb_buf, tag):
            k = nxt('xn', [0, 1])
            ss, ssb = stat2()
            rt, rtb = stat2()
            rs, rsb = stat2()
            pg.op('act', lambda e: e.activation(out=junkbf[0:rows, 0:D], in_=src2d[0:rows], func=AF.Square,
                                                accum_out=ss[0:rows]),
                  reads=src_bufs, writes=ssb + [B('junkbf')])
            pg.op('act', lambda e: e.activation(out=rt[0:rows], in_=ss[0:rows], func=AF.Sqrt, scale=1.0 / D,
                                                bias=eps_t[0:rows]),
                  reads=ssb + [B('eps')], writes=rtb)
            pg.op('dve', lambda e: e.reciprocal(out=rs[0:rows], in_=rt[0:rows]), reads=rtb, writes=rsb)
            pg.op('dve', lambda e: e.scalar_tensor_tensor(out=xn[k][0:rows], in0=src2d[0:rows],
                                                          scalar=rs[0:rows], in1=gb[0:rows],
                                                          op0=ALU.mult, op1=ALU.mult),
                  reads=src_bufs + rsb + [gb_buf], writes=[B('xn', k)])
            bk = nxt('tp', [0, 1])

            def tps(e):
                last = None
                for kc in range(KC):
                    last = e.transpose(out=banks_bf[bk][:, kc * 128: kc * 128 + rows],
                                       in_=xn[k][0:rows].rearrange("t (p k) -> t k p", k=KC)[:, kc, :],
                                       identity=ident_bf[0:rows, 0:rows])
                return last
            pg.op('pe', tps, reads=[B('xn', k), B('ident_bf')], writes=[PB[bk]])
            src = banks_bf[bk][:, 0:1024].rearrange("p (a b) -> p a b", a=KC)[:, :, 0:rows]

            def back():
                pg.op('act', lambda e: e.copy(out=uT[:, :, col0:col0 + rows], in_=src),
                      reads=[PB[bk]], writes=[B('uT', tag)])
            return back

        eps_t = carve('R0', F32, [P, 1])
        pg.op('pool', lambda e: e.memset(eps_t, EPS), writes=[B('eps')])

        reset('R2')
        reset('R3')
        pvT = carve('R3', F32, [P, 4, T])
        wslab = carve('R2', BF16, [P, KC, 512])
        wsmall = carve('R2', BF16, [P, KC, 136])
        wik = carve('R2', BF16, [P, KC, 96])
        w_iq2 = carve('R2', BF16, [P, KC, 256])
        wpool = carve('R2', BF16, [P, 4, 128])
        ptmp = [carve('R2', F32, [P, T]) for _ in range(2)]
        dT = [carve('R2', BF16, [P, T]) for _ in range(2)]
        tmpc = carve('R2', F32, [P, 16])

        xt4 = xt + [ptmp[0][:, 0:D], ptmp[1][:, 0:D]]
        tiles = [-1] + list(range(NT))
        pend1 = None
        for j in tiles:
            rows = 16 if j < 0 else 128
            col0 = 0 if j < 0 else 16 + 128 * j
            k = nxt('xt4', [0, 1, 2, 3])
            src = meta_d if j < 0 else x_d[128 * j:128 * (j + 1), :]
            pg.dma('sp', xt4[k][0:rows], src, writes=[B('xt', k)])
            if j == 1:
                late_consts()
            bk_ = rmsnorm_to_uT(xt4[k], [B('xt', k)], rows, col0, gbc[0], B('gbc', 0), j)
            if pend1 is not None:
                pend1()
            pend1 = bk_
        pend1()

        def uT_bufs(c0, c1):
            res = []
            if c0 < 16:
                res.append(B('uT', -1))
            for j in range(NT):
                a, b_ = 16 + 128 * j, 16 + 128 * (j + 1)
                if a < c1 and b_ > c0:
                    res.append(B('uT', j))
            return res

        win_p = win_d.rearrange("(p k) n -> p k n", k=KC)

        pg.dma('pool', wsmall[:, :, 0:128], win_p[:, :, 512:640], writes=[B('wsmall')])
        pg.dma('pool', wsmall[:, :, 128:136], win_p[:, :, 928:936], reads=[], writes=[B('wsmall2')])
        for r_ in range(3):
            pg.dma('pool', wik[:, :, 32 * r_:32 * r_ + 32], win_p[:, :, 896:928], writes=[B('wik', r_)])
        pg.dma('pool', w_iq2, win_p[:, :, 640:896], writes=[B('w_iq2')])
        pg.dma('pool', wslab, win_p[:, :, 936:1448], writes=[B('wslab')])
        pg.dma('pool', wpool, wpool_d.rearrange("g c d -> c g d"), writes=[B('wpool')])

        def p2a_f1(j):
            rows = 16 if j < 0 else 128
            col0 = 0 if j < 0 else 16 + 128 * j
            chunk = 16 if j < 0 else j
            bk = nxt('p2a', [2, 3])

            def mm(e):
                last = None
                for kc in range(KC):
                    last = e.matmul(banks[bk][0:rows, 0:136], lhsT=uT[:, kc, col0:col0 + rows],
                                    rhs=wsmall[:, kc, :], start=(kc == 0), stop=(kc == KC - 1))
                return last
            pg.op('pe', mm, reads=[B('uT', j), B('wsmall'), B('wsmall2')], writes=[PB[bk]])
            ss, ssb = stat2()
            rt, rtb = stat2()
            rs, rsb = stat2()
            pg.op('act', lambda e: e.activation(
                out=junkbf[0:rows, 0:128], in_=banks[bk][0:rows, 0:128], func=AF.Square, accum_out=ss[0:rows]),
                reads=[PB[bk]], writes=ssb + [B('junkbf')])
            pg.op('act', lambda e: e.activation(
                out=rt[0:rows], in_=ss[0:rows], func=AF.Sqrt, scale=1.0 / 128, bias=eps_t[0:rows]),
                reads=ssb + [B('eps')], writes=rtb)
            pg.op('dve', lambda e: e.reciprocal(out=rs[0:rows], in_=rt[0:rows]), reads=rtb, writes=rsb)
            pg.op('dve', lambda e: e.scalar_tensor_tensor(
                out=ckeys[0:rows, chunk, 0:128], in0=banks[bk][0:rows, 0:128], scalar=rs[0:rows],
                in1=gkv_bc[0:rows], op0=ALU.mult, op1=ALU.mult),
                reads=[PB[bk], B('gkv_bc')] + rsb, writes=[B('ckeys', chunk)])
            if j >= 0:
                pg.op('dve', lambda e: e.tensor_scalar(
                    out=iw_all[:, j, :], in0=banks[bk][:, 128:136], scalar1=IDX_SCALE, scalar2=None,
                    op0=ALU.mult), reads=[PB[bk]], writes=[B('iw', j)])

            def f2():
                bt = nxt('p2at', [4, 5])
                pg.op('pe', lambda e: e.transpose(
                    out=banks_bf[bt][:, 0:rows], in_=ckeys[0:rows, chunk, 0:128], identity=ident_bf[0:rows, 0:rows]),
                    reads=[B('ckeys', chunk), B('ident_bf')], writes=[PB[bt]])

                def f3():
                    pg.op('act', lambda e: e.copy(
                        out=cnT[:, col0:col0 + rows], in_=banks_bf[bt][:, 0:rows]),
                        reads=[PB[bt]], writes=[B('cnT', j)])
                return f3
            return f2

        p_f2, p_f3 = None, None
        for j in tiles:
            f2 = p2a_f1(j)
            f3 = p_f2() if p_f2 is not None else None
            if p_f3 is not None:
                p_f3()
            p_f2, p_f3 = f2, f3
        f3 = p_f2()
        if p_f3 is not None:
            p_f3()
        f3()

        blocks = [(0, 16)] + [(16 + 512 * b, 16 + 512 * (b + 1)) for b in range(4)]
        allb = list(range(8))
        evac_i = [0]

        def evac_copy(out_ap, in_ap, reads, writes):
            evac_i[0] += 1
            if evac_i[0] % 2 == 0:
                pg.op('act', lambda e: e.copy(out=out_ap, in_=in_ap), reads=reads, writes=writes)
            else:
                pg.op('dve', lambda e: e.tensor_copy(out=out_ap, in_=in_ap), reads=reads, writes=writes)

        def act_evac(out_ap, in_ap, reads, writes):
            pg.op('act', lambda e: e.copy(out=out_ap, in_=in_ap), reads=reads, writes=writes)

        def p2b_ik():
            for bi, (c0, c1) in enumerate(blocks):
                bk = nxt('gen', allb)
                n = c1 - c0

                def mm(e, bk=bk, c0=c0, c1=c1, n=n):
                    last = None
                    for kc in range(KC):
                        last = e.matmul(banks[bk][0:96, 0:n], lhsT=wik[:, kc, :], rhs=uT[:, kc, c0:c1],
                                        start=(kc == 0), stop=(kc == KC - 1))
                    return last
                pg.op('pe', mm, reads=uT_bufs(c0, c1) + [B('wik', 0), B('wik', 1), B('wik', 2)], writes=[PB[bk]])
                act_evac(ikT[0:96, c0:c1], banks[bk][0:96, 0:n], [PB[bk]], [B('ikT', bi)])

        def p2b_iq():
            for g in range(3):
                M = 96 if g < 2 else 64
                for b in range(4):
                    bk = nxt('gen', allb)
                    c0 = 16 + 512 * b

                    def mmi(e, bk=bk, g=g, M=M, c0=c0):
                        last = None
                        for kc in range(KC):
                            last = e.matmul(banks[bk][0:M, 0:512], lhsT=w_iq2[:, kc, 96 * g:96 * g + M],
                                            rhs=uT[:, kc, c0:c0 + 512], start=(kc == 0), stop=(kc == KC - 1))
                        return last
                    pg.op('pe', mmi, reads=uT_bufs(c0, c0 + 512) + [B('w_iq2')], writes=[PB[bk]])
                    act_evac(iqT_all[0:M, g, 512 * b:512 * (b + 1)], banks[bk][0:M, 0:512], [PB[bk]],
                             [B('iqT_all', g, b)])

        def p2b_pv(m):
            for bi, (c0, c1) in enumerate(blocks):
                bk = nxt('gen', allb)
                n = c1 - c0

                def mm(e, bk=bk, c0=c0, c1=c1, n=n):
                    last = None
                    for kc in range(KC):
                        last = e.matmul(banks[bk][:, 0:n], lhsT=wslab[:, kc, m * 128:(m + 1) * 128],
                                        rhs=uT[:, kc, c0:c1], start=(kc == 0), stop=(kc == KC - 1))
                    return last
                pg.op('pe', mm, reads=uT_bufs(c0, c1) + [B('wslab')], writes=[PB[bk]])
                act_evac(pvT[:, m, c0:c1], banks[bk][:, 0:n], [PB[bk]], [B('pvT', m)])

        p3_di = {}

        def p3_chain(g):
            w = 2 << g
            src = pvT[:, g, :]
            cur = src
            curb = [B('pvT', g)]
            for k in [1, 2, 4, 8][:g + 1]:
                pi = nxt('ptmp', [0, 1])
                dst = ptmp[pi]
                pg.op('pool', lambda e, dst=dst, cur=cur, k=k: e.tensor_tensor(
                    out=dst[:, k:1024], in0=cur[:, k:1024], in1=cur[:, 0:1024 - k], op=ALU.add),
                    reads=curb, writes=[B('ptmp', pi), B('xt', 2 + pi)])
                pg.op('dve', lambda e, dst=dst, cur=cur, k=k: e.tensor_tensor(
                    out=dst[:, 1024:T], in0=cur[:, 1024:T], in1=cur[:, 1024 - k:T - k], op=ALU.add),
                    reads=curb, writes=[B('ptmp', pi, 'hi')])
                pg.op('pool', lambda e, dst=dst, cur=cur, k=k: e.tensor_copy(out=dst[:, 0:k], in_=cur[:, 0:k]),
                      reads=curb, writes=[B('ptmp', pi, 'head'), B('xt', 2 + pi)])
                cur = dst
                curb = [B('ptmp', pi), B('ptmp', pi, 'hi'), B('ptmp', pi, 'head')]
            di = nxt('dT', [0, 1])
            p3_di[g] = di
            cb = curb
            pg.op('dve', lambda e: e.scalar_tensor_tensor(
                out=dT[di][:, w - 1:T], in0=cur[:, w - 1:T], scalar=1.0 / w, in1=src[:, w - 1:T],
                op0=ALU.mult, op1=ALU.subtract),
                reads=cb + [B('pvT', g)], writes=[B('dT', di)])
            pg.op('dve', lambda e: e.tensor_tensor(
                out=tmpc[:, 0:w - 1], in0=cur[:, 0:w - 1], in1=invcnt[:, 0:w - 1], op=ALU.mult),
                reads=cb + [B('invcnt')], writes=[B('tmpc')])
            pg.op('dve', lambda e: e.tensor_tensor(
                out=dT[di][:, 0:w - 1], in0=tmpc[:, 0:w - 1], in1=src[:, 0:w - 1], op=ALU.subtract),
                reads=[B('tmpc'), B('pvT', g)], writes=[B('dT', di, 'head')])

        def p3_mm(g):
            di = p3_di[g]
            for b in range(4):
                bk = nxt('gen', allb)
                pg.op('pe', lambda e, bk=bk, b=b: e.matmul(
                    banks[bk][:, 0:512], lhsT=wpool[:, g, :], rhs=dT[di][:, 16 + 512 * b:16 + 512 * (b + 1)],
                    start=True, stop=True),
                    reads=[B('wpool'), B('dT', di), B('dT', di, 'head')], writes=[PB[bk]])
                pg.op('act', lambda e, bk=bk, b=b: e.activation(
                    out=poolT[:, g, 512 * b:512 * (b + 1)], in_=banks[bk][:, 0:512], func=AF.Identity,
                    scale=pscale[:, g:g + 1]),
                    reads=[PB[bk], B('pscale')], writes=[B('poolT', b)])

        W_Q_OFF = 25344
        w_q = carve_at('R3', W_Q_OFF, BF16, [P, KC, 512])
        p2b_pv(3)
        p2b_pv(2)
        p3_chain(3)
        p2b_pv(1)
        p3_chain(2)
        if stop_after >= 4:
            pg.dma('pool', w_q, win_p[:, :, 0:512], writes=[B('w_q'), B('pvT', 3)])
        p2b_pv(0)
        p2b_ik()
        p2b_iq()
        p3_mm(3)
        p3_mm(2)
        p3_chain(1)
        p3_mm(1)
        p3_chain(0)
        p3_mm(0)

        if stop_after <= 3:
            pass
        pg.barrier()
        reset('R2')
        reset('R3')
        w_ba = carve_at('R0', off_idle, BF16, [P, 4, D])
        w_bp = carve_at('R0', off_idle + 8192, BF16, [P, 4, D])
        score = carve('R3', F32, [P, T])
        mb = carve('R3', BF16, [P, T])
        mbT = carve('R3', BF16, [P, 17, 128])
        ukT_pad = carve('R2', BF16, [P, 8, 128])
        uv_pad = carve('R2', BF16, [P, 8, 128])
        wuk_bf = carve('R2', BF16, [P, 512])
        qT_t = carve('R2', BF16, [P, 4, 128])
        qabsT_t = carve('R2', BF16, [P, 8, 128])
        absq = carve('R2', BF16, [P, 8, 128])
        iqT_t = carve('R2', BF16, [P, 8, 128])
        rbuf = [carve('R2', BF16, [P, 512]) for _ in range(4)]
        pT = [carve('R2', BF16, [P, 512]) for _ in range(3)]
        diag = carve('R2', BF16, [P, 8, 128])
        olatT_t = carve('R2', BF16, [P, 8, 128])
        negm8 = carve('R2', BF16, [P, 128])
        bis = carve('R2', F32, [P, 4 * (NIT + 2)])
        rsum = carve('R2', F32, [P, 8])
        thr = carve('R2', F32, [P, 2])

        if stop_after >= 4:
            pg.dma('pool', wuk_bf, wuk_d, writes=[B('wuk_bf')])
            pg.dma('pool', w_ba, wba_d.rearrange("(m p) n -> p m n", p=128),
                   writes=[B('w_ba'), B('gbc', 0), B('gbc', 1)])
            pg.dma('pool', w_bp, wbp_d.rearrange("(m p) n -> p m n", p=128),
                   writes=[B('w_bp'), B('xt', 0), B('xt', 1)])
            pg.op('pool', lambda e: e.memset(ukT_pad, 0.0), writes=[B('ukT_pad')])
            pg.op('pool', lambda e: e.memset(uv_pad, 0.0), writes=[B('uv_pad')])
            for hp in range(2):
                src = wuv_d.rearrange("c (m two d) -> c m two d", two=2, d=64)[:, :, hp, :]
                dst = uv_pad.rearrange("c (m two) n -> c m two n", two=2)[:, :, hp, 64 * hp:64 * hp + 64]
                pg.dma('pool', dst, src, reads=[B('uv_pad')], writes=[B('uv_pad', hp)])
            for m in range(4):
                bk = nxt('gen', allb)
                pg.op('pe', lambda e, bk=bk, m=m: e.transpose(
                    out=banks_bf[bk][:, 0:128], in_=wuk_bf[:, m * 128:(m + 1) * 128], identity=ident_bf),
                    reads=[B('wuk_bf'), B('ident_bf')], writes=[PB[bk]])
                for hp in range(2):
                    h = 2 * m + hp
                    pg.op('dve', lambda e, bk=bk, h=h, hp=hp: e.tensor_copy(
                        out=ukT_pad[64 * hp:64 * hp + 64, h, :], in_=banks_bf[bk][64 * hp:64 * hp + 64, 0:128]),
                        reads=[PB[bk], B('ukT_pad')], writes=[B('ukT_pad', h)])
            pg.op('dve', lambda e: e.tensor_reduce(out=cmax, in_=cnT, axis=AX.X, op=ALU.max,
                                                   apply_absolute_value=True),
                  reads=[B('cnT', j) for j in tiles], writes=[B('cmax')])
            pg.op('pool', lambda e: e.memset(ones_bf, 1.0), writes=[B('ones_bf')])
            pg.op('dve', lambda e: e.tensor_copy(out=cmaxB, in_=mk_ap(cmax, 0, [[pstep(cmax), 128], [0, 128]])),
                  reads=[B('cmax')], writes=[B('cmaxB')])
            pg.op('pool', lambda e: e.memset(cmaxsel, 0.0), writes=[B('cmaxsel')])
            pg.op('pool', lambda e: e.memset(mbT[:, 16, :], NEG), writes=[B('mbT', 16)])
            for h in range(8):
                pg.op('dve', lambda e, h=h: e.tensor_copy(out=cmaxsel[:, h, h:h + 1], in_=cmax),
                      reads=[B('cmax'), B('cmaxsel')], writes=[B('cmaxsel', h)])
        ukb = [B('ukT_pad')] + [B('ukT_pad', h) for h in range(8)]
        uvb = [B('uv_pad'), B('uv_pad', 0), B('uv_pad', 1)]
        cmb = [B('cmaxsel')] + [B('cmaxsel', h) for h in range(8)]
        ikb = [B('ikT', bi) for bi in range(5)]
        cnb = [B('cnT', j) for j in tiles]
        ckb = [B('ckeys', c) for c in range(17)] + [B('ckeys_ones')]

        mids = bis[:, 0:NIT + 2]
        cnts = bis[:, NIT + 2:2 * (NIT + 2)]
        dds = bis[:, 2 * (NIT + 2):3 * (NIT + 2)]
        Wc = bis[:, 3 * (NIT + 2):4 * (NIT + 2)]

        n_attn_tiles = NT if stop_after >= 4 else 0
        n_attn_tiles = debug.get('n_attn_tiles', n_attn_tiles)
        qabs2 = [qabsT_t, carve('R2', BF16, [P, 8, 128]), carve('R2', BF16, [P, 8, 128])]
        mb2 = [mb, carve('R3', BF16, [P, T])]
        assert cursor['R3'] <= W_Q_OFF, cursor['R3']
        score2 = [score, carve('R2', F32, [P, T])]
        negsh = carve('R2', F32, [P, 8])
        lnS = carve('R2', F32, [P, 512])
        rcpS = carve('R2', F32, [P, 512])
        ROT4 = [0, 1, 2, 3]
        PSS = [6, 7]
        ACC = [4, 5]

        def act_copy(out_ap, in_ap, reads, writes):
            pg.op('act', lambda e: e.copy(out=out_ap, in_=in_ap), reads=reads, writes=writes)

        def chunks_of(j):
            S = 16 + 128 * (j + 1)
            return [(0, 16)] + [(16 + 512 * m, min(16 + 512 * (m + 1), S)) for m in range((S - 16 + 511) // 512)]

        def stageA(j, tick=None):
            par = j % 2
            par3 = j % 3
            sc, qa = score2[par], qabs2[par3]
            qc = 16 + 128 * j
            ub = [B('uT', j)]
            for h in range(8):
                pg.op('act', lambda e, h=h, j=j: e.activation(out=diag[:, h, :], in_=ident_f, func=AF.Identity,
                                                              scale=iw_all[:, j, h:h + 1]),
                      reads=[B('ident_f'), B('iw', j)], writes=[B('diag', h)])
            items = []
            for (c0, c1) in chunks_of(j):
                bs = nxt('pss', PSS)
                for hp_ in range(4):
                    items.append((c0, c1, bs, hp_))

            def emit_x(it):
                c0, c1, bs, hp_ = it
                n = c1 - c0
                res = []
                for hh in range(2):
                    h = 2 * hp_ + hh
                    bx = nxt('rot4', ROT4)
                    g_, a_ = h // 3, h % 3
                    pg.op('pe', lambda e, bx=bx, g_=g_, a_=a_, c0=c0, c1=c1, n=n: e.matmul(
                        banks[bx][:, 0:n], lhsT=iqT_all[32 * a_:32 * a_ + 32, g_, 128 * j:128 * (j + 1)],
                        rhs=ikT[32 * a_:32 * a_ + 32, c0:c1], start=True, stop=True),
                        reads=[B('iqT_all', g_, j // 4)] + ikb, writes=[PB[bx]])
                    ri = nxt('rbuf', [0, 1, 2, 3])
                    pg.op('act', lambda e, bx=bx, ri=ri, n=n: e.activation(
                        out=rbuf[ri][:, 0:n], in_=banks[bx][:, 0:n], func=AF.Relu),
                        reads=[PB[bx]], writes=[B('rbuf', ri)])
                    res.append((h, ri))
                return res

            def emit_d(it, res):
                c0, c1, bs, hp_ = it
                n = c1 - c0
                for (h, ri) in res:
                    pg.op('pe', lambda e, bs=bs, h=h, ri=ri, n=n: e.matmul(
                        banks[bs][:, 0:n], lhsT=diag[:, h, :], rhs=rbuf[ri][:, 0:n], start=(h == 0), stop=(h == 7)),
                        reads=[B('diag', h), B('rbuf', ri)], writes=[PB[bs]])
                if hp_ == 3:
                    act_copy(sc[:, c0:c1], banks[bs][:, 0:n], [PB[bs]], [B('score', par, c0)])

            prev = None
            for it in items:
                res = emit_x(it)
                if prev is not None:
                    emit_d(*prev)
                prev = (it, res)
                if tick is not None:
                    tick()
            emit_d(*prev)
            bk = nxt('rot4', ROT4)

            def mmq(e, bk=bk, qc=qc):
                last = None
                for m in range(4):
                    for kc in range(KC):
                        last = e.matmul(banks[bk][:, m * 128:(m + 1) * 128], lhsT=w_q[:, kc, m * 128:(m + 1) * 128],
                                        rhs=uT[:, kc, qc:qc + 128], start=(kc == 0), stop=(kc == KC - 1))
                return last
            pg.op('pe', mmq, reads=ub + [B('w_q')], writes=[PB[bk]])
            act_copy(qT_t, banks[bk][:, 0:512].rearrange("p (a b) -> p a b", a=4), [PB[bk]], [B('qT_t')])
            for half in range(2):
                bk = nxt('rot4', ROT4)

                def mma(e, bk=bk, half=half):
                    last = None
                    for hh in range(4):
                        h = 4 * half + hh
                        last = e.matmul(banks[bk][:, hh * 128:(hh + 1) * 128], lhsT=ukT_pad[:, h, :],
                                        rhs=qT_t[:, h // 2, :], start=True, stop=True)
                    return last
                pg.op('pe', mma, reads=ukb + [B('qT_t')], writes=[PB[bk]])
                src3 = banks[bk][:, 0:512].rearrange("p (a b) -> p a b", a=4)
                act_copy(qa[:, 4 * half:4 * half + 4, :], src3, [PB[bk]], [B('qabsT_t', par3, half)])
                pg.op('dve', lambda e, half=half, qa=qa: e.scalar_tensor_tensor(
                    out=absq[:, 4 * half:4 * half + 4, :], in0=qa[:, 4 * half:4 * half + 4, :], scalar=-1.0,
                    in1=qa[:, 4 * half:4 * half + 4, :], op0=ALU.mult, op1=ALU.max),
                    reads=[B('qabsT_t', par3, half)], writes=[B('absq', half)])

        def stageA2(j):
            par = j % 3
            for half in range(2):
                bk = nxt('rot4', ROT4)

                def mmb(e, bk=bk, half=half):
                    last = None
                    for hh in range(4):
                        last = e.matmul(banks[bk][:, hh * 128:(hh + 1) * 128], lhsT=cmaxB,
                                        rhs=absq[:, 4 * half + hh, :], start=True, stop=True)
                    return last
                pg.op('pe', mmb, reads=[B('cmaxB'), B('absq', half)], writes=[PB[bk]])
                pg.op('dve', lambda e, bk=bk, half=half: e.tensor_reduce(
                    out=negsh[:, 4 + half:5 + half], in_=banks[bk][:, 0:512], axis=AX.X, op=ALU.max),
                    reads=[PB[bk]], writes=[B('negsh_t', half)])
            pg.op('dve', lambda e: e.tensor_tensor(out=negsh[:, 6:7], in0=negsh[:, 4:5], in1=negsh[:, 5:6], op=ALU.max),
                  reads=[B('negsh_t', 0), B('negsh_t', 1)], writes=[B('negsh_m')])
            pg.op('dve', lambda e, par=par: e.tensor_scalar(out=negsh[:, par:par + 1], in0=negsh[:, 6:7],
                                                            scalar1=-ATTN_SCALE, scalar2=None, op0=ALU.mult),
                  reads=[B('negsh_m')], writes=[B('negsh', par)])

        def scbufs(j):
            return [B('score', j % 2, c0) for (c0, c1) in chunks_of(j)]

        def stageB_init(j):
            S = 16 + 128 * (j + 1)
            sc = score2[j % 2]
            scb = scbufs(j)
            Rr, Rb = stat2()
            pg.op('dve', lambda e: e.tensor_reduce(out=Rr, in_=sc[:, 0:S], axis=AX.X, op=ALU.max,
                                                   apply_absolute_value=True),
                  reads=scb, writes=Rb)
            pg.op('dve', lambda e: e.tensor_tensor(out=sc[:, S - 128:S], in0=sc[:, S - 128:S],
                                                   in1=blockbias, op=ALU.add),
                  reads=scb + [B('blockbias')], writes=[B('score', j % 2, 16 + 512 * (j // 4))])
            pg.op('dve', lambda e: e.tensor_scalar(out=Wc, in0=pow2, scalar1=Rr, scalar2=1e-30,
                                                   op0=ALU.mult, op1=ALU.add),
                  reads=Rb + [B('pow2')], writes=[B('Wc')])
            pg.op('dve', lambda e: e.memset(mids[:, 0:1], 0.0), writes=[B('mid', 0)])

        def stageB_iter(j, n_):
            S = 16 + 128 * (j + 1)
            sc = score2[j % 2]
            pg.op('dve', lambda e: e.tensor_scalar(
                out=junkbf[:, 0:S], in0=sc[:, 0:S], scalar1=mids[:, n_ - 1:n_], scalar2=None,
                op0=ALU.is_ge, op1=ALU.add, accum_out=cnts[:, n_ - 1:n_]),
                reads=scbufs(j) + [B('mid', n_ - 1)], writes=[B('junkbf'), B('cnt', n_)])
            pg.op('dve', lambda e: e.scalar_tensor_tensor(
                out=dds[:, n_ - 1:n_], in0=cnts[:, n_ - 1:n_], scalar=KTOP - 0.5, in1=Wc[:, n_ - 1:n_],
                op0=ALU.is_ge, op1=ALU.mult),
                reads=[B('cnt', n_), B('Wc')], writes=[B('dd', n_)])
            pg.op('dve', lambda e: e.scalar_tensor_tensor(
                out=mids[:, n_:n_ + 1], in0=dds[:, n_ - 1:n_], scalar=Wc[:, n_:n_ + 1], in1=mids[:, n_ - 1:n_],
                op0=ALU.subtract, op1=ALU.add),
                reads=[B('dd', n_), B('Wc'), B('mid', n_ - 1)], writes=[B('mid', n_)])

        def stageB_fin(j):
            pg.op('dve', lambda e: e.tensor_tensor(out=thr[:, 0:1], in0=mids[:, NIT:NIT + 1], in1=Wc[:, NIT:NIT + 1],
                                                   op=ALU.subtract),
                  reads=[B('mid', NIT), B('Wc')], writes=[B('thr')])
            S = 16 + 128 * (j + 1)
            sc = score2[j % 2]
            mbj = mb2[j % 2]
            pg.op('dve', lambda e: e.tensor_scalar(out=mbj[:, 0:S], in0=sc[:, 0:S], scalar1=thr[:, 0:1],
                                                   scalar2=NEG, op0=ALU.is_lt, op1=ALU.mult),
                  reads=scbufs(j) + [B('thr')], writes=[B('mb', j % 2)])

        def stageC(j):
            mbj = mb2[j % 2]
            klist = [16] + list(range(j + 1))
            for g0 in range(0, len(klist), 8):
                grp = klist[g0:g0 + 8]
                bk = nxt('rot4', ROT4)

                def tp(e, grp=grp, bk=bk):
                    last = None
                    for si, i in enumerate(grp):
                        c_ = 0 if i == 16 else 16 + 128 * i
                        last = e.transpose(out=banks_bf[bk][:, si * 128:(si + 1) * 128], in_=mbj[:, c_:c_ + 128],
                                           identity=ident_bf)
                    return last
                pg.op('pe', tp, reads=[B('mb', j % 2), B('ident_bf')], writes=[PB[bk]])
                si0 = 0
                if grp[0] == 16:
                    pg.op('dve', lambda e, bk=bk: e.tensor_copy(out=mbT[0:16, 16, :], in_=banks_bf[bk][0:16, 0:128]),
                          reads=[PB[bk]], writes=[B('mbT', 16)])
                    si0 = 1
                nn_ = len(grp) - si0
                if nn_ > 0:
                    i0 = grp[si0]
                    srcv = banks_bf[bk][:, si0 * 128:(si0 + nn_) * 128].rearrange("p (a b) -> p a b", a=nn_)
                    pg.op('dve', lambda e, i0=i0, nn_=nn_, srcv=srcv: e.tensor_copy(out=mbT[:, i0:i0 + nn_, :], in_=srcv),
                          reads=[PB[bk]], writes=[B('mbT', i) for i in range(i0, i0 + nn_)])

        def stageD_lg(j, q, ci):
            par = j % 3
            qa = qabs2[par]
            xch = [16] + list(range(j + 1))
            i = xch[ci]
            bl = nxt('rot4', ROT4)
            kc0 = 0 if i == 16 else 16 + 128 * i
            mrow = mk_ap(mbT, i * 128, [[pstep(mbT), 128], [0, 4], [1, 128]])
            qrhs = qa[:, 4 * q:4 * q + 4, :]

            def lg(e):
                e.matmul(banks[bl][:, 0:512], lhsT=cnT[:, kc0:kc0 + 128], rhs=qrhs, start=True, stop=False)
                return e.matmul(banks[bl][:, 0:512], lhsT=ident_bf, rhs=mrow, start=False, stop=True)
            pg.op('pe', lg, reads=cnb + [B('qabsT_t', par, q), B('ident_bf'), B('mbT', i)], writes=[PB[bl]])
            pi = nxt('pT', [0, 1, 2])
            pg.op('act', lambda e: e.activation(out=pT[pi][:, 0:512], in_=banks[bl][:, 0:512], func=AF.Exp,
                                                scale=ATTN_SCALE, bias=negsh[:, par:par + 1]),
                  reads=[PB[bl], B('negsh', par)], writes=[B('pT', pi)])
            return pi

        def stageD_pv(j, q, ci, pi):
            xch = [16] + list(range(j + 1))
            i = xch[ci]
            bO, bS = ACC
            first, last_ = (ci == 0), (ci == len(xch) - 1)

            def pv(e):
                e.matmul(banks[bO][:, 0:512], lhsT=ckeys[:, i, 0:128], rhs=pT[pi][:, 0:512], start=first, stop=last_)
                return e.matmul(banks[bS][:, 0:512], lhsT=ones_bf, rhs=pT[pi][:, 0:512], start=first, stop=last_)
            pg.op('pe', pv, reads=[B('pT', pi), B('ones_bf'), B('ckeys', i)], writes=[PB[bO], PB[bS]])

        def stageD_qfin(j, q):
            bO, bS = ACC
            pg.op('act', lambda e: e.activation(out=lnS, in_=banks[bS][:, 0:512], func=AF.Ln),
                  reads=[PB[bS]], writes=[B('lnS')])
            pg.op('act', lambda e: e.activation(out=rcpS, in_=lnS, func=AF.Exp, scale=-1.0),
                  reads=[B('lnS')], writes=[B('rcpS')])
            pg.op('dve', lambda e: e.tensor_tensor(
                out=olatT_t[:, 4 * q:4 * q + 4, :], in0=banks[bO][:, 0:512].rearrange("p (a b) -> p a b", a=4),
                in1=rcpS.rearrange("p (a b) -> p a b", a=4), op=ALU.mult),
                reads=[PB[bO], B('rcpS')], writes=[B('olatT_t', q)])

        def stageD_fin(j):
            bk = nxt('rot4', ROT4)

            def mmo(e, bk=bk):
                last = None
                for m in range(4):
                    o = banks[bk][:, m * 128:(m + 1) * 128]
                    e.matmul(o, lhsT=uv_pad[:, 2 * m, :], rhs=olatT_t[:, 2 * m, :], start=True, stop=False)
                    last = e.matmul(o, lhsT=uv_pad[:, 2 * m + 1, :], rhs=olatT_t[:, 2 * m + 1, :], start=False, stop=True)
                return last
            pg.op('pe', mmo, reads=uvb + [B('olatT_t', 0), B('olatT_t', 1)], writes=[PB[bk]])
            act_copy(attnT[:, :, 128 * j:128 * (j + 1)], banks[bk][:, 0:512].rearrange("p (a b) -> p a b", a=4),
                     [PB[bk]], [B('attnT', j)])

        nA = n_attn_tiles

        def fullB(j):
            stageB_init(j)
            if 16 + 128 * (j + 1) <= KTOP:
                pg.op('dve', lambda e: e.tensor_reduce(out=cnts[:, 0:1], in_=Wc[:, 1:NIT + 1], axis=AX.X, op=ALU.add),
                      reads=[B('Wc')], writes=[B('cnt', 1)])
                pg.op('dve', lambda e: e.tensor_scalar(out=mids[:, NIT:NIT + 1], in0=cnts[:, 0:1], scalar1=-1.0,
                                                       scalar2=None, op0=ALU.mult),
                      reads=[B('cnt', 1)], writes=[B('mid', NIT)])
            else:
                for n_ in range(1, NIT + 1):
                    stageB_iter(j, n_)
            stageB_fin(j)

        if nA > 2:
            stageA(0)
            fullB(0)
            stageA(1)
            stageB_init(1)
            stageA2(0)
            for n_ in (1, 2, 3):
                stageB_iter(1, n_)
            stageA2(1)
            for n_ in (4, 5, 6):
                stageB_iter(1, n_)
            stageC(0)
            for n_ in (7, 8):
                stageB_iter(1, n_)
            assert 4 * len(chunks_of(2)) == 8
            pro_n = [8]

            def pro_tick():
                pro_n[0] += 1
                if pro_n[0] <= NIT:
                    stageB_iter(1, pro_n[0])
            stageA(2, pro_tick)
            for n_ in range(pro_n[0] + 1, NIT + 1):
                stageB_iter(1, n_)
            stageA2(2)
            stageB_fin(1)
        elif nA > 0:
            stageA(0)
            fullB(0)
            if nA > 1:
                stageA(1)
            stageA2(0)
            if nA > 1:
                stageA2(1)
            stageC(0)
            if nA > 1:
                fullB(1)
        carry_sched = None
        for i in range(nA):
            hasB = i + 2 < nA
            hasA = i + 3 < nA
            nch = i + 2
            seq = [(q, ci) for q in range(2) for ci in range(nch)]
            steps = len(seq)
            n_items = 4 * len(chunks_of(i + 3)) if hasA else 0
            wD, wA = 1.0 * steps, 1.7 * n_items
            ticks = [('D', k) for k in range(steps)] + [('A', k) for k in range(n_items)]
            wts = [1.0] * steps + [1.7] * n_items
            split_last = hasB and not hasA and i + 1 < nA
            if split_last:
                steps2 = 2 * (i + 3)
                ticks = ticks + [('E', k) for k in range(steps2)]
                wts = wts + [1.0] * steps2
            tot = sum(wts)
            sched = {}
            acc, done = 0.0, 0
            for tk, w in zip(ticks, wts):
                acc += w
                upto = int(round(NIT * acc / tot))
                sched[tk] = list(range(done + 1, upto + 1))
                done = upto
            if hasB:
                stageB_init(i + 2)
            pis = {}
            pis[0] = stageD_lg(i, *seq[0])
            for k, (q, ci) in enumerate(seq):
                if k + 1 < steps:
                    pis[k + 1] = stageD_lg(i, *seq[k + 1])
                stageD_pv(i, q, ci, pis[k])
                if hasB:
                    for n_ in sched[('D', k)]:
                        stageB_iter(i + 2, n_)
                if carry_sched is not None:
                    for n_ in carry_sched[('E', k)]:
                        stageB_iter(i + 1, n_)
                if ci == nch - 1:
                    stageD_qfin(i, q)
            stageD_fin(i)
            if hasA:
                cnt = [0]

                def tick(cnt=cnt):
                    if hasB:
                        for n_ in sched[('A', cnt[0])]:
                            stageB_iter(i + 2, n_)
                    cnt[0] += 1
                stageA(i + 3, tick)
            if hasB and not split_last:
                stageB_fin(i + 2)
            if carry_sched is not None:
                stageB_fin(i + 1)
            carry_sched = sched if split_last else None
            if i + 1 < nA:
                stageC(i + 1)
            if hasA:
                stageA2(i + 3)

        pg.barrier()
        reset('R2')
        reset('R3')
        mergedT = carve('R3', BF16, [P, 8, TX])
        wgA2 = [carve('R2', BF16, [P, KC, 256]) for _ in range(2)]
        wgP2 = [carve('R2', BF16, [P, KC, 256]) for _ in range(2)]
        w_out = carve('R2', BF16, [P, 8, D])
        off_wout_end = cursor['R2']
        sA = [carve('R2', F32, [P, 512]) for _ in range(2)]
        sP = [carve('R2', F32, [P, 512]) for _ in range(2)]
        run_tail = stop_after >= 5

        def load_wg(grp):
            s_ = grp % 2
            pg.dma('pool', wgA2[s_], win_p[:, :, 1448 + 256 * grp:1448 + 256 * (grp + 1)], writes=[B('wgA', s_)])
            pg.dma('pool', wgP2[s_], win_p[:, :, 2472 + 256 * grp:2472 + 256 * (grp + 1)], writes=[B('wgP', s_)])

        if run_tail:
            load_wg(0)
            load_wg(1)
            pg.dma('pool', w_out, wout_d.rearrange("(m p) n -> p m n", p=128), writes=[B('w_out')])
            for grp in range(4):
                gs = grp % 2
                wgA, wgP = wgA2[gs], wgP2[gs]
                if 1 <= grp < 3:
                    load_wg(grp + 1)
                for n4 in range(2):
                    nn = 2 * grp + n4
                    for b in range(4):
                        bset = nxt('tailA', [[0, 1, 2, 3], [4, 5, 6, 7]])
                        c0 = 16 + 512 * b
                        bA, bP, bgA, bgP = bset

                        def mmA(e, bA=bA, nn=nn, b=b):
                            last = None
                            for m in range(4):
                                last = e.matmul(banks[bA][:, 0:512], lhsT=w_ba[:, m, nn * 128:(nn + 1) * 128],
                                                rhs=attnT[:, m, 512 * b:512 * (b + 1)], start=(m == 0), stop=(m == 3))
                            return last
                        pg.op('pe', mmA, reads=[B('w_ba')] + [B('attnT', 4 * b + q) for q in range(4)], writes=[PB[bA]])

                        def mmP(e, bP=bP, nn=nn, b=b):
                            last = None
                            for m in range(4):
                                last = e.matmul(banks[bP][:, 0:512], lhsT=w_bp[:, m, nn * 128:(nn + 1) * 128],
                                                rhs=poolT[:, m, 512 * b:512 * (b + 1)], start=(m == 0), stop=(m == 3))
                            return last
                        pg.op('pe', mmP, reads=[B('w_bp'), B('poolT', b)], writes=[PB[bP]])

                        def mmg(e, bk, wt, n4=n4, c0=c0):
                            last = None
                            for kc in range(KC):
                                last = e.matmul(banks[bk][:, 0:512], lhsT=wt[:, kc, n4 * 128:(n4 + 1) * 128],
                                                rhs=uT[:, kc, c0:c0 + 512], start=(kc == 0), stop=(kc == KC - 1))
                            return last
                        pg.op('pe', lambda e, bgA=bgA, f=mmg, wgA=wgA: f(e, bgA, wgA),
                              reads=uT_bufs(c0, c0 + 512) + [B('wgA', gs)], writes=[PB[bgA]])
                        pg.op('pe', lambda e, bgP=bgP, f=mmg, wgP=wgP: f(e, bgP, wgP),
                              reads=uT_bufs(c0, c0 + 512) + [B('wgP', gs)], writes=[PB[bgP]])
                        si = nxt('sAP', [0, 1])
                        pg.op('act', lambda e, bgA=bgA, si=si: e.activation(out=sA[si], in_=banks[bgA][:, 0:512], func=AF.Sigmoid),
                              reads=[PB[bgA]], writes=[B('sA', si)])
                        pg.op('act', lambda e, bgP=bgP, si=si: e.activation(out=sP[si], in_=banks[bgP][:, 0:512], func=AF.Sigmoid),
                              reads=[PB[bgP]], writes=[B('sP', si)])
                        pg.op('dve', lambda e, bA=bA, si=si: e.tensor_tensor(out=sA[si], in0=sA[si], in1=banks[bA][:, 0:512], op=ALU.mult),
                              reads=[B('sA', si), PB[bA]], writes=[B('sA', si)])
                        pg.op('dve', lambda e, bP=bP, si=si: e.tensor_tensor(out=sP[si], in0=sP[si], in1=banks[bP][:, 0:512], op=ALU.mult),
                              reads=[B('sP', si), PB[bP]], writes=[B('sP', si)])
                        pg.op('dve', lambda e, si=si, nn=nn, b=b: e.tensor_tensor(
                            out=mergedT[:, nn, 512 * b:512 * (b + 1)], in0=sA[si], in1=sP[si], op=ALU.add),
                            reads=[B('sA', si), B('sP', si)], writes=[B('mergedT', nn, b)])

        pg.barrier()
        reset('R1')
        reset('R2')
        hacc = carve('R1', F32, [P, NT, D])
        w_r = carve('R2', BF16, [P, KC, 36])
        rbias = carve('R2', F32, [P, 36])
        gate_all = carve('R2', F32, [P, NT, 32])
        rt_ = carve('R2', F32, [P, 256])
        cur_save = cursor['R2']
        lg_all = carve('R2', F32, [P, NT, 36])
        ssq = carve('R2', F32, [P, NT])
        rstd2 = carve('R2', F32, [P, NT])
        rv = carve('R2', F32, [P, 1600])
        assert cursor['R2'] <= off_wout_end - 16384, cursor['R2']
        if run_tail:
            pg.dma('pool', w_r[:, :, 0:4], wgr_d.rearrange("(p k) n -> p k n", k=KC), writes=[B('w_r', 0)])
            pg.dma('pool', w_r[:, :, 4:36], wer_d.rearrange("(p k) n -> p k n", k=KC), writes=[B('w_r', 1)])
            pg.dma('sp', rbias[:, 0:4], bgr_d.partition_broadcast(128), writes=[B('rbias', 0)])
            pg.dma('sp', rbias[:, 4:36], ber_d.partition_broadcast(128), writes=[B('rbias', 1)])
            pg.dma('sp', gbc[1], g2_d.partition_broadcast(128), writes=[B('gbc', 1)])
            for j in range(NT):
                b = j // 4
                k = nxt('xt', [0, 1])
                pg.dma('sp', xt[k], x_d[128 * j:128 * (j + 1), :], writes=[B('xt', k)])
                for hh in range(2):
                    bk = nxt('tailB', [0, 1, 2, 3])

                    def mmh(e, bk=bk, j=j, hh=hh):
                        last = None
                        for nn in range(8):
                            last = e.matmul(banks[bk][:, 0:512], lhsT=mergedT[:, nn, 128 * j:128 * (j + 1)],
                                            rhs=w_out[:, nn, 512 * hh:512 * (hh + 1)], start=(nn == 0), stop=(nn == 7))
                        return last
                    pg.op('pe', mmh, reads=[B('mergedT', nn, b) for nn in range(8)] + [B('w_out')] + [B('R3a', q) for q in range(6)],
                          writes=[PB[bk]])
                    pg.op('dve', lambda e, bk=bk, j=j, hh=hh, k=k: e.tensor_tensor(
                        out=hacc[:, j, 512 * hh:512 * (hh + 1)], in0=banks[bk][:, 0:512], in1=xt[k][:, 512 * hh:512 * (hh + 1)],
                        op=ALU.add), reads=[PB[bk], B('xt', k)], writes=[B('hacc', j, hh)])
                pg.op('act', lambda e, j=j: e.activation(out=junkbf[:, 0:D], in_=hacc[:, j, :], func=AF.Square,
                                                         accum_out=ssq[:, j:j + 1]),
                      reads=[B('hacc', j, 0), B('hacc', j, 1)], writes=[B('junkbf'), B('ssq', j)])
            pg.op('act', lambda e: e.activation(out=rstd2, in_=ssq, func=AF.Sqrt, scale=1.0 / D, bias=eps_t),
                  reads=[B('ssq', j) for j in range(NT)] + [B('eps')], writes=[B('rstd2s')])
            pg.op('dve', lambda e: e.reciprocal(out=rstd2, in_=rstd2), reads=[B('rstd2s')], writes=[B('rstd2')])
            def s2_front(j):
                k = nxt('xn', [0, 1])
                pg.op('dve', lambda e: e.scalar_tensor_tensor(
                    out=xn[k], in0=hacc[:, j, :], scalar=rstd2[:, j:j + 1], in1=gbc[1], op0=ALU.mult, op1=ALU.mult),
                    reads=[B('hacc', j, 0), B('hacc', j, 1), B('rstd2'), B('gbc', 1)], writes=[B('xn', k)])
                bk = nxt('tp', [4, 5])

                def tps(e):
                    last = None
                    for kc in range(KC):
                        last = e.transpose(out=banks_bf[bk][:, kc * 128:(kc + 1) * 128],
                                           in_=xn[k].rearrange("t (p k) -> t k p", k=KC)[:, kc, :], identity=ident_bf)
                    return last
                pg.op('pe', tps, reads=[B('xn', k), B('ident_bf')], writes=[PB[bk]])
                srcv = banks_bf[bk][:, 0:1024].rearrange("p (a b) -> p a b", a=KC)
                pg.op('act', lambda e: e.copy(out=uT[:, :, 16 + 128 * j:16 + 128 * (j + 1)], in_=srcv),
                      reads=[PB[bk]], writes=[B('uT', j)])

            s2_bk = {}

            def s2_back(j):
                bk2 = nxt('rt', [6, 7])
                s2_bk[j] = bk2

                def mmr(e):
                    last = None
                    for kc in range(KC):
                        last = e.matmul(banks[bk2][:, 0:36], lhsT=uT[:, kc, 16 + 128 * j:16 + 128 * (j + 1)],
                                        rhs=w_r[:, kc, :], start=(kc == 0), stop=(kc == KC - 1))
                    return last
                pg.op('pe', mmr, reads=[B('uT', j), B('w_r', 0), B('w_r', 1)], writes=[PB[bk2]])

            def s2_add(j):
                bk2 = s2_bk[j]
                pg.op('dve', lambda e: e.tensor_tensor(out=lg_all[:, j, :], in0=banks[bk2][:, 0:36], in1=rbias,
                                                       op=ALU.add),
                      reads=[PB[bk2], B('rbias', 0), B('rbias', 1)], writes=[B('lg_all', j)])

            for t_ in range(NT + 3):
                if t_ >= 3:
                    s2_add(t_ - 3)
                if 2 <= t_ < NT + 2:
                    s2_back(t_ - 2)
                if t_ < NT:
                    s2_front(t_)
            RV = B('rv')
            off = [0]

            def rvv(n):
                a_ = rv[:, off[0]:off[0] + n]
                off[0] += n
                return a_
            gmax, gsum, pgp, m1, m2, dm, ed, den, p1, p2 = [rvv(NT) for _ in range(10)]
            goh, gex = rvv(NT * 4), rvv(NT * 4)
            esel, oh1, esel2, oh2, t1, t2 = [rvv(NT * 8) for _ in range(6)]
            tmp32 = rvv(NT * 32)
            pl = pstep(lg_all)
            pr = pstep(rv)

            def v3(ap, n):
                return ap.rearrange("p (a b) -> p a b", a=NT)

            def bc_last(ap, n):
                return mk_ap(ap, 0, [[pr, 128], [1, NT], [0, n]])
            gl = lg_all[:, :, 0:4]
            el4 = mk_ap(lg_all, 4, [[pl, 128], [36, NT], [8, 4], [1, 8]])
            lgb = [B('lg_all', j) for j in range(NT)]

            def dv(fn, extra=()):
                pg.op('dve', fn, reads=[RV] + list(extra), writes=[RV])
            dv(lambda e: e.tensor_reduce(out=gmax, in_=gl, axis=AX.X, op=ALU.max), lgb)
            dv(lambda e: e.tensor_tensor(out=v3(goh, 4), in0=gl, in1=bc_last(gmax, 4), op=ALU.is_ge), lgb)
            dv(lambda e: e.tensor_tensor(out=v3(gex, 4), in0=gl, in1=bc_last(gmax, 4), op=ALU.subtract), lgb)
            pg.op('act', lambda e: e.activation(out=gex, in_=gex, func=AF.Exp), reads=[RV], writes=[RV])
            dv(lambda e: e.tensor_reduce(out=gsum, in_=v3(gex, 4), axis=AX.X, op=ALU.add))
            dv(lambda e: e.reciprocal(out=pgp, in_=gsum))
            goh4 = mk_ap(goh, 0, [[pr, 128], [4, NT], [1, 4], [0, 8]])
            t4 = mk_ap(tmp32, 0, [[pr, 128], [32, NT], [8, 4], [1, 8]])
            dv(lambda e: e.tensor_tensor(out=t4, in0=el4, in1=goh4, op=ALU.mult), lgb)
            t4T = mk_ap(tmp32, 0, [[pr, 128], [32, NT], [1, 8], [8, 4]])
            dv(lambda e: e.tensor_reduce(out=v3(esel, 8), in_=t4T, axis=AX.X, op=ALU.add))
            dv(lambda e: e.tensor_reduce(out=m1, in_=v3(esel, 8), axis=AX.X, op=ALU.max))
            dv(lambda e: e.tensor_tensor(out=v3(oh1, 8), in0=v3(esel, 8), in1=bc_last(m1, 8), op=ALU.is_ge))
            dv(lambda e: e.scalar_tensor_tensor(out=esel2, in0=oh1, scalar=-BIG, in1=esel, op0=ALU.mult, op1=ALU.add))
            dv(lambda e: e.tensor_reduce(out=m2, in_=v3(esel2, 8), axis=AX.X, op=ALU.max))
            dv(lambda e: e.tensor_tensor(out=v3(oh2, 8), in0=v3(esel2, 8), in1=bc_last(m2, 8), op=ALU.is_ge))
            dv(lambda e: e.tensor_tensor(out=dm, in0=m2, in1=m1, op=ALU.subtract))
            pg.op('act', lambda e: e.activation(out=ed, in_=dm, func=AF.Exp), reads=[RV], writes=[RV])
            dv(lambda e: e.tensor_scalar(out=den, in0=ed, scalar1=1.0, scalar2=None, op0=ALU.add))
            dv(lambda e: e.reciprocal(out=p1, in_=den))
            dv(lambda e: e.tensor_tensor(out=p2, in0=ed, in1=p1, op=ALU.mult))
            dv(lambda e: e.tensor_tensor(out=p1, in0=p1, in1=pgp, op=ALU.mult))
            dv(lambda e: e.tensor_tensor(out=p2, in0=p2, in1=pgp, op=ALU.mult))
            dv(lambda e: e.tensor_tensor(out=v3(t1, 8), in0=v3(oh1, 8), in1=bc_last(p1, 8), op=ALU.mult))
            dv(lambda e: e.tensor_tensor(out=v3(t2, 8), in0=v3(oh2, 8), in1=bc_last(p2, 8), op=ALU.mult))
            dv(lambda e: e.tensor_tensor(out=t1, in0=t1, in1=t2, op=ALU.add))
            wpg4 = mk_ap(t1, 0, [[pr, 128], [8, NT], [0, 4], [1, 8]])
            gout = gate_all.rearrange("p a (g e) -> p a g e", g=4)
            pg.op('dve', lambda e: e.tensor_tensor(out=gout, in0=goh4, in1=wpg4, op=ALU.mult),
                  reads=[RV], writes=[B('gate', j) for j in range(NT)])
        cursor['R2'] = cur_save

        reset('R3')
        n_exp = debug.get('n_exp', N_EXP if stop_after >= 6 else 0)
        wg_e = [carve('R3', BF16, [P, KC, 256]) for _ in range(2)]
        wu_e = [carve('R3', BF16, [P, KC, 256]) for _ in range(2)]
        wd_e = [carve('R3', BF16, [P, 2, D]) for _ in range(2)]
        silt = [carve('R3', F32, [P, 512]) for _ in range(2)]
        hid = [carve('R2', BF16, [P, 2, TX]) for _ in range(2)]

        def load_gu(e_):
            s = e_ % 2
            al = (lambda q: [B('R3a', q)]) if e_ < 2 else (lambda q: [])
            pg.dma('pool', wg_e[s], weg_d[e_].rearrange("(p k) n -> p k n", k=KC), writes=[B('wg_e', s)] + al(2 * s))
            pg.dma('pool', wu_e[s], weu_d[e_].rearrange("(p k) n -> p k n", k=KC), writes=[B('wu_e', s)] + al(2 * s + 1))

        def load_d(e_):
            s = e_ % 2
            pg.dma('pool', wd_e[s], wed_d[e_].rearrange("(f p) n -> p f n", p=128),
                   writes=[B('wd_e', s)] + ([B('R3a', 4 + s)] if e_ < 2 else []))

        def gu_group(e_, b, fh):
            s = e_ % 2
            c0 = 16 + 512 * b
            bG, bU = nxt('moeGU', [[0, 1], [2, 3]])

            def mmGU(e, bk, wt):
                last = None
                for kc in range(KC):
                    last = e.matmul(banks[bk][:, 0:512], lhsT=wt[:, kc, fh * 128:(fh + 1) * 128],
                                    rhs=uT[:, kc, c0:c0 + 512], start=(kc == 0), stop=(kc == KC - 1))
                return last
            pg.op('pe', lambda e: mmGU(e, bG, wg_e[s]), reads=uT_bufs(c0, c0 + 512) + [B('wg_e', s)],
                  writes=[PB[bG]])
            pg.op('pe', lambda e: mmGU(e, bU, wu_e[s]), reads=uT_bufs(c0, c0 + 512) + [B('wu_e', s)],
                  writes=[PB[bU]])
            si = nxt('silt', [0, 1])
            pg.op('act', lambda e: e.activation(out=silt[si], in_=banks[bG][:, 0:512], func=AF.Silu),
                  reads=[PB[bG]], writes=[B('silt', si)])
            pg.op('dve', lambda e: e.tensor_tensor(
                out=hid[s][:, fh, 512 * b:512 * (b + 1)], in0=silt[si], in1=banks[bU][:, 0:512], op=ALU.mult),
                reads=[B('silt', si), PB[bU]], writes=[B('hid', s, b)])

        def y_tile(e_, j):
            s = e_ % 2
            for hh in range(2):
                bk = nxt('moeY', [4, 5, 6, 7])

                def mmy(e, bk=bk, hh=hh):
                    e.matmul(banks[bk][:, 0:512], lhsT=hid[s][:, 0, 128 * j:128 * (j + 1)],
                             rhs=wd_e[s][:, 0, 512 * hh:512 * (hh + 1)], start=True, stop=False)
                    return e.matmul(banks[bk][:, 0:512], lhsT=hid[s][:, 1, 128 * j:128 * (j + 1)],
                                    rhs=wd_e[s][:, 1, 512 * hh:512 * (hh + 1)], start=False, stop=True)
                pg.op('pe', mmy, reads=[B('hid', s, j // 4), B('wd_e', s)], writes=[PB[bk]])
                pg.op('dve', lambda e, bk=bk, hh=hh: e.scalar_tensor_tensor(
                    out=hacc[:, j, 512 * hh:512 * (hh + 1)], in0=banks[bk][:, 0:512], scalar=gate_all[:, j, e_:e_ + 1],
                    in1=hacc[:, j, 512 * hh:512 * (hh + 1)], op0=ALU.mult, op1=ALU.add),
                    reads=[PB[bk], B('gate', j), B('hacc', j, hh)], writes=[B('hacc', j, hh)])

        LAG = 4
        G_ = n_exp * 8
        if n_exp > 0:
            load_gu(0)
            load_d(0)
            if n_exp > 1:
                load_d(1)
        for t_ in range(G_ + LAG if n_exp > 0 else 0):
            if t_ < G_:
                e_, gi = t_ // 8, t_ % 8
                if gi == 0 and e_ + 1 < n_exp:
                    load_gu(e_ + 1)
                gu_group(e_, gi // 2, gi % 2)
            u_ = t_ - LAG
            if u_ >= 0:
                e2, m_ = u_ // 8, u_ % 8
                y_tile(e2, 2 * m_)
                y_tile(e2, 2 * m_ + 1)
                if m_ == 7 and e2 + 2 < n_exp:
                    load_d(e2 + 2)

        if run_tail:
            pg.dma('sp', gbc[0], gf_d.partition_broadcast(128), writes=[B('gbc', 0)])
            for j in range(NT):
                hb = [B('hacc', j, 0), B('hacc', j, 1)]
                pg.op('act', lambda e, j=j: e.activation(out=junkbf[:, 0:D], in_=hacc[:, j, :], func=AF.Square,
                                                         accum_out=ssq[:, j:j + 1]),
                      reads=hb, writes=[B('ssq', j), B('junkbf')])
            pg.op('act', lambda e: e.activation(out=rstd2, in_=ssq, func=AF.Sqrt, scale=1.0 / D, bias=eps_t),
                  reads=[B('ssq', j) for j in range(NT)] + [B('eps')], writes=[B('rstd2s')])
            pg.op('dve', lambda e: e.reciprocal(out=rstd2, in_=rstd2), reads=[B('rstd2s')], writes=[B('rstd2')])
            obuf = [carve_at('R0', 4096 * i, F32, [P, D]) for i in range(8)]
            for j in range(NT):
                hb = [B('hacc', j, 0), B('hacc', j, 1)]
                k = j % 8
                if False:
                    pg.op('act', lambda e, j=j, k=k: e.activation(
                        out=obuf[k], in_=hacc[:, j, :], func=AF.Identity, scale=rstd2[:, j:j + 1]),
                        reads=hb + [B('rstd2')], writes=[B('obuf', k)])
                    pg.op('pool', lambda e, k=k: e.tensor_tensor(out=obuf[k], in0=obuf[k], in1=gbc[0], op=ALU.mult),
                          reads=[B('obuf', k), B('gbc', 0)], writes=[B('obuf', k)])
                else:
                    pg.op('dve', lambda e, j=j, k=k: e.scalar_tensor_tensor(
                        out=obuf[k], in0=hacc[:, j, :], scalar=rstd2[:, j:j + 1], in1=gbc[0], op0=ALU.mult, op1=ALU.mult),
                        reads=hb + [B('rstd2'), B('gbc', 0)], writes=[B('obuf', k)])
                pg.dma('sp', out_d[128 * j:128 * (j + 1), :], obuf[k], reads=[B('obuf', k)], writes=[B('out', j)])

        pg.barrier()
        local = dict(uT=uT, ckeys=ckeys, cnT=cnT, ikT=ikT, poolT=poolT, attnT=attnT, iw_all=iw_all, score=score,
                     mb=mb, hacc=hacc, gate_all=gate_all, mergedT=mergedT, qabsT_t=qabsT_t,
                     bis=bis, thr=thr, iqT_t=iqT_t, diag=diag, mbT=mbT)
        for name in debug.get('dump', []):
            ap = local[name]
            shp = list(ap.shape)
            n = 1
            for s_ in shp[1:]:
                n *= s_
            dd = nc.dram_tensor("dbg_" + name, [shp[0], n], F32, kind="ExternalOutput").ap()
            flat = ap
            if len(shp) == 3:
                flat = ap.rearrange("p a b -> p (a b)")
            for c0 in range(0, n, 2048):
                c1 = min(n, c0 + 2048)
                pg.dma('pool', dd[:, c0:c1], flat[:, c0:c1], reads=[], writes=[B('dbg', name, c0)])
        pg.barrier()
        pg.emit(esem, dsem)
    return nc


_CACHE = {}


def _consts():
    ident = np.eye(128, dtype=np.float32)
    p = np.arange(128)[:, None]
    s = np.arange(128)[None, :]
    blockbias = np.where((s < 64) | (p >= 64), 0.0, -BIG).astype(np.float32)
    sel8 = np.zeros((32, 8, 128), np.float32)
    for h in range(8):
        sel8[h, h, :] = 1.0
    invcnt = np.broadcast_to((1.0 / np.arange(1, 17, dtype=np.float32))[None, :], (128, 16)).copy()
    pow2 = np.broadcast_to((1.0001 * 2.0 ** (-np.arange(NIT + 2, dtype=np.float64))).astype(np.float32)[None, :],
                           (128, NIT + 2)).copy()
    return dict(c_ident=ident, c_blockbias=blockbias, c_sel8=sel8.reshape(32, 1024), c_invcnt=invcnt, c_pow2=pow2)


def make_in_maps(inputs):
    f = lambda a: np.ascontiguousarray(np.asarray(a, dtype=np.float32))
    shared = dict(
        meta=f(inputs['meta_tokens']),
        norm1_g=f(inputs['norm1_g']).reshape(1, D),
        w_in=f(inputs['w_in']).reshape(D, 3496),
        kv_norm_g=f(inputs['kv_norm_g']).reshape(1, 128),
        w_uk=f(inputs['w_uk']).reshape(128, 512),
        w_uv=f(inputs['w_uv']).reshape(128, 512),
        w_pool=f(inputs['w_pool']).reshape(4, 128, 128),
        pool_scale=f(inputs['pool_scale']).reshape(1, 512),
        w_ba=f(inputs['w_branch_attn']).reshape(512, D),
        w_bp=f(inputs['w_branch_pool']).reshape(512, D),
        w_out=f(inputs['w_out']).reshape(D, D),
        norm2_g=f(inputs['norm2_g']).reshape(1, D),
        w_gr=f(inputs['w_group_router']).reshape(D, 4),
        b_gr=f(inputs['b_group_router']).reshape(1, 4),
        w_er=f(inputs['w_expert_router']).reshape(D, 32),
        b_er=f(inputs['b_expert_router']).reshape(1, 32),
        w_eg=f(inputs['w_expert_gate']).reshape(N_EXP, D, 256),
        w_eu=f(inputs['w_expert_up']).reshape(N_EXP, D, 256),
        w_ed=f(inputs['w_expert_down']).reshape(N_EXP, 256, D),
        final_g=f(inputs['final_norm_g']).reshape(1, D),
    )
    shared.update(_consts())
    x = f(inputs['x'])
    return [dict(shared, x=x[b]) for b in range(8)]


def kernel(**inputs):
    if 'nc' not in _CACHE:
        _CACHE['nc'] = build()
    nc = _CACHE['nc']
    in_maps = make_in_maps(inputs)
    res = run_bass_kernel_spmd(nc, in_maps, core_ids=list(range(8)))
    return np.stack([np.asarray(r["out"], dtype=np.float32).reshape(TX, D) for r in res.results], axis=0)
```

```python
import math
from contextlib import ExitStack

import numpy as np
import concourse.bass as bass
import concourse.mybir as mybir
from concourse.bass_utils import run_bass_kernel_spmd

F32 = mybir.dt.float32
BF16 = mybir.dt.bfloat16
ALU = mybir.AluOpType
AF = mybir.ActivationFunctionType
AX = mybir.AxisListType

P = 128
D = 1024
KC = 8
NT = 16
TX = 2048
T = 2064
EPS = 1e-6
ATTN_SCALE = 64 ** -0.5
IDX_SCALE = (8 ** -0.5) * (32 ** -0.5)
KTOP = 256
NIT = 16
NEG = -30000.0
BIG = 1.0e30
N_EXP = 32
ENGS = ['pe', 'act', 'dve', 'pool', 'sp']
NDS = 24


class Buf:
    __slots__ = ('name', 'lw', 'rd')

    def __init__(self, name):
        self.name = name
        self.lw = None
        self.rd = {}


class Op:
    __slots__ = ('fn', 'waits', 'dma')

    def __init__(self, fn, waits, dma):
        self.fn = fn
        self.waits = waits
        self.dma = dma


class Prog:
    def __init__(self, nc):
        self.nc = nc
        self.ops = {e: [] for e in ENGS}
        self.seen = {e: {} for e in ENGS}
        self.seen_dma = {e: set() for e in ENGS}
        self.dma_info = []
        self.dma_uses = [0] * NDS
        self.dma_hist = {}
        self.bufs = {}

    def B(self, *key):
        b = self.bufs.get(key)
        if b is None:
            b = Buf(key)
            self.bufs[key] = b
        return b

    def _deps(self, reads, writes):
        toks = set()
        for b in reads:
            if b.lw is not None:
                toks.add(b.lw)
        for b in writes:
            if b.lw is not None:
                toks.add(b.lw)
            toks.update(b.rd.values())
        return toks

    def _resolve(self, eng, toks):
        best = {}
        out = []
        for t in toks:
            if t[0] == 'c':
                if t[1] == eng and eng == 'pe':
                    continue
                if best.get(t[1], -1) < t[2]:
                    best[t[1]] = t[2]
            else:
                if t[1] not in self.seen_dma[eng]:
                    self.seen_dma[eng].add(t[1])
                    out.append(t)
        for pe_, i in best.items():
            if self.seen[eng].get(pe_, -1) >= i:
                continue
            self.seen[eng][pe_] = i
            out.append(('c', pe_, i))
        return out

    def _commit(self, tok, key, reads, writes):
        for b in reads:
            b.rd[key] = tok
        for b in writes:
            b.lw = tok
            b.rd = {}

    def op(self, eng, fn, reads=(), writes=()):
        ps = [b for b in reads if b.name[0] == 'psum']
        if ps:
            writes = list(writes) + [b for b in ps if b not in writes]
            reads = [b for b in reads if b.name[0] != 'psum']
        idx = len(self.ops[eng])
        tok = ('c', eng, idx)
        waits = self._resolve(eng, self._deps(reads, writes))
        self.ops[eng].append(Op(fn, waits, None))
        self._commit(tok, eng, reads, writes)
        return tok

    def dma(self, q, out, in_, reads=(), writes=(), **kw):
        did = len(self.dma_info)
        half = NDS // 2
        base = half if q == 'pool' else 0
        hist = self.dma_hist.setdefault(base, [])
        s = base + len(hist) % half
        self.dma_uses[s] += 1
        self.dma_info.append((s, 16 * self.dma_uses[s]))
        toks = self._deps(reads, writes)
        if len(hist) >= half:
            toks.add(('d', hist[len(hist) - half]))
        hist.append(did)
        waits = self._resolve(q, toks)
        self.ops[q].append(Op(lambda e, o=out, i=in_, k=kw: e.dma_start(out=o, in_=i, **k), waits, did))
        tok = ('d', did)
        self._commit(tok, tok, reads, writes)
        return tok

    def barrier(self):
        toks = set()
        for e in ENGS:
            if e != 'sp' and self.ops[e]:
                for i in range(len(self.ops[e]) - 1, -1, -1):
                    if self.ops[e][i].dma is None and self.ops[e][i].fn is not None:
                        toks.add(('c', e, i))
                        break
        for hist in self.dma_hist.values():
            for d in hist[-(NDS // 2):]:
                toks.add(('d', d))
        for e in ENGS:
            mine = set(t for t in toks if not (t[0] == 'c' and t[1] == e and e == 'pe'))
            waits = self._resolve(e, mine)
            if waits:
                self.ops[e].append(Op(None, waits, None))

    def emit(self, esem, dsem):
        sig = {e: set() for e in ENGS}
        for e in ENGS:
            for o in self.ops[e]:
                for t in o.waits:
                    if t[0] == 'c':
                        sig[t[1]].add(t[2])
        signo = {e: {} for e in ENGS}
        for e in ENGS:
            for n, i in enumerate(sorted(sig[e])):
                signo[e][i] = n + 1
        prog = self

        def stream(ename, h):
            for i, o in enumerate(prog.ops[ename]):
                for t in o.waits:
                    if t[0] == 'c':
                        h.wait_ge(esem[t[1]], signo[t[1]][t[2]])
                    else:
                        s, val = prog.dma_info[t[1]]
                        h.wait_ge(dsem[s], val)
                if o.fn is None:
                    continue
                inst = o.fn(h)
                if o.dma is not None:
                    inst.then_inc(dsem[prog.dma_info[o.dma][0]], 16)
                elif i in sig[ename]:
                    inst.then_inc(esem[ename], 1)

        with self.nc.Block() as block:
            @block.tensor
            def _(h):
                stream('pe', h)

            @block.scalar
            def _(h):
                stream('act', h)

            @block.vector
            def _(h):
                stream('dve', h)

            @block.gpsimd
            def _(h):
                stream('pool', h)

            @block.sync
            def _(h):
                stream('sp', h)


def mk_ap(base, extra_off, dims):
    return bass.AP(base.tensor, base.offset + extra_off, dims)


def pstep(ap):
    return ap.ap[0][0]


def build(debug=None):
    debug = debug or {}
    stop_after = debug.get('stop_after', 99)
    nc = bass.Bass("TRN2", target_bir_lowering=False)

    def din(name, shape):
        return nc.dram_tensor(name, list(shape), F32, kind="ExternalInput").ap()

    x_d = din("x", [TX, D])
    meta_d = din("meta", [16, D])
    g1_d = din("norm1_g", [1, D])
    win_d = din("w_in", [D, 3496])
    gkv_d = din("kv_norm_g", [1, 128])
    wuk_d = din("w_uk", [128, 512])
    wuv_d = din("w_uv", [128, 512])
    wpool_d = din("w_pool", [4, 128, 128])
    pscale_d = din("pool_scale", [1, 512])
    wba_d = din("w_ba", [512, D])
    wbp_d = din("w_bp", [512, D])
    wout_d = din("w_out", [D, D])
    g2_d = din("norm2_g", [1, D])
    wgr_d = din("w_gr", [D, 4])
    bgr_d = din("b_gr", [1, 4])
    wer_d = din("w_er", [D, 32])
    ber_d = din("b_er", [1, 32])
    weg_d = din("w_eg", [N_EXP, D, 256])
    weu_d = din("w_eu", [N_EXP, D, 256])
    wed_d = din("w_ed", [N_EXP, 256, D])
    gf_d = din("final_g", [1, D])
    cid_d = din("c_ident", [128, 128])
    cbb_d = din("c_blockbias", [128, 128])
    csel_d = din("c_sel8", [32, 8 * 128])
    cinv_d = din("c_invcnt", [128, 16])
    cpow_d = din("c_pow2", [128, NIT + 2])
    out_d = nc.dram_tensor("out", [TX, D], F32, kind="ExternalOutput").ap()
    dbg_out = {}

    pg = Prog(nc)
    B = pg.B

    with ExitStack() as es:
        esem = {e: es.enter_context(nc.semaphore("sem_" + e)) for e in ENGS}
        dsem = [es.enter_context(nc.semaphore("dsem%d" % i)) for i in range(NDS)]

        R0_B, R1_B, R2_B, R3_B = 62 * 1024, 64 * 1024, 42 * 1024, 34 * 1024
        arenas = {}
        for nm, nb in (('R0', R0_B), ('R1', R1_B), ('R2', R2_B), ('R3', R3_B)):
            arenas[nm] = (es.enter_context(nc.sbuf_tensor(nm, [P, nb // 4], F32)), nb)
        cursor = {}

        def reset(nm):
            cursor[nm] = 0

        def carve(nm, dtype, shape):
            h, nb = arenas[nm]
            esz = 4 if dtype == F32 else 2
            n = 1
            for s in shape[1:]:
                n *= s
            off = (cursor[nm] + 63) // 64 * 64
            assert off + n * esz <= nb, (nm, off, n * esz, nb, shape)
            cursor[nm] = off + n * esz
            hv = h if dtype == F32 else h.bitcast(dtype)
            ap = hv[0:shape[0], off // esz: off // esz + n]
            if len(shape) == 3:
                ap = ap.rearrange("p (a b) -> p a b", a=shape[1])
            elif len(shape) == 4:
                ap = ap.rearrange("p (a b c) -> p a b c", a=shape[1], b=shape[2])
            return ap

        def carve_at(nm, off, dtype, shape):
            save = cursor[nm]
            cursor[nm] = off
            ap = carve(nm, dtype, shape)
            assert (off + 63) // 64 * 64 == off
            cursor[nm] = save
            return ap

        for nm in arenas:
            reset(nm)

        banks = [es.enter_context(nc.psum_tensor("ps%d" % i, [P, 512], F32)) for i in range(8)]
        banks_bf = [b.bitcast(BF16) for b in banks]
        PB = [B('psum', i) for i in range(8)]

        uT = carve('R0', BF16, [P, KC, T])
        off_idle = (cursor['R0'] + 63) // 64 * 64
        gbc = [carve('R0', F32, [P, D]) for _ in range(2)]
        xt = [carve('R0', F32, [P, D]) for _ in range(2)]
        xn = [carve('R0', BF16, [P, D]) for _ in range(2)]
        junkbf = carve('R0', BF16, [P, T])
        ident_f = carve('R0', F32, [P, 128])
        ident_bf = carve('R0', BF16, [P, 128])
        stats = carve('R0', F32, [P, 512])
        stat_i = [0]

        def stat(n=1):
            i = stat_i[0]
            if i + n > 512:
                i = 0
            stat_i[0] = i + n
            return stats[:, i:i + n], B('stat', i, n)

        def stat_bufs(i, n):
            return [B('statc', c) for c in range(i, i + n)]

        def stat2(n=1):
            i = stat_i[0]
            if i + n > 512:
                i = 0
            stat_i[0] = i + n
            return stats[:, i:i + n], stat_bufs(i, n)

        ckeys = carve('R1', BF16, [P, 17, 129])
        cnT = carve('R1', BF16, [P, T])
        ikT = carve('R1', BF16, [P, T])
        poolT = carve('R1', BF16, [P, 4, TX])
        attnT = carve('R1', BF16, [P, 4, TX])
        iw_all = carve('R1', F32, [P, NT, 8])
        gkv_bc = carve('R1', F32, [P, 128])
        blockbias = carve('R1', F32, [P, 128])
        sel8 = carve('R1', BF16, [P, 8, 128])
        invcnt = carve('R1', F32, [P, 16])
        pow2 = carve('R1', F32, [P, NIT + 2])
        pscale = carve('R1', F32, [P, 4])
        cmaxsel = carve('R1', BF16, [P, 8, 32])
        cmax = carve('R1', F32, [P, 1])
        ones_bf = carve('R1', BF16, [P, 128])
        iqT_all = carve('R1', BF16, [P, 3, TX])
        cmaxB = carve('R1', BF16, [P, 128])

        pg.dma('sp', ident_f, cid_d, writes=[B('ident_f')])
        pg.op('dve', lambda e: e.tensor_copy(out=ident_bf, in_=ident_f), reads=[B('ident_f')], writes=[B('ident_bf')])
        pg.dma('pool', sel8[0:32].rearrange("p a b -> p (a b)"), csel_d, writes=[B('sel8')])
        pg.dma('sp', gbc[0], g1_d.partition_broadcast(128), writes=[B('gbc', 0)])
        pg.op('pool', lambda e: e.memset(ckeys[:, 16, 0:128], 0.0), writes=[B('ckeys', 16)])
        pg.op('pool', lambda e: e.memset(ckeys[:, :, 128:129], 1.0), writes=[B('ckeys_ones')])

        def late_consts():
            pg.dma('sp', blockbias, cbb_d, writes=[B('blockbias')])
            pg.dma('sp', invcnt, cinv_d, writes=[B('invcnt')])
            pg.dma('sp', pow2, cpow_d, writes=[B('pow2')])
            pg.dma('sp', pscale, pscale_d.rearrange("o (g p) -> p (o g)", p=128), writes=[B('pscale')],
                   allow_slow_non_contiguous=True)
            pg.dma('sp', gkv_bc, gkv_d.partition_broadcast(128), writes=[B('gkv_bc')])

        def perm3(ap_2d, rows):
            return ap_2d[0:rows].rearrange("t (p k) -> t p k", k=KC)

        def permout(ap_2d, rows):
            return ap_2d[0:rows].rearrange("t (k p) -> t p k", k=KC)

        rot = {}

        def nxt(name, lst):
            i = rot.get(name, 0)
            rot[name] = i + 1
            return lst[i % len(lst)]

        def rmsnorm_to_uT(src2d, src_bufs, rows, col0, gb, gb_buf, tag):
            k = nxt('xn', [0, 1])
            ss, ssb = stat2()
            rt, rtb = stat2()
            rs, rsb = stat2()
            pg.op('act', lambda e: e.activation(out=junkbf[0:rows, 0:D], in_=src2d[0:rows], func=AF.Square,
                                                accum_out=ss[0:rows]),
                  reads=src_bufs, writes=ssb + [B('junkbf')])
            pg.op('act', lambda e: e.activation(out=rt[0:rows], in_=ss[0:rows], func=AF.Sqrt, scale=1.0 / D,
                                                bias=eps_t[0:rows]),
                  reads=ssb + [B('eps')], writes=rtb)
            pg.op('dve', lambda e: e.reciprocal(out=rs[0:rows], in_=rt[0:rows]), reads=rtb, writes=rsb)
            pg.op('dve', lambda e: e.scalar_tensor_tensor(out=xn[k][0:rows], in0=src2d[0:rows],
                                                          scalar=rs[0:rows], in1=gb[0:rows],
                                                          op0=ALU.mult, op1=ALU.mult),
                  reads=src_bufs + rsb + [gb_buf], writes=[B('xn', k)])
            bk = nxt('tp', [0, 1])

            def tps(e):
                last = None
                for kc in range(KC):
                    last = e.transpose(out=banks_bf[bk][:, kc * 128: kc * 128 + rows],
                                       in_=xn[k][0:rows].rearrange("t (p k) -> t k p", k=KC)[:, kc, :],
                                       identity=ident_bf[0:rows, 0:rows])
                return last
            pg.op('pe', tps, reads=[B('xn', k), B('ident_bf')], writes=[PB[bk]])
            src = banks_bf[bk][:, 0:1024].rearrange("p (a b) -> p a b", a=KC)[:, :, 0:rows]

            def back():
                pg.op('act', lambda e: e.copy(out=uT[:, :, col0:col0 + rows], in_=src),
                      reads=[PB[bk]], writes=[B('uT', tag)])
            return back

        eps_t = carve('R0', F32, [P, 1])
        pg.op('pool', lambda e: e.memset(eps_t, EPS), writes=[B('eps')])

        reset('R2')
        reset('R3')
        pvT = carve('R3', F32, [P, 4, T])
        wslab = carve('R2', BF16, [P, KC, 512])
        wsmall = carve('R2', BF16, [P, KC, 136])
        wik = carve('R2', BF16, [P, KC, 96])
        w_iq2 = carve('R2', BF16, [P, KC, 256])
        wpool = carve('R2', BF16, [P, 4, 128])
        ptmp = [carve('R2', F32, [P, T]) for _ in range(2)]
        dT = [carve('R2', BF16, [P, T]) for _ in range(2)]
        tmpc = carve('R2', F32, [P, 16])

        xt4 = xt + [ptmp[0][:, 0:D], ptmp[1][:, 0:D]]
        tiles = [-1] + list(range(NT))
        pend1 = None
        for j in tiles:
            rows = 16 if j < 0 else 128
            col0 = 0 if j < 0 else 16 + 128 * j
            k = nxt('xt4', [0, 1, 2, 3])
            src = meta_d if j < 0 else x_d[128 * j:128 * (j + 1), :]
            pg.dma('sp', xt4[k][0:rows], src, writes=[B('xt', k)])
            if j == 1:
                late_consts()
            bk_ = rmsnorm_to_uT(xt4[k], [B('xt', k)], rows, col0, gbc[0], B('gbc', 0), j)
            if pend1 is not None:
                pend1()
            pend1 = bk_
        pend1()

        def uT_bufs(c0, c1):
            res = []
            if c0 < 16:
                res.append(B('uT', -1))
            for j in range(NT):
                a, b_ = 16 + 128 * j, 16 + 128 * (j + 1)
                if a < c1 and b_ > c0:
                    res.append(B('uT', j))
            return res

        win_p = win_d.rearrange("(p k) n -> p k n", k=KC)

        pg.dma('pool', wsmall[:, :, 0:128], win_p[:, :, 512:640], writes=[B('wsmall')])
        pg.dma('pool', wsmall[:, :, 128:136], win_p[:, :, 928:936], reads=[], writes=[B('wsmall2')])
        for r_ in range(3):
            pg.dma('pool', wik[:, :, 32 * r_:32 * r_ + 32], win_p[:, :, 896:928], writes=[B('wik', r_)])
        pg.dma('pool', w_iq2, win_p[:, :, 640:896], writes=[B('w_iq2')])
        pg.dma('pool', wslab, win_p[:, :, 936:1448], writes=[B('wslab')])
        pg.dma('pool', wpool, wpool_d.rearrange("g c d -> c g d"), writes=[B('wpool')])

        def p2a_f1(j):
            rows = 16 if j < 0 else 128
            col0 = 0 if j < 0 else 16 + 128 * j
            chunk = 16 if j < 0 else j
            bk = nxt('p2a', [2, 3])

            def mm(e):
                last = None
                for kc in range(KC):
                    last = e.matmul(banks[bk][0:rows, 0:136], lhsT=uT[:, kc, col0:col0 + rows],
                                    rhs=wsmall[:, kc, :], start=(kc == 0), stop=(kc == KC - 1))
                return last
            pg.op('pe', mm, reads=[B('uT', j), B('wsmall'), B('wsmall2')], writes=[PB[bk]])
            ss, ssb = stat2()
            rt, rtb = stat2()
            rs, rsb = stat2()
            pg.op('act', lambda e: e.activation(
                out=junkbf[0:rows, 0:128], in_=banks[bk][0:rows, 0:128], func=AF.Square, accum_out=ss[0:rows]),
                reads=[PB[bk]], writes=ssb + [B('junkbf')])
            pg.op('act', lambda e: e.activation(
                out=rt[0:rows], in_=ss[0:rows], func=AF.Sqrt, scale=1.0 / 128, bias=eps_t[0:rows]),
                reads=ssb + [B('eps')], writes=rtb)
            pg.op('dve', lambda e: e.reciprocal(out=rs[0:rows], in_=rt[0:rows]), reads=rtb, writes=rsb)
            pg.op('dve', lambda e: e.scalar_tensor_tensor(
                out=ckeys[0:rows, chunk, 0:128], in0=banks[bk][0:rows, 0:128], scalar=rs[0:rows],
                in1=gkv_bc[0:rows], op0=ALU.mult, op1=ALU.mult),
                reads=[PB[bk], B('gkv_bc')] + rsb, writes=[B('ckeys', chunk)])
            if j >= 0:
                pg.op('dve', lambda e: e.tensor_scalar(
                    out=iw_all[:, j, :], in0=banks[bk][:, 128:136], scalar1=IDX_SCALE, scalar2=None,
                    op0=ALU.mult), reads=[PB[bk]], writes=[B('iw', j)])

            def f2():
                bt = nxt('p2at', [4, 5])
                pg.op('pe', lambda e: e.transpose(
                    out=banks_bf[bt][:, 0:rows], in_=ckeys[0:rows, chunk, 0:128], identity=ident_bf[0:rows, 0:rows]),
                    reads=[B('ckeys', chunk), B('ident_bf')], writes=[PB[bt]])

                def f3():
                    pg.op('act', lambda e: e.copy(
                        out=cnT[:, col0:col0 + rows], in_=banks_bf[bt][:, 0:rows]),
                        reads=[PB[bt]], writes=[B('cnT', j)])
                return f3
            return f2

        p_f2, p_f3 = None, None
        for j in tiles:
            f2 = p2a_f1(j)
            f3 = p_f2() if p_f2 is not None else None
            if p_f3 is not None:
                p_f3()
            p_f2, p_f3 = f2, f3
        f3 = p_f2()
        if p_f3 is not None:
            p_f3()
        f3()

        blocks = [(0, 16)] + [(16 + 512 * b, 16 + 512 * (b + 1)) for b in range(4)]
        allb = list(range(8))
        evac_i = [0]

        def evac_copy(out_ap, in_ap, reads, writes):
            evac_i[0] += 1
            if evac_i[0] % 2 == 0:
                pg.op('act', lambda e: e.copy(out=out_ap, in_=in_ap), reads=reads, writes=writes)
            else:
                pg.op('dve', lambda e: e.tensor_copy(out=out_ap, in_=in_ap), reads=reads, writes=writes)

        def act_evac(out_ap, in_ap, reads, writes):
            pg.op('act', lambda e: e.copy(out=out_ap, in_=in_ap), reads=reads, writes=writes)

        def p2b_ik():
            for bi, (c0, c1) in enumerate(blocks):
                bk = nxt('gen', allb)
                n = c1 - c0

                def mm(e, bk=bk, c0=c0, c1=c1, n=n):
                    last = None
                    for kc in range(KC):
                        last = e.matmul(banks[bk][0:96, 0:n], lhsT=wik[:, kc, :], rhs=uT[:, kc, c0:c1],
                                        start=(kc == 0), stop=(kc == KC - 1))
                    return last
                pg.op('pe', mm, reads=uT_bufs(c0, c1) + [B('wik', 0), B('wik', 1), B('wik', 2)], writes=[PB[bk]])
                act_evac(ikT[0:96, c0:c1], banks[bk][0:96, 0:n], [PB[bk]], [B('ikT', bi)])

        def p2b_iq():
            for g in range(3):
                M = 96 if g < 2 else 64
                for b in range(4):
                    bk = nxt('gen', allb)
                    c0 = 16 + 512 * b

                    def mmi(e, bk=bk, g=g, M=M, c0=c0):
                        last = None
                        for kc in range(KC):
                            last = e.matmul(banks[bk][0:M, 0:512], lhsT=w_iq2[:, kc, 96 * g:96 * g + M],
                                            rhs=uT[:, kc, c0:c0 + 512], start=(kc == 0), stop=(kc == KC - 1))
                        return last
                    pg.op('pe', mmi, reads=uT_bufs(c0, c0 + 512) + [B('w_iq2')], writes=[PB[bk]])
                    act_evac(iqT_all[0:M, g, 512 * b:512 * (b + 1)], banks[bk][0:M, 0:512], [PB[bk]],
                             [B('iqT_all', g, b)])

        def p2b_pv(m):
            for bi, (c0, c1) in enumerate(blocks):
                bk = nxt('gen', allb)
                n = c1 - c0

                def mm(e, bk=bk, c0=c0, c1=c1, n=n):
                    last = None
                    for kc in range(KC):
                        last = e.matmul(banks[bk][:, 0:n], lhsT=wslab[:, kc, m * 128:(m + 1) * 128],
                                        rhs=uT[:, kc, c0:c1], start=(kc == 0), stop=(kc == KC - 1))
                    return last
                pg.op('pe', mm, reads=uT_bufs(c0, c1) + [B('wslab')], writes=[PB[bk]])
                act_evac(pvT[:, m, c0:c1], banks[bk][:, 0:n], [PB[bk]], [B('pvT', m)])

        p3_di = {}

        def p3_chain(g):
            w = 2 << g
            src = pvT[:, g, :]
            cur = src
            curb = [B('pvT', g)]
            for k in [1, 2, 4, 8][:g + 1]:
                pi = nxt('ptmp', [0, 1])
                dst = ptmp[pi]
                pg.op('pool', lambda e, dst=dst, cur=cur, k=k: e.tensor_tensor(
                    out=dst[:, k:1024], in0=cur[:, k:1024], in1=cur[:, 0:1024 - k], op=ALU.add),
                    reads=curb, writes=[B('ptmp', pi), B('xt', 2 + pi)])
                pg.op('dve', lambda e, dst=dst, cur=cur, k=k: e.tensor_tensor(
                    out=dst[:, 1024:T], in0=cur[:, 1024:T], in1=cur[:, 1024 - k:T - k], op=ALU.add),
                    reads=curb, writes=[B('ptmp', pi, 'hi')])
                pg.op('pool', lambda e, dst=dst, cur=cur, k=k: e.tensor_copy(out=dst[:, 0:k], in_=cur[:, 0:k]),
                      reads=curb, writes=[B('ptmp', pi, 'head'), B('xt', 2 + pi)])
                cur = dst
                curb = [B('ptmp', pi), B('ptmp', pi, 'hi'), B('ptmp', pi, 'head')]
            di = nxt('dT', [0, 1])
            p3_di[g] = di
            cb = curb
            pg.op('dve', lambda e: e.scalar_tensor_tensor(
                out=dT[di][:, w - 1:T], in0=cur[:, w - 1:T], scalar=1.0 / w, in1=src[:, w - 1:T],
                op0=ALU.mult, op1=ALU.subtract),
                reads=cb + [B('pvT', g)], writes=[B('dT', di)])
            pg.op('dve', lambda e: e.tensor_tensor(
                out=tmpc[:, 0:w - 1], in0=cur[:, 0:w - 1], in1=invcnt[:, 0:w - 1], op=ALU.mult),
                reads=cb + [B('invcnt')], writes=[B('tmpc')])
            pg.op('dve', lambda e: e.tensor_tensor(
                out=dT[di][:, 0:w - 1], in0=tmpc[:, 0:w - 1], in1=src[:, 0:w - 1], op=ALU.subtract),
                reads=[B('tmpc'), B('pvT', g)], writes=[B('dT', di, 'head')])

        def p3_mm(g):
            di = p3_di[g]
            for b in range(4):
                bk = nxt('gen', allb)
                pg.op('pe', lambda e, bk=bk, b=b: e.matmul(
                    banks[bk][:, 0:512], lhsT=wpool[:, g, :], rhs=dT[di][:, 16 + 512 * b:16 + 512 * (b + 1)],
                    start=True, stop=True),
                    reads=[B('wpool'), B('dT', di), B('dT', di, 'head')], writes=[PB[bk]])
                pg.op('act', lambda e, bk=bk, b=b: e.activation(
                    out=poolT[:, g, 512 * b:512 * (b + 1)], in_=banks[bk][:, 0:512], func=AF.Identity,
                    scale=pscale[:, g:g + 1]),
                    reads=[PB[bk], B('pscale')], writes=[B('poolT', b)])

        W_Q_OFF = 25344
        w_q = carve_at('R3', W_Q_OFF, BF16, [P, KC, 512])
        p2b_pv(3)
        p2b_pv(2)
        p3_chain(3)
        p2b_pv(1)
        p3_chain(2)
        if stop_after >= 4:
            pg.dma('pool', w_q, win_p[:, :, 0:512], writes=[B('w_q'), B('pvT', 3)])
        p2b_pv(0)
        p2b_ik()
        p2b_iq()
        p3_mm(3)
        p3_mm(2)
        p3_chain(1)
        p3_mm(1)
        p3_chain(0)
        p3_mm(0)

        if stop_after <= 3:
            pass
        pg.barrier()
        reset('R2')
        reset('R3')
        w_ba = carve_at('R0', off_idle, BF16, [P, 4, D])
        w_bp = carve_at('R0', off_idle + 8192, BF16, [P, 4, D])
        score = carve('R3', F32, [P, T])
        mb = carve('R3', BF16, [P, T])
        mbT = carve('R3', BF16, [P, 17, 128])
        ukT_pad = carve('R2', BF16, [P, 8, 128])
        uv_pad = carve('R2', BF16, [P, 8, 128])
        wuk_bf = carve('R2', BF16, [P, 512])
        qT_t = carve('R2', BF16, [P, 4, 128])
        qabsT_t = carve('R2', BF16, [P, 8, 128])
        absq = carve('R2', BF16, [P, 8, 128])
        iqT_t = carve('R2', BF16, [P, 8, 128])
        rbuf = [carve('R2', BF16, [P, 512]) for _ in range(4)]
        pT = [carve('R2', BF16, [P, 512]) for _ in range(3)]
        diag = carve('R2', BF16, [P, 8, 128])
        olatT_t = carve('R2', BF16, [P, 8, 128])
        negm8 = carve('R2', BF16, [P, 128])
        bis = carve('R2', F32, [P, 4 * (NIT + 2)])
        rsum = carve('R2', F32, [P, 8])
        thr = carve('R2', F32, [P, 2])

        if stop_after >= 4:
            pg.dma('pool', wuk_bf, wuk_d, writes=[B('wuk_bf')])
            pg.dma('pool', w_ba, wba_d.rearrange("(m p) n -> p m n", p=128),
                   writes=[B('w_ba'), B('gbc', 0), B('gbc', 1)])
            pg.dma('pool', w_bp, wbp_d.rearrange("(m p) n -> p m n", p=128),
                   writes=[B('w_bp'), B('xt', 0), B('xt', 1)])
            pg.op('pool', lambda e: e.memset(ukT_pad, 0.0), writes=[B('ukT_pad')])
            pg.op('pool', lambda e: e.memset(uv_pad, 0.0), writes=[B('uv_pad')])
            for hp in range(2):
                src = wuv_d.rearrange("c (m two d) -> c m two d", two=2, d=64)[:, :, hp, :]
                dst = uv_pad.rearrange("c (m two) n -> c m two n", two=2)[:, :, hp, 64 * hp:64 * hp + 64]
                pg.dma('pool', dst, src, reads=[B('uv_pad')], writes=[B('uv_pad', hp)])
            for m in range(4):
                bk = nxt('gen', allb)
                pg.op('pe', lambda e, bk=bk, m=m: e.transpose(
                    out=banks_bf[bk][:, 0:128], in_=wuk_bf[:, m * 128:(m + 1) * 128], identity=ident_bf),
                    reads=[B('wuk_bf'), B('ident_bf')], writes=[PB[bk]])
                for hp in range(2):
                    h = 2 * m + hp
                    pg.op('dve', lambda e, bk=bk, h=h, hp=hp: e.tensor_copy(
                        out=ukT_pad[64 * hp:64 * hp + 64, h, :], in_=banks_bf[bk][64 * hp:64 * hp + 64, 0:128]),
                        reads=[PB[bk], B('ukT_pad')], writes=[B('ukT_pad', h)])
            pg.op('dve', lambda e: e.tensor_reduce(out=cmax, in_=cnT, axis=AX.X, op=ALU.max,
                                                   apply_absolute_value=True),
                  reads=[B('cnT', j) for j in tiles], writes=[B('cmax')])
            pg.op('pool', lambda e: e.memset(ones_bf, 1.0), writes=[B('ones_bf')])
            pg.op('dve', lambda e: e.tensor_copy(out=cmaxB, in_=mk_ap(cmax, 0, [[pstep(cmax), 128], [0, 128]])),
                  reads=[B('cmax')], writes=[B('cmaxB')])
            pg.op('pool', lambda e: e.memset(cmaxsel, 0.0), writes=[B('cmaxsel')])
            pg.op('pool', lambda e: e.memset(mbT[:, 16, :], NEG), writes=[B('mbT', 16)])
            for h in range(8):
                pg.op('dve', lambda e, h=h: e.tensor_copy(out=cmaxsel[:, h, h:h + 1], in_=cmax),
                      reads=[B('cmax'), B('cmaxsel')], writes=[B('cmaxsel', h)])
        ukb = [B('ukT_pad')] + [B('ukT_pad', h) for h in range(8)]
        uvb = [B('uv_pad'), B('uv_pad', 0), B('uv_pad', 1)]
        cmb = [B('cmaxsel')] + [B('cmaxsel', h) for h in range(8)]
        ikb = [B('ikT', bi) for bi in range(5)]
        cnb = [B('cnT', j) for j in tiles]
        ckb = [B('ckeys', c) for c in range(17)] + [B('ckeys_ones')]

        mids = bis[:, 0:NIT + 2]
        cnts = bis[:, NIT + 2:2 * (NIT + 2)]
        dds = bis[:, 2 * (NIT + 2):3 * (NIT + 2)]
        Wc = bis[:, 3 * (NIT + 2):4 * (NIT + 2)]

        n_attn_tiles = NT if stop_after >= 4 else 0
        n_attn_tiles = debug.get('n_attn_tiles', n_attn_tiles)
        qabs2 = [qabsT_t, carve('R2', BF16, [P, 8, 128]), carve('R2', BF16, [P, 8, 128])]
        mb2 = [mb, carve('R3', BF16, [P, T])]
        assert cursor['R3'] <= W_Q_OFF, cursor['R3']
        score2 = [score, carve('R2', F32, [P, T])]
        negsh = carve('R2', F32, [P, 8])
        lnS = carve('R2', F32, [P, 512])
        rcpS = carve('R2', F32, [P, 512])
        ROT4 = [0, 1, 2, 3]
        PSS = [6, 7]
        ACC = [4, 5]

        def act_copy(out_ap, in_ap, reads, writes):
            pg.op('act', lambda e: e.copy(out=out_ap, in_=in_ap), reads=reads, writes=writes)

        def chunks_of(j):
            S = 16 + 128 * (j + 1)
            return [(0, 16)] + [(16 + 512 * m, min(16 + 512 * (m + 1), S)) for m in range((S - 16 + 511) // 512)]

        def stageA(j, tick=None):
            par = j % 2
            par3 = j % 3
            sc, qa = score2[par], qabs2[par3]
            qc = 16 + 128 * j
            ub = [B('uT', j)]
            for h in range(8):
                pg.op('act', lambda e, h=h, j=j: e.activation(out=diag[:, h, :], in_=ident_f, func=AF.Identity,
                                                              scale=iw_all[:, j, h:h + 1]),
                      reads=[B('ident_f'), B('iw', j)], writes=[B('diag', h)])
            items = []
            for (c0, c1) in chunks_of(j):
                bs = nxt('pss', PSS)
                for hp_ in range(4):
                    items.append((c0, c1, bs, hp_))

            def emit_x(it):
                c0, c1, bs, hp_ = it
                n = c1 - c0
                res = []
                for hh in range(2):
                    h = 2 * hp_ + hh
                    bx = nxt('rot4', ROT4)
                    g_, a_ = h // 3, h % 3
                    pg.op('pe', lambda e, bx=bx, g_=g_, a_=a_, c0=c0, c1=c1, n=n: e.matmul(
                        banks[bx][:, 0:n], lhsT=iqT_all[32 * a_:32 * a_ + 32, g_, 128 * j:128 * (j + 1)],
                        rhs=ikT[32 * a_:32 * a_ + 32, c0:c1], start=True, stop=True),
                        reads=[B('iqT_all', g_, j // 4)] + ikb, writes=[PB[bx]])
                    ri = nxt('rbuf', [0, 1, 2, 3])
                    pg.op('act', lambda e, bx=bx, ri=ri, n=n: e.activation(
                        out=rbuf[ri][:, 0:n], in_=banks[bx][:, 0:n], func=AF.Relu),
                        reads=[PB[bx]], writes=[B('rbuf', ri)])
                    res.append((h, ri))
                return res

            def emit_d(it, res):
                c0, c1, bs, hp_ = it
                n = c1 - c0
                for (h, ri) in res:
                    pg.op('pe', lambda e, bs=bs, h=h, ri=ri, n=n: e.matmul(
                        banks[bs][:, 0:n], lhsT=diag[:, h, :], rhs=rbuf[ri][:, 0:n], start=(h == 0), stop=(h == 7)),
                        reads=[B('diag', h), B('rbuf', ri)], writes=[PB[bs]])
                if hp_ == 3:
                    act_copy(sc[:, c0:c1], banks[bs][:, 0:n], [PB[bs]], [B('score', par, c0)])

            prev = None
            for it in items:
                res = emit_x(it)
                if prev is not None:
                    emit_d(*prev)
                prev = (it, res)
                if tick is not None:
                    tick()
            emit_d(*prev)
            bk = nxt('rot4', ROT4)

            def mmq(e, bk=bk, qc=qc):
                last = None
                for m in range(4):
                    for kc in range(KC):
                        last = e.matmul(banks[bk][:, m * 128:(m + 1) * 128], lhsT=w_q[:, kc, m * 128:(m + 1) * 128],
                                        rhs=uT[:, kc, qc:qc + 128], start=(kc == 0), stop=(kc == KC - 1))
                return last
            pg.op('pe', mmq, reads=ub + [B('w_q')], writes=[PB[bk]])
            act_copy(qT_t, banks[bk][:, 0:512].rearrange("p (a b) -> p a b", a=4), [PB[bk]], [B('qT_t')])
            for half in range(2):
                bk = nxt('rot4', ROT4)

                def mma(e, bk=bk, half=half):
                    last = None
                    for hh in range(4):
                        h = 4 * half + hh
                        last = e.matmul(banks[bk][:, hh * 128:(hh + 1) * 128], lhsT=ukT_pad[:, h, :],
                                        rhs=qT_t[:, h // 2, :], start=True, stop=True)
                    return last
                pg.op('pe', mma, reads=ukb + [B('qT_t')], writes=[PB[bk]])
                src3 = banks[bk][:, 0:512].rearrange("p (a b) -> p a b", a=4)
                act_copy(qa[:, 4 * half:4 * half + 4, :], src3, [PB[bk]], [B('qabsT_t', par3, half)])
                pg.op('dve', lambda e, half=half, qa=qa: e.scalar_tensor_tensor(
                    out=absq[:, 4 * half:4 * half + 4, :], in0=qa[:, 4 * half:4 * half + 4, :], scalar=-1.0,
                    in1=qa[:, 4 * half:4 * half + 4, :], op0=ALU.mult, op1=ALU.max),
                    reads=[B('qabsT_t', par3, half)], writes=[B('absq', half)])

        def stageA2(j):
            par = j % 3
            for half in range(2):
                bk = nxt('rot4', ROT4)

                def mmb(e, bk=bk, half=half):
                    last = None
                    for hh in range(4):
                        last = e.matmul(banks[bk][:, hh * 128:(hh + 1) * 128], lhsT=cmaxB,
                                        rhs=absq[:, 4 * half + hh, :], start=True, stop=True)
                    return last
                pg.op('pe', mmb, reads=[B('cmaxB'), B('absq', half)], writes=[PB[bk]])
                pg.op('dve', lambda e, bk=bk, half=half: e.tensor_reduce(
                    out=negsh[:, 4 + half:5 + half], in_=banks[bk][:, 0:512], axis=AX.X, op=ALU.max),
                    reads=[PB[bk]], writes=[B('negsh_t', half)])
            pg.op('dve', lambda e: e.tensor_tensor(out=negsh[:, 6:7], in0=negsh[:, 4:5], in1=negsh[:, 5:6], op=ALU.max),
                  reads=[B('negsh_t', 0), B('negsh_t', 1)], writes=[B('negsh_m')])
            pg.op('dve', lambda e, par=par: e.tensor_scalar(out=negsh[:, par:par + 1], in0=negsh[:, 6:7],
                                                            scalar1=-ATTN_SCALE, scalar2=None, op0=ALU.mult),
                  reads=[B('negsh_m')], writes=[B('negsh', par)])

        def scbufs(j):
            return [B('score', j % 2, c0) for (c0, c1) in chunks_of(j)]

        def stageB_init(j):
            S = 16 + 128 * (j + 1)
            sc = score2[j % 2]
            scb = scbufs(j)
            Rr, Rb = stat2()
            pg.op('dve', lambda e: e.tensor_reduce(out=Rr, in_=sc[:, 0:S], axis=AX.X, op=ALU.max,
                                                   apply_absolute_value=True),
                  reads=scb, writes=Rb)
            pg.op('dve', lambda e: e.tensor_tensor(out=sc[:, S - 128:S], in0=sc[:, S - 128:S],
                                                   in1=blockbias, op=ALU.add),
                  reads=scb + [B('blockbias')], writes=[B('score', j % 2, 16 + 512 * (j // 4))])
            pg.op('dve', lambda e: e.tensor_scalar(out=Wc, in0=pow2, scalar1=Rr, scalar2=1e-30,
                                                   op0=ALU.mult, op1=ALU.add),
                  reads=Rb + [B('pow2')], writes=[B('Wc')])
            pg.op('dve', lambda e: e.memset(mids[:, 0:1], 0.0), writes=[B('mid', 0)])

        def stageB_iter(j, n_):
            S = 16 + 128 * (j + 1)
            sc = score2[j % 2]
            pg.op('dve', lambda e: e.tensor_scalar(
                out=junkbf[:, 0:S], in0=sc[:, 0:S], scalar1=mids[:, n_ - 1:n_], scalar2=None,
                op0=ALU.is_ge, op1=ALU.add, accum_out=cnts[:, n_ - 1:n_]),
                reads=scbufs(j) + [B('mid', n_ - 1)], writes=[B('junkbf'), B('cnt', n_)])
            pg.op('dve', lambda e: e.scalar_tensor_tensor(
                out=dds[:, n_ - 1:n_], in0=cnts[:, n_ - 1:n_], scalar=KTOP - 0.5, in1=Wc[:, n_ - 1:n_],
                op0=ALU.is_ge, op1=ALU.mult),
                reads=[B('cnt', n_), B('Wc')], writes=[B('dd', n_)])
            pg.op('dve', lambda e: e.scalar_tensor_tensor(
                out=mids[:, n_:n_ + 1], in0=dds[:, n_ - 1:n_], scalar=Wc[:, n_:n_ + 1], in1=mids[:, n_ - 1:n_],
                op0=ALU.subtract, op1=ALU.add),
                reads=[B('dd', n_), B('Wc'), B('mid', n_ - 1)], writes=[B('mid', n_)])

        def stageB_fin(j):
            pg.op('dve', lambda e: e.tensor_tensor(out=thr[:, 0:1], in0=mids[:, NIT:NIT + 1], in1=Wc[:, NIT:NIT + 1],
                                                   op=ALU.subtract),
                  reads=[B('mid', NIT), B('Wc')], writes=[B('thr')])
            S = 16 + 128 * (j + 1)
            sc = score2[j % 2]
            mbj = mb2[j % 2]
            pg.op('dve', lambda e: e.tensor_scalar(out=mbj[:, 0:S], in0=sc[:, 0:S], scalar1=thr[:, 0:1],
                                                   scalar2=NEG, op0=ALU.is_lt, op1=ALU.mult),
                  reads=scbufs(j) + [B('thr')], writes=[B('mb', j % 2)])

        def stageC(j):
            mbj = mb2[j % 2]
            klist = [16] + list(range(j + 1))
            for g0 in range(0, len(klist), 8):
                grp = klist[g0:g0 + 8]
                bk = nxt('rot4', ROT4)

                def tp(e, grp=grp, bk=bk):
                    last = None
                    for si, i in enumerate(grp):
                        c_ = 0 if i == 16 else 16 + 128 * i
                        last = e.transpose(out=banks_bf[bk][:, si * 128:(si + 1) * 128], in_=mbj[:, c_:c_ + 128],
                                           identity=ident_bf)
                    return last
                pg.op('pe', tp, reads=[B('mb', j % 2), B('ident_bf')], writes=[PB[bk]])
                si0 = 0
                if grp[0] == 16:
                    pg.op('dve', lambda e, bk=bk: e.tensor_copy(out=mbT[0:16, 16, :], in_=banks_bf[bk][0:16, 0:128]),
                          reads=[PB[bk]], writes=[B('mbT', 16)])
                    si0 = 1
                nn_ = len(grp) - si0
                if nn_ > 0:
                    i0 = grp[si0]
                    srcv = banks_bf[bk][:, si0 * 128:(si0 + nn_) * 128].rearrange("p (a b) -> p a b", a=nn_)
                    pg.op('dve', lambda e, i0=i0, nn_=nn_, srcv=srcv: e.tensor_copy(out=mbT[:, i0:i0 + nn_, :], in_=srcv),
                          reads=[PB[bk]], writes=[B('mbT', i) for i in range(i0, i0 + nn_)])

        def stageD_lg(j, q, ci):
            par = j % 3
            qa = qabs2[par]
            xch = [16] + list(range(j + 1))
            i = xch[ci]
            bl = nxt('rot4', ROT4)
            kc0 = 0 if i == 16 else 16 + 128 * i
            mrow = mk_ap(mbT, i * 128, [[pstep(mbT), 128], [0, 4], [1, 128]])
            qrhs = qa[:, 4 * q:4 * q + 4, :]

            def lg(e):
                e.matmul(banks[bl][:, 0:512], lhsT=cnT[:, kc0:kc0 + 128], rhs=qrhs, start=True, stop=False)
                return e.matmul(banks[bl][:, 0:512], lhsT=ident_bf, rhs=mrow, start=False, stop=True)
            pg.op('pe', lg, reads=cnb + [B('qabsT_t', par, q), B('ident_bf'), B('mbT', i)], writes=[PB[bl]])
            pi = nxt('pT', [0, 1, 2])
            pg.op('act', lambda e: e.activation(out=pT[pi][:, 0:512], in_=banks[bl][:, 0:512], func=AF.Exp,
                                                scale=ATTN_SCALE, bias=negsh[:, par:par + 1]),
                  reads=[PB[bl], B('negsh', par)], writes=[B('pT', pi)])
            return pi

        def stageD_pv(j, q, ci, pi):
            xch = [16] + list(range(j + 1))
            i = xch[ci]
            bO, bS = ACC
            first, last_ = (ci == 0), (ci == len(xch) - 1)

            def pv(e):
                e.matmul(banks[bO][:, 0:512], lhsT=ckeys[:, i, 0:128], rhs=pT[pi][:, 0:512], start=first, stop=last_)
                return e.matmul(banks[bS][:, 0:512], lhsT=ones_bf, rhs=pT[pi][:, 0:512], start=first, stop=last_)
            pg.op('pe', pv, reads=[B('pT', pi), B('ones_bf'), B('ckeys', i)], writes=[PB[bO], PB[bS]])

        def stageD_qfin(j, q):
            bO, bS = ACC
            pg.op('act', lambda e: e.activation(out=lnS, in_=banks[bS][:, 0:512], func=AF.Ln),
                  reads=[PB[bS]], writes=[B('lnS')])
            pg.op('act', lambda e: e.activation(out=rcpS, in_=lnS, func=AF.Exp, scale=-1.0),
                  reads=[B('lnS')], writes=[B('rcpS')])
            pg.op('dve', lambda e: e.tensor_tensor(
                out=olatT_t[:, 4 * q:4 * q + 4, :], in0=banks[bO][:, 0:512].rearrange("p (a b) -> p a b", a=4),
                in1=rcpS.rearrange("p (a b) -> p a b", a=4), op=ALU.mult),
                reads=[PB[bO], B('rcpS')], writes=[B('olatT_t', q)])

        def stageD_fin(j):
            bk = nxt('rot4', ROT4)

            def mmo(e, bk=bk):
                last = None
                for m in range(4):
                    o = banks[bk][:, m * 128:(m + 1) * 128]
                    e.matmul(o, lhsT=uv_pad[:, 2 * m, :], rhs=olatT_t[:, 2 * m, :], start=True, stop=False)
                    last = e.matmul(o, lhsT=uv_pad[:, 2 * m + 1, :], rhs=olatT_t[:, 2 * m + 1, :], start=False, stop=True)
                return last
            pg.op('pe', mmo, reads=uvb + [B('olatT_t', 0), B('olatT_t', 1)], writes=[PB[bk]])
            act_copy(attnT[:, :, 128 * j:128 * (j + 1)], banks[bk][:, 0:512].rearrange("p (a b) -> p a b", a=4),
                     [PB[bk]], [B('attnT', j)])

        nA = n_attn_tiles

        def fullB(j):
            stageB_init(j)
            if 16 + 128 * (j + 1) <= KTOP:
                pg.op('dve', lambda e: e.tensor_reduce(out=cnts[:, 0:1], in_=Wc[:, 1:NIT + 1], axis=AX.X, op=ALU.add),
                      reads=[B('Wc')], writes=[B('cnt', 1)])
                pg.op('dve', lambda e: e.tensor_scalar(out=mids[:, NIT:NIT + 1], in0=cnts[:, 0:1], scalar1=-1.0,
                                                       scalar2=None, op0=ALU.mult),
                      reads=[B('cnt', 1)], writes=[B('mid', NIT)])
            else:
                for n_ in range(1, NIT + 1):
                    stageB_iter(j, n_)
            stageB_fin(j)

        if nA > 2:
            stageA(0)
            fullB(0)
            stageA(1)
            stageB_init(1)
            stageA2(0)
            for n_ in (1, 2, 3):
                stageB_iter(1, n_)
            stageA2(1)
            for n_ in (4, 5, 6):
                stageB_iter(1, n_)
            stageC(0)
            for n_ in (7, 8):
                stageB_iter(1, n_)
            assert 4 * len(chunks_of(2)) == 8
            pro_n = [8]

            def pro_tick():
                pro_n[0] += 1
                if pro_n[0] <= NIT:
                    stageB_iter(1, pro_n[0])
            stageA(2, pro_tick)
            for n_ in range(pro_n[0] + 1, NIT + 1):
                stageB_iter(1, n_)
            stageA2(2)
            stageB_fin(1)
        elif nA > 0:
            stageA(0)
            fullB(0)
            if nA > 1:
                stageA(1)
            stageA2(0)
            if nA > 1:
                stageA2(1)
            stageC(0)
            if nA > 1:
                fullB(1)
        carry_sched = None
        for i in range(nA):
            hasB = i + 2 < nA
            hasA = i + 3 < nA
            nch = i + 2
            seq = [(q, ci) for q in range(2) for ci in range(nch)]
            steps = len(seq)
            n_items = 4 * len(chunks_of(i + 3)) if hasA else 0
            wD, wA = 1.0 * steps, 1.7 * n_items
            ticks = [('D', k) for k in range(steps)] + [('A', k) for k in range(n_items)]
            wts = [1.0] * steps + [1.7] * n_items
            split_last = hasB and not hasA and i + 1 < nA
            if split_last:
                steps2 = 2 * (i + 3)
                ticks = ticks + [('E', k) for k in range(steps2)]
                wts = wts + [1.0] * steps2
            tot = sum(wts)
            sched = {}
            acc, done = 0.0, 0
            for tk, w in zip(ticks, wts):
                acc += w
                upto = int(round(NIT * acc / tot))
                sched[tk] = list(range(done + 1, upto + 1))
                done = upto
            if hasB:
                stageB_init(i + 2)
            pis = {}
            pis[0] = stageD_lg(i, *seq[0])
            for k, (q, ci) in enumerate(seq):
                if k + 1 < steps:
                    pis[k + 1] = stageD_lg(i, *seq[k + 1])
                stageD_pv(i, q, ci, pis[k])
                if hasB:
                    for n_ in sched[('D', k)]:
                        stageB_iter(i + 2, n_)
                if carry_sched is not None:
                    for n_ in carry_sched[('E', k)]:
                        stageB_iter(i + 1, n_)
                if ci == nch - 1:
                    stageD_qfin(i, q)
            stageD_fin(i)
            if hasA:
                cnt = [0]

                def tick(cnt=cnt):
                    if hasB:
                        for n_ in sched[('A', cnt[0])]:
                            stageB_iter(i + 2, n_)
                    cnt[0] += 1
                stageA(i + 3, tick)
            if hasB and not split_last:
                stageB_fin(i + 2)
            if carry_sched is not None:
                stageB_fin(i + 1)
            carry_sched = sched if split_last else None
            if i + 1 < nA:
                stageC(i + 1)
            if hasA:
                stageA2(i + 3)

        pg.barrier()
        reset('R2')
        reset('R3')
        mergedT = carve('R3', BF16, [P, 8, TX])
        wgA2 = [carve('R2', BF16, [P, KC, 256]) for _ in range(2)]
        wgP2 = [carve('R2', BF16, [P, KC, 256]) for _ in range(2)]
        w_out = carve('R2', BF16, [P, 8, D])
        off_wout_end = cursor['R2']
        sA = [carve('R2', F32, [P, 512]) for _ in range(2)]
        sP = [carve('R2', F32, [P, 512]) for _ in range(2)]
        run_tail = stop_after >= 5

        def load_wg(grp):
            s_ = grp % 2
            pg.dma('pool', wgA2[s_], win_p[:, :, 1448 + 256 * grp:1448 + 256 * (grp + 1)], writes=[B('wgA', s_)])
            pg.dma('pool', wgP2[s_], win_p[:, :, 2472 + 256 * grp:2472 + 256 * (grp + 1)], writes=[B('wgP', s_)])

        if run_tail:
            load_wg(0)
            load_wg(1)
            pg.dma('pool', w_out, wout_d.rearrange("(m p) n -> p m n", p=128), writes=[B('w_out')])
            for grp in range(4):
                gs = grp % 2
                wgA, wgP = wgA2[gs], wgP2[gs]
                if 1 <= grp < 3:
                    load_wg(grp + 1)
                for n4 in range(2):
                    nn = 2 * grp + n4
                    for b in range(4):
                        bset = nxt('tailA', [[0, 1, 2, 3], [4, 5, 6, 7]])
                        c0 = 16 + 512 * b
                        bA, bP, bgA, bgP = bset

                        def mmA(e, bA=bA, nn=nn, b=b):
                            last = None
                            for m in range(4):
                                last = e.matmul(banks[bA][:, 0:512], lhsT=w_ba[:, m, nn * 128:(nn + 1) * 128],
                                                rhs=attnT[:, m, 512 * b:512 * (b + 1)], start=(m == 0), stop=(m == 3))
                            return last
                        pg.op('pe', mmA, reads=[B('w_ba')] + [B('attnT', 4 * b + q) for q in range(4)], writes=[PB[bA]])

                        def mmP(e, bP=bP, nn=nn, b=b):
                            last = None
                            for m in range(4):
                                last = e.matmul(banks[bP][:, 0:512], lhsT=w_bp[:, m, nn * 128:(nn + 1) * 128],
                                                rhs=poolT[:, m, 512 * b:512 * (b + 1)], start=(m == 0), stop=(m == 3))
                            return last
                        pg.op('pe', mmP, reads=[B('w_bp'), B('poolT', b)], writes=[PB[bP]])

                        def mmg(e, bk, wt, n4=n4, c0=c0):
                            last = None
                            for kc in range(KC):
                                last = e.matmul(banks[bk][:, 0:512], lhsT=wt[:, kc, n4 * 128:(n4 + 1) * 128],
                                                rhs=uT[:, kc, c0:c0 + 512], start=(kc == 0), stop=(kc == KC - 1))
                            return last
                        pg.op('pe', lambda e, bgA=bgA, f=mmg, wgA=wgA: f(e, bgA, wgA),
                              reads=uT_bufs(c0, c0 + 512) + [B('wgA', gs)], writes=[PB[bgA]])
                        pg.op('pe', lambda e, bgP=bgP, f=mmg, wgP=wgP: f(e, bgP, wgP),
                              reads=uT_bufs(c0, c0 + 512) + [B('wgP', gs)], writes=[PB[bgP]])
                        si = nxt('sAP', [0, 1])
                        pg.op('act', lambda e, bgA=bgA, si=si: e.activation(out=sA[si], in_=banks[bgA][:, 0:512], func=AF.Sigmoid),
                              reads=[PB[bgA]], writes=[B('sA', si)])
                        pg.op('act', lambda e, bgP=bgP, si=si: e.activation(out=sP[si], in_=banks[bgP][:, 0:512], func=AF.Sigmoid),
                              reads=[PB[bgP]], writes=[B('sP', si)])
                        pg.op('dve', lambda e, bA=bA, si=si: e.tensor_tensor(out=sA[si], in0=sA[si], in1=banks[bA][:, 0:512], op=ALU.mult),
                              reads=[B('sA', si), PB[bA]], writes=[B('sA', si)])
                        pg.op('dve', lambda e, bP=bP, si=si: e.tensor_tensor(out=sP[si], in0=sP[si], in1=banks[bP][:, 0:512], op=ALU.mult),
                              reads=[B('sP', si), PB[bP]], writes=[B('sP', si)])
                        pg.op('dve', lambda e, si=si, nn=nn, b=b: e.tensor_tensor(
                            out=mergedT[:, nn, 512 * b:512 * (b + 1)], in0=sA[si], in1=sP[si], op=ALU.add),
                            reads=[B('sA', si), B('sP', si)], writes=[B('mergedT', nn, b)])

        pg.barrier()
        reset('R1')
        reset('R2')
        hacc = carve('R1', F32, [P, NT, D])
        w_r = carve('R2', BF16, [P, KC, 36])
        rbias = carve('R2', F32, [P, 36])
        gate_all = carve('R2', F32, [P, NT, 32])
        rt_ = carve('R2', F32, [P, 256])
        cur_save = cursor['R2']
        lg_all = carve('R2', F32, [P, NT, 36])
        ssq = carve('R2', F32, [P, NT])
        rstd2 = carve('R2', F32, [P, NT])
        rv = carve('R2', F32, [P, 1600])
        assert cursor['R2'] <= off_wout_end - 16384, cursor['R2']
        if run_tail:
            pg.dma('pool', w_r[:, :, 0:4], wgr_d.rearrange("(p k) n -> p k n", k=KC), writes=[B('w_r', 0)])
            pg.dma('pool', w_r[:, :, 4:36], wer_d.rearrange("(p k) n -> p k n", k=KC), writes=[B('w_r', 1)])
            pg.dma('sp', rbias[:, 0:4], bgr_d.partition_broadcast(128), writes=[B('rbias', 0)])
            pg.dma('sp', rbias[:, 4:36], ber_d.partition_broadcast(128), writes=[B('rbias', 1)])
            pg.dma('sp', gbc[1], g2_d.partition_broadcast(128), writes=[B('gbc', 1)])
            for j in range(NT):
                b = j // 4
                k = nxt('xt', [0, 1])
                pg.dma('sp', xt[k], x_d[128 * j:128 * (j + 1), :], writes=[B('xt', k)])
                for hh in range(2):
                    bk = nxt('tailB', [0, 1, 2, 3])

                    def mmh(e, bk=bk, j=j, hh=hh):
                        last = None
                        for nn in range(8):
                            last = e.matmul(banks[bk][:, 0:512], lhsT=mergedT[:, nn, 128 * j:128 * (j + 1)],
                                            rhs=w_out[:, nn, 512 * hh:512 * (hh + 1)], start=(nn == 0), stop=(nn == 7))
                        return last
                    pg.op('pe', mmh, reads=[B('mergedT', nn, b) for nn in range(8)] + [B('w_out')] + [B('R3a', q) for q in range(6)],
                          writes=[PB[bk]])
                    pg.op('dve', lambda e, bk=bk, j=j, hh=hh, k=k: e.tensor_tensor(
                        out=hacc[:, j, 512 * hh:512 * (hh + 1)], in0=banks[bk][:, 0:512], in1=xt[k][:, 512 * hh:512 * (hh + 1)],
                        op=ALU.add), reads=[PB[bk], B('xt', k)], writes=[B('hacc', j, hh)])
                pg.op('act', lambda e, j=j: e.activation(out=junkbf[:, 0:D], in_=hacc[:, j, :], func=AF.Square,
                                                         accum_out=ssq[:, j:j + 1]),
                      reads=[B('hacc', j, 0), B('hacc', j, 1)], writes=[B('junkbf'), B('ssq', j)])
            pg.op('act', lambda e: e.activation(out=rstd2, in_=ssq, func=AF.Sqrt, scale=1.0 / D, bias=eps_t),
                  reads=[B('ssq', j) for j in range(NT)] + [B('eps')], writes=[B('rstd2s')])
            pg.op('dve', lambda e: e.reciprocal(out=rstd2, in_=rstd2), reads=[B('rstd2s')], writes=[B('rstd2')])
            def s2_front(j):
                k = nxt('xn', [0, 1])
                pg.op('dve', lambda e: e.scalar_tensor_tensor(
                    out=xn[k], in0=hacc[:, j, :], scalar=rstd2[:, j:j + 1], in1=gbc[1], op0=ALU.mult, op1=ALU.mult),
                    reads=[B('hacc', j, 0), B('hacc', j, 1), B('rstd2'), B('gbc', 1)], writes=[B('xn', k)])
                bk = nxt('tp', [4, 5])

                def tps(e):
                    last = None
                    for kc in range(KC):
                        last = e.transpose(out=banks_bf[bk][:, kc * 128:(kc + 1) * 128],
                                           in_=xn[k].rearrange("t (p k) -> t k p", k=KC)[:, kc, :], identity=ident_bf)
                    return last
                pg.op('pe', tps, reads=[B('xn', k), B('ident_bf')], writes=[PB[bk]])
                srcv = banks_bf[bk][:, 0:1024].rearrange("p (a b) -> p a b", a=KC)
                pg.op('act', lambda e: e.copy(out=uT[:, :, 16 + 128 * j:16 + 128 * (j + 1)], in_=srcv),
                      reads=[PB[bk]], writes=[B('uT', j)])

            s2_bk = {}

            def s2_back(j):
                bk2 = nxt('rt', [6, 7])
                s2_bk[j] = bk2

                def mmr(e):
                    last = None
                    for kc in range(KC):
                        last = e.matmul(banks[bk2][:, 0:36], lhsT=uT[:, kc, 16 + 128 * j:16 + 128 * (j + 1)],
                                        rhs=w_r[:, kc, :], start=(kc == 0), stop=(kc == KC - 1))
                    return last
                pg.op('pe', mmr, reads=[B('uT', j), B('w_r', 0), B('w_r', 1)], writes=[PB[bk2]])

            def s2_add(j):
                bk2 = s2_bk[j]
                pg.op('dve', lambda e: e.tensor_tensor(out=lg_all[:, j, :], in0=banks[bk2][:, 0:36], in1=rbias,
                                                       op=ALU.add),
                      reads=[PB[bk2], B('rbias', 0), B('rbias', 1)], writes=[B('lg_all', j)])

            for t_ in range(NT + 3):
                if t_ >= 3:
                    s2_add(t_ - 3)
                if 2 <= t_ < NT + 2:
                    s2_back(t_ - 2)
                if t_ < NT:
                    s2_front(t_)
            RV = B('rv')
            off = [0]

            def rvv(n):
                a_ = rv[:, off[0]:off[0] + n]
                off[0] += n
                return a_
            gmax, gsum, pgp, m1, m2, dm, ed, den, p1, p2 = [rvv(NT) for _ in range(10)]
            goh, gex = rvv(NT * 4), rvv(NT * 4)
            esel, oh1, esel2, oh2, t1, t2 = [rvv(NT * 8) for _ in range(6)]
            tmp32 = rvv(NT * 32)
            pl = pstep(lg_all)
            pr = pstep(rv)

            def v3(ap, n):
                return ap.rearrange("p (a b) -> p a b", a=NT)

            def bc_last(ap, n):
                return mk_ap(ap, 0, [[pr, 128], [1, NT], [0, n]])
            gl = lg_all[:, :, 0:4]
            el4 = mk_ap(lg_all, 4, [[pl, 128], [36, NT], [8, 4], [1, 8]])
            lgb = [B('lg_all', j) for j in range(NT)]

            def dv(fn, extra=()):
                pg.op('dve', fn, reads=[RV] + list(extra), writes=[RV])
            dv(lambda e: e.tensor_reduce(out=gmax, in_=gl, axis=AX.X, op=ALU.max), lgb)
            dv(lambda e: e.tensor_tensor(out=v3(goh, 4), in0=gl, in1=bc_last(gmax, 4), op=ALU.is_ge), lgb)
            dv(lambda e: e.tensor_tensor(out=v3(gex, 4), in0=gl, in1=bc_last(gmax, 4), op=ALU.subtract), lgb)
            pg.op('act', lambda e: e.activation(out=gex, in_=gex, func=AF.Exp), reads=[RV], writes=[RV])
            dv(lambda e: e.tensor_reduce(out=gsum, in_=v3(gex, 4), axis=AX.X, op=ALU.add))
            dv(lambda e: e.reciprocal(out=pgp, in_=gsum))
            goh4 = mk_ap(goh, 0, [[pr, 128], [4, NT], [1, 4], [0, 8]])
            t4 = mk_ap(tmp32, 0, [[pr, 128], [32, NT], [8, 4], [1, 8]])
            dv(lambda e: e.tensor_tensor(out=t4, in0=el4, in1=goh4, op=ALU.mult), lgb)
            t4T = mk_ap(tmp32, 0, [[pr, 128], [32, NT], [1, 8], [8, 4]])
            dv(lambda e: e.tensor_reduce(out=v3(esel, 8), in_=t4T, axis=AX.X, op=ALU.add))
            dv(lambda e: e.tensor_reduce(out=m1, in_=v3(esel, 8), axis=AX.X, op=ALU.max))
            dv(lambda e: e.tensor_tensor(out=v3(oh1, 8), in0=v3(esel, 8), in1=bc_last(m1, 8), op=ALU.is_ge))
            dv(lambda e: e.scalar_tensor_tensor(out=esel2, in0=oh1, scalar=-BIG, in1=esel, op0=ALU.mult, op1=ALU.add))
            dv(lambda e: e.tensor_reduce(out=m2, in_=v3(esel2, 8), axis=AX.X, op=ALU.max))
            dv(lambda e: e.tensor_tensor(out=v3(oh2, 8), in0=v3(esel2, 8), in1=bc_last(m2, 8), op=ALU.is_ge))
            dv(lambda e: e.tensor_tensor(out=dm, in0=m2, in1=m1, op=ALU.subtract))
            pg.op('act', lambda e: e.activation(out=ed, in_=dm, func=AF.Exp), reads=[RV], writes=[RV])
            dv(lambda e: e.tensor_scalar(out=den, in0=ed, scalar1=1.0, scalar2=None, op0=ALU.add))
            dv(lambda e: e.reciprocal(out=p1, in_=den))
            dv(lambda e: e.tensor_tensor(out=p2, in0=ed, in1=p1, op=ALU.mult))
            dv(lambda e: e.tensor_tensor(out=p1, in0=p1, in1=pgp, op=ALU.mult))
            dv(lambda e: e.tensor_tensor(out=p2, in0=p2, in1=pgp, op=ALU.mult))
            dv(lambda e: e.tensor_tensor(out=v3(t1, 8), in0=v3(oh1, 8), in1=bc_last(p1, 8), op=ALU.mult))
            dv(lambda e: e.tensor_tensor(out=v3(t2, 8), in0=v3(oh2, 8), in1=bc_last(p2, 8), op=ALU.mult))
            dv(lambda e: e.tensor_tensor(out=t1, in0=t1, in1=t2, op=ALU.add))
            wpg4 = mk_ap(t1, 0, [[pr, 128], [8, NT], [0, 4], [1, 8]])
            gout = gate_all.rearrange("p a (g e) -> p a g e", g=4)
            pg.op('dve', lambda e: e.tensor_tensor(out=gout, in0=goh4, in1=wpg4, op=ALU.mult),
                  reads=[RV], writes=[B('gate', j) for j in range(NT)])
        cursor['R2'] = cur_save

        reset('R3')
        n_exp = debug.get('n_exp', N_EXP if stop_after >= 6 else 0)
        wg_e = [carve('R3', BF16, [P, KC, 256]) for _ in range(2)]
        wu_e = [carve('R3', BF16, [P, KC, 256]) for _ in range(2)]
        wd_e = [carve('R3', BF16, [P, 2, D]) for _ in range(2)]
        silt = [carve('R3', F32, [P, 512]) for _ in range(2)]
        hid = [carve('R2', BF16, [P, 2, TX]) for _ in range(2)]

        def load_gu(e_):
            s = e_ % 2
            al = (lambda q: [B('R3a', q)]) if e_ < 2 else (lambda q: [])
            pg.dma('pool', wg_e[s], weg_d[e_].rearrange("(p k) n -> p k n", k=KC), writes=[B('wg_e', s)] + al(2 * s))
            pg.dma('pool', wu_e[s], weu_d[e_].rearrange("(p k) n -> p k n", k=KC), writes=[B('wu_e', s)] + al(2 * s + 1))

        def load_d(e_):
            s = e_ % 2
            pg.dma('pool', wd_e[s], wed_d[e_].rearrange("(f p) n -> p f n", p=128),
                   writes=[B('wd_e', s)] + ([B('R3a', 4 + s)] if e_ < 2 else []))

        def gu_group(e_, b, fh):
            s = e_ % 2
            c0 = 16 + 512 * b
            bG, bU = nxt('moeGU', [[0, 1], [2, 3]])

            def mmGU(e, bk, wt):
                last = None
                for kc in range(KC):
                    last = e.matmul(banks[bk][:, 0:512], lhsT=wt[:, kc, fh * 128:(fh + 1) * 128],
                                    rhs=uT[:, kc, c0:c0 + 512], start=(kc == 0), stop=(kc == KC - 1))
                return last
            pg.op('pe', lambda e: mmGU(e, bG, wg_e[s]), reads=uT_bufs(c0, c0 + 512) + [B('wg_e', s)],
                  writes=[PB[bG]])
            pg.op('pe', lambda e: mmGU(e, bU, wu_e[s]), reads=uT_bufs(c0, c0 + 512) + [B('wu_e', s)],
                  writes=[PB[bU]])
            si = nxt('silt', [0, 1])
            pg.op('act', lambda e: e.activation(out=silt[si], in_=banks[bG][:, 0:512], func=AF.Silu),
                  reads=[PB[bG]], writes=[B('silt', si)])
            pg.op('dve', lambda e: e.tensor_tensor(
                out=hid[s][:, fh, 512 * b:512 * (b + 1)], in0=silt[si], in1=banks[bU][:, 0:512], op=ALU.mult),
                reads=[B('silt', si), PB[bU]], writes=[B('hid', s, b)])

        def y_tile(e_, j):
            s = e_ % 2
            for hh in range(2):
                bk = nxt('moeY', [4, 5, 6, 7])

                def mmy(e, bk=bk, hh=hh):
                    e.matmul(banks[bk][:, 0:512], lhsT=hid[s][:, 0, 128 * j:128 * (j + 1)],
                             rhs=wd_e[s][:, 0, 512 * hh:512 * (hh + 1)], start=True, stop=False)
                    return e.matmul(banks[bk][:, 0:512], lhsT=hid[s][:, 1, 128 * j:128 * (j + 1)],
                                    rhs=wd_e[s][:, 1, 512 * hh:512 * (hh + 1)], start=False, stop=True)
                pg.op('pe', mmy, reads=[B('hid', s, j // 4), B('wd_e', s)], writes=[PB[bk]])
                pg.op('dve', lambda e, bk=bk, hh=hh: e.scalar_tensor_tensor(
                    out=hacc[:, j, 512 * hh:512 * (hh + 1)], in0=banks[bk][:, 0:512], scalar=gate_all[:, j, e_:e_ + 1],
                    in1=hacc[:, j, 512 * hh:512 * (hh + 1)], op0=ALU.mult, op1=ALU.add),
                    reads=[PB[bk], B('gate', j), B('hacc', j, hh)], writes=[B('hacc', j, hh)])

        LAG = 3
        G_ = n_exp * 8
        if n_exp > 0:
            load_gu(0)
            load_d(0)
            if n_exp > 1:
                load_d(1)
        for t_ in range(G_ + LAG if n_exp > 0 else 0):
            if t_ < G_:
                e_, gi = t_ // 8, t_ % 8
                if gi == 0 and e_ + 1 < n_exp:
                    load_gu(e_ + 1)
                gu_group(e_, gi // 2, gi % 2)
            u_ = t_ - LAG
            if u_ >= 0:
                e2, m_ = u_ // 8, u_ % 8
                y_tile(e2, 2 * m_)
                y_tile(e2, 2 * m_ + 1)
                if m_ == 7 and e2 + 2 < n_exp:
                    load_d(e2 + 2)

        if run_tail:
            pg.dma('sp', gbc[0], gf_d.partition_broadcast(128), writes=[B('gbc', 0)])
            for hf in range(2):
                for j in range(8 * hf, 8 * hf + 8):
                    hb = [B('hacc', j, 0), B('hacc', j, 1)]
                    pg.op('act', lambda e, j=j: e.activation(out=junkbf[:, 0:D], in_=hacc[:, j, :], func=AF.Square,
                                                             accum_out=ssq[:, j:j + 1]),
                          reads=hb, writes=[B('ssq', j), B('junkbf')])
                sl = slice(8 * hf, 8 * hf + 8)
                pg.op('act', lambda e, sl=sl: e.activation(out=rstd2[:, sl], in_=ssq[:, sl], func=AF.Sqrt,
                                                            scale=1.0 / D, bias=eps_t),
                      reads=[B('ssq', j) for j in range(8 * hf, 8 * hf + 8)] + [B('eps')],
                      writes=[B('rstd2s', 'f', hf)])
            obuf = [carve_at('R0', 4096 * i, F32, [P, D]) for i in range(8)]
            for j in range(NT):
                hb = [B('hacc', j, 0), B('hacc', j, 1)]
                k = j % 8
                if j % 8 == 0:
                    sl = slice(j, j + 8)
                    pg.op('dve', lambda e, sl=sl: e.reciprocal(out=rstd2[:, sl], in_=rstd2[:, sl]),
                          reads=[B('rstd2s', 'f', j // 8)], writes=[B('rstd2', 'f', j // 8)])
                if False:
                    pg.op('act', lambda e, j=j, k=k: e.activation(
                        out=obuf[k], in_=hacc[:, j, :], func=AF.Identity, scale=rstd2[:, j:j + 1]),
                        reads=hb + [B('rstd2')], writes=[B('obuf', k)])
                    pg.op('pool', lambda e, k=k: e.tensor_tensor(out=obuf[k], in0=obuf[k], in1=gbc[0], op=ALU.mult),
                          reads=[B('obuf', k), B('gbc', 0)], writes=[B('obuf', k)])
                else:
                    pg.op('dve', lambda e, j=j, k=k: e.scalar_tensor_tensor(
                        out=obuf[k], in0=hacc[:, j, :], scalar=rstd2[:, j:j + 1], in1=gbc[0], op0=ALU.mult, op1=ALU.mult),
                        reads=hb + [B('rstd2', 'f', j // 8), B('gbc', 0)], writes=[B('obuf', k)])
                pg.dma('sp', out_d[128 * j:128 * (j + 1), :], obuf[k], reads=[B('obuf', k)], writes=[B('out', j)])

        pg.barrier()
        local = dict(uT=uT, ckeys=ckeys, cnT=cnT, ikT=ikT, poolT=poolT, attnT=attnT, iw_all=iw_all, score=score,
                     mb=mb, hacc=hacc, gate_all=gate_all, mergedT=mergedT, qabsT_t=qabsT_t,
                     bis=bis, thr=thr, iqT_t=iqT_t, diag=diag, mbT=mbT)
        for name in debug.get('dump', []):
            ap = local[name]
            shp = list(ap.shape)
            n = 1
            for s_ in shp[1:]:
                n *= s_
            dd = nc.dram_tensor("dbg_" + name, [shp[0], n], F32, kind="ExternalOutput").ap()
            flat = ap
            if len(shp) == 3:
                flat = ap.rearrange("p a b -> p (a b)")
            for c0 in range(0, n, 2048):
                c1 = min(n, c0 + 2048)
                pg.dma('pool', dd[:, c0:c1], flat[:, c0:c1], reads=[], writes=[B('dbg', name, c0)])
        pg.barrier()
        pg.emit(esem, dsem)
    return nc


_CACHE = {}


def _consts():
    ident = np.eye(128, dtype=np.float32)
    p = np.arange(128)[:, None]
    s = np.arange(128)[None, :]
    blockbias = np.where((s < 64) | (p >= 64), 0.0, -BIG).astype(np.float32)
    sel8 = np.zeros((32, 8, 128), np.float32)
    for h in range(8):
        sel8[h, h, :] = 1.0
    invcnt = np.broadcast_to((1.0 / np.arange(1, 17, dtype=np.float32))[None, :], (128, 16)).copy()
    pow2 = np.broadcast_to((1.0001 * 2.0 ** (-np.arange(NIT + 2, dtype=np.float64))).astype(np.float32)[None, :],
                           (128, NIT + 2)).copy()
    return dict(c_ident=ident, c_blockbias=blockbias, c_sel8=sel8.reshape(32, 1024), c_invcnt=invcnt, c_pow2=pow2)


def make_in_maps(inputs):
    f = lambda a: np.ascontiguousarray(np.asarray(a, dtype=np.float32))
    shared = dict(
        meta=f(inputs['meta_tokens']),
        norm1_g=f(inputs['norm1_g']).reshape(1, D),
        w_in=f(inputs['w_in']).reshape(D, 3496),
        kv_norm_g=f(inputs['kv_norm_g']).reshape(1, 128),
        w_uk=f(inputs['w_uk']).reshape(128, 512),
        w_uv=f(inputs['w_uv']).reshape(128, 512),
        w_pool=f(inputs['w_pool']).reshape(4, 128, 128),
        pool_scale=f(inputs['pool_scale']).reshape(1, 512),
        w_ba=f(inputs['w_branch_attn']).reshape(512, D),
        w_bp=f(inputs['w_branch_pool']).reshape(512, D),
        w_out=f(inputs['w_out']).reshape(D, D),
        norm2_g=f(inputs['norm2_g']).reshape(1, D),
        w_gr=f(inputs['w_group_router']).reshape(D, 4),
        b_gr=f(inputs['b_group_router']).reshape(1, 4),
        w_er=f(inputs['w_expert_router']).reshape(D, 32),
        b_er=f(inputs['b_expert_router']).reshape(1, 32),
        w_eg=f(inputs['w_expert_gate']).reshape(N_EXP, D, 256),
        w_eu=f(inputs['w_expert_up']).reshape(N_EXP, D, 256),
        w_ed=f(inputs['w_expert_down']).reshape(N_EXP, 256, D),
        final_g=f(inputs['final_norm_g']).reshape(1, D),
    )
    shared.update(_consts())
    x = f(inputs['x'])
    return [dict(shared, x=x[b]) for b in range(8)]


def kernel(**inputs):
    if 'nc' not in _CACHE:
        _CACHE['nc'] = build()
    nc = _CACHE['nc']
    in_maps = make_in_maps(inputs)
    res = run_bass_kernel_spmd(nc, in_maps, core_ids=list(range(8)))
    return np.stack([np.asarray(r["out"], dtype=np.float32).reshape(TX, D) for r in res.results], axis=0)
```
